# Optimizing a Trainium2 kernel written in Bass

```python
import math
import jax, jax.numpy as jnp
from jax import lax
import numpy as np

D_MODEL = 1024
BATCH = 16
SEQ = 256
DEPTH = 4
DEC_BATCH = 8
DEC_SEQ = 4096
PAST_LEN = 512

GRID_W = 64
N_EVEN = (DEPTH + 1) // 2
N_ODD = DEPTH // 2
MIX_W = D_MODEL
DH = 64
HA = D_MODEL // 128
NA_KH = 8
NA_KW = 16
DQK = 64
DVB = 2 * DQK
HB = D_MODEL // 256
C_W = D_MODEL // 2
C_GH = 16
C_G = C_W // C_GH
C_P = 64
DT_MIN = 1e-3
DT_MAX = 1e-1
D_W = D_MODEL // 2
HY_ORDER = 2
HY_EMB_BANDS = 16
HY_EMB = 1 + 2 * HY_EMB_BANDS
HY_HID = 64
HY_BAND_MIN = 1e-4
HY_MIN_DECAY = 3.07
HY_MAX_DECAY = 15.35
D_FF = -(-8 * D_MODEL // (3 * 256)) * 256
EVEN_IN = 3 * HA * DH + 2 * HB * 2 * DQK + HB * DVB
ODD_IN = C_W + 3 * D_W
ROPE_BASE = 10000.0
EPS = 1e-6
Q_BLOCK = 128

kernel_name = 'hybrid_diffusion_na_diff_s5_hyena'


def rmsnorm(x, g):
    x32 = x.astype(jnp.float32)
    y = x32 * lax.rsqrt(jnp.mean(x32 * x32, axis=-1, keepdims=True) + EPS)
    return (y * g.astype(jnp.float32)).astype(x.dtype)


def modulation(cond, w, b):
    m = jax.nn.silu(cond) @ w + b
    return [t[:, None, :] for t in jnp.split(m, 6, axis=-1)]


def swiglu(h, wg, wu, wd):
    return (jax.nn.silu(h @ wg) * (h @ wu)) @ wd


def axial_rope(x):
    L, d = x.shape[1], x.shape[-1]
    half = d // 2
    nf = half // 2
    t = jnp.arange(L)
    inv = ROPE_BASE ** (-jnp.arange(nf, dtype=jnp.float32) / nf)
    bshape = (1, L) + (1,) * (x.ndim - 3) + (nf,)

    def rot(xp, pos):
        ang = pos.astype(jnp.float32)[:, None] * inv
        cos = jnp.cos(ang).reshape(bshape)
        sin = jnp.sin(ang).reshape(bshape)
        x1, x2 = xp[..., :nf], xp[..., nf:]
        return jnp.concatenate([x1 * cos - x2 * sin, x1 * sin + x2 * cos], axis=-1)

    out = jnp.concatenate([rot(x[..., :half], t // GRID_W), rot(x[..., half:], t % GRID_W)], axis=-1)
    return out.astype(x.dtype)


def softmax_attn(q, k, v):
    B, Lq, H, Dh = q.shape
    nb = Lq // Q_BLOCK
    qb = jnp.moveaxis(q.reshape(B, nb, Q_BLOCK, H, Dh), 1, 0)
    scale = Dh ** -0.5

    def blk(qi):
        s = jnp.einsum('bqhd,bkhd->bhqk', qi, k, preferred_element_type=jnp.float32) * scale
        p = jax.nn.softmax(s, axis=-1).astype(v.dtype)
        return jnp.einsum('bhqk,bkhd->bqhd', p, v)

    out = lax.map(blk, qb)
    return jnp.moveaxis(out, 0, 1).reshape(B, Lq, H * Dh)


def diff_lambda(lq1, lk1, lq2, lk2, lam_init):
    f32 = jnp.float32
    return (jnp.exp(jnp.sum(lq1.astype(f32) * lk1.astype(f32)))
            - jnp.exp(jnp.sum(lq2.astype(f32) * lk2.astype(f32))) + lam_init)


def diff_attn(q, k, v, lam):
    B, Lq = q.shape[:2]
    nb = Lq // Q_BLOCK
    qb = jnp.moveaxis(q.reshape((B, nb, Q_BLOCK) + q.shape[2:]), 1, 0)
    scale = DQK ** -0.5

    def blk(qi):
        s = jnp.einsum('bqhsd,bkhsd->bhsqk', qi, k, preferred_element_type=jnp.float32) * scale
        p = jax.nn.softmax(s, axis=-1)
        w = (p[:, :, 0] - lam * p[:, :, 1]).astype(v.dtype)
        return jnp.einsum('bhqk,bkhd->bqhd', w, v)

    out = lax.map(blk, qb)
    return jnp.moveaxis(out, 0, 1).reshape(B, Lq, HB, DVB)


def diff_heads_out(o, g, lam_init):
    B, L = o.shape[:2]
    return (rmsnorm(o, g) * (1.0 - lam_init)).reshape(B, L, HB * DVB)


def neighbourhood_attn(q, k, v, kc, vc, rpb):
    B, L, H, Dh = q.shape
    rows = L // GRID_W
    kh, kw = min(NA_KH, rows), NA_KW
    scale = Dh ** -0.5
    qg = q.reshape(B, rows, GRID_W, H, Dh)
    kg = k.reshape(B, rows, GRID_W, H, Dh)
    vg = v.reshape(B, rows, GRID_W, H, Dh)
    cols = np.arange(GRID_W)
    col_idx = np.clip(cols - kw // 2, 0, GRID_W - kw)[:, None] + np.arange(kw)[None, :]
    bias_cols = rpb[:, :, col_idx - cols[:, None] + (NA_KW - 1)]

    def row_block(r):
        rs = jnp.clip(r - kh // 2, 0, rows - kh)
        qr = lax.dynamic_index_in_dim(qg, r, axis=1, keepdims=False)
        kn = lax.dynamic_slice_in_dim(kg, rs, kh, axis=1)[:, :, col_idx]
        vn = lax.dynamic_slice_in_dim(vg, rs, kh, axis=1)[:, :, col_idx]
        drow = rs + jnp.arange(kh) - r + (NA_KH - 1)
        bias = jnp.transpose(bias_cols[:, drow], (0, 2, 1, 3))
        s_loc = jnp.einsum('bqhd,bmqnhd->bhqmn', qr, kn, preferred_element_type=jnp.float32) * scale + bias
        s_ctx = jnp.einsum('bqhd,bkhd->bhqk', qr, kc, preferred_element_type=jnp.float32) * scale
        s = jnp.concatenate([s_loc.reshape(B, H, GRID_W, kh * kw), s_ctx], axis=-1)
        p = jax.nn.softmax(s, axis=-1).astype(v.dtype)
        p_loc = p[..., :kh * kw].reshape(B, H, GRID_W, kh, kw)
        return (jnp.einsum('bhqmn,bmqnhd->bqhd', p_loc, vn)
                + jnp.einsum('bhqk,bkhd->bqhd', p[..., kh * kw:], vc))

    out = lax.map(row_block, jnp.arange(rows))
    return jnp.moveaxis(out, 0, 1).reshape(B, L, H * Dh)


def split_even(p):
    B, L, _ = p.shape
    wa, wq = HA * DH, HB * 2 * DQK
    qa, ka, va, qb, kb, vb = jnp.split(p, [wa, 2 * wa, 3 * wa, 3 * wa + wq, 3 * wa + 2 * wq], axis=-1)
    return (qa.reshape(B, L, HA, DH), ka.reshape(B, L, HA, DH), va.reshape(B, L, HA, DH),
            qb.reshape(B, L, HB, 2, DQK), kb.reshape(B, L, HB, 2, DQK), vb.reshape(B, L, HB, DVB))


def even_mixer_ctx(h, w_in, w_out, lam, lam_init, g_subln):
    B, L, _ = h.shape
    qa, ka, va, qb, kb, vb = split_even(h @ w_in)
    oa = softmax_attn(qa, ka, va)
    ob = diff_heads_out(diff_attn(qb, kb, vb, lam), g_subln, lam_init)
    out = jnp.concatenate([oa, ob], axis=-1) @ w_out
    return out, ka, va, kb.reshape(B, L, HB, 2 * DQK), vb


def even_mixer_lat(h, ctx_ak, ctx_av, ctx_bk, ctx_bv, w_in, w_out, rpb, lam, lam_init, g_subln):
    B, L, _ = h.shape
    qa, ka, va, qb, kb, vb = split_even(h @ w_in)
    oa = neighbourhood_attn(qa, ka, va, ctx_ak, ctx_av, rpb)
    k_all = jnp.concatenate([axial_rope(kb), ctx_bk.reshape(B, ctx_bk.shape[1], HB, 2, DQK)], axis=1)
    v_all = jnp.concatenate([vb, ctx_bv], axis=1)
    ob = diff_heads_out(diff_attn(axial_rope(qb), k_all, v_all, lam), g_subln, lam_init)
    return jnp.concatenate([oa, ob], axis=-1) @ w_out


def _complex_linear_combine(e1, e2):
    a1r, a1i, b1r, b1i = e1
    a2r, a2i, b2r, b2i = e2
    return (a2r * a1r - a2i * a1i, a2r * a1i + a2i * a1r,
            a2r * b1r - a2i * b1i + b2r, a2r * b1i + a2i * b1r + b2i)


def s5_direction(u, lam_re, lam_im, log_dt, b_re, b_im, c_re, c_im, h0, reverse):
    f32 = jnp.float32
    lr, li = lam_re.astype(f32), lam_im.astype(f32)
    dt = jnp.exp(log_dt.astype(f32))[:, None]
    mag = jnp.exp(lr * dt)
    ab_re, ab_im = mag * jnp.cos(li * dt), mag * jnp.sin(li * dt)
    den = lr * lr + li * li
    k_re = ((ab_re - 1.0) * lr + ab_im * li) / den
    k_im = (ab_im * lr - (ab_re - 1.0) * li) / den
    br, bi = b_re.astype(f32), b_im.astype(f32)
    bb_re = k_re[..., None] * br - k_im[..., None] * bi
    bb_im = k_re[..., None] * bi + k_im[..., None] * br
    bu_re = jnp.einsum('gph,blgh->blgp', bb_re, u)
    bu_im = jnp.einsum('gph,blgh->blgp', bb_im, u)
    if h0 is not None:
        h_re, h_im = h0[0].astype(f32), h0[1].astype(f32)
        first = -1 if reverse else 0
        bu_re = bu_re.at[:, first].add(ab_re * h_re - ab_im * h_im)
        bu_im = bu_im.at[:, first].add(ab_re * h_im + ab_im * h_re)
    L = u.shape[1]
    a_re = jnp.broadcast_to(ab_re, (1, L) + ab_re.shape)
    a_im = jnp.broadcast_to(ab_im, (1, L) + ab_im.shape)
    _, _, x_re, x_im = lax.associative_scan(_complex_linear_combine, (a_re, a_im, bu_re, bu_im),
                                            reverse=reverse, axis=1)
    y = (jnp.einsum('ghp,blgp->blgh', c_re.astype(f32), x_re)
         - jnp.einsum('ghp,blgp->blgh', c_im.astype(f32), x_im))
    return y, x_re, x_im


def s5_bidir(u, h0_re, h0_im, lam_re, lam_im, log_dt, b_re, b_im, c_re, c_im, d_skip):
    B, L, _ = u.shape
    u32 = u.astype(jnp.float32)
    ug = u32.reshape(B, L, C_G, C_GH)
    y = d_skip.astype(jnp.float32) * u32
    finals_re, finals_im = [], []
    for dr in range(2):
        h0 = None if h0_re is None else (h0_re[:, dr], h0_im[:, dr])
        yd, x_re, x_im = s5_direction(ug, lam_re[dr], lam_im[dr], log_dt[dr], b_re[dr], b_im[dr],
                                      c_re[dr], c_im[dr], h0, reverse=(dr == 1))
        y = y + yd.reshape(B, L, C_W)
        if h0_re is None:
            last = 0 if dr == 1 else L - 1
            finals_re.append(x_re[:, last])
            finals_im.append(x_im[:, last])
    if h0_re is None:
        return y, jnp.stack(finals_re, axis=1), jnp.stack(finals_im, axis=1)
    return y, None, None


def hyena_filter_spectra(L, w1, b1, fr1, w2, b2, fr2, w3, decay):
    f32 = jnp.float32
    tn = jnp.arange(L, dtype=f32) / L
    bands = jnp.linspace(HY_BAND_MIN, HY_EMB_BANDS - 1, HY_EMB_BANDS, dtype=f32)
    ang = (2.0 * math.pi) * tn[:, None] * bands
    feats = jnp.concatenate([tn[:, None], jnp.cos(ang), jnp.sin(ang)], axis=-1)
    z = jnp.sin(fr1.astype(f32) * (feats @ w1.astype(f32) + b1.astype(f32)))
    z = jnp.sin(fr2.astype(f32) * (z @ w2.astype(f32) + b2.astype(f32)))
    hf = (z @ w3.astype(f32)).reshape(L, 2, HY_ORDER, D_W)
    hf = hf * jnp.exp(-tn[:, None, None, None] * jnp.abs(decay.astype(f32)))
    k = jnp.concatenate([hf[:, 0], jnp.zeros((1, HY_ORDER, D_W), f32), hf[:0:-1, 1]], axis=0)
    return jnp.fft.rfft(k, axis=0)


def hyena(u3, conv_w, conv_b, w1, b1, fr1, w2, b2, fr2, w3, decay, bias):
    B, L, _ = u3.shape
    f32 = jnp.float32
    up = jnp.pad(u3.astype(f32), ((0, 0), (1, 1), (0, 0)))
    cw = conv_w.astype(f32)
    s = up[:, :-2] * cw[0] + up[:, 1:-1] * cw[1] + up[:, 2:] * cw[2] + conv_b.astype(f32)
    v, x1, x2 = jnp.split(s, 3, axis=-1)
    spec = hyena_filter_spectra(L, w1, b1, fr1, w2, b2, fr2, w3, decay)
    bias32 = bias.astype(f32)

    def long_conv(z, n):
        zf = jnp.fft.rfft(z, n=2 * L, axis=1)
        return jnp.fft.irfft(zf * spec[None, :, n], n=2 * L, axis=1)[:, :L]

    z = x1 * (long_conv(v, 0) + bias32[0] * v)
    z = x2 * (long_conv(z, 1) + bias32[1] * z)
    return z.astype(u3.dtype)


def odd_mixer(h, h0_re, h0_im, w_in, w_out, lam_re, lam_im, log_dt, b_re, b_im, c_re, c_im, d_skip,
              w_glu, b_glu, conv_w, conv_b, w1, b1, fr1, w2, b2, fr2, w3, decay, bias):
    p = h @ w_in
    u, hy_in = p[..., :C_W], p[..., C_W:]
    y_s5, fin_re, fin_im = s5_bidir(u, h0_re, h0_im, lam_re, lam_im, log_dt, b_re, b_im, c_re, c_im, d_skip)
    g = jax.nn.gelu(y_s5)
    y_c = (g * jax.nn.sigmoid(g @ w_glu + b_glu)).astype(h.dtype)
    y_d = hyena(hy_in, conv_w, conv_b, w1, b1, fr1, w2, b2, fr2, w3, decay, bias)
    out = jnp.concatenate([y_c, y_d], axis=-1) @ w_out
    return out, fin_re, fin_im


def setup_inputs(seed: int = 0) -> dict:
    key = jax.random.key(seed)
    keys = jax.random.split(key, 80)
    counter = [0]

    def nk():
        counter[0] += 1
        return keys[counter[0] - 1]

    def nrm(shape, s):
        return s * jax.random.normal(nk(), shape, jnp.float32)

    D, F = D_MODEL, D_FF
    inp = {}
    inp['x_prompt'] = nrm((BATCH, SEQ, D), 1.0)
    inp['x_sample'] = nrm((DEC_BATCH, DEC_SEQ, D), 1.0)
    inp['cache_a_k'] = nrm((DEC_BATCH, N_EVEN, PAST_LEN, HA, DH), 1.0)
    inp['cache_a_v'] = nrm((DEC_BATCH, N_EVEN, PAST_LEN, HA, DH), 1.0)
    inp['cache_b_k'] = nrm((DEC_BATCH, N_EVEN, PAST_LEN, HB, 2 * DQK), 1.0)
    inp['cache_b_v'] = nrm((DEC_BATCH, N_EVEN, PAST_LEN, HB, DVB), 1.0)
    inp['state_c_re'] = nrm((DEC_BATCH, N_ODD, 2, C_G, C_P), 0.1)
    inp['state_c_im'] = nrm((DEC_BATCH, N_ODD, 2, C_G, C_P), 0.1)
    inp['c'] = nrm((DEC_BATCH, D), 1.0)
    inp['c_ctx'] = nrm((D,), 1.0)
    inp['w_mod'] = nrm((DEPTH, D, 6 * D), 0.5 * D ** -0.5)
    inp['b_mod'] = nrm((DEPTH, 6 * D), 0.02)
    inp['g_mix_pre'] = 1.0 + nrm((DEPTH, D), 0.02)
    inp['g_mix_post'] = 1.0 + nrm((DEPTH, D), 0.02)
    inp['g_ffn_pre'] = 1.0 + nrm((DEPTH, D), 0.02)
    inp['g_ffn_post'] = 1.0 + nrm((DEPTH, D), 0.02)
    inp['w_ffn_gate'] = nrm((DEPTH, D, F), D ** -0.5)
    inp['w_ffn_up'] = nrm((DEPTH, D, F), D ** -0.5)
    inp['w_ffn_down'] = nrm((DEPTH, F, D), F ** -0.5)
    inp['w_in_e'] = nrm((N_EVEN, D, EVEN_IN), D ** -0.5)
    inp['w_out_e'] = nrm((N_EVEN, MIX_W, D), MIX_W ** -0.5)
    inp['na_rpb'] = nrm((N_EVEN, HA, 2 * NA_KH - 1, 2 * NA_KW - 1), 0.1)
    inp['lam_q1'] = nrm((N_EVEN, DQK), 0.1)
    inp['lam_k1'] = nrm((N_EVEN, DQK), 0.1)
    inp['lam_q2'] = nrm((N_EVEN, DQK), 0.1)
    inp['lam_k2'] = nrm((N_EVEN, DQK), 0.1)
    inp['g_subln'] = 1.0 + nrm((N_EVEN, DVB), 0.02)
    inp['w_in_o'] = nrm((N_ODD, D, ODD_IN), D ** -0.5)
    inp['w_out_o'] = nrm((N_ODD, MIX_W, D), MIX_W ** -0.5)
    inp['ssm_lam_re'] = -0.5 + nrm((N_ODD, 2, C_G, C_P), 0.01)
    inp['ssm_lam_im'] = jnp.pi * jnp.arange(C_P, dtype=jnp.float32) + nrm((N_ODD, 2, C_G, C_P), 0.01)
    inp['ssm_log_dt'] = jax.random.uniform(nk(), (N_ODD, 2, C_G), jnp.float32,
                                           minval=math.log(DT_MIN), maxval=math.log(DT_MAX))
    inp['ssm_b_re'] = nrm((N_ODD, 2, C_G, C_P, C_GH), (2 * C_GH) ** -0.5)
    inp['ssm_b_im'] = nrm((N_ODD, 2, C_G, C_P, C_GH), (2 * C_GH) ** -0.5)
    inp['ssm_c_re'] = nrm((N_ODD, 2, C_G, C_GH, C_P), 0.5)
    inp['ssm_c_im'] = nrm((N_ODD, 2, C_G, C_GH, C_P), 0.5)
    inp['ssm_d'] = nrm((N_ODD, C_W), 1.0)
    inp['w_glu'] = nrm((N_ODD, C_W, C_W), C_W ** -0.5)
    inp['b_glu'] = nrm((N_ODD, C_W), 0.02)
    inp['hy_conv_w'] = nrm((N_ODD, 3, 3 * D_W), 3 ** -0.5)
    inp['hy_conv_b'] = nrm((N_ODD, 3 * D_W), 0.02)
    inp['hy_w1'] = nrm((N_ODD, HY_EMB, HY_HID), HY_EMB ** -0.5)
    inp['hy_b1'] = nrm((N_ODD, HY_HID), 0.1)
    inp['hy_fr1'] = 1.0 + nrm((N_ODD, HY_HID), 0.02)
    inp['hy_w2'] = nrm((N_ODD, HY_HID, HY_HID), HY_HID ** -0.5)
    inp['hy_b2'] = nrm((N_ODD, HY_HID), 0.1)
    inp['hy_fr2'] = 1.0 + nrm((N_ODD, HY_HID), 0.02)
    inp['hy_w3'] = nrm((N_ODD, HY_HID, 2 * HY_ORDER * D_W), 0.1 * HY_HID ** -0.5)
    inp['hy_decay'] = (jnp.linspace(HY_MIN_DECAY, HY_MAX_DECAY, D_W, dtype=jnp.float32)
                       + nrm((N_ODD, 2, HY_ORDER, D_W), 0.1))
    inp['hy_bias'] = nrm((N_ODD, HY_ORDER, D_W), 1.0)
    return inp


def reference(x_prompt, x_sample, cache_a_k, cache_a_v, cache_b_k, cache_b_v, state_c_re, state_c_im,
              c, c_ctx, w_mod, b_mod, g_mix_pre, g_mix_post, g_ffn_pre, g_ffn_post,
              w_ffn_gate, w_ffn_up, w_ffn_down, w_in_e, w_out_e, na_rpb,
              lam_q1, lam_k1, lam_q2, lam_k2, g_subln, w_in_o, w_out_o,
              ssm_lam_re, ssm_lam_im, ssm_log_dt, ssm_b_re, ssm_b_im, ssm_c_re, ssm_c_im, ssm_d,
              w_glu, b_glu, hy_conv_w, hy_conv_b, hy_w1, hy_b1, hy_fr1, hy_w2, hy_b2, hy_fr2,
              hy_w3, hy_decay, hy_bias):
    xc, xs = x_prompt, x_sample
    new_ak, new_av, new_bk, new_bv, new_sre, new_sim = [], [], [], [], [], []
    for l in range(DEPTH):
        mod_c = modulation(c_ctx[None, :], w_mod[l], b_mod[l])
        mod_s = modulation(c, w_mod[l], b_mod[l])
        hc = rmsnorm(xc, g_mix_pre[l]) * (1.0 + mod_c[1]) + mod_c[0]
        hs = rmsnorm(xs, g_mix_pre[l]) * (1.0 + mod_s[1]) + mod_s[0]
        if l % 2 == 0:
            e = l // 2
            lam_init = 0.8 - 0.6 * math.exp(-0.3 * l)
            lam = diff_lambda(lam_q1[e], lam_k1[e], lam_q2[e], lam_k2[e], lam_init)
            oc, ak, av, bk, bv = even_mixer_ctx(hc, w_in_e[e], w_out_e[e], lam, lam_init, g_subln[e])
            os_ = even_mixer_lat(hs, cache_a_k[:, e], cache_a_v[:, e], cache_b_k[:, e], cache_b_v[:, e],
                                 w_in_e[e], w_out_e[e], na_rpb[e], lam, lam_init, g_subln[e])
            new_ak.append(ak)
            new_av.append(av)
            new_bk.append(bk)
            new_bv.append(bv)
        else:
            o = l // 2
            odd_p = (w_in_o[o], w_out_o[o], ssm_lam_re[o], ssm_lam_im[o], ssm_log_dt[o], ssm_b_re[o],
                     ssm_b_im[o], ssm_c_re[o], ssm_c_im[o], ssm_d[o], w_glu[o], b_glu[o],
                     hy_conv_w[o], hy_conv_b[o], hy_w1[o], hy_b1[o], hy_fr1[o], hy_w2[o], hy_b2[o],
                     hy_fr2[o], hy_w3[o], hy_decay[o], hy_bias[o])
            oc, sre, sim = odd_mixer(hc, None, None, *odd_p)
            os_, _, _ = odd_mixer(hs, state_c_re[:, o], state_c_im[:, o], *odd_p)
            new_sre.append(sre)
            new_sim.append(sim)
        xc = xc + mod_c[2] * rmsnorm(oc, g_mix_post[l])
        xs = xs + mod_s[2] * rmsnorm(os_, g_mix_post[l])
        hc = rmsnorm(xc, g_ffn_pre[l]) * (1.0 + mod_c[4]) + mod_c[3]
        hs = rmsnorm(xs, g_ffn_pre[l]) * (1.0 + mod_s[4]) + mod_s[3]
        xc = xc + mod_c[5] * rmsnorm(swiglu(hc, w_ffn_gate[l], w_ffn_up[l], w_ffn_down[l]), g_ffn_post[l])
        xs = xs + mod_s[5] * rmsnorm(swiglu(hs, w_ffn_gate[l], w_ffn_up[l], w_ffn_down[l]), g_ffn_post[l])
    new_a_k = jnp.stack(new_ak, axis=1)
    new_a_v = jnp.stack(new_av, axis=1)
    new_b_k = jnp.stack(new_bk, axis=1)
    new_b_v = jnp.stack(new_bv, axis=1)
    new_s_re = jnp.stack(new_sre, axis=1)
    new_s_im = jnp.stack(new_sim, axis=1)
    return (xc, xs, new_a_k, new_a_v, new_b_k, new_b_v, new_s_re, new_s_im)
```

```python
import math
import contextlib
import numpy as np
import ml_dtypes
import concourse.bass as bass
import concourse.mybir as mybir
from concourse.bass_utils import run_bass_kernel_spmd

F32 = mybir.dt.float32
BF16 = mybir.dt.bfloat16
I32 = mybir.dt.int32
AF = mybir.ActivationFunctionType
ALU = mybir.AluOpType

D = 1024
NS = 4096
NP_ = 512
NT = NS + NP_
DFF = 2816
NFC = 22
EPS = 1e-6
DEPTH = 4
ENGS = ("pe", "dve", "act", "pool", "sp")
SEM_ROLL = 12000
NDMA_SEM = 28
TWO_PI = 2.0 * math.pi


class Prog:
    def __init__(self, nc, top):
        self.nc = nc
        self.top = top
        self.stack = top
        self.q = {e: [] for e in ENGS}
        self.cnt = {e: 0 for e in ENGS}
        self.esems = {e: [] for e in ENGS}
        self.last_tok = {e: None for e in ENGS}
        self.last_w = {}
        self.readers = {}
        self.waited = {e: {} for e in ENGS}
        self.dma_sems = [top.enter_context(nc.semaphore("dq%d" % i)) for i in range(NDMA_SEM)]
        self.dma_uses = [0] * NDMA_SEM
        self.dma_i = 0
        self.sem_ids = {}
        self.nbuf = 0
        self.ninst = 0

    def sbuf(self, shape, dtype, name="sb"):
        self.nbuf += 1
        return self.stack.enter_context(self.nc.sbuf_tensor("%s_%d" % (name, self.nbuf), list(shape), dtype))

    def psum(self, shape, dtype=F32, name="ps"):
        self.nbuf += 1
        return self.stack.enter_context(self.nc.psum_tensor("%s_%d" % (name, self.nbuf), list(shape), dtype))

    def _esem(self, eng, k):
        while len(self.esems[eng]) <= k:
            s = self.top.enter_context(self.nc.semaphore("e_%s_%d" % (eng, len(self.esems[eng]))))
            self.esems[eng].append(s)
        return self.esems[eng][k]

    def _need(self, eng, tok, waits):
        if tok is None:
            return
        sem, val, teng = tok
        if teng == "pe" and eng == "pe":
            return
        key = id(sem)
        self.sem_ids[key] = sem
        if self.waited[eng].get(key, 0) >= val:
            return
        if val > waits.get(key, 0):
            waits[key] = val

    def op(self, eng, fn, reads=(), writes=(), dma=False):
        waits = {}
        for r in reads:
            self._need(eng, self.last_w.get(r), waits)
        for w in writes:
            self._need(eng, self.last_w.get(w), waits)
            for t in self.readers.get(w, ()):
                self._need(eng, t, waits)
        if dma:
            slot = self.dma_i % NDMA_SEM
            self.dma_i += 1
            sem = self.dma_sems[slot]
            if self.dma_uses[slot] > 0:
                self._need(eng, (sem, 16 * self.dma_uses[slot], "dma"), waits)
            self.dma_uses[slot] += 1
            tok = (sem, 16 * self.dma_uses[slot], "dma")
            inc = 16
        else:
            n = self.cnt[eng]
            self.cnt[eng] += 1
            sem = self._esem(eng, n // SEM_ROLL)
            tok = (sem, (n % SEM_ROLL) + 1, eng)
            inc = 1
            self.last_tok[eng] = tok
        wl = []
        for key, val in waits.items():
            self.waited[eng][key] = val
            wl.append((self.sem_ids[key], val))
        self.q[eng].append((wl, fn, tok[0], inc))
        self.ninst += 1
        for r in reads:
            self.readers.setdefault(r, []).append(tok)
        for w in writes:
            self.last_w[w] = tok
            self.readers[w] = []
        return tok

    def dma(self, out, in_, reads=(), writes=(), eng="sp", **kw):
        return self.op(eng, lambda e: e.dma_start(out=out, in_=in_, **kw), reads, writes, dma=True)

    def barrier(self):
        toks = [self.last_tok[e] for e in ENGS if self.last_tok[e] is not None]
        for i, s in enumerate(self.dma_sems):
            if self.dma_uses[i] > 0:
                toks.append((s, 16 * self.dma_uses[i], "dma"))
        for e in ENGS:
            waits = {}
            for t in toks:
                if t[2] == e:
                    continue
                self._need(e, t, waits)
            wl = []
            for key, val in waits.items():
                self.waited[e][key] = val
                wl.append((self.sem_ids[key], val))
            if wl:
                self.q[e].append((wl, None, None, 0))
        self.last_w.clear()
        self.readers.clear()

    def flush(self):
        q = self.q

        def run(engname, e):
            for wl, fn, sem, inc in q[engname]:
                for s, v in wl:
                    e.wait_ge(s, v)
                if fn is not None:
                    fn(e).then_inc(sem, inc)

        with self.nc.Block() as block:
            @block.tensor
            def _(e):
                run("pe", e)

            @block.vector
            def _(e):
                run("dve", e)

            @block.scalar
            def _(e):
                run("act", e)

            @block.gpsimd
            def _(e):
                run("pool", e)

            @block.sync
            def _(e):
                run("sp", e)
        self.q = {e: [] for e in ENGS}

    @contextlib.contextmanager
    def phase(self):
        with contextlib.ExitStack() as st:
            old = self.stack
            self.stack = st
            yield
            self.barrier()
            self.flush()
            self.stack = old


class RR:
    def __init__(self, p, n, shape, dtype, name, psum=False):
        self.t = [(p.psum(shape, dtype, name) if psum else p.sbuf(shape, dtype, name)) for _ in range(n)]
        self.k = ["%s@%d_%d" % (name, p.nbuf, i) for i in range(n)]
        self.n = n
        self.i = 0

    def next(self):
        j = self.i % self.n
        self.i += 1
        return self.t[j], self.k[j]


def mm(p, out, lhsT, rhs, start, stop, r, w):
    p.op("pe", lambda e: e.matmul(out, lhsT=lhsT, rhs=rhs, start=start, stop=stop), r, w)


def trp(p, out, in_, ident, r, w):
    p.op("pe", lambda e: e.transpose(out, in_, ident), r, w)


def act(p, out, in_, func, r, w, scale=None, bias=None, accum=None):
    kw = {}
    if scale is not None:
        kw["scale"] = scale
    if bias is not None:
        kw["bias"] = bias
    if accum is not None:
        kw["accum_out"] = accum
    p.op("act", lambda e: e.activation(out=out, in_=in_, func=func, **kw), r, w)


def tt(p, eng, out, a, b, op, r, w):
    p.op(eng, lambda e: e.tensor_tensor(out=out, in0=a, in1=b, op=op), r, w)


def ts(p, eng, out, a, s1, s2, op0, op1, r, w):
    if op1 is None:
        p.op(eng, lambda e: e.tensor_scalar(out=out, in0=a, scalar1=s1, scalar2=None, op0=op0), r, w)
    else:
        p.op(eng, lambda e: e.tensor_scalar(out=out, in0=a, scalar1=s1, scalar2=s2, op0=op0, op1=op1), r, w)


def stt(p, out, a, s, b, op0, op1, r, w):
    p.op("dve", lambda e: e.scalar_tensor_tensor(out=out, in0=a, scalar=s, in1=b, op0=op0, op1=op1), r, w)


def cp(p, eng, out, in_, r, w):
    if eng == "act":
        p.op("act", lambda e: e.activation(out=out, in_=in_, func=AF.Copy), r, w)
    else:
        p.op(eng, lambda e: e.tensor_copy(out=out, in_=in_), r, w)


def mset(p, eng, ap, val, w):
    p.op(eng, lambda e: e.memset(ap, val), (), w)


def recip(p, out, in_, r, w):
    p.op("dve", lambda e: e.reciprocal(out=out, in_=in_), r, w)


_CONST = {}


def _dft_blocks(L):
    N = 2 * L
    nb = L // 128
    s = np.arange(L, dtype=np.int64)
    idx = (s[:, None] * s[None, :]) % N
    ang = idx.astype(np.float64) * (TWO_PI / N)
    C = np.cos(ang)
    S = np.sin(ang)

    def blk(M):
        return np.ascontiguousarray(M.reshape(nb, 128, nb, 128).transpose(2, 1, 0, 3)).astype(ml_dtypes.bfloat16)

    return blk(C), blk(S)


def _feats(L):
    tn = (np.arange(L, dtype=np.float32) / np.float32(L)).astype(np.float32)
    bands = np.linspace(1e-4, 15.0, 16, dtype=np.float32)
    ang = (np.float32(TWO_PI) * tn[:, None]).astype(np.float32) * bands
    f = np.concatenate([tn[:, None], np.cos(ang), np.sin(ang)], axis=-1).astype(np.float32)
    tnl = np.ascontiguousarray(tn.reshape(L // 128, 128).T)
    return np.ascontiguousarray(f.T), tnl


def consts():
    if _CONST:
        return _CONST
    c = _CONST
    c["identb"] = np.eye(128, dtype=np.float32).astype(ml_dtypes.bfloat16)
    c["identf"] = np.eye(128, dtype=np.float32)
    t = np.arange(NS)
    inv = (10000.0 ** (-np.arange(16, dtype=np.float32) / 16)).astype(np.float32)
    rc = np.zeros((64, NS), np.float32)
    rs = np.zeros((64, NS), np.float32)
    for d in range(64):
        pos = (t // 64) if d < 32 else (t % 64)
        ang = pos.astype(np.float32) * inv[d % 16]
        rc[d] = np.cos(ang)
        rs[d] = np.sin(ang)
    c["ropeC"] = np.concatenate([rc, rc], 0)
    c["ropeS"] = np.concatenate([rs, rs], 0)
    kc = np.arange(64)[:, None]
    qc = 63 - np.arange(64)[None, :]
    cs = np.clip(qc - 8, 0, 48)
    wm = ((kc >= cs) & (kc < cs + 16)).astype(np.float32)
    c["winmask"] = np.concatenate([wm, wm], 0)
    c["dftC4096"], c["dftS4096"] = _dft_blocks(4096)
    c["dftC256"], c["dftS256"] = _dft_blocks(256)
    c["feats4096"], c["tn4096"] = _feats(4096)
    c["feats256"], c["tn256"] = _feats(256)
    sg = np.where(np.arange(128) % 2 == 0, 1.0, -1.0).astype(np.float32)
    c["sgnp"] = np.tile(sg[:, None], (1, 32)).astype(ml_dtypes.bfloat16)
    c["sgnj"] = sg[None, :].astype(ml_dtypes.bfloat16)
    c["iota128"] = np.tile(np.arange(1, 129, dtype=np.float32)[None, :], (128, 1))
    return c


IN_SPECS = {}


def build_program(nlayers=DEPTH):
    nc = bass.Bass("TRN2", target_bir_lowering=False)
    T = {}

    def din(name, shape, dt=F32):
        T[name] = nc.dram_tensor(name, list(shape), dt, kind="ExternalInput").ap()
        IN_SPECS[name] = (tuple(shape), dt)
        return T[name]

    def dout(name, shape):
        T[name] = nc.dram_tensor(name, list(shape), F32, kind="ExternalOutput").ap()
        return T[name]

    def dscr(name, shape, dt=F32):
        T[name] = nc.dram_tensor(name, list(shape), dt).ap()
        return T[name]

    x0 = din("x0", [NT, D])
    cak = din("cak", [2, 512, 512]); cav = din("cav", [2, 512, 512])
    cbk = din("cbk", [2, 512, 512]); cbv = din("cbv", [2, 512, 512])
    sre = din("sre", [2, 2, 128, 16]); sim = din("sim", [2, 2, 128, 16])
    cond = din("cond", [128, 8, 2])
    w_mod = din("w_mod", [4, D, 6 * D]); b_mod = din("b_mod", [4, 128, 48])
    gpre1 = din("gpre1", [4, 128, 8]); gpost1 = din("gpost1", [4, 128, 8])
    gpre2 = din("gpre2", [4, 128, 8]); gpost2 = din("gpost2", [4, 128, 8])
    wg_in = din("wg", [4, D, DFF]); wu_in = din("wu", [4, D, DFF]); wd_in = din("wd", [4, DFF, D])
    w_in_e = din("w_in_e", [2, D, 3072]); w_out_e = din("w_out_e", [2, D, D])
    rpb = din("rpb", [2, 8, 15, 31])
    lamv = din("lamv", [2, 4, 64]); gsub = din("gsub", [2, 128])
    w_in_o = din("w_in_o", [2, D, 2048]); w_out_o = din("w_out_o", [2, D, D])
    s_lre = din("s_lre", [2, 2, 128, 16]); s_lim = din("s_lim", [2, 2, 128, 16]); s_ldt = din("s_ldt", [2, 2, 128, 16])
    s_bre = din("s_bre", [2, 2, 128, 256]); s_bim = din("s_bim", [2, 2, 128, 256])
    s_cre = din("s_cre", [2, 2, 128, 256]); s_cim = din("s_cim", [2, 2, 128, 256])
    s_d = din("s_d", [2, 128, 4])
    w_glu = din("w_glu", [2, 512, 512]); b_glu = din("b_glu", [2, 128, 4])
    hcw = din("hcw", [2, 128, 12, 3]); hcb = din("hcb", [2, 128, 12])
    hw1 = din("hw1", [2, 33, 64]); hb1 = din("hb1", [2, 64, 1]); hf1 = din("hf1", [2, 64, 1])
    hw2 = din("hw2", [2, 64, 64]); hb2 = din("hb2", [2, 64, 1]); hf2 = din("hf2", [2, 64, 1])
    hw3 = din("hw3", [2, 64, 2048]); hdec = din("hdec", [2, 4, 512]); hbias = din("hbias", [2, 2, 512])
    identb_d = din("identb", [128, 128], BF16); identf_d = din("identf", [128, 128])
    ropeC = din("ropeC", [128, NS]); ropeS = din("ropeS", [128, NS])
    winmask = din("winmask", [128, 64])
    dftC = {4096: din("dftC4096", [32, 128, 32, 128], BF16), 256: din("dftC256", [2, 128, 2, 128], BF16)}
    dftS = {4096: din("dftS4096", [32, 128, 32, 128], BF16), 256: din("dftS256", [2, 128, 2, 128], BF16)}
    feats = {4096: din("feats4096", [33, 4096]), 256: din("feats256", [33, 256])}
    tnl = {4096: din("tn4096", [128, 32]), 256: din("tn256", [128, 2])}
    iota_d = din("iota128", [128, 128])
    din("sgnp", [128, 32], BF16); din("sgnj", [1, 128], BF16)

    o_yp = dout("o_yp", [NP_, D]); o_ys = dout("o_ys", [NS, D])
    o_ak = dout("o_ak", [2, 2, 256, 512]); o_av = dout("o_av", [2, 2, 256, 512])
    o_bk = dout("o_bk", [2, 2, 256, 512]); o_bv = dout("o_bv", [2, 2, 256, 512])
    o_sre = dout("o_sre", [2, 2, 2, 16, 128]); o_sim = dout("o_sim", [2, 2, 2, 16, 128])

    X = dscr("X", [NT, D])
    WG = dscr("WG", [4, D, DFF], BF16); WU = dscr("WU", [4, D, DFF], BF16); WD = dscr("WD", [4, DFF, D], BF16)
    QAT = dscr("QAT", [512, 5120], BF16); KAT = dscr("KAT", [512, 5120], BF16); VA = dscr("VA", [5120, 512], BF16)
    QBT = dscr("QBT", [512, 5120], BF16); KBT = dscr("KBT", [512, 5120], BF16); VB = dscr("VB", [5120, 512], BF16)
    MIXT = dscr("MIXT", [NT, D], BF16)
    MIXF = dscr("MIXF", [D, NT], BF16)
    RPBP = dscr("RPBP", [8, 15, 288])
    UT32 = dscr("UT32", [512, NT]); UTB = dscr("UTB", [512, NT], BF16)
    HYT = dscr("HYT", [1536, NT]); HYTM = dscr("HYTM", [NT, 1536])
    YS = dscr("YS", [2, 512, NT])
    KSP = {4096: dscr("KSP4096", [2, 2, 4096, 512]), 256: dscr("KSP256", [2, 2, 256, 512])}
    Z1F = dscr("Z1F", [NT, 512])

    fin_keys = []

    with contextlib.ExitStack() as top:
        p = Prog(nc, top)
        identb = p.sbuf([128, 128], BF16, "identb")
        identf = p.sbuf([128, 128], F32, "identf")
        condT = p.sbuf([128, 8, 2], F32, "condT")
        modA1 = p.sbuf([128, 8, 2], F32, "modA1"); modB1 = p.sbuf([128, 8, 2], F32, "modB1")
        modA2 = p.sbuf([128, 8, 2], F32, "modA2"); modB2 = p.sbuf([128, 8, 2], F32, "modB2")
        ggbc1 = [p.sbuf([128, D], F32, "ggbc1") for _ in range(2)]
        ggbc2 = [p.sbuf([128, D], F32, "ggbc2") for _ in range(2)]
        epsT = p.sbuf([128, 1], F32, "epsT")
        EPS_T[0] = epsT

        with p.phase():
            p.dma(identb[:], identb_d, (), ["identb"])
            p.dma(identf[:], identf_d, (), ["identf"])
            p.dma(condT[:], cond, (), ["condT"])
            mset(p, "dve", epsT[:], EPS, ["epsT"])
            act(p, condT[:], condT[:], AF.Silu, ["condT"], ["condT"])
            for i in range(9):
                p.dma(X[i * 512:(i + 1) * 512, :], x0[i * 512:(i + 1) * 512, :], (), ["X%d" % i], eng=("sp" if i % 2 else "act"))
            st32 = RR(p, 3, [128, DFF], F32, "st32")
            st16 = RR(p, 3, [128, DFF], BF16, "st16")
            k = 0
            for l in range(nlayers):
                for (src, dst, rows, cols) in ((wg_in, WG, D, DFF), (wu_in, WU, D, DFF), (wd_in, WD, DFF, D)):
                    for rb in range(rows // 128):
                        a, ak = st32.next()
                        b, bk = st16.next()
                        p.dma(a[:, 0:cols], src[l][rb * 128:(rb + 1) * 128, :], (), [ak], eng=("sp" if k % 2 else "act"))
                        eng = ("dve", "pool", "act")[k % 3]
                        cp(p, eng, b[:, 0:cols], a[:, 0:cols], [ak], [bk])
                        p.dma(dst[l][rb * 128:(rb + 1) * 128, :], b[:, 0:cols], [bk], ["wscr"], eng="sp")
                        k += 1

        for l in range(nlayers):
            even = (l % 2 == 0)
            li = l // 2
            lam_init = 0.8 - 0.6 * math.exp(-0.3 * l)
            build_mod(p, T, l, condT, identf, (modA1, modB1, modA2, modB2, ggbc1, ggbc2))
            if even:
                build_even_proj(p, T, l, li, identb, identf, epsT, modA1, modB1)
                build_even_attn(p, T, l, li, lam_init)
                tm_chunks = list(range(8))
            else:
                build_odd_proj(p, T, l, li, identb, epsT, modA1, modB1)
                build_s5(p, T, l, li, identf)
                build_hyena(p, T, l, li, identf)
                tm_chunks = [4, 5, 6, 7]
            build_out_ffn(p, T, l, li, even, tm_chunks, identb, epsT, modA2, modB2, ggbc1, ggbc2)

        with p.phase():
            for i in range(8):
                p.dma(o_ys[i * 512:(i + 1) * 512, :], X[i * 512:(i + 1) * 512, :], (), ["oys%d" % i], eng=("sp" if i % 2 else "act"))
            p.dma(o_yp[:, :], X[NS:NT, :], (), ["oyp"])
    return nc


def build_mod(p, T, l, condT, identf, outs):
    modA1, modB1, modA2, modB2, ggbc1, ggbc2 = outs
    w_mod, b_mod = T["w_mod"], T["b_mod"]
    with p.phase():
        mfm = p.sbuf([128, 48, 2], F32, "mfm")
        bm = p.sbuf([128, 48], F32, "bm")
        g4 = p.sbuf([128, 4, 8], F32, "g4")
        gg = p.sbuf([128, 8, 2], F32, "gg")
        lhs = RR(p, 2, [128, 128], F32, "bclhs")
        wp = RR(p, 2, [128, 8, 512], F32, "wmodp")
        pm = RR(p, 2, [128, 512], F32, "pm", psum=True)
        pb = RR(p, 2, [128, 512], F32, "pbc", psum=True)
        p.dma(bm[:], b_mod[l], (), ["bm"])
        for j, nm in enumerate(("gpre1", "gpost1", "gpre2", "gpost2")):
            p.dma(g4[:, j, :], T[nm][l], (), ["g4"], eng="act")
        for pc in range(12):
            w, wk = wp.next()
            p.dma(w[:], w_mod[l].rearrange("(kc q) n -> q kc n", q=128)[:, :, pc * 512:(pc + 1) * 512], (), [wk],
                  eng=("sp" if pc % 2 else "act"))
            ps, pk = pm.next()
            for jc in range(4):
                for kc in range(8):
                    mm(p, ps[:, jc * 2:jc * 2 + 2], w[:, kc, jc * 128:(jc + 1) * 128], condT[:, kc, :], kc == 0, kc == 7,
                       [wk, "condT"], [pk])
            for cd in range(2):
                tt(p, "dve", mfm[:, pc * 4:pc * 4 + 4, cd], ps[:, cd:8:2], bm[:, pc * 4:pc * 4 + 4], ALU.add,
                   [pk, "bm"], ["mfm"])
        for cd in range(2):
            ts(p, "dve", modA1[:, :, cd], mfm[:, 8:16, cd], 1.0, None, ALU.add, None, ["mfm"], ["modA1"])
            tt(p, "dve", modA1[:, :, cd], modA1[:, :, cd], g4[:, 0, :], ALU.mult, ["modA1", "g4"], ["modA1"])
            cp(p, "dve", modB1[:, :, cd], mfm[:, 0:8, cd], ["mfm"], ["modB1"])
            ts(p, "dve", modA2[:, :, cd], mfm[:, 32:40, cd], 1.0, None, ALU.add, None, ["mfm"], ["modA2"])
            tt(p, "dve", modA2[:, :, cd], modA2[:, :, cd], g4[:, 2, :], ALU.mult, ["modA2", "g4"], ["modA2"])
            cp(p, "dve", modB2[:, :, cd], mfm[:, 24:32, cd], ["mfm"], ["modB2"])
            for which, (sp_, gj, dst) in enumerate(((2, 1, ggbc1), (5, 3, ggbc2))):
                tt(p, "dve", gg[:, :, cd], mfm[:, sp_ * 8:sp_ * 8 + 8, cd], g4[:, gj, :], ALU.mult, ["mfm", "g4"], ["gg"])
                for half in range(2):
                    ps, pk = pb.next()
                    for kk in range(4):
                        kc = half * 4 + kk
                        lt, lk = lhs.next()
                        cp(p, "dve", lt[:], gg[:, kc, cd:cd + 1].to_broadcast([128, 128]), ["gg"], [lk])
                        mm(p, ps[:, kk * 128:(kk + 1) * 128], lt[:], identf[:], True, True, [lk, "identf"], [pk])
                    cp(p, "act", dst[cd][:, half * 512:(half + 1) * 512], ps[:], [pk], ["ggbc"])


class NormCtx:
    def __init__(self, p, identb, epsT):
        self.p = p
        self.identb = identb
        self.epsT = epsT
        self.junk = RR(p, 2, [128, D], BF16, "junk")
        self.ss = RR(p, 2, [128, 4], F32, "ss")
        self.xn = RR(p, 2, [128, 4, D], BF16, "xn")
        self.pst = RR(p, 2, [128, 1024], BF16, "pst", psum=True)

    def run(self, xs, xkey, A, B, cd, hT, hkey, ncol=512):
        p = self.p
        nt = ncol // 128
        ss, sk = self.ss.next()
        xn, xk = self.xn.next()
        for i in range(nt):
            j, jk = self.junk.next()
            act(p, j[:], xs[:, i, :], AF.Square, [xkey], [jk, sk], accum=ss[:, i:i + 1])
        act(p, ss[:, 0:nt], ss[:, 0:nt], AF.Sqrt, [sk], [sk], scale=1.0 / D, bias=self.epsT[:])
        recip(p, ss[:, 0:nt], ss[:, 0:nt], [sk], [sk])
        for i in range(nt):
            ts(p, "dve" if i % 2 == 0 else "pool", xn[:, i, :], xs[:, i, :], ss[:, i:i + 1], None, ALU.mult, None,
               [xkey, sk], [xk])
        for kc in range(8):
            ps, pk = self.pst.next()
            for i in range(nt):
                trp(p, ps[:, i * 128:(i + 1) * 128], xn[:, i, kc * 128:(kc + 1) * 128], self.identb[:], [xk, "identb"], [pk])
            act(p, hT[:, kc, 0:ncol], ps[:, 0:ncol], AF.Identity, [pk, "modA", "modB"], [hkey],
                scale=A[:, kc, cd:cd + 1], bias=B[:, kc, cd:cd + 1])


def load_w_bf16(p, dst, dkey, src2d, ncols, st, nkc=8):
    k = 0
    for kc in range(nkc):
        c0 = 0
        while c0 < ncols:
            cw = min(1024, ncols - c0)
            a, ak = st.next()
            p.dma(a[:, 0:cw], src2d[kc * 128:(kc + 1) * 128, c0:c0 + cw], (), [ak], eng=("sp" if k % 2 else "act"))
            cp(p, ("dve", "pool")[k % 2], dst[:, kc, c0:c0 + cw], a[:, 0:cw], [ak], [dkey])
            c0 += cw
            k += 1


def unit_cd(u):
    return 0 if u < 8 else 1


def xview(X, t0, n=512):
    return X[t0:t0 + n, :].rearrange("(i q) c -> q i c", q=128)


class PostNorm:
    def __init__(self, p, epsT):
        self.p = p
        self.epsT = epsT
        self.ss = RR(p, 4, [128, 4], F32, "pnss")
        self.junk = RR(p, 2, [128, D], BF16, "pnjunk")
        self.tmp = RR(p, 2, [128, D], F32, "pntmp")

    def run(self, po0, k0, po1, k1, x, xk, gg):
        p = self.p
        ss, sk = self.ss.next()
        j, jk = self.junk.next()
        act(p, j[:, 0:512], po0[:], AF.Square, [k0], [jk, sk], accum=ss[:, 0:1])
        act(p, j[:, 512:1024], po1[:], AF.Square, [k1], [jk, sk], accum=ss[:, 1:2])
        tt(p, "dve", ss[:, 2:3], ss[:, 0:1], ss[:, 1:2], ALU.add, [sk], [sk])
        act(p, ss[:, 2:3], ss[:, 2:3], AF.Sqrt, [sk], [sk], scale=1.0 / D, bias=self.epsT[:])
        recip(p, ss[:, 3:4], ss[:, 2:3], [sk], [sk])
        t, tk = self.tmp.next()
        stt(p, t[:, 0:512], po0[:], ss[:, 3:4], gg[:, 0:512], ALU.mult, ALU.mult, [k0, sk, "ggbc"], [tk])
        stt(p, t[:, 512:1024], po1[:], ss[:, 3:4], gg[:, 512:1024], ALU.mult, ALU.mult, [k1, sk, "ggbc"], [tk])
        tt(p, "pool", x, x, t[:], ALU.add, [tk, xk], [xk])


def build_out_ffn(p, T, l, li, even, tm_chunks, identb, epsT, modA2, modB2, ggbc1, ggbc2):
    X, MIXT, MIXF = T["X"], T["MIXT"], T["MIXF"]
    w_out = T["w_out_e"][li] if even else T["w_out_o"][li]
    ntm = len(tm_chunks)
    c0 = tm_chunks[0]
    with p.phase():
        st = RR(p, 2, [128, 1024], F32, "wst")
        wout = p.sbuf([128, 8, D], BF16, "wout")
        load_w_bf16(p, wout, "wout", w_out, D, st)
        pn = PostNorm(p, epsT)
        xs = RR(p, 2, [128, 4, D], F32, "xs")
        mT = RR(p, 2, [128, 8, 512], BF16, "mT")
        mx = RR(p, 2, [128, 4, ntm * 128], BF16, "mx")
        pso = RR(p, 4, [128, 512], F32, "pso", psum=True)
        pst = RR(p, 2, [128, 1024], BF16, "pst", psum=True)
        for u in range(9):
            t0 = u * 512
            cd = unit_cd(u)
            xt, xk = xs.next()
            p.dma(xt[:], xview(X, t0), ["X%d" % u], [xk], eng="act")
            m, mk = mT.next()
            for kc in range(8):
                if kc not in tm_chunks:
                    p.dma(m[:, kc, :], MIXF[kc * 128:(kc + 1) * 128, t0:t0 + 512], ["MIXF"], [mk], eng="sp")
            mxt, mxk = mx.next()
            p.dma(mxt[:], MIXT[t0:t0 + 512, c0 * 128:(c0 + ntm) * 128].rearrange("(i q) c -> q i c", q=128), ["MIXT"], [mxk])
            for kc in tm_chunks:
                ps, pk = pst.next()
                for i in range(4):
                    trp(p, ps[:, i * 128:(i + 1) * 128], mxt[:, i, (kc - c0) * 128:(kc - c0 + 1) * 128], identb[:], [mxk, "identb"], [pk])
                cp(p, "act" if kc % 2 else "dve", m[:, kc, :], ps[:, 0:512], [pk], [mk])
            for i in range(4):
                po0, k0 = pso.next()
                po1, k1 = pso.next()
                for half, (po, pk) in enumerate(((po0, k0), (po1, k1))):
                    for kc in range(8):
                        mm(p, po[:], m[:, kc, i * 128:(i + 1) * 128], wout[:, kc, half * 512:(half + 1) * 512], kc == 0, kc == 7,
                           [mk, "wout"], [pk])
                pn.run(po0, k0, po1, k1, xt[:, i, :], xk, ggbc1[cd])
            p.dma(xview(X, t0), xt[:], [xk], ["X%d" % u], eng="sp")
    WG, WU = T["WG"][l], T["WU"][l]
    with p.phase():
        st = RR(p, 2, [128, 1024], F32, "wst")
        wd = p.sbuf([128, NFC, D], BF16, "wd")
        load_w_bf16(p, wd, "wd", T["wd"][l], D, st, nkc=NFC)
        pn = PostNorm(p, epsT)
        nctx = NormCtx(p, identb, epsT)
        xs = RR(p, 2, [128, 4, D], F32, "xs")
        hT = RR(p, 1, [128, 8, 512], BF16, "hT")
        actT = p.sbuf([128, NFC, 512], BF16, "actT")
        wgu = RR(p, 2, [128, 2, 8, 256], BF16, "wgu")
        sg = RR(p, 2, [128, 512], F32, "sg")
        pso = RR(p, 4, [128, 512], F32, "pso", psum=True)
        for u in range(9):
            t0 = u * 512
            cd = unit_cd(u)
            xt, xk = xs.next()
            p.dma(xt[:], xview(X, t0), ["X%d" % u], [xk], eng="act")
            h, hk = hT.next()
            nctx.run(xt, xk, modA2, modB2, cd, h, hk)
            for pc in range(11):
                w, wk = wgu.next()
                p.dma(w[:, 0], WG.rearrange("(kc q) f -> q kc f", q=128)[:, :, pc * 256:(pc + 1) * 256], ["wscr"], [wk], eng="sp")
                p.dma(w[:, 1], WU.rearrange("(kc q) f -> q kc f", q=128)[:, :, pc * 256:(pc + 1) * 256], ["wscr"], [wk], eng="sp")
                for f2 in range(2):
                    fc = pc * 2 + f2
                    pg, kg = pso.next()
                    pu, ku = pso.next()
                    for kc in range(8):
                        mm(p, pg[:], w[:, 0, kc, f2 * 128:(f2 + 1) * 128], h[:, kc, :], kc == 0, kc == 7, [wk, hk], [kg])
                    for kc in range(8):
                        mm(p, pu[:], w[:, 1, kc, f2 * 128:(f2 + 1) * 128], h[:, kc, :], kc == 0, kc == 7, [wk, hk], [ku])
                    s, sk = sg.next()
                    act(p, s[:], pg[:], AF.Silu, [kg], [sk])
                    tt(p, "dve", actT[:, fc, :], pu[:], s[:], ALU.mult, [ku, sk], ["actT"])
            for i in range(4):
                po0, k0 = pso.next()
                po1, k1 = pso.next()
                for half, (po, pk) in enumerate(((po0, k0), (po1, k1))):
                    for fc in range(NFC):
                        mm(p, po[:], actT[:, fc, i * 128:(i + 1) * 128], wd[:, fc, half * 512:(half + 1) * 512], fc == 0, fc == NFC - 1,
                           ["actT", "wd"], [pk])
                pn.run(po0, k0, po1, k1, xt[:, i, :], xk, ggbc2[cd])
            p.dma(xview(X, t0), xt[:], [xk], ["X%d" % u], eng="sp")


def build_even_proj(p, T, l, li, identb, identf, epsT, modA1, modB1):
    X = T["X"]
    QAT, KAT, VA, QBT, KBT, VB = T["QAT"], T["KAT"], T["VA"], T["QBT"], T["KBT"], T["VB"]
    with p.phase():
        st = RR(p, 2, [128, 1024], F32, "wst")
        win = p.sbuf([128, 8, 3072], BF16, "win")
        wrot = p.sbuf([128, 8, 1024], BF16, "wrot")
        load_w_bf16(p, win, "win", T["w_in_e"][li], 3072, st)
        for (s0, d0) in ((1536, 0), (2048, 512)):
            sv = win[:, :, s0:s0 + 512].rearrange("q k (b two s) -> q k b two s", b=16, two=2, s=16)
            dv = wrot[:, :, d0:d0 + 512].rearrange("q k (b two s) -> q k b two s", b=16, two=2, s=16)
            ts(p, "dve", dv[:, :, :, 0, :], sv[:, :, :, 1, :], -1.0, None, ALU.mult, None, ["win"], ["wrot"])
            cp(p, "pool", dv[:, :, :, 1, :], sv[:, :, :, 0, :], ["win"], ["wrot"])
        nctx = NormCtx(p, identb, epsT)
        xs = RR(p, 1, [128, 4, D], F32, "xs")
        hT = RR(p, 2, [128, 8, 512], BF16, "hT")
        rcs = RR(p, 2, [128, 2, 512], F32, "rcs")
        obf = RR(p, 4, [128, 512], BF16, "obf")
        o32 = RR(p, 2, [128, 512], F32, "o32")
        t12 = RR(p, 4, [128, 512], F32, "t12")
        pso = RR(p, 5, [128, 512], F32, "pso", psum=True)
        pst = nctx.pst
        ctile = RR(p, 2, [128, 512], F32, "ctile")
        cb16 = RR(p, 2, [128, 512], BF16, "cb16")
        ktmp = RR(p, 2, [128, 4, 128], BF16, "ktmp")
        for (ck, cv, KTd, Vd) in ((T["cak"], T["cav"], KAT, VA), (T["cbk"], T["cbv"], KBT, VB)):
            for i in range(4):
                a, ak = ctile.next()
                p.dma(a[:], ck[li, i * 128:(i + 1) * 128, :], (), [ak], eng="act")
                b, bk = cb16.next()
                cp(p, "dve", b[:], a[:], [ak], [bk])
                kt, kk = ktmp.next()
                ps, pk = pst.next()
                for c in range(4):
                    trp(p, ps[:, c * 128:(c + 1) * 128], b[:, c * 128:(c + 1) * 128], identb[:], [bk, "identb"], [pk])
                cp(p, "act", kt[:].rearrange("q c t -> q (c t)"), ps[:, 0:512], [pk], [kk])
                p.dma(KTd[:, NS + i * 128:NS + (i + 1) * 128].rearrange("(c q) t -> q c t", q=128), kt[:], [kk], ["KT"])
                a, ak = ctile.next()
                p.dma(a[:], cv[li, i * 128:(i + 1) * 128, :], (), [ak], eng="act")
                b, bk = cb16.next()
                cp(p, "pool", b[:], a[:], [ak], [bk])
                p.dma(Vd[NS + i * 128:NS + (i + 1) * 128, :], b[:], [bk], ["V"])

        def fm_group(W, col0, h, hk, n):
            ps, pk = pso.next()
            for kc in range(8):
                mm(p, ps[:, 0:n], W[:, kc, col0:col0 + 128], h[:, kc, 0:n], kc == 0, kc == 7, ["win", "wrot", hk], [pk])
            return ps, pk

        for u in range(9):
            t0 = u * 512
            cd = unit_cd(u)
            xt, xk = xs.next()
            p.dma(xt[:], xview(X, t0), ["X%d" % u], [xk], eng="act")
            h, hk = hT.next()
            nctx.run(xt, xk, modA1, modB1, cd, h, hk)
            dcol = t0 if cd == 0 else NT
            if cd == 0:
                rc, rk = rcs.next()
                p.dma(rc[:, 0, :], T["ropeC"][:, t0:t0 + 512], (), [rk])
                p.dma(rc[:, 1, :], T["ropeS"][:, t0:t0 + 512], (), [rk])
            k = 0
            for (dst, wc0, rot0, dkey) in ((QAT, 0, None, "QT"), (KAT, 512, None, "KT"), (QBT, 1536, 0, "QT"), (KBT, 2048, 512, "KT")):
                for c in range(4):
                    ps, pk = fm_group(win, wc0 + c * 128, h, hk, 512)
                    o, ok = obf.next()
                    if cd == 0 and rot0 is not None:
                        ps2, pk2 = fm_group(wrot, rot0 + c * 128, h, hk, 512)
                        a, ak = t12.next()
                        b, bk = t12.next()
                        tt(p, "dve", a[:], ps[:], rc[:, 0, :], ALU.mult, [pk, rk], [ak])
                        tt(p, "dve", b[:], ps2[:], rc[:, 1, :], ALU.mult, [pk2, rk], [bk])
                        tt(p, "pool", o[:], a[:], b[:], ALU.add, [ak, bk], [ok])
                    else:
                        cp(p, "act" if k % 2 else "dve", o[:], ps[:], [pk], [ok])
                    k += 1
                    p.dma(dst[c * 128:(c + 1) * 128, dcol:dcol + 512], o[:], [ok], [dkey])
            for i in range(4):
                for (wc0, Vd, outk, outv) in ((512, None, "o_ak", None), (1024, VA, None, "o_av"), (2048, None, "o_bk", None), (2560, VB, None, "o_bv")):
                    if cd == 0 and Vd is None:
                        continue
                    ps, pk = pso.next()
                    for kc in range(8):
                        mm(p, ps[:], h[:, kc, i * 128:(i + 1) * 128], win[:, kc, wc0:wc0 + 512], kc == 0, kc == 7, ["win", hk], [pk])
                    if Vd is not None:
                        o, ok = obf.next()
                        cp(p, "act", o[:], ps[:], [pk], [ok])
                        p.dma(Vd[dcol + i * 128:dcol + (i + 1) * 128, :], o[:], [ok], ["V"])
                    if cd == 1:
                        o2, ok2 = o32.next()
                        cp(p, "act", o2[:], ps[:], [pk], [ok2])
                        nm = outk or outv
                        p.dma(T[nm][i // 2, li, (i % 2) * 128:(i % 2 + 1) * 128, :], o2[:], [ok2], [nm + str(li)])


class AttnCtx:
    def __init__(self, p):
        self.p = p
        self.pss = RR(p, 3, [128, 512], F32, "pss", psum=True)
        self.pacc = RR(p, 4, [128, 512], F32, "pacc", psum=True)
        self.E = RR(p, 3, [128, 512], BF16, "E")
        self.small = RR(p, 8, [128, 4], F32, "asmall")


def attn_block(p, C, KT, ktkey, QT, qkey, nq, Vaug, vkey, nkt, dv, finish):
    nqt = (nq + 127) // 128
    accs = [C.pacc.next() for _ in range(nqt)]

    def qk(kt):
        ps, pk = C.pss.next()
        mm(p, ps[:, 0:nq], KT[:, kt * 128:(kt + 1) * 128], QT[:, 0:nq], True, True, [ktkey, qkey], [pk])
        e, ek = C.E.next()
        act(p, e[:, 0:nq], ps[:, 0:nq], AF.Exp, [pk], [ek], scale=0.125)
        return e, ek

    pend = qk(0)
    for kt in range(nkt):
        nxt = qk(kt + 1) if kt + 1 < nkt else None
        e, ek = pend
        for i in range(nqt):
            mm(p, accs[i][0][:, 0:dv + 1], e[:, i * 128:(i + 1) * 128], Vaug[:, kt, :], kt == 0, kt == nkt - 1,
               [ek, vkey], [accs[i][1]])
        pend = nxt
    for i in range(nqt):
        finish(i, accs[i][0], accs[i][1])


def build_even_attn(p, T, l, li, lam_init):
    QAT, KAT, VA, QBT, KBT, VB = T["QAT"], T["KAT"], T["VA"], T["QBT"], T["KBT"], T["VB"]
    MIXT, RPBP = T["MIXT"], T["RPBP"]
    with p.phase():
        C = AttnCtx(p)
        lamt = p.sbuf([128, 4, 64], F32, "lamt")
        lsm = p.sbuf([128, 8], F32, "lsm")
        gsb = p.sbuf([128, 128], F32, "gsb")
        lj = p.sbuf([128, 64], F32, "lj")
        p.dma(lamt[:].rearrange("q a b -> q (a b)"), T["lamv"][li:li + 1].rearrange("o a b -> o (a b)").to_broadcast([128, 256]), (), ["lamt"])
        p.dma(gsb[:], T["gsub"][li:li + 1, :].to_broadcast([128, 128]), (), ["gsb"])
        for j in range(2):
            tt(p, "dve", lj[:], lamt[:, 2 * j, :], lamt[:, 2 * j + 1, :], ALU.mult, ["lamt"], ["lj"])
            act(p, lj[:], lj[:], AF.Identity, ["lj"], ["lj", "lsm"], accum=lsm[:, j:j + 1])
        act(p, lsm[:, 2:4], lsm[:, 0:2], AF.Exp, ["lsm"], ["lsm"])
        tt(p, "dve", lsm[:, 4:5], lsm[:, 3:4], lsm[:, 2:3], ALU.subtract, ["lsm"], ["lsm"])
        ts(p, "dve", lsm[:, 5:6], lsm[:, 4:5], -lam_init, None, ALU.add, None, ["lsm"], ["lsm"])
        ts(p, "dve", gsb[:], gsb[:], 1.0 - lam_init, None, ALU.mult, None, ["gsb"], ["gsb"])
        neglam = lsm[:, 5:6]

        om = [p.sbuf([128, 4, 128], F32, "om%d" % s) for s in range(2)]
        ocomb = RR(p, 2, [128, 128], F32, "ocomb")
        ojunk = RR(p, 2, [128, 128], F32, "ojunk")

        def diff_finish(s):
            def f(i, acc, ak):
                sm, sk = C.small.next()
                recip(p, sm[:, 0:1], acc[:, 128:129], [ak], [sk])
                ts(p, "dve", om[s][:, i, :], acc[:, 0:128], sm[:, 0:1], None, ALU.mult, None, [ak, sk], ["om%d" % s])
            return f

        def diff_combine(i, dst, dkey):
            o, ok = ocomb.next()
            stt(p, o[:], om[1][:, i, :], neglam, om[0][:, i, :], ALU.mult, ALU.add, ["om0", "om1", "lsm"], [ok])
            sm, sk = C.small.next()
            j, jk = ojunk.next()
            act(p, j[:], o[:], AF.Square, [ok], [jk, sk], accum=sm[:, 0:1])
            act(p, sm[:, 1:2], sm[:, 0:1], AF.Sqrt, [sk], [sk], scale=1.0 / 128, bias=EPS_T[0][:])
            recip(p, sm[:, 2:3], sm[:, 1:2], [sk], [sk])
            stt(p, dst, o[:], sm[:, 2:3], gsb[:], ALU.mult, ALU.mult, [ok, sk, "gsb"], [dkey])

        ktb = RR(p, 2, [64, NT], BF16, "ktb")
        vaug = RR(p, 2, [128, 36, 129], BF16, "vaug")
        qtb = RR(p, 2, [64, 512], BF16, "qtb")
        obuf = RR(p, 2, [128, 4, 128], BF16, "obufB")
        for h in range(4):
            v, vk = vaug.next()
            mset(p, "pool", v[:], 1.0, [vk])
            for t9 in range(4):
                p.dma(v[:, t9 * 9:(t9 + 1) * 9, 0:128],
                      VB[t9 * 1152:(t9 + 1) * 1152, h * 128:(h + 1) * 128].rearrange("(t q) d -> q t d", q=128), ["V"], [vk],
                      eng=("act" if t9 % 2 else "sp"))
            kts = []
            for s in range(2):
                kt_, kk = ktb.next()
                p.dma(kt_[:], KBT[h * 128 + s * 64:h * 128 + (s + 1) * 64, 0:NT], ["KT"], [kk], eng="act")
                kts.append((kt_, kk))
            for qb in range(8):
                for s in range(2):
                    q, qk = qtb.next()
                    p.dma(q[:], QBT[h * 128 + s * 64:h * 128 + (s + 1) * 64, qb * 512:(qb + 1) * 512], ["QT"], [qk])
                    attn_block(p, C, kts[s][0], kts[s][1], q, qk, 512, v, vk, 36, 128, diff_finish(s))
                ob, obk = obuf.next()
                for i in range(4):
                    diff_combine(i, ob[:, i, :], obk)
                p.dma(MIXT[qb * 512:(qb + 1) * 512, 512 + h * 128:512 + (h + 1) * 128].rearrange("(i q) c -> q i c", q=128), ob[:],
                      [obk], ["MIXT"])

        ktp = RR(p, 2, [64, 256], BF16, "ktp")
        qtp = RR(p, 2, [64, 256], BF16, "qtp")
        vap = RR(p, 2, [128, 2, 65], BF16, "vap")
        vbp = RR(p, 2, [128, 2, 129], BF16, "vbp")
        obp = RR(p, 2, [128, 2, D], BF16, "obp")
        for js in range(2):
            tb = NT + 256 * js
            ob, obk = obp.next()
            for h in range(8):
                v, vk = vap.next()
                mset(p, "pool", v[:], 1.0, [vk])
                p.dma(v[:, :, 0:64], VA[tb:tb + 256, h * 64:(h + 1) * 64].rearrange("(t q) d -> q t d", q=128), ["V"], [vk], eng="act")
                kt_, kk = ktp.next()
                p.dma(kt_[:], KAT[h * 64:(h + 1) * 64, tb:tb + 256], ["KT"], [kk], eng="act")
                q, qk = qtp.next()
                p.dma(q[:], QAT[h * 64:(h + 1) * 64, tb:tb + 256], ["QT"], [qk])

                def fin_a(i, acc, ak, h=h, ob=ob, obk=obk):
                    sm, sk = C.small.next()
                    recip(p, sm[:, 0:1], acc[:, 64:65], [ak], [sk])
                    ts(p, "dve", ob[:, i, h * 64:(h + 1) * 64], acc[:, 0:64], sm[:, 0:1], None, ALU.mult, None, [ak, sk], [obk])
                attn_block(p, C, kt_, kk, q, qk, 256, v, vk, 2, 64, fin_a)
            for h in range(4):
                v, vk = vbp.next()
                mset(p, "pool", v[:], 1.0, [vk])
                p.dma(v[:, :, 0:128], VB[tb:tb + 256, h * 128:(h + 1) * 128].rearrange("(t q) d -> q t d", q=128), ["V"], [vk], eng="act")
                for s in range(2):
                    kt_, kk = ktp.next()
                    p.dma(kt_[:], KBT[h * 128 + s * 64:h * 128 + (s + 1) * 64, tb:tb + 256], ["KT"], [kk], eng="act")
                    q, qk = qtp.next()
                    p.dma(q[:], QBT[h * 128 + s * 64:h * 128 + (s + 1) * 64, tb:tb + 256], ["QT"], [qk])
                    attn_block(p, C, kt_, kk, q, qk, 256, v, vk, 2, 128, diff_finish(s))
                for i in range(2):
                    diff_combine(i, ob[:, i, 512 + h * 128:512 + (h + 1) * 128], obk)
            p.dma(MIXT[NS + 256 * js:NS + 256 * (js + 1), :].rearrange("(i q) c -> q i c", q=128), ob[:], [obk], ["MIXT"])

        zt = p.sbuf([120, 288], F32, "zt")
        mset(p, "dve", zt[:], 0.0, ["zt"])
        p.dma(RPBP.rearrange("h r c -> (h r) c"), zt[:], ["zt"], ["RPBP"])
        p.dma(RPBP[:, :, 128:159], T["rpb"][li], (), ["RPBP"])
        wm = p.sbuf([128, 64], F32, "wm")
        p.dma(wm[:], T["winmask"], (), ["wm"])
        mraw = RR(p, 2, [128, 32, 64], F32, "mraw")
        mh = RR(p, 2, [128, 32, 64], BF16, "mh")
        kta = RR(p, 2, [64, NT], BF16, "kta")
        qta = RR(p, 2, [64, NS], BF16, "qta")
        va_e = RR(p, 2, [128, 36, 65], BF16, "va_e")
        va_o = RR(p, 2, [128, 35, 65], BF16, "va_o")
        oba = RR(p, 2, [64, 64, 64], BF16, "oba")
        En = RR(p, 3, [128, 8, 64], BF16, "En")
        for h in range(8):
            mr, mrk = mraw.next()
            kq = 0
            for off in range(8):
                for j in range(4):
                    for mlow in range(2):
                        dr = 2 * j + mlow + off
                        base = RPBP[h, dr, 80:144]
                        src = bass.AP(base.tensor, base.offset, [[1, 64], [1, 64]])
                        p.dma(mr[mlow * 64:(mlow + 1) * 64, off * 4 + j, :], src, ["RPBP"], [mrk], eng=("sp" if kq % 2 else "act"))
                        kq += 1
            act(p, mr[:], mr[:], AF.Exp, [mrk], [mrk])
            m_, mk_ = mh.next()
            tt(p, "pool", m_[:], mr[:], wm[:].unsqueeze(1).to_broadcast([128, 32, 64]), ALU.mult, [mrk, "wm"], [mk_])
            kt_, kk = kta.next()
            p.dma(kt_[:], KAT[h * 64:(h + 1) * 64, 0:NT], ["KT"], [kk], eng="act")
            q, qk = qta.next()
            p.dma(q[:], QAT[h * 64:(h + 1) * 64, 0:NS], ["QT"], [qk], eng="act")
            ve, vek = va_e.next()
            mset(p, "pool", ve[:], 1.0, [vek])
            for t9 in range(4):
                p.dma(ve[:, t9 * 9:(t9 + 1) * 9, 0:64],
                      VA[t9 * 1152:(t9 + 1) * 1152, h * 64:(h + 1) * 64].rearrange("(t q) d -> q t d", q=128), ["V"], [vek],
                      eng=("act" if t9 % 2 else "sp"))
            vo, vok = va_o.next()
            mset(p, "pool", vo[:], 1.0, [vok])
            for t9 in range(5):
                p.dma(vo[:, t9 * 7:(t9 + 1) * 7, 0:64],
                      VA[64 + t9 * 896:64 + (t9 + 1) * 896, h * 64:(h + 1) * 64].rearrange("(t q) d -> q t d", q=128), ["V"], [vok],
                      eng=("act" if t9 % 2 else "sp"))
            ob, obk = oba.next()
            def na_stage1(r):
                rs = min(max(r - 4, 0), 56)
                off = rs - r + 7
                ps, pk = C.pss.next()
                psv = ps[:].rearrange("q (j c) -> q j c", c=64)
                for j in range(4):
                    k0 = rs * 64 + 128 * j
                    mm(p, psv[:, j, :], kt_[:, k0:k0 + 128], q[:, r * 64:(r + 1) * 64], True, True, [kk, qk], [pk])
                for j in range(4):
                    k0 = NS + 128 * j
                    mm(p, psv[:, 4 + j, :], kt_[:, k0:k0 + 128], q[:, r * 64:(r + 1) * 64], True, True, [kk, qk], [pk])
                e, ek = En.next()
                act(p, e[:], psv, AF.Exp, [pk], [ek], scale=0.125)
                tt(p, "dve", e[:, 0:4, :], e[:, 0:4, :], m_[:, off * 4:off * 4 + 4, ::-1], ALU.mult, [ek, mk_], [ek])
                return e, ek

            def na_stage2(r, e, ek):
                rs = min(max(r - 4, 0), 56)
                acc, ak = C.pacc.next()
                for j in range(8):
                    if j < 4:
                        if rs % 2 == 0:
                            vt = ve[:, rs // 2 + j, :]
                            vkk = vek
                        else:
                            vt = vo[:, (rs - 1) // 2 + j, :]
                            vkk = vok
                    else:
                        vt = ve[:, 32 + (j - 4), :]
                        vkk = vek
                    mm(p, acc[0:64, 0:65], e[:, j, :], vt, j == 0, j == 7, [ek, vkk], [ak])
                sm, sk = C.small.next()
                recip(p, sm[0:64, 0:1], acc[0:64, 64:65], [ak], [sk])
                ts(p, "dve", ob[:, r, :], acc[0:64, 0:64], sm[0:64, 0:1], None, ALU.mult, None, [ak, sk], [obk])

            pend = na_stage1(0)
            for r in range(64):
                nxt = na_stage1(r + 1) if r + 1 < 64 else None
                na_stage2(r, pend[0], pend[1])
                pend = nxt
            for r4 in range(4):
                p.dma(MIXT[r4 * 1024:(r4 + 1) * 1024, h * 64:(h + 1) * 64].rearrange("(r q) d -> q r d", q=64), ob[:, r4 * 16:(r4 + 1) * 16, :],
                      [obk], ["MIXT"])


EPS_T = [None]


def _fm(v, nch):
    v = np.asarray(v, np.float32)
    return np.ascontiguousarray(np.swapaxes(v.reshape(v.shape[:-1] + (nch, 128)), -1, -2))


def _pairs(a):
    a = np.asarray(a, np.float32)
    sh = a.shape[:-2]
    a = a.reshape(sh + (16, 2, 64))
    a = np.moveaxis(a, -3, -1)
    return np.ascontiguousarray(a.reshape(sh + (128, 16)))


def shared_inputs(inp):
    f = lambda a: np.ascontiguousarray(np.asarray(a, np.float32))
    S = {}
    S["w_mod"] = f(inp["w_mod"])
    S["b_mod"] = _fm(inp["b_mod"], 48)
    S["gpre1"] = _fm(inp["g_mix_pre"], 8); S["gpost1"] = _fm(inp["g_mix_post"], 8)
    S["gpre2"] = _fm(inp["g_ffn_pre"], 8); S["gpost2"] = _fm(inp["g_ffn_post"], 8)
    S["wg"] = f(inp["w_ffn_gate"]); S["wu"] = f(inp["w_ffn_up"]); S["wd"] = f(inp["w_ffn_down"])
    S["w_in_e"] = f(inp["w_in_e"]); S["w_out_e"] = f(inp["w_out_e"])
    S["rpb"] = f(inp["na_rpb"])
    S["lamv"] = np.ascontiguousarray(np.stack([f(inp["lam_q1"]), f(inp["lam_k1"]), f(inp["lam_q2"]), f(inp["lam_k2"])], axis=1))
    S["gsub"] = f(inp["g_subln"])
    S["w_in_o"] = f(inp["w_in_o"]); S["w_out_o"] = f(inp["w_out_o"])
    S["s_lre"] = _pairs(inp["ssm_lam_re"]); S["s_lim"] = _pairs(inp["ssm_lam_im"])
    ldt = np.asarray(inp["ssm_log_dt"], np.float32)
    S["s_ldt"] = _pairs(np.repeat(ldt[..., None], 64, axis=-1))
    for nm, src in (("s_bre", "ssm_b_re"), ("s_bim", "ssm_b_im")):
        b = np.asarray(inp[src], np.float32)
        b = np.moveaxis(b, -1, 2)
        b = _pairs(b)
        S[nm] = np.ascontiguousarray(np.moveaxis(b, 2, -1).reshape(2, 2, 128, 256))
    for nm, src in (("s_cre", "ssm_c_re"), ("s_cim", "ssm_c_im")):
        c = np.asarray(inp[src], np.float32)
        c = np.moveaxis(c, 3, 2)
        c = _pairs(c)
        S[nm] = np.ascontiguousarray(np.moveaxis(c, 2, -1).reshape(2, 2, 128, 256))
    S["s_d"] = _fm(inp["ssm_d"], 4)
    S["w_glu"] = f(inp["w_glu"]); S["b_glu"] = _fm(inp["b_glu"], 4)
    cw = np.asarray(inp["hy_conv_w"], np.float32)
    S["hcw"] = np.ascontiguousarray(np.moveaxis(_fm(cw, 12), 1, -1))
    S["hcb"] = _fm(inp["hy_conv_b"], 12)
    S["hw1"] = f(inp["hy_w1"]); S["hb1"] = f(inp["hy_b1"])[..., None]; S["hf1"] = f(inp["hy_fr1"])[..., None]
    S["hw2"] = f(inp["hy_w2"]); S["hb2"] = f(inp["hy_b2"])[..., None]; S["hf2"] = f(inp["hy_fr2"])[..., None]
    S["hw3"] = f(inp["hy_w3"])
    S["hdec"] = np.ascontiguousarray(np.asarray(inp["hy_decay"], np.float32).reshape(2, 4, 512))
    S["hbias"] = f(inp["hy_bias"])
    S.update(consts())
    return S


def core_inputs(inp, cid, S):
    m = dict(S)
    xs = np.asarray(inp["x_sample"][cid], np.float32)
    xp = np.asarray(inp["x_prompt"][2 * cid:2 * cid + 2], np.float32).reshape(512, D)
    m["x0"] = np.ascontiguousarray(np.concatenate([xs, xp], 0))
    for nm, src in (("cak", "cache_a_k"), ("cav", "cache_a_v"), ("cbk", "cache_b_k"), ("cbv", "cache_b_v")):
        m[nm] = np.ascontiguousarray(np.asarray(inp[src][cid], np.float32).reshape(2, 512, 512))
    m["sre"] = _pairs(inp["state_c_re"][cid]); m["sim"] = _pairs(inp["state_c_im"][cid])
    cc = np.stack([np.asarray(inp["c"][cid], np.float32), np.asarray(inp["c_ctx"], np.float32)], -1)
    m["cond"] = np.ascontiguousarray(cc.reshape(8, 128, 2).transpose(1, 0, 2))
    return m


_PROG = {}


def run_device(inputs, nlayers=DEPTH, cores=8):
    if nlayers not in _PROG:
        _PROG[nlayers] = build_program(nlayers)
    nc = _PROG[nlayers]
    S = shared_inputs(inputs)
    in_maps = []
    for cid in range(cores):
        m = core_inputs(inputs, cid, S)
        in_maps.append({k: m[k] for k in IN_SPECS})
    res = run_bass_kernel_spmd(nc, in_maps, core_ids=list(range(cores)))
    return res.results


def kernel(**inputs):
    R = run_device(inputs)
    yp = np.concatenate([r["o_yp"].reshape(2, 256, D) for r in R], 0)
    ys = np.stack([r["o_ys"] for r in R], 0)
    outs = [yp.astype(np.float32), ys.astype(np.float32)]
    for nm, (hh, dd) in (("o_ak", (8, 64)), ("o_av", (8, 64)), ("o_bk", (4, 128)), ("o_bv", (4, 128))):
        a = np.concatenate([r[nm] for r in R], 0)
        outs.append(np.ascontiguousarray(a.reshape(16, 2, 256, hh, dd)).astype(np.float32))
    for nm in ("o_sre", "o_sim"):
        a = np.concatenate([r[nm] for r in R], 0)
        outs.append(np.ascontiguousarray(a.reshape(16, 2, 2, 32, 64)).astype(np.float32))
    return tuple(outs)


def build_odd_proj(p, T, l, li, identb, epsT, modA1, modB1):
    X = T["X"]
    UT32, UTB, HYT = T["UT32"], T["UTB"], T["HYT"]
    with p.phase():
        st = RR(p, 2, [128, 1024], F32, "wst")
        win = p.sbuf([128, 8, 2048], BF16, "win")
        load_w_bf16(p, win, "win", T["w_in_o"][li], 2048, st)
        nctx = NormCtx(p, identb, epsT)
        xs = RR(p, 2, [128, 4, D], F32, "xs")
        hT = RR(p, 2, [128, 8, 512], BF16, "hT")
        o32 = RR(p, 3, [128, 512], F32, "o32")
        obf = RR(p, 2, [128, 512], BF16, "obf")
        pso = RR(p, 4, [128, 512], F32, "pso", psum=True)
        for u in range(9):
            t0 = u * 512
            cd = unit_cd(u)
            xt, xk = xs.next()
            p.dma(xt[:], xview(X, t0), ["X%d" % u], [xk], eng="act")
            h, hk = hT.next()
            nctx.run(xt, xk, modA1, modB1, cd, h, hk)
            for c in range(16):
                ps, pk = pso.next()
                for kc in range(8):
                    mm(p, ps[:], win[:, kc, c * 128:(c + 1) * 128], h[:, kc, :], kc == 0, kc == 7, ["win", hk], [pk])
                o, ok = o32.next()
                cp(p, "act" if c % 2 else "dve", o[:], ps[:], [pk], [ok])
                if c < 4:
                    p.dma(UT32[c * 128:(c + 1) * 128, t0:t0 + 512], o[:], [ok], ["UT32"])
                    b, bk = obf.next()
                    cp(p, "pool", b[:], o[:], [ok], [bk])
                    p.dma(UTB[c * 128:(c + 1) * 128, t0:t0 + 512], b[:], [bk], ["UTB"])
                else:
                    p.dma(HYT[(c - 4) * 128:(c - 3) * 128, t0:t0 + 512], o[:], [ok], ["HYT"])


def sin_turns(p, eng_i, out, turns, tmp_i, tmp_f, keys_r, keys_w, shift=0.0):
    r = list(keys_r)
    if shift != 0.0:
        ts(p, "dve", tmp_f, turns, shift, None, ALU.add, None, r, keys_w)
        src = tmp_f
    else:
        src = turns
    cp(p, "dve", tmp_i, src, r + list(keys_w), keys_w)
    cp(p, "dve", out, tmp_i, keys_w, keys_w)
    tt(p, "dve", out, src, out, ALU.subtract, r + list(keys_w), keys_w)
    act(p, out, out, AF.Sin, keys_w, keys_w, scale=TWO_PI)


SEQS = ((0, 4096, True), (4096, 256, False), (4352, 256, False))


def build_s5(p, T, l, li, identf):
    UTB, UT32, YS, MIXF = T["UTB"], T["UT32"], T["YS"], T["MIXF"]
    with p.phase():
        iota = p.sbuf([128, 128], F32, "iota")
        p.dma(iota[:], T["iota128"], (), ["iota"])
        prm = p.sbuf([128, 2, 3, 16], F32, "prm")
        wk_ = p.sbuf([128, 2, 12, 16], F32, "wk")
        wki = p.sbuf([128, 16], I32, "wki")
        Bl = p.sbuf([128, 2, 16, 2, 128], BF16, "Bl")
        Cl = p.sbuf([128, 2, 16, 2, 128], BF16, "Cl")
        cosT = p.sbuf([128, 2, 16, 128], F32, "cosT")
        sinT = p.sbuf([128, 2, 16, 128], F32, "sinT")
        rT = p.sbuf([128, 2, 16, 128], F32, "rT")
        bc = RR(p, 2, [128, 2, 16, 16], F32, "bc")
        bb = RR(p, 2, [128, 2, 16, 16], F32, "bb")
        spd = RR(p, 2, [128, 128], F32, "spd")
        ti = p.sbuf([128, 128], I32, "ti")
        tf = p.sbuf([128, 128], F32, "tf")
        ptr = RR(p, 1, [128, 512], F32, "ptr", psum=True)
        pbu = RR(p, 4, [128, 512], F32, "pbu", psum=True)
        pcy = RR(p, 2, [128, 512], F32, "pcy", psum=True)
        for d in range(2):
            for j, nm in enumerate(("s_lre", "s_lim", "s_ldt")):
                p.dma(prm[:, d, j, :], T[nm][li, d], (), ["prm"])
            lre, lim, ldt = prm[:, d, 0, :], prm[:, d, 1, :], prm[:, d, 2, :]
            W = lambda i: wk_[:, d, i, :]
            R, Wk = ["prm", "wk"], ["wk"]
            act(p, W(0), ldt, AF.Exp, R, Wk)
            tt(p, "dve", W(1), lre, W(0), ALU.mult, R, Wk)
            act(p, W(1), W(1), AF.Exp, R, Wk)
            tt(p, "dve", W(2), lim, W(0), ALU.mult, R, Wk)
            ts(p, "dve", W(2), W(2), 1.0 / TWO_PI, None, ALU.mult, None, R, Wk)
            cp(p, "dve", wki[:], W(2), R, ["wki"])
            cp(p, "dve", W(3), wki[:], ["wki"], Wk)
            tt(p, "dve", W(2), W(2), W(3), ALU.subtract, R, Wk)
            act(p, W(4), W(2), AF.Sin, R, Wk, scale=TWO_PI)
            ts(p, "dve", W(3), W(2), 0.25, None, ALU.add, None, R, Wk)
            cp(p, "dve", wki[:], W(3), R, ["wki"])
            cp(p, "dve", W(5), wki[:], ["wki"], Wk)
            tt(p, "dve", W(3), W(3), W(5), ALU.subtract, R, Wk)
            act(p, W(5), W(3), AF.Sin, R, Wk, scale=TWO_PI)
            tt(p, "dve", W(6), W(1), W(5), ALU.mult, R, Wk)
            tt(p, "dve", W(7), W(1), W(4), ALU.mult, R, Wk)
            tt(p, "dve", W(3), lre, lre, ALU.mult, R, Wk)
            tt(p, "dve", W(4), lim, lim, ALU.mult, R, Wk)
            tt(p, "dve", W(3), W(3), W(4), ALU.add, R, Wk)
            recip(p, W(3), W(3), R, Wk)
            ts(p, "dve", W(6), W(6), -1.0, None, ALU.add, None, R, Wk)
            tt(p, "dve", W(4), W(6), lre, ALU.mult, R, Wk)
            tt(p, "dve", W(5), W(7), lim, ALU.mult, R, Wk)
            tt(p, "dve", W(4), W(4), W(5), ALU.add, R, Wk)
            tt(p, "dve", W(8), W(4), W(3), ALU.mult, R, Wk)
            tt(p, "dve", W(4), W(7), lre, ALU.mult, R, Wk)
            tt(p, "dve", W(5), W(6), lim, ALU.mult, R, Wk)
            tt(p, "dve", W(4), W(4), W(5), ALU.subtract, R, Wk)
            tt(p, "dve", W(9), W(4), W(3), ALU.mult, R, Wk)
            b_, bk = bc.next()
            p.dma(b_[:, 0].rearrange("q k h -> q (k h)"), T["s_bre"][li, d], (), [bk])
            p.dma(b_[:, 1].rearrange("q k h -> q (k h)"), T["s_bim"][li, d], (), [bk])
            o_, ok = bb.next()
            kre = W(8).unsqueeze(2).to_broadcast([128, 16, 16])
            kim = W(9).unsqueeze(2).to_broadcast([128, 16, 16])
            t1, t1k = bb.next()
            tt(p, "dve", o_[:, 0], b_[:, 0], kre, ALU.mult, [bk] + R, [ok])
            tt(p, "dve", t1[:, 0], b_[:, 1], kim, ALU.mult, [bk] + R, [t1k])
            tt(p, "dve", o_[:, 0], o_[:, 0], t1[:, 0], ALU.subtract, [t1k], [ok])
            tt(p, "dve", o_[:, 1], b_[:, 1], kre, ALU.mult, [bk] + R, [ok])
            tt(p, "dve", t1[:, 1], b_[:, 0], kim, ALU.mult, [bk] + R, [t1k])
            tt(p, "dve", o_[:, 1], o_[:, 1], t1[:, 1], ALU.add, [t1k], [ok])
            c_, ck = bc.next()
            p.dma(c_[:, 0].rearrange("q k h -> q (k h)"), T["s_cre"][li, d], (), [ck])
            p.dma(c_[:, 1].rearrange("q k h -> q (k h)"), T["s_cim"][li, d], (), [ck])
            mset(p, "pool", Cl[:, d].rearrange("q k r m -> q (k r m)"), 0.0, ["Cl"])
            for k in range(16):
                gl0, gl1 = (2 * k) % 8, (2 * k + 1) % 8
                for ri in range(2):
                    s_, sk = spd.next()
                    mset(p, "pool", s_[:], 0.0, [sk])
                    cp(p, "dve", s_[0:64, gl0 * 16:(gl0 + 1) * 16], o_[0:64, ri, k, :], [ok], [sk])
                    cp(p, "dve", s_[64:128, gl1 * 16:(gl1 + 1) * 16], o_[64:128, ri, k, :], [ok], [sk])
                    ps, pk = ptr.next()
                    trp(p, ps[:, 0:128], s_[:], identf[:], [sk, "identf"], [pk])
                    cp(p, "act", Bl[:, d, k, ri, :], ps[:, 0:128], [pk], ["Bl"])
                    if ri == 0:
                        cp(p, "dve", Cl[0:64, d, k, 0, gl0 * 16:(gl0 + 1) * 16], c_[0:64, 0, k, :], [ck, "Cl"], ["Cl"])
                        cp(p, "dve", Cl[64:128, d, k, 0, gl1 * 16:(gl1 + 1) * 16], c_[64:128, 0, k, :], [ck, "Cl"], ["Cl"])
                    else:
                        ts(p, "dve", Cl[0:64, d, k, 1, gl0 * 16:(gl0 + 1) * 16], c_[0:64, 1, k, :], -1.0, None, ALU.mult, None, [ck, "Cl"], ["Cl"])
                        ts(p, "dve", Cl[64:128, d, k, 1, gl1 * 16:(gl1 + 1) * 16], c_[64:128, 1, k, :], -1.0, None, ALU.mult, None, [ck, "Cl"], ["Cl"])
                ts(p, "dve", tf[:], iota[:], wk_[:, d, 2, k:k + 1], None, ALU.mult, None, ["iota"] + R, ["tf"])
                cp(p, "dve", ti[:], tf[:], ["tf"], ["ti"])
                cp(p, "dve", sinT[:, d, k, :], ti[:], ["ti"], ["tab"])
                tt(p, "dve", tf[:], tf[:], sinT[:, d, k, :], ALU.subtract, ["tab"], ["tf"])
                act(p, sinT[:, d, k, :], tf[:], AF.Sin, ["tf"], ["tab"], scale=TWO_PI)
                ts(p, "dve", tf[:], tf[:], 0.25, None, ALU.add, None, ["tf"], ["tf"])
                cp(p, "dve", ti[:], tf[:], ["tf"], ["ti"])
                cp(p, "dve", cosT[:, d, k, :], ti[:], ["ti"], ["tab"])
                tt(p, "dve", tf[:], tf[:], cosT[:, d, k, :], ALU.subtract, ["tab"], ["tf"])
                act(p, cosT[:, d, k, :], tf[:], AF.Sin, ["tf"], ["tab"], scale=TWO_PI)
                cp(p, "pool", rT[:, d, k, :], wk_[:, d, 1, k:k + 1].to_broadcast([128, 128]), R, ["tab"])

        ut = RR(p, 2, [128, 4, 512], BF16, "ut")
        xre = p.sbuf([128, 16, 512], BF16, "xre")
        xim = p.sbuf([128, 16, 512], BF16, "xim")
        m4 = RR(p, 8, [128, 4, 128], F32, "m4")
        wz = RR(p, 8, [128, 4, 128], F32, "wz")
        carry = p.sbuf([128, 2, 16], F32, "carry")
        cst = RR(p, 4, [128, 2], F32, "cst")
        o32 = RR(p, 2, [128, 512], F32, "o32")
        fst = RR(p, 2, [16, 128], F32, "fst")
        for si, (toff, L, has_init) in enumerate(SEQS):
            bs = min(512, L)
            nblk = L // bs
            nch = bs // 128
            for d in range(2):
                if has_init:
                    p.dma(carry[:, 0, :], T["sre"][li, d], (), ["carry"])
                    p.dma(carry[:, 1, :], T["sim"][li, d], (), ["carry"])
                else:
                    mset(p, "dve", carry[:].rearrange("q a k -> q (a k)"), 0.0, ["carry"])
                blks = range(nblk) if d == 0 else range(nblk - 1, -1, -1)
                for blk in blks:
                    c0 = toff + blk * bs
                    u_, uk = ut.next()
                    p.dma(u_[:, :, 0:bs], UTB[:, c0:c0 + bs].rearrange("(c q) t -> q c t", q=128), ["UTB"], [uk])
                    for k in range(16):
                        pr, prk = pbu.next()
                        pi_, pik = pbu.next()
                        mm(p, pr[:, 0:bs], Bl[:, d, k, 0, :], u_[:, k // 4, 0:bs], True, True, ["Bl", uk], [prk])
                        mm(p, pi_[:, 0:bs], Bl[:, d, k, 1, :], u_[:, k // 4, 0:bs], True, True, ["Bl", uk], [pik])
                        cs_, sn_, r_ = cosT[:, d, k, :], sinT[:, d, k, :], rT[:, d, k, :]
                        csb = cs_.unsqueeze(1).to_broadcast([128, nch, 128])
                        snb = sn_.unsqueeze(1).to_broadcast([128, nch, 128])

                        def V3(a, d=d):
                            v = a.rearrange("q (c j) -> q c j", j=128)
                            return v if d == 0 else v[:, :, ::-1]
                        bre, bim = V3(pr[:, 0:bs]), V3(pi_[:, 0:bs])
                        a1, a1k = m4.next(); a2, a2k = m4.next(); a3, a3k = m4.next(); a4, a4k = m4.next()
                        tt(p, "dve", a1[:, 0:nch, :], bre, csb, ALU.mult, [prk, "tab"], [a1k])
                        tt(p, "dve", a2[:, 0:nch, :], bim, snb, ALU.mult, [pik, "tab"], [a2k])
                        tt(p, "dve", a3[:, 0:nch, :], bim, csb, ALU.mult, [pik, "tab"], [a3k])
                        tt(p, "dve", a4[:, 0:nch, :], bre, snb, ALU.mult, [prk, "tab"], [a4k])
                        wr, wrk = wz.next(); wi, wik = wz.next()
                        tt(p, "pool", wr[:, 0:nch, :], a1[:, 0:nch, :], a2[:, 0:nch, :], ALU.add, [a1k, a2k], [wrk])
                        tt(p, "pool", wi[:, 0:nch, :], a3[:, 0:nch, :], a4[:, 0:nch, :], ALU.subtract, [a3k, a4k], [wik])
                        zr, zrk = wz.next(); zi, zik = wz.next()
                        chs = range(nch) if d == 0 else range(nch - 1, -1, -1)
                        for ch in chs:
                            p.op("dve", lambda e, zr=zr, wr=wr, r_=r_, k=k, ch=ch: e.tensor_tensor_scan(out=zr[:, ch, :], data0=r_, data1=wr[:, ch, :], initial=carry[:, 0, k:k + 1], op0=ALU.mult, op1=ALU.add),
                                 [wrk, "tab", "carry"], [zrk])
                            p.op("dve", lambda e, zi=zi, wi=wi, r_=r_, k=k, ch=ch: e.tensor_tensor_scan(out=zi[:, ch, :], data0=r_, data1=wi[:, ch, :], initial=carry[:, 1, k:k + 1], op0=ALU.mult, op1=ALU.add),
                                 [wik, "tab", "carry"], [zik])
                            c2, c2k = cst.next()
                            tt(p, "dve", c2[:, 0:1], zi[:, ch, 127:128], sn_[:, 127:128], ALU.mult, [zik, "tab"], [c2k])
                            tt(p, "dve", c2[:, 1:2], zi[:, ch, 127:128], cs_[:, 127:128], ALU.mult, [zik, "tab"], [c2k])
                            stt(p, carry[:, 0, k:k + 1], zr[:, ch, 127:128], cs_[:, 127:128], c2[:, 0:1], ALU.mult, ALU.subtract, [zrk, c2k, "tab"], ["carry"])
                            stt(p, carry[:, 1, k:k + 1], zr[:, ch, 127:128], sn_[:, 127:128], c2[:, 1:2], ALU.mult, ALU.add, [zrk, c2k, "tab"], ["carry"])
                        n1, n1k = m4.next(); n2, n2k = m4.next(); n3, n3k = m4.next(); n4, n4k = m4.next()
                        tt(p, "pool", n1[:, 0:nch, :], zr[:, 0:nch, :], csb, ALU.mult, [zrk, "tab"], [n1k])
                        tt(p, "pool", n2[:, 0:nch, :], zi[:, 0:nch, :], snb, ALU.mult, [zik, "tab"], [n2k])
                        tt(p, "dve", n3[:, 0:nch, :], zr[:, 0:nch, :], snb, ALU.mult, [zrk, "tab"], [n3k])
                        tt(p, "pool", n4[:, 0:nch, :], zi[:, 0:nch, :], csb, ALU.mult, [zik, "tab"], [n4k])
                        tt(p, "pool", V3(xre[:, k, 0:bs]), n1[:, 0:nch, :], n2[:, 0:nch, :], ALU.subtract, [n1k, n2k], ["xre"])
                        tt(p, "pool", V3(xim[:, k, 0:bs]), n3[:, 0:nch, :], n4[:, 0:nch, :], ALU.add, [n3k, n4k], ["xim"])
                    for cc in range(4):
                        ps, pk = pcy.next()
                        for kk in range(4):
                            k = cc * 4 + kk
                            mm(p, ps[:, 0:bs], Cl[:, d, k, 0, :], xre[:, k, 0:bs], kk == 0, False, ["Cl", "xre"], [pk])
                            mm(p, ps[:, 0:bs], Cl[:, d, k, 1, :], xim[:, k, 0:bs], False, kk == 3, ["Cl", "xim"], [pk])
                        o, ok = o32.next()
                        cp(p, "act", o[:, 0:bs], ps[:, 0:bs], [pk], [ok])
                        p.dma(YS[d, cc * 128:(cc + 1) * 128, c0:c0 + bs], o[:, 0:bs], [ok], ["YS"])
                if not has_init:
                    for ri, nm in enumerate(("o_sre", "o_sim")):
                        ps, pk = ptr.next()
                        trp(p, ps[0:16, 0:128], carry[:, ri, :], identf[:], ["carry", "identf"], [pk])
                        f_, fk = fst.next()
                        cp(p, "act", f_[:], ps[0:16, 0:128], [pk], [fk])
                        p.dma(T[nm][si - 1, li, d], f_[:], [fk], [nm + "%d_%d_%d" % (si, li, d)])

    with p.phase():
        st = RR(p, 2, [128, 1024], F32, "wst")
        wgl = p.sbuf([128, 4, 512], BF16, "wgl")
        load_w_bf16(p, wgl, "wgl", T["w_glu"][li], 512, st, nkc=4)
        dsk = p.sbuf([128, 4], F32, "dsk"); bgl = p.sbuf([128, 4], F32, "bgl")
        p.dma(dsk[:], T["s_d"][li], (), ["dsk"]); p.dma(bgl[:], T["b_glu"][li], (), ["bgl"])
        yl = RR(p, 3, [128, 3, 512], F32, "yl")
        g32 = RR(p, 2, [128, 4, 512], F32, "g32")
        gb = RR(p, 2, [128, 4, 512], BF16, "gb")
        sg = RR(p, 2, [128, 512], F32, "sg")
        ob = RR(p, 2, [128, 512], BF16, "ob")
        pso = RR(p, 3, [128, 512], F32, "pso", psum=True)
        for u in range(9):
            t0 = u * 512
            g, gk = g32.next()
            g2, g2k = gb.next()
            for cc in range(4):
                y, yk = yl.next()
                p.dma(y[:, 0, :], YS[0, cc * 128:(cc + 1) * 128, t0:t0 + 512], ["YS"], [yk], eng="act")
                p.dma(y[:, 1, :], YS[1, cc * 128:(cc + 1) * 128, t0:t0 + 512], ["YS"], [yk], eng="act")
                p.dma(y[:, 2, :], UT32[cc * 128:(cc + 1) * 128, t0:t0 + 512], ["UT32"], [yk], eng="sp")
                tt(p, "pool", y[:, 0, :], y[:, 0, :], y[:, 1, :], ALU.add, [yk], [yk])
                stt(p, y[:, 0, :], y[:, 2, :], dsk[:, cc:cc + 1], y[:, 0, :], ALU.mult, ALU.add, [yk, "dsk"], [yk])
                act(p, g[:, cc, :], y[:, 0, :], AF.Gelu, [yk], [gk])
                cp(p, "pool", g2[:, cc, :], g[:, cc, :], [gk], [g2k])
            for oc in range(4):
                ps, pk = pso.next()
                for kc in range(4):
                    mm(p, ps[:], wgl[:, kc, oc * 128:(oc + 1) * 128], g2[:, kc, :], kc == 0, kc == 3, ["wgl", g2k], [pk])
                s, sk = sg.next()
                act(p, s[:], ps[:], AF.Sigmoid, [pk, "bgl"], [sk], bias=bgl[:, oc:oc + 1])
                o, ok = ob.next()
                tt(p, "dve", o[:], g[:, oc, :], s[:], ALU.mult, [gk, sk], [ok])
                p.dma(MIXF[oc * 128:(oc + 1) * 128, t0:t0 + 512], o[:], [ok], ["MIXF"])


def build_hyena(p, T, l, li, identf):
    HYT, HYTM, Z1F, MIXT = T["HYT"], T["HYTM"], T["Z1F"], T["MIXT"]
    with p.phase():
        cw = p.sbuf([128, 12, 3], F32, "cw"); cb = p.sbuf([128, 12], F32, "cb")
        p.dma(cw[:], T["hcw"][li], (), ["cw"]); p.dma(cb[:], T["hcb"][li], (), ["cw"])
        raw = RR(p, 2, [128, 4098], F32, "raw")
        sv = RR(p, 2, [128, 4096], F32, "sv")
        stg = RR(p, 3, [128, 4, 128], F32, "stg")
        ptr = RR(p, 4, [128, 512], F32, "ptr", psum=True)
        for cc in range(12):
            for (toff, L, _) in SEQS:
                r_, rk = raw.next()
                mset(p, "pool", r_[:, 0:1], 0.0, [rk])
                mset(p, "pool", r_[:, L + 1:L + 2], 0.0, [rk])
                p.dma(r_[:, 1:L + 1], HYT[cc * 128:(cc + 1) * 128, toff:toff + L], ["HYT"], [rk], eng="act")
                s_, sk = sv.next()
                act(p, s_[:, 0:L], r_[:, 1:L + 1], AF.Identity, [rk, "cw"], [sk], scale=cw[:, cc, 1:2], bias=cb[:, cc:cc + 1])
                stt(p, s_[:, 0:L], r_[:, 0:L], cw[:, cc, 0:1], s_[:, 0:L], ALU.mult, ALU.add, [rk, "cw"], [sk])
                stt(p, s_[:, 0:L], r_[:, 2:L + 2], cw[:, cc, 2:3], s_[:, 0:L], ALU.mult, ALU.add, [rk, "cw"], [sk])
                for t4 in range(L // 512 if L >= 512 else 1):
                    n = min(4, L // 128)
                    ps, pk = ptr.next()
                    for i in range(n):
                        trp(p, ps[:, i * 128:(i + 1) * 128], s_[:, (t4 * 4 + i) * 128:(t4 * 4 + i + 1) * 128], identf[:], [sk, "identf"], [pk])
                    g, gk = stg.next()
                    cp(p, "act" if t4 % 2 else "dve", g[:, 0:n, :].rearrange("q i c -> q (i c)"), ps[:, 0:n * 128], [pk], [gk])
                    r0 = toff + t4 * 512
                    p.dma(HYTM[r0:r0 + n * 128, cc * 128:(cc + 1) * 128].rearrange("(i q) c -> q i c", q=128), g[:, 0:n, :], [gk], ["HYTM"])

    for L in (4096, 256):
        nb = L // 128
        N = 2 * L
        KSP = T["KSP%d" % L]
        Cb, Sb = T["dftC%d" % L], T["dftS%d" % L]
        with p.phase():
            ft = p.sbuf([33, L], F32, "ft")
            p.dma(ft[:], T["feats%d" % L], (), ["ft"])
            w1 = p.sbuf([33, 64], F32, "w1"); w2 = p.sbuf([64, 64], F32, "w2"); w3 = p.sbuf([64, 2048], F32, "w3")
            p.dma(w1[:], T["hw1"][li], (), ["w1"]); p.dma(w2[:], T["hw2"][li], (), ["w2"]); p.dma(w3[:], T["hw3"][li], (), ["w3"])
            sc = p.sbuf([64, 8], F32, "sc")
            for j, nm in enumerate(("hb1", "hf1", "hb2", "hf2")):
                p.dma(sc[:, j:j + 1], T[nm][li], (), ["sc"])
            ts(p, "dve", sc[:, 4:5], sc[:, 1:2], 1.0 / TWO_PI, None, ALU.mult, None, ["sc"], ["sc"])
            ts(p, "dve", sc[:, 5:6], sc[:, 3:4], 1.0 / TWO_PI, None, ALU.mult, None, ["sc"], ["sc"])
            ntn = p.sbuf([128, nb], F32, "ntn")
            p.dma(ntn[:], T["tn%d" % L], (), ["ntn"])
            ts(p, "dve", ntn[:], ntn[:], -1.0, None, ALU.mult, None, ["ntn"], ["ntn"])
            adec = p.sbuf([128, 4, 512], F32, "adec")
            p.dma(adec[:].rearrange("q a c -> q (a c)"), T["hdec"][li:li + 1].rearrange("o a c -> o (a c)").to_broadcast([128, 2048]), (), ["adec"])
            act(p, adec[:].rearrange("q a c -> q (a c)"), adec[:].rearrange("q a c -> q (a c)"), AF.Abs, ["adec"], ["adec"])
            z1 = p.sbuf([64, L], F32, "z1"); z2 = p.sbuf([64, L], F32, "z2")
            tfm = RR(p, 2, [64, 512], F32, "tfm")
            tim = RR(p, 2, [64, 512], I32, "tim")
            pz = RR(p, 2, [128, 512], F32, "pz", psum=True)
            ph = RR(p, 2, [128, 512], F32, "ph", psum=True)
            pk_ = RR(p, 3, [128, 512], F32, "pk", psum=True)
            bw = min(512, L)
            for (src, srck, W, K, dst, dk, bcol, fcol) in ((ft, "ft", w1, 33, z1, "z1", 0, 4), (z1, "z1", w2, 64, z2, "z2", 2, 5)):
                for b in range(L // bw):
                    ps, pk = pz.next()
                    mm(p, ps[0:64, 0:bw], W[0:K, :], src[0:K, b * bw:(b + 1) * bw], True, True, [srck, "w1", "w2"], [pk])
                    t_, tk = tfm.next()
                    i_, ik = tim.next()
                    ts(p, "dve", t_[:, 0:bw], ps[0:64, 0:bw], sc[:, bcol:bcol + 1], sc[:, fcol:fcol + 1], ALU.add, ALU.mult, [pk, "sc"], [tk])
                    cp(p, "dve", i_[:, 0:bw], t_[:, 0:bw], [tk], [ik])
                    d_ = dst[:, b * bw:(b + 1) * bw]
                    cp(p, "dve", d_, i_[:, 0:bw], [ik], [dk])
                    tt(p, "dve", d_, t_[:, 0:bw], d_, ALU.subtract, [tk, dk], [dk])
                    act(p, d_, d_, AF.Sin, [dk], [dk], scale=TWO_PI)
            ab = [p.sbuf([128, nb, 512], BF16, "ab%d" % j) for j in range(2)]
            hfs = RR(p, 4, [128, 512], F32, "hfs")
            dcy = RR(p, 2, [128, 512], F32, "dcy")
            cbk = RR(p, 2, [128, nb, 128], BF16, "cbk")
            ko = RR(p, 3, [128, 512], F32, "ko")
            sgp = p.sbuf([128, 32], BF16, "sgp")
            p.dma(sgp[:], T["sgnp"], (), ["sgp"])
            for order in range(2):
                for tb in range(nb):
                    hh = []
                    for side in range(2):
                        so = side * 2 + order
                        ps, pk = ph.next()
                        mm(p, ps[:], z2[0:64, tb * 128:(tb + 1) * 128], w3[0:64, so * 512:(so + 1) * 512], True, True, ["z2", "w3"], [pk])
                        dc, dck = dcy.next()
                        act(p, dc[:], adec[:, so, :], AF.Exp, ["adec", "ntn"], [dck], scale=ntn[:, tb:tb + 1])
                        h_, hk = hfs.next()
                        tt(p, "dve", h_[:], ps[:], dc[:], ALU.mult, [pk, dck], [hk])
                        if side == 1 and tb == 0:
                            mset(p, "dve", h_[0:1, :], 0.0, [hk])
                        hh.append((h_, hk))
                    tt(p, "pool", ab[0][:, tb, :], hh[0][0][:], hh[1][0][:], ALU.add, [hh[0][1], hh[1][1]], ["ab0"])
                    tt(p, "pool", ab[1][:, tb, :], hh[0][0][:], hh[1][0][:], ALU.subtract, [hh[0][1], hh[1][1]], ["ab1"])
                for which, (Mb, a_, akey) in enumerate(((Cb, ab[0], "ab0"), (Sb, ab[1], "ab1"))):
                    for fb in range(nb):
                        m_, mk = cbk.next()
                        p.dma(m_[:], Mb[fb], (), [mk], eng=("sp" if fb % 2 else "act"))
                        ps, pk = pk_.next()
                        for sc_ in range(nb):
                            mm(p, ps[:], m_[:, sc_, :], a_[:, sc_, :], sc_ == 0, sc_ == nb - 1, [mk, akey], [pk])
                        o, ok = ko.next()
                        ts(p, "dve", o[:], ps[:], 2.0 / N, None, ALU.mult, None, [pk], [ok])
                        if fb == 0:
                            if which == 0:
                                ts(p, "dve", o[0:1, :], ps[0:1, :], 1.0 / N, None, ALU.mult, None, [pk], [ok])
                            else:
                                ps2, pk2 = pk_.next()
                                for sc_ in range(nb):
                                    mm(p, ps2[0:1, :], sgp[:, 0:1], ab[0][:, sc_, :], sc_ == 0, sc_ == nb - 1, [mk, "ab0", "sgp"], [pk2])
                                ts(p, "dve", o[0:1, :], ps2[0:1, :], 1.0 / N, None, ALU.mult, None, [pk2], [ok])
                        p.dma(KSP[order, which, fb * 128:(fb + 1) * 128, :], o[:], [ok], ["KSP"])

    with p.phase():
        bbc = p.sbuf([128, 2, 512], F32, "bbc")
        p.dma(bbc[:].rearrange("q a c -> q (a c)"), T["hbias"][li:li + 1].rearrange("o a c -> o (a c)").to_broadcast([128, 1024]), (), ["bbc"])
        src_t = p.sbuf([128, 32, 512], BF16, "srct")
        Yre = p.sbuf([128, 32, 512], BF16, "Yre")
        Yp = p.sbuf([128, 32, 512], BF16, "Yp")
        cbk = RR(p, 2, [128, 32, 128], BF16, "cbk")
        sbk = RR(p, 2, [128, 32, 128], BF16, "sbk")
        hv = RR(p, 2, [128, 1536], F32, "hv")
        z1t = RR(p, 2, [128, 512], F32, "z1t")
        t4_ = RR(p, 4, [128, 512], F32, "t4")
        kk_ = RR(p, 2, [128, 2, 512], F32, "kk")
        ob = RR(p, 2, [128, 512], BF16, "ob")
        pf = RR(p, 4, [128, 512], F32, "pf", psum=True)
        pi = RR(p, 2, [128, 512], F32, "pi", psum=True)
        sgp = p.sbuf([128, 32], BF16, "sgp")
        sgj = p.sbuf([1, 128], BF16, "sgj")
        p.dma(sgp[:], T["sgnp"], (), ["sgp"])
        p.dma(sgj[:], T["sgnj"], (), ["sgp"])
        for (toff, L, _) in SEQS:
            nb = L // 128
            KSP = T["KSP%d" % L]
            Cb, Sb = T["dftC%d" % L], T["dftS%d" % L]
            for tb in range(nb):
                h_, hk = hv.next()
                p.dma(h_[:, 0:512], HYTM[toff + tb * 128:toff + (tb + 1) * 128, 0:512], ["HYTM"], [hk], eng="act")
                cp(p, "pool", src_t[:, tb, :], h_[:, 0:512], [hk], ["srct"])
            for order in range(2):
                for fb in range(nb):
                    c_, ck = cbk.next(); s_, sk = sbk.next()
                    p.dma(c_[:, 0:nb, :], Cb[fb], (), [ck], eng="act")
                    p.dma(s_[:, 0:nb, :], Sb[fb], (), [sk], eng="sp")
                    if fb == 0:
                        cp(p, "dve", s_[:, 0:nb, 0], sgp[:, 0:nb], [sk, "sgp"], [sk])
                    k_, kk = kk_.next()
                    p.dma(k_[:, 0, :], KSP[order, 0, fb * 128:(fb + 1) * 128, :], ["KSP"], [kk], eng="act")
                    p.dma(k_[:, 1, :], KSP[order, 1, fb * 128:(fb + 1) * 128, :], ["KSP"], [kk], eng="sp")
                    pre, prk = pf.next(); pp, ppk = pf.next()
                    for sc_ in range(nb):
                        mm(p, pre[:], c_[:, sc_, :], src_t[:, sc_, :], sc_ == 0, sc_ == nb - 1, [ck, "srct"], [prk])
                    for sc_ in range(nb):
                        mm(p, pp[:], s_[:, sc_, :], src_t[:, sc_, :], sc_ == 0, sc_ == nb - 1, [sk, "srct"], [ppk])
                    a1, a1k = t4_.next(); a2, a2k = t4_.next(); a3, a3k = t4_.next(); a4, a4k = t4_.next()
                    tt(p, "dve", a1[:], pre[:], k_[:, 0, :], ALU.mult, [prk, kk], [a1k])
                    tt(p, "dve", a2[:], pp[:], k_[:, 1, :], ALU.mult, [ppk, kk], [a2k])
                    tt(p, "dve", a3[:], pre[:], k_[:, 1, :], ALU.mult, [prk, kk], [a3k])
                    tt(p, "dve", a4[:], pp[:], k_[:, 0, :], ALU.mult, [ppk, kk], [a4k])
                    tt(p, "pool", Yre[:, fb, :], a1[:], a2[:], ALU.subtract, [a1k, a2k], ["Yre"])
                    tt(p, "pool", Yp[:, fb, :], a3[:], a4[:], ALU.add, [a3k, a4k], ["Yp"])
                    if fb == 0:
                        cp(p, "pool", Yre[0:1, 0, :], a1[0:1, :], [a1k, "Yre"], ["Yre"])
                        cp(p, "pool", Yp[0:1, 0, :], a2[0:1, :], [a2k, "Yp"], ["Yp"])
                for tb in range(nb):
                    c_, ck = cbk.next(); s_, sk = sbk.next()
                    p.dma(c_[:, 0:nb, :], Cb[tb], (), [ck], eng="act")
                    p.dma(s_[:, 0:nb, :], Sb[tb], (), [sk], eng="sp")
                    cp(p, "dve", s_[0:1, 0, :], sgj[0:1, :], [sk, "sgp"], [sk])
                    ps, pk = pi.next()
                    for fc in range(nb):
                        mm(p, ps[:], c_[:, fc, :], Yre[:, fc, :], fc == 0, False, [ck, "Yre"], [pk])
                    for fc in range(nb):
                        mm(p, ps[:], s_[:, fc, :], Yp[:, fc, :], False, fc == nb - 1, [sk, "Yp"], [pk])
                    r0 = toff + tb * 128
                    h_, hk = hv.next()
                    p.dma(h_[:], HYTM[r0:r0 + 128, :], ["HYTM"], [hk], eng="act")
                    a1, a1k = t4_.next(); a2, a2k = t4_.next()
                    if order == 0:
                        tt(p, "pool", a1[:], h_[:, 0:512], bbc[:, 0, :], ALU.mult, [hk, "bbc"], [a1k])
                        tt(p, "dve", a2[:], ps[:], a1[:], ALU.add, [pk, a1k], [a2k])
                        z_, zk = z1t.next()
                        tt(p, "dve", z_[:], a2[:], h_[:, 512:1024], ALU.mult, [a2k, hk], [zk])
                        p.dma(Z1F[r0:r0 + 128, :], z_[:], [zk], ["Z1F"])
                        cp(p, "pool", src_t[:, tb, :], z_[:], [zk], ["srct"])
                    else:
                        z_, zk = z1t.next()
                        p.dma(z_[:], Z1F[r0:r0 + 128, :], ["Z1F"], [zk], eng="sp")
                        tt(p, "pool", a1[:], z_[:], bbc[:, 1, :], ALU.mult, [zk, "bbc"], [a1k])
                        tt(p, "dve", a2[:], ps[:], a1[:], ALU.add, [pk, a1k], [a2k])
                        o, ok = ob.next()
                        tt(p, "dve", o[:], a2[:], h_[:, 1024:1536], ALU.mult, [a2k, hk], [ok])
                        p.dma(MIXT[r0:r0 + 128, 512:1024], o[:], [ok], ["MIXT"])
```

```python
import math
import contextlib
import numpy as np
import ml_dtypes
import concourse.bass as bass
import concourse.mybir as mybir
from concourse.bass_utils import run_bass_kernel_spmd

F32 = mybir.dt.float32
BF16 = mybir.dt.bfloat16
I32 = mybir.dt.int32
AF = mybir.ActivationFunctionType
ALU = mybir.AluOpType

D = 1024
NS = 4096
NP_ = 512
NT = NS + NP_
DFF = 2816
NFC = 22
EPS = 1e-6
DEPTH = 4
ENGS = ("pe", "dve", "act", "pool", "sp")
SEM_ROLL = 12000
NDMA_SEM = 28
TWO_PI = 2.0 * math.pi


class Prog:
    def __init__(self, nc, top):
        self.nc = nc
        self.top = top
        self.stack = top
        self.q = {e: [] for e in ENGS}
        self.cnt = {e: 0 for e in ENGS}
        self.esems = {e: [] for e in ENGS}
        self.last_tok = {e: None for e in ENGS}
        self.last_w = {}
        self.readers = {}
        self.waited = {e: {} for e in ENGS}
        self.dma_sems = [top.enter_context(nc.semaphore("dq%d" % i)) for i in range(NDMA_SEM)]
        self.dma_uses = [0] * NDMA_SEM
        self.dma_i = 0
        self.sem_ids = {}
        self.nbuf = 0
        self.ninst = 0

    def sbuf(self, shape, dtype, name="sb"):
        self.nbuf += 1
        return self.stack.enter_context(self.nc.sbuf_tensor("%s_%d" % (name, self.nbuf), list(shape), dtype))

    def psum(self, shape, dtype=F32, name="ps"):
        self.nbuf += 1
        return self.stack.enter_context(self.nc.psum_tensor("%s_%d" % (name, self.nbuf), list(shape), dtype))

    def _esem(self, eng, k):
        while len(self.esems[eng]) <= k:
            s = self.top.enter_context(self.nc.semaphore("e_%s_%d" % (eng, len(self.esems[eng]))))
            self.esems[eng].append(s)
        return self.esems[eng][k]

    def _need(self, eng, tok, waits):
        if tok is None:
            return
        sem, val, teng = tok
        if teng == "pe" and eng == "pe":
            return
        key = id(sem)
        self.sem_ids[key] = sem
        if self.waited[eng].get(key, 0) >= val:
            return
        if val > waits.get(key, 0):
            waits[key] = val

    def op(self, eng, fn, reads=(), writes=(), dma=False):
        waits = {}
        for r in reads:
            self._need(eng, self.last_w.get(r), waits)
        for w in writes:
            self._need(eng, self.last_w.get(w), waits)
            for t in self.readers.get(w, ()):
                self._need(eng, t, waits)
        if dma:
            slot = self.dma_i % NDMA_SEM
            self.dma_i += 1
            sem = self.dma_sems[slot]
            if self.dma_uses[slot] > 0:
                self._need(eng, (sem, 16 * self.dma_uses[slot], "dma"), waits)
            self.dma_uses[slot] += 1
            tok = (sem, 16 * self.dma_uses[slot], "dma")
            inc = 16
        else:
            n = self.cnt[eng]
            self.cnt[eng] += 1
            sem = self._esem(eng, n // SEM_ROLL)
            tok = (sem, (n % SEM_ROLL) + 1, eng)
            inc = 1
            self.last_tok[eng] = tok
        wl = []
        for key, val in waits.items():
            self.waited[eng][key] = val
            wl.append((self.sem_ids[key], val))
        self.q[eng].append((wl, fn, tok[0], inc))
        self.ninst += 1
        for r in reads:
            self.readers.setdefault(r, []).append(tok)
        for w in writes:
            self.last_w[w] = tok
            self.readers[w] = []
        return tok

    def dma(self, out, in_, reads=(), writes=(), eng="sp", **kw):
        return self.op(eng, lambda e: e.dma_start(out=out, in_=in_, **kw), reads, writes, dma=True)

    def barrier(self):
        toks = [self.last_tok[e] for e in ENGS if self.last_tok[e] is not None]
        for i, s in enumerate(self.dma_sems):
            if self.dma_uses[i] > 0:
                toks.append((s, 16 * self.dma_uses[i], "dma"))
        for e in ENGS:
            waits = {}
            for t in toks:
                if t[2] == e:
                    continue
                self._need(e, t, waits)
            wl = []
            for key, val in waits.items():
                self.waited[e][key] = val
                wl.append((self.sem_ids[key], val))
            if wl:
                self.q[e].append((wl, None, None, 0))
        self.last_w.clear()
        self.readers.clear()

    def flush(self):
        q = self.q

        def run(engname, e):
            for wl, fn, sem, inc in q[engname]:
                for s, v in wl:
                    e.wait_ge(s, v)
                if fn is not None:
                    fn(e).then_inc(sem, inc)

        with self.nc.Block() as block:
            @block.tensor
            def _(e):
                run("pe", e)

            @block.vector
            def _(e):
                run("dve", e)

            @block.scalar
            def _(e):
                run("act", e)

            @block.gpsimd
            def _(e):
                run("pool", e)

            @block.sync
            def _(e):
                run("sp", e)
        self.q = {e: [] for e in ENGS}

    @contextlib.contextmanager
    def phase(self):
        with contextlib.ExitStack() as st:
            old = self.stack
            self.stack = st
            yield
            self.barrier()
            self.flush()
            self.stack = old


class RR:
    def __init__(self, p, n, shape, dtype, name, psum=False):
        self.t = [(p.psum(shape, dtype, name) if psum else p.sbuf(shape, dtype, name)) for _ in range(n)]
        self.k = ["%s@%d_%d" % (name, p.nbuf, i) for i in range(n)]
        self.n = n
        self.i = 0

    def next(self):
        j = self.i % self.n
        self.i += 1
        return self.t[j], self.k[j]


def mm(p, out, lhsT, rhs, start, stop, r, w):
    p.op("pe", lambda e: e.matmul(out, lhsT=lhsT, rhs=rhs, start=start, stop=stop), r, w)


def trp(p, out, in_, ident, r, w):
    p.op("pe", lambda e: e.transpose(out, in_, ident), r, w)


def act(p, out, in_, func, r, w, scale=None, bias=None, accum=None):
    kw = {}
    if scale is not None:
        kw["scale"] = scale
    if bias is not None:
        kw["bias"] = bias
    if accum is not None:
        kw["accum_out"] = accum
    p.op("act", lambda e: e.activation(out=out, in_=in_, func=func, **kw), r, w)


def tt(p, eng, out, a, b, op, r, w):
    p.op(eng, lambda e: e.tensor_tensor(out=out, in0=a, in1=b, op=op), r, w)


def ts(p, eng, out, a, s1, s2, op0, op1, r, w):
    if op1 is None:
        p.op(eng, lambda e: e.tensor_scalar(out=out, in0=a, scalar1=s1, scalar2=None, op0=op0), r, w)
    else:
        p.op(eng, lambda e: e.tensor_scalar(out=out, in0=a, scalar1=s1, scalar2=s2, op0=op0, op1=op1), r, w)


def stt(p, out, a, s, b, op0, op1, r, w):
    p.op("dve", lambda e: e.scalar_tensor_tensor(out=out, in0=a, scalar=s, in1=b, op0=op0, op1=op1), r, w)


def cp(p, eng, out, in_, r, w):
    if eng == "act":
        p.op("act", lambda e: e.activation(out=out, in_=in_, func=AF.Copy), r, w)
    else:
        p.op(eng, lambda e: e.tensor_copy(out=out, in_=in_), r, w)


def mset(p, eng, ap, val, w):
    p.op(eng, lambda e: e.memset(ap, val), (), w)


def recip(p, out, in_, r, w):
    p.op("dve", lambda e: e.reciprocal(out=out, in_=in_), r, w)


_CONST = {}


def _dft_blocks(L):
    N = 2 * L
    nb = L // 128
    s = np.arange(L, dtype=np.int64)
    idx = (s[:, None] * s[None, :]) % N
    ang = idx.astype(np.float64) * (TWO_PI / N)
    C = np.cos(ang)
    S = np.sin(ang)

    def blk(M):
        return np.ascontiguousarray(M.reshape(nb, 128, nb, 128).transpose(2, 1, 0, 3)).astype(ml_dtypes.bfloat16)

    return blk(C), blk(S)


def _feats(L):
    tn = (np.arange(L, dtype=np.float32) / np.float32(L)).astype(np.float32)
    bands = np.linspace(1e-4, 15.0, 16, dtype=np.float32)
    ang = (np.float32(TWO_PI) * tn[:, None]).astype(np.float32) * bands
    f = np.concatenate([tn[:, None], np.cos(ang), np.sin(ang)], axis=-1).astype(np.float32)
    tnl = np.ascontiguousarray(tn.reshape(L // 128, 128).T)
    return np.ascontiguousarray(f.T), tnl


def consts():
    if _CONST:
        return _CONST
    c = _CONST
    c["identb"] = np.eye(128, dtype=np.float32).astype(ml_dtypes.bfloat16)
    c["identf"] = np.eye(128, dtype=np.float32)
    t = np.arange(NS)
    inv = (10000.0 ** (-np.arange(16, dtype=np.float32) / 16)).astype(np.float32)
    rc = np.zeros((64, NS), np.float32)
    rs = np.zeros((64, NS), np.float32)
    for d in range(64):
        pos = (t // 64) if d < 32 else (t % 64)
        ang = pos.astype(np.float32) * inv[d % 16]
        rc[d] = np.cos(ang)
        rs[d] = np.sin(ang)
    c["ropeC"] = np.concatenate([rc, rc], 0)
    c["ropeS"] = np.concatenate([rs, rs], 0)
    kc = np.arange(64)[:, None]
    qc = 63 - np.arange(64)[None, :]
    cs = np.clip(qc - 8, 0, 48)
    wm = ((kc >= cs) & (kc < cs + 16)).astype(np.float32)
    c["winmask"] = np.concatenate([wm, wm], 0)
    c["dftC4096"], c["dftS4096"] = _dft_blocks(4096)
    c["dftC256"], c["dftS256"] = _dft_blocks(256)
    c["feats4096"], c["tn4096"] = _feats(4096)
    c["feats256"], c["tn256"] = _feats(256)
    sg = np.where(np.arange(128) % 2 == 0, 1.0, -1.0).astype(np.float32)
    c["sgnp"] = np.tile(sg[:, None], (1, 32)).astype(ml_dtypes.bfloat16)
    c["sgnj"] = sg[None, :].astype(ml_dtypes.bfloat16)
    c["iota128"] = np.tile(np.arange(1, 129, dtype=np.float32)[None, :], (128, 1))
    return c


IN_SPECS = {}


def build_program(nlayers=DEPTH):
    nc = bass.Bass("TRN2", target_bir_lowering=False)
    T = {}

    def din(name, shape, dt=F32):
        T[name] = nc.dram_tensor(name, list(shape), dt, kind="ExternalInput").ap()
        IN_SPECS[name] = (tuple(shape), dt)
        return T[name]

    def dout(name, shape):
        T[name] = nc.dram_tensor(name, list(shape), F32, kind="ExternalOutput").ap()
        return T[name]

    def dscr(name, shape, dt=F32):
        T[name] = nc.dram_tensor(name, list(shape), dt).ap()
        return T[name]

    x0 = din("x0", [NT, D])
    cak = din("cak", [2, 512, 512]); cav = din("cav", [2, 512, 512])
    cbk = din("cbk", [2, 512, 512]); cbv = din("cbv", [2, 512, 512])
    sre = din("sre", [2, 2, 128, 16]); sim = din("sim", [2, 2, 128, 16])
    cond = din("cond", [128, 8, 2])
    w_mod = din("w_mod", [4, D, 6 * D]); b_mod = din("b_mod", [4, 128, 48])
    gpre1 = din("gpre1", [4, 128, 8]); gpost1 = din("gpost1", [4, 128, 8])
    gpre2 = din("gpre2", [4, 128, 8]); gpost2 = din("gpost2", [4, 128, 8])
    wg_in = din("wg", [4, D, DFF]); wu_in = din("wu", [4, D, DFF]); wd_in = din("wd", [4, DFF, D])
    w_in_e = din("w_in_e", [2, D, 3072]); w_out_e = din("w_out_e", [2, D, D])
    rpb = din("rpb", [2, 8, 15, 31])
    lamv = din("lamv", [2, 4, 64]); gsub = din("gsub", [2, 128])
    w_in_o = din("w_in_o", [2, D, 2048]); w_out_o = din("w_out_o", [2, D, D])
    s_lre = din("s_lre", [2, 2, 128, 16]); s_lim = din("s_lim", [2, 2, 128, 16]); s_ldt = din("s_ldt", [2, 2, 128, 16])
    s_bre = din("s_bre", [2, 2, 128, 256]); s_bim = din("s_bim", [2, 2, 128, 256])
    s_cre = din("s_cre", [2, 2, 128, 256]); s_cim = din("s_cim", [2, 2, 128, 256])
    s_d = din("s_d", [2, 128, 4])
    w_glu = din("w_glu", [2, 512, 512]); b_glu = din("b_glu", [2, 128, 4])
    hcw = din("hcw", [2, 128, 12, 3]); hcb = din("hcb", [2, 128, 12])
    hw1 = din("hw1", [2, 33, 64]); hb1 = din("hb1", [2, 64, 1]); hf1 = din("hf1", [2, 64, 1])
    hw2 = din("hw2", [2, 64, 64]); hb2 = din("hb2", [2, 64, 1]); hf2 = din("hf2", [2, 64, 1])
    hw3 = din("hw3", [2, 64, 2048]); hdec = din("hdec", [2, 4, 512]); hbias = din("hbias", [2, 2, 512])
    identb_d = din("identb", [128, 128], BF16); identf_d = din("identf", [128, 128])
    ropeC = din("ropeC", [128, NS]); ropeS = din("ropeS", [128, NS])
    winmask = din("winmask", [128, 64])
    dftC = {4096: din("dftC4096", [32, 128, 32, 128], BF16), 256: din("dftC256", [2, 128, 2, 128], BF16)}
    dftS = {4096: din("dftS4096", [32, 128, 32, 128], BF16), 256: din("dftS256", [2, 128, 2, 128], BF16)}
    feats = {4096: din("feats4096", [33, 4096]), 256: din("feats256", [33, 256])}
    tnl = {4096: din("tn4096", [128, 32]), 256: din("tn256", [128, 2])}
    iota_d = din("iota128", [128, 128])
    din("sgnp", [128, 32], BF16); din("sgnj", [1, 128], BF16)

    o_yp = dout("o_yp", [NP_, D]); o_ys = dout("o_ys", [NS, D])
    o_ak = dout("o_ak", [2, 2, 256, 512]); o_av = dout("o_av", [2, 2, 256, 512])
    o_bk = dout("o_bk", [2, 2, 256, 512]); o_bv = dout("o_bv", [2, 2, 256, 512])
    o_sre = dout("o_sre", [2, 2, 2, 16, 128]); o_sim = dout("o_sim", [2, 2, 2, 16, 128])

    X = dscr("X", [NT, D])
    WG = dscr("WG", [4, 11, 128, 8, 256], BF16); WU = dscr("WU", [4, 11, 128, 8, 256], BF16)
    QAT = dscr("QAT", [512, 5120], BF16); KAT = dscr("KAT", [512, 5120], BF16); VA = dscr("VA", [5120, 512], BF16)
    QBT = dscr("QBT", [512, 5120], BF16); KBT = dscr("KBT", [512, 5120], BF16); VB = dscr("VB", [5120, 512], BF16)
    MIXT = dscr("MIXT", [NT, D], BF16)
    MIXF = dscr("MIXF", [D, NT], BF16)
    RPBP = dscr("RPBP", [8, 15, 288])
    UT32 = dscr("UT32", [512, NT]); UTB = dscr("UTB", [512, NT], BF16)
    HYT = dscr("HYT", [1536, NT]); HYTM = dscr("HYTM", [NT, 1536])
    YS = dscr("YS", [2, 512, NT])
    KSP = {4096: dscr("KSP4096", [2, 2, 4096, 512]), 256: dscr("KSP256", [2, 2, 256, 512])}
    Z1F = dscr("Z1F", [NT, 512])

    fin_keys = []

    with contextlib.ExitStack() as top:
        p = Prog(nc, top)
        identb = p.sbuf([128, 128], BF16, "identb")
        identf = p.sbuf([128, 128], F32, "identf")
        condT = p.sbuf([128, 8, 2], F32, "condT")
        modA1 = p.sbuf([128, 8, 2], F32, "modA1"); modB1 = p.sbuf([128, 8, 2], F32, "modB1")
        modA2 = p.sbuf([128, 8, 2], F32, "modA2"); modB2 = p.sbuf([128, 8, 2], F32, "modB2")
        ggbc1 = [p.sbuf([128, D], F32, "ggbc1") for _ in range(2)]
        ggbc2 = [p.sbuf([128, D], F32, "ggbc2") for _ in range(2)]
        epsT = p.sbuf([128, 1], F32, "epsT")
        EPS_T[0] = epsT

        with p.phase():
            p.dma(identb[:], identb_d, (), ["identb"])
            p.dma(identf[:], identf_d, (), ["identf"])
            p.dma(condT[:], cond, (), ["condT"])
            mset(p, "dve", epsT[:], EPS, ["epsT"])
            act(p, condT[:], condT[:], AF.Silu, ["condT"], ["condT"])
            for i in range(9):
                p.dma(X[i * 512:(i + 1) * 512, :], x0[i * 512:(i + 1) * 512, :], (), ["X%d" % i], eng=("sp" if i % 2 else "act"))
            st32 = RR(p, 3, [128, DFF], F32, "st32")
            st16 = RR(p, 3, [128, DFF], BF16, "st16")
            k = 0
            for l in range(nlayers):
                for (src, dst) in ((wg_in, WG), (wu_in, WU)):
                    for rb in range(8):
                        a, ak = st32.next()
                        b, bk = st16.next()
                        p.dma(a[:], src[l][rb * 128:(rb + 1) * 128, :], (), [ak], eng=("sp" if k % 2 else "act"))
                        eng = ("dve", "pool", "act")[k % 3]
                        cp(p, eng, b[:], a[:], [ak], [bk])
                        p.dma(dst[l].rearrange("pc q kc f -> q kc pc f")[:, rb], b[:].rearrange("q (pc f) -> q pc f", f=256), [bk], ["wscr"], eng="sp")
                        k += 1

        for l in range(nlayers):
            even = (l % 2 == 0)
            li = l // 2
            lam_init = 0.8 - 0.6 * math.exp(-0.3 * l)
            build_mod(p, T, l, condT, identf, (modA1, modB1, modA2, modB2, ggbc1, ggbc2))
            if even:
                build_even_proj(p, T, l, li, identb, identf, epsT, modA1, modB1)
                build_even_attn(p, T, l, li, lam_init)
                tm_chunks = list(range(8))
            else:
                build_odd_proj(p, T, l, li, identb, epsT, modA1, modB1)
                build_s5(p, T, l, li, identf)
                build_hyena(p, T, l, li, identf)
                tm_chunks = [4, 5, 6, 7]
            build_out_ffn(p, T, l, li, even, tm_chunks, identb, epsT, modA2, modB2, ggbc1, ggbc2)

        with p.phase():
            for i in range(8):
                p.dma(o_ys[i * 512:(i + 1) * 512, :], X[i * 512:(i + 1) * 512, :], (), ["oys%d" % i], eng=("sp" if i % 2 else "act"))
            p.dma(o_yp[:, :], X[NS:NT, :], (), ["oyp"])
    return nc


def build_mod(p, T, l, condT, identf, outs):
    modA1, modB1, modA2, modB2, ggbc1, ggbc2 = outs
    w_mod, b_mod = T["w_mod"], T["b_mod"]
    with p.phase():
        mfm = p.sbuf([128, 48, 2], F32, "mfm")
        bm = p.sbuf([128, 48], F32, "bm")
        g4 = p.sbuf([128, 4, 8], F32, "g4")
        gg = p.sbuf([128, 8, 2], F32, "gg")
        lhs = RR(p, 2, [128, 128], F32, "bclhs")
        wp = RR(p, 2, [128, 8, 512], F32, "wmodp")
        pm = RR(p, 2, [128, 512], F32, "pm", psum=True)
        pb = RR(p, 2, [128, 512], F32, "pbc", psum=True)
        p.dma(bm[:], b_mod[l], (), ["bm"])
        for j, nm in enumerate(("gpre1", "gpost1", "gpre2", "gpost2")):
            p.dma(g4[:, j, :], T[nm][l], (), ["g4"], eng="act")
        for pc in range(12):
            w, wk = wp.next()
            p.dma(w[:], w_mod[l].rearrange("(kc q) n -> q kc n", q=128)[:, :, pc * 512:(pc + 1) * 512], (), [wk],
                  eng=("sp" if pc % 2 else "act"))
            ps, pk = pm.next()
            for jc in range(4):
                for kc in range(8):
                    mm(p, ps[:, jc * 2:jc * 2 + 2], w[:, kc, jc * 128:(jc + 1) * 128], condT[:, kc, :], kc == 0, kc == 7,
                       [wk, "condT"], [pk])
            for cd in range(2):
                tt(p, "dve", mfm[:, pc * 4:pc * 4 + 4, cd], ps[:, cd:8:2], bm[:, pc * 4:pc * 4 + 4], ALU.add,
                   [pk, "bm"], ["mfm"])
        for cd in range(2):
            ts(p, "dve", modA1[:, :, cd], mfm[:, 8:16, cd], 1.0, None, ALU.add, None, ["mfm"], ["modA1"])
            tt(p, "dve", modA1[:, :, cd], modA1[:, :, cd], g4[:, 0, :], ALU.mult, ["modA1", "g4"], ["modA1"])
            cp(p, "dve", modB1[:, :, cd], mfm[:, 0:8, cd], ["mfm"], ["modB1"])
            ts(p, "dve", modA2[:, :, cd], mfm[:, 32:40, cd], 1.0, None, ALU.add, None, ["mfm"], ["modA2"])
            tt(p, "dve", modA2[:, :, cd], modA2[:, :, cd], g4[:, 2, :], ALU.mult, ["modA2", "g4"], ["modA2"])
            cp(p, "dve", modB2[:, :, cd], mfm[:, 24:32, cd], ["mfm"], ["modB2"])
            for which, (sp_, gj, dst) in enumerate(((2, 1, ggbc1), (5, 3, ggbc2))):
                tt(p, "dve", gg[:, :, cd], mfm[:, sp_ * 8:sp_ * 8 + 8, cd], g4[:, gj, :], ALU.mult, ["mfm", "g4"], ["gg"])
                for half in range(2):
                    ps, pk = pb.next()
                    for kk in range(4):
                        kc = half * 4 + kk
                        lt, lk = lhs.next()
                        cp(p, "dve", lt[:], gg[:, kc, cd:cd + 1].to_broadcast([128, 128]), ["gg"], [lk])
                        mm(p, ps[:, kk * 128:(kk + 1) * 128], lt[:], identf[:], True, True, [lk, "identf"], [pk])
                    cp(p, "act", dst[cd][:, half * 512:(half + 1) * 512], ps[:], [pk], ["ggbc"])


class NormCtx:
    def __init__(self, p, identb, epsT):
        self.p = p
        self.identb = identb
        self.epsT = epsT
        self.junk = RR(p, 2, [128, D], BF16, "junk")
        self.ss = RR(p, 2, [128, 4], F32, "ss")
        self.xn = RR(p, 2, [128, 4, D], BF16, "xn")
        self.pst = RR(p, 2, [128, 1024], BF16, "pst", psum=True)

    def run(self, xs, xkey, A, B, cd, hT, hkey, ncol=512):
        p = self.p
        nt = ncol // 128
        ss, sk = self.ss.next()
        xn, xk = self.xn.next()
        for i in range(nt):
            j, jk = self.junk.next()
            act(p, j[:], xs[:, i, :], AF.Square, [xkey], [jk, sk], accum=ss[:, i:i + 1])
        act(p, ss[:, 0:nt], ss[:, 0:nt], AF.Sqrt, [sk], [sk], scale=1.0 / D, bias=self.epsT[:])
        recip(p, ss[:, 0:nt], ss[:, 0:nt], [sk], [sk])
        for i in range(nt):
            ts(p, "dve" if i % 2 == 0 else "pool", xn[:, i, :], xs[:, i, :], ss[:, i:i + 1], None, ALU.mult, None,
               [xkey, sk], [xk])
        for kc in range(8):
            ps, pk = self.pst.next()
            for i in range(nt):
                trp(p, ps[:, i * 128:(i + 1) * 128], xn[:, i, kc * 128:(kc + 1) * 128], self.identb[:], [xk, "identb"], [pk])
            act(p, hT[:, kc, 0:ncol], ps[:, 0:ncol], AF.Identity, [pk, "modA", "modB"], [hkey],
                scale=A[:, kc, cd:cd + 1], bias=B[:, kc, cd:cd + 1])


def load_w_bf16(p, dst, dkey, src2d, ncols, st, nkc=8):
    k = 0
    for kc in range(nkc):
        c0 = 0
        while c0 < ncols:
            cw = min(1024, ncols - c0)
            a, ak = st.next()
            p.dma(a[:, 0:cw], src2d[kc * 128:(kc + 1) * 128, c0:c0 + cw], (), [ak], eng=("sp" if k % 2 else "act"))
            cp(p, ("dve", "pool")[k % 2], dst[:, kc, c0:c0 + cw], a[:, 0:cw], [ak], [dkey])
            c0 += cw
            k += 1


def unit_cd(u):
    return 0 if u < 8 else 1


def xview(X, t0, n=512):
    return X[t0:t0 + n, :].rearrange("(i q) c -> q i c", q=128)


class PostNorm:
    def __init__(self, p, epsT):
        self.p = p
        self.epsT = epsT
        self.ss = RR(p, 4, [128, 4], F32, "pnss")
        self.junk = RR(p, 2, [128, D], BF16, "pnjunk")
        self.tmp = RR(p, 2, [128, D], F32, "pntmp")

    def run(self, po0, k0, po1, k1, x, xk, gg):
        p = self.p
        ss, sk = self.ss.next()
        j, jk = self.junk.next()
        act(p, j[:, 0:512], po0[:], AF.Square, [k0], [jk, sk], accum=ss[:, 0:1])
        act(p, j[:, 512:1024], po1[:], AF.Square, [k1], [jk, sk], accum=ss[:, 1:2])
        tt(p, "dve", ss[:, 2:3], ss[:, 0:1], ss[:, 1:2], ALU.add, [sk], [sk])
        act(p, ss[:, 2:3], ss[:, 2:3], AF.Sqrt, [sk], [sk], scale=1.0 / D, bias=self.epsT[:])
        recip(p, ss[:, 3:4], ss[:, 2:3], [sk], [sk])
        t, tk = self.tmp.next()
        stt(p, t[:, 0:512], po0[:], ss[:, 3:4], gg[:, 0:512], ALU.mult, ALU.mult, [k0, sk, "ggbc"], [tk])
        stt(p, t[:, 512:1024], po1[:], ss[:, 3:4], gg[:, 512:1024], ALU.mult, ALU.mult, [k1, sk, "ggbc"], [tk])
        tt(p, "pool", x, x, t[:], ALU.add, [tk, xk], [xk])


def build_out_ffn(p, T, l, li, even, tm_chunks, identb, epsT, modA2, modB2, ggbc1, ggbc2):
    X, MIXT, MIXF = T["X"], T["MIXT"], T["MIXF"]
    w_out = T["w_out_e"][li] if even else T["w_out_o"][li]
    ntm = len(tm_chunks)
    c0 = tm_chunks[0]
    with p.phase():
        st = RR(p, 2, [128, 1024], F32, "wst")
        wout = p.sbuf([128, 8, D], BF16, "wout")
        load_w_bf16(p, wout, "wout", w_out, D, st)
        pn = PostNorm(p, epsT)
        xs = RR(p, 2, [128, 4, D], F32, "xs")
        mT = RR(p, 2, [128, 8, 512], BF16, "mT")
        mx = RR(p, 2, [128, 4, ntm * 128], BF16, "mx")
        pso = RR(p, 4, [128, 512], F32, "pso", psum=True)
        pst = RR(p, 2, [128, 1024], BF16, "pst", psum=True)
        for u in range(9):
            t0 = u * 512
            cd = unit_cd(u)
            xt, xk = xs.next()
            p.dma(xt[:], xview(X, t0), ["X%d" % u], [xk], eng="act")
            m, mk = mT.next()
            for kc in range(8):
                if kc not in tm_chunks:
                    p.dma(m[:, kc, :], MIXF[kc * 128:(kc + 1) * 128, t0:t0 + 512], ["MIXF"], [mk], eng="sp")
            mxt, mxk = mx.next()
            p.dma(mxt[:], MIXT[t0:t0 + 512, c0 * 128:(c0 + ntm) * 128].rearrange("(i q) c -> q i c", q=128), ["MIXT"], [mxk])
            for kc in tm_chunks:
                ps, pk = pst.next()
                for i in range(4):
                    trp(p, ps[:, i * 128:(i + 1) * 128], mxt[:, i, (kc - c0) * 128:(kc - c0 + 1) * 128], identb[:], [mxk, "identb"], [pk])
                cp(p, "act" if kc % 2 else "dve", m[:, kc, :], ps[:, 0:512], [pk], [mk])
            for i in range(4):
                po0, k0 = pso.next()
                po1, k1 = pso.next()
                for half, (po, pk) in enumerate(((po0, k0), (po1, k1))):
                    for kc in range(8):
                        mm(p, po[:], m[:, kc, i * 128:(i + 1) * 128], wout[:, kc, half * 512:(half + 1) * 512], kc == 0, kc == 7,
                           [mk, "wout"], [pk])
                pn.run(po0, k0, po1, k1, xt[:, i, :], xk, ggbc1[cd])
            p.dma(xview(X, t0), xt[:], [xk], ["X%d" % u], eng="sp")
    WG, WU = T["WG"][l], T["WU"][l]
    with p.phase():
        st = RR(p, 2, [128, 1024], F32, "wst")
        wd = p.sbuf([128, NFC, D], BF16, "wd")
        load_w_bf16(p, wd, "wd", T["wd"][l], D, st, nkc=NFC)
        pn = PostNorm(p, epsT)
        nctx = NormCtx(p, identb, epsT)
        xs = RR(p, 2, [128, 4, D], F32, "xs")
        hT = RR(p, 1, [128, 8, 512], BF16, "hT")
        actT = p.sbuf([128, NFC, 512], BF16, "actT")
        wgu = RR(p, 2, [128, 2, 8, 256], BF16, "wgu")
        sg = RR(p, 2, [128, 512], F32, "sg")
        pso = RR(p, 4, [128, 512], F32, "pso", psum=True)
        for u in range(9):
            t0 = u * 512
            cd = unit_cd(u)
            xt, xk = xs.next()
            p.dma(xt[:], xview(X, t0), ["X%d" % u], [xk], eng="act")
            h, hk = hT.next()
            nctx.run(xt, xk, modA2, modB2, cd, h, hk)
            for pc in range(11):
                w, wk = wgu.next()
                p.dma(w[:, 0], WG[pc], ["wscr"], [wk], eng="sp")
                p.dma(w[:, 1], WU[pc], ["wscr"], [wk], eng="act")
                for f2 in range(2):
                    fc = pc * 2 + f2
                    pg, kg = pso.next()
                    pu, ku = pso.next()
                    for kc in range(8):
                        mm(p, pg[:], w[:, 0, kc, f2 * 128:(f2 + 1) * 128], h[:, kc, :], kc == 0, kc == 7, [wk, hk], [kg])
                    for kc in range(8):
                        mm(p, pu[:], w[:, 1, kc, f2 * 128:(f2 + 1) * 128], h[:, kc, :], kc == 0, kc == 7, [wk, hk], [ku])
                    s, sk = sg.next()
                    act(p, s[:], pg[:], AF.Silu, [kg], [sk])
                    tt(p, "dve", actT[:, fc, :], pu[:], s[:], ALU.mult, [ku, sk], ["actT"])
            for i in range(4):
                po0, k0 = pso.next()
                po1, k1 = pso.next()
                for half, (po, pk) in enumerate(((po0, k0), (po1, k1))):
                    for fc in range(NFC):
                        mm(p, po[:], actT[:, fc, i * 128:(i + 1) * 128], wd[:, fc, half * 512:(half + 1) * 512], fc == 0, fc == NFC - 1,
                           ["actT", "wd"], [pk])
                pn.run(po0, k0, po1, k1, xt[:, i, :], xk, ggbc2[cd])
            p.dma(xview(X, t0), xt[:], [xk], ["X%d" % u], eng="sp")


def build_even_proj(p, T, l, li, identb, identf, epsT, modA1, modB1):
    X = T["X"]
    QAT, KAT, VA, QBT, KBT, VB = T["QAT"], T["KAT"], T["VA"], T["QBT"], T["KBT"], T["VB"]
    with p.phase():
        st = RR(p, 2, [128, 1024], F32, "wst")
        win = p.sbuf([128, 8, 3072], BF16, "win")
        wrot = p.sbuf([128, 8, 1024], BF16, "wrot")
        load_w_bf16(p, win, "win", T["w_in_e"][li], 3072, st)
        for (s0, d0) in ((1536, 0), (2048, 512)):
            sv = win[:, :, s0:s0 + 512].rearrange("q k (b two s) -> q k b two s", b=16, two=2, s=16)
            dv = wrot[:, :, d0:d0 + 512].rearrange("q k (b two s) -> q k b two s", b=16, two=2, s=16)
            ts(p, "dve", dv[:, :, :, 0, :], sv[:, :, :, 1, :], -1.0, None, ALU.mult, None, ["win"], ["wrot"])
            cp(p, "pool", dv[:, :, :, 1, :], sv[:, :, :, 0, :], ["win"], ["wrot"])
        nctx = NormCtx(p, identb, epsT)
        xs = RR(p, 1, [128, 4, D], F32, "xs")
        hT = RR(p, 2, [128, 8, 512], BF16, "hT")
        rcs = RR(p, 2, [128, 2, 512], F32, "rcs")
        obf = RR(p, 4, [128, 512], BF16, "obf")
        o32 = RR(p, 2, [128, 512], F32, "o32")
        t12 = RR(p, 4, [128, 512], F32, "t12")
        pso = RR(p, 5, [128, 512], F32, "pso", psum=True)
        pst = nctx.pst
        ctile = RR(p, 2, [128, 512], F32, "ctile")
        cb16 = RR(p, 2, [128, 512], BF16, "cb16")
        ktmp = RR(p, 2, [128, 4, 128], BF16, "ktmp")
        for (ck, cv, KTd, Vd) in ((T["cak"], T["cav"], KAT, VA), (T["cbk"], T["cbv"], KBT, VB)):
            for i in range(4):
                a, ak = ctile.next()
                p.dma(a[:], ck[li, i * 128:(i + 1) * 128, :], (), [ak], eng="act")
                b, bk = cb16.next()
                cp(p, "dve", b[:], a[:], [ak], [bk])
                kt, kk = ktmp.next()
                ps, pk = pst.next()
                for c in range(4):
                    trp(p, ps[:, c * 128:(c + 1) * 128], b[:, c * 128:(c + 1) * 128], identb[:], [bk, "identb"], [pk])
                cp(p, "act", kt[:].rearrange("q c t -> q (c t)"), ps[:, 0:512], [pk], [kk])
                p.dma(KTd[:, NS + i * 128:NS + (i + 1) * 128].rearrange("(c q) t -> q c t", q=128), kt[:], [kk], ["KT"])
                a, ak = ctile.next()
                p.dma(a[:], cv[li, i * 128:(i + 1) * 128, :], (), [ak], eng="act")
                b, bk = cb16.next()
                cp(p, "pool", b[:], a[:], [ak], [bk])
                p.dma(Vd[NS + i * 128:NS + (i + 1) * 128, :], b[:], [bk], ["V"])

        def fm_group(W, col0, h, hk, n):
            ps, pk = pso.next()
            for kc in range(8):
                mm(p, ps[:, 0:n], W[:, kc, col0:col0 + 128], h[:, kc, 0:n], kc == 0, kc == 7, ["win", "wrot", hk], [pk])
            return ps, pk

        for u in range(9):
            t0 = u * 512
            cd = unit_cd(u)
            xt, xk = xs.next()
            p.dma(xt[:], xview(X, t0), ["X%d" % u], [xk], eng="act")
            h, hk = hT.next()
            nctx.run(xt, xk, modA1, modB1, cd, h, hk)
            dcol = t0 if cd == 0 else NT
            if cd == 0:
                rc, rk = rcs.next()
                p.dma(rc[:, 0, :], T["ropeC"][:, t0:t0 + 512], (), [rk])
                p.dma(rc[:, 1, :], T["ropeS"][:, t0:t0 + 512], (), [rk])
            k = 0
            for (dst, wc0, rot0, dkey) in ((QAT, 0, None, "QT"), (KAT, 512, None, "KT"), (QBT, 1536, 0, "QT"), (KBT, 2048, 512, "KT")):
                for c in range(4):
                    ps, pk = fm_group(win, wc0 + c * 128, h, hk, 512)
                    o, ok = obf.next()
                    if cd == 0 and rot0 is not None:
                        ps2, pk2 = fm_group(wrot, rot0 + c * 128, h, hk, 512)
                        a, ak = t12.next()
                        b, bk = t12.next()
                        tt(p, "dve", a[:], ps[:], rc[:, 0, :], ALU.mult, [pk, rk], [ak])
                        tt(p, "dve", b[:], ps2[:], rc[:, 1, :], ALU.mult, [pk2, rk], [bk])
                        tt(p, "pool", o[:], a[:], b[:], ALU.add, [ak, bk], [ok])
                    else:
                        cp(p, "act" if k % 2 else "dve", o[:], ps[:], [pk], [ok])
                    k += 1
                    p.dma(dst[c * 128:(c + 1) * 128, dcol:dcol + 512], o[:], [ok], [dkey])
            for i in range(4):
                for (wc0, Vd, outk, outv) in ((512, None, "o_ak", None), (1024, VA, None, "o_av"), (2048, None, "o_bk", None), (2560, VB, None, "o_bv")):
                    if cd == 0 and Vd is None:
                        continue
                    ps, pk = pso.next()
                    for kc in range(8):
                        mm(p, ps[:], h[:, kc, i * 128:(i + 1) * 128], win[:, kc, wc0:wc0 + 512], kc == 0, kc == 7, ["win", hk], [pk])
                    if Vd is not None:
                        o, ok = obf.next()
                        cp(p, "act", o[:], ps[:], [pk], [ok])
                        p.dma(Vd[dcol + i * 128:dcol + (i + 1) * 128, :], o[:], [ok], ["V"])
                    if cd == 1:
                        o2, ok2 = o32.next()
                        cp(p, "act", o2[:], ps[:], [pk], [ok2])
                        nm = outk or outv
                        p.dma(T[nm][i // 2, li, (i % 2) * 128:(i % 2 + 1) * 128, :], o2[:], [ok2], [nm + str(li)])


class AttnCtx:
    def __init__(self, p):
        self.p = p
        self.pss = RR(p, 3, [128, 512], F32, "pss", psum=True)
        self.pacc = RR(p, 4, [128, 512], F32, "pacc", psum=True)
        self.E = RR(p, 3, [128, 512], BF16, "E")
        self.small = RR(p, 8, [128, 4], F32, "asmall")


def attn_block(p, C, KT, ktkey, QT, qkey, nq, Vaug, vkey, nkt, dv, finish):
    nqt = (nq + 127) // 128
    accs = [C.pacc.next() for _ in range(nqt)]

    def qk(kt):
        ps, pk = C.pss.next()
        mm(p, ps[:, 0:nq], KT[:, kt * 128:(kt + 1) * 128], QT[:, 0:nq], True, True, [ktkey, qkey], [pk])
        e, ek = C.E.next()
        act(p, e[:, 0:nq], ps[:, 0:nq], AF.Exp, [pk], [ek], scale=0.125)
        return e, ek

    pend = qk(0)
    for kt in range(nkt):
        nxt = qk(kt + 1) if kt + 1 < nkt else None
        e, ek = pend
        for i in range(nqt):
            mm(p, accs[i][0][:, 0:dv + 1], e[:, i * 128:(i + 1) * 128], Vaug[:, kt, :], kt == 0, kt == nkt - 1,
               [ek, vkey], [accs[i][1]])
        pend = nxt
    for i in range(nqt):
        finish(i, accs[i][0], accs[i][1])


def build_even_attn(p, T, l, li, lam_init):
    QAT, KAT, VA, QBT, KBT, VB = T["QAT"], T["KAT"], T["VA"], T["QBT"], T["KBT"], T["VB"]
    MIXT, RPBP = T["MIXT"], T["RPBP"]
    with p.phase():
        C = AttnCtx(p)
        lamt = p.sbuf([128, 4, 64], F32, "lamt")
        lsm = p.sbuf([128, 8], F32, "lsm")
        gsb = p.sbuf([128, 128], F32, "gsb")
        lj = p.sbuf([128, 64], F32, "lj")
        p.dma(lamt[:].rearrange("q a b -> q (a b)"), T["lamv"][li:li + 1].rearrange("o a b -> o (a b)").to_broadcast([128, 256]), (), ["lamt"])
        p.dma(gsb[:], T["gsub"][li:li + 1, :].to_broadcast([128, 128]), (), ["gsb"])
        for j in range(2):
            tt(p, "dve", lj[:], lamt[:, 2 * j, :], lamt[:, 2 * j + 1, :], ALU.mult, ["lamt"], ["lj"])
            act(p, lj[:], lj[:], AF.Identity, ["lj"], ["lj", "lsm"], accum=lsm[:, j:j + 1])
        act(p, lsm[:, 2:4], lsm[:, 0:2], AF.Exp, ["lsm"], ["lsm"])
        tt(p, "dve", lsm[:, 4:5], lsm[:, 3:4], lsm[:, 2:3], ALU.subtract, ["lsm"], ["lsm"])
        ts(p, "dve", lsm[:, 5:6], lsm[:, 4:5], -lam_init, None, ALU.add, None, ["lsm"], ["lsm"])
        ts(p, "dve", gsb[:], gsb[:], 1.0 - lam_init, None, ALU.mult, None, ["gsb"], ["gsb"])
        neglam = lsm[:, 5:6]

        om = [p.sbuf([128, 4, 128], F32, "om%d" % s) for s in range(2)]
        ocomb = RR(p, 2, [128, 128], F32, "ocomb")
        ojunk = RR(p, 2, [128, 128], F32, "ojunk")

        def diff_finish(s):
            def f(i, acc, ak):
                sm, sk = C.small.next()
                recip(p, sm[:, 0:1], acc[:, 128:129], [ak], [sk])
                ts(p, "dve", om[s][:, i, :], acc[:, 0:128], sm[:, 0:1], None, ALU.mult, None, [ak, sk], ["om%d" % s])
            return f

        def diff_combine(i, dst, dkey):
            o, ok = ocomb.next()
            stt(p, o[:], om[1][:, i, :], neglam, om[0][:, i, :], ALU.mult, ALU.add, ["om0", "om1", "lsm"], [ok])
            sm, sk = C.small.next()
            j, jk = ojunk.next()
            act(p, j[:], o[:], AF.Square, [ok], [jk, sk], accum=sm[:, 0:1])
            act(p, sm[:, 1:2], sm[:, 0:1], AF.Sqrt, [sk], [sk], scale=1.0 / 128, bias=EPS_T[0][:])
            recip(p, sm[:, 2:3], sm[:, 1:2], [sk], [sk])
            stt(p, dst, o[:], sm[:, 2:3], gsb[:], ALU.mult, ALU.mult, [ok, sk, "gsb"], [dkey])

        ktb = RR(p, 2, [64, NT], BF16, "ktb")
        vaug = RR(p, 2, [128, 36, 129], BF16, "vaug")
        qtb = RR(p, 2, [64, 512], BF16, "qtb")
        obuf = RR(p, 2, [128, 4, 128], BF16, "obufB")
        for h in range(4):
            v, vk = vaug.next()
            mset(p, "pool", v[:], 1.0, [vk])
            for t9 in range(4):
                p.dma(v[:, t9 * 9:(t9 + 1) * 9, 0:128],
                      VB[t9 * 1152:(t9 + 1) * 1152, h * 128:(h + 1) * 128].rearrange("(t q) d -> q t d", q=128), ["V"], [vk],
                      eng=("act" if t9 % 2 else "sp"))
            kts = []
            for s in range(2):
                kt_, kk = ktb.next()
                p.dma(kt_[:], KBT[h * 128 + s * 64:h * 128 + (s + 1) * 64, 0:NT], ["KT"], [kk], eng="act")
                kts.append((kt_, kk))
            for qb in range(8):
                for s in range(2):
                    q, qk = qtb.next()
                    p.dma(q[:], QBT[h * 128 + s * 64:h * 128 + (s + 1) * 64, qb * 512:(qb + 1) * 512], ["QT"], [qk])
                    attn_block(p, C, kts[s][0], kts[s][1], q, qk, 512, v, vk, 36, 128, diff_finish(s))
                ob, obk = obuf.next()
                for i in range(4):
                    diff_combine(i, ob[:, i, :], obk)
                p.dma(MIXT[qb * 512:(qb + 1) * 512, 512 + h * 128:512 + (h + 1) * 128].rearrange("(i q) c -> q i c", q=128), ob[:],
                      [obk], ["MIXT"])

        ktp = RR(p, 2, [64, 256], BF16, "ktp")
        qtp = RR(p, 2, [64, 256], BF16, "qtp")
        vap = RR(p, 2, [128, 2, 65], BF16, "vap")
        vbp = RR(p, 2, [128, 2, 129], BF16, "vbp")
        obp = RR(p, 2, [128, 2, D], BF16, "obp")
        for js in range(2):
            tb = NT + 256 * js
            ob, obk = obp.next()
            for h in range(8):
                v, vk = vap.next()
                mset(p, "pool", v[:], 1.0, [vk])
                p.dma(v[:, :, 0:64], VA[tb:tb + 256, h * 64:(h + 1) * 64].rearrange("(t q) d -> q t d", q=128), ["V"], [vk], eng="act")
                kt_, kk = ktp.next()
                p.dma(kt_[:], KAT[h * 64:(h + 1) * 64, tb:tb + 256], ["KT"], [kk], eng="act")
                q, qk = qtp.next()
                p.dma(q[:], QAT[h * 64:(h + 1) * 64, tb:tb + 256], ["QT"], [qk])

                def fin_a(i, acc, ak, h=h, ob=ob, obk=obk):
                    sm, sk = C.small.next()
                    recip(p, sm[:, 0:1], acc[:, 64:65], [ak], [sk])
                    ts(p, "dve", ob[:, i, h * 64:(h + 1) * 64], acc[:, 0:64], sm[:, 0:1], None, ALU.mult, None, [ak, sk], [obk])
                attn_block(p, C, kt_, kk, q, qk, 256, v, vk, 2, 64, fin_a)
            for h in range(4):
                v, vk = vbp.next()
                mset(p, "pool", v[:], 1.0, [vk])
                p.dma(v[:, :, 0:128], VB[tb:tb + 256, h * 128:(h + 1) * 128].rearrange("(t q) d -> q t d", q=128), ["V"], [vk], eng="act")
                for s in range(2):
                    kt_, kk = ktp.next()
                    p.dma(kt_[:], KBT[h * 128 + s * 64:h * 128 + (s + 1) * 64, tb:tb + 256], ["KT"], [kk], eng="act")
                    q, qk = qtp.next()
                    p.dma(q[:], QBT[h * 128 + s * 64:h * 128 + (s + 1) * 64, tb:tb + 256], ["QT"], [qk])
                    attn_block(p, C, kt_, kk, q, qk, 256, v, vk, 2, 128, diff_finish(s))
                for i in range(2):
                    diff_combine(i, ob[:, i, 512 + h * 128:512 + (h + 1) * 128], obk)
            p.dma(MIXT[NS + 256 * js:NS + 256 * (js + 1), :].rearrange("(i q) c -> q i c", q=128), ob[:], [obk], ["MIXT"])

        zt = p.sbuf([120, 288], F32, "zt")
        mset(p, "dve", zt[:], 0.0, ["zt"])
        p.dma(RPBP.rearrange("h r c -> (h r) c"), zt[:], ["zt"], ["RPBP"])
        p.dma(RPBP[:, :, 128:159], T["rpb"][li], (), ["RPBP"])
        wm = p.sbuf([128, 64], F32, "wm")
        p.dma(wm[:], T["winmask"], (), ["wm"])
        mraw = RR(p, 2, [128, 32, 64], F32, "mraw")
        mh = RR(p, 2, [128, 32, 64], BF16, "mh")
        kta = RR(p, 2, [64, NT], BF16, "kta")
        qta = RR(p, 2, [64, NS], BF16, "qta")
        va_e = RR(p, 2, [128, 36, 65], BF16, "va_e")
        va_o = RR(p, 2, [128, 35, 65], BF16, "va_o")
        oba = RR(p, 2, [64, 64, 64], BF16, "oba")
        En = RR(p, 3, [128, 8, 64], BF16, "En")
        for h in range(8):
            mr, mrk = mraw.next()
            kq = 0
            for off in range(8):
                for j in range(4):
                    for mlow in range(2):
                        dr = 2 * j + mlow + off
                        base = RPBP[h, dr, 80:144]
                        src = bass.AP(base.tensor, base.offset, [[1, 64], [1, 64]])
                        p.dma(mr[mlow * 64:(mlow + 1) * 64, off * 4 + j, :], src, ["RPBP"], [mrk], eng=("sp" if kq % 2 else "act"))
                        kq += 1
            act(p, mr[:], mr[:], AF.Exp, [mrk], [mrk])
            m_, mk_ = mh.next()
            tt(p, "pool", m_[:], mr[:], wm[:].unsqueeze(1).to_broadcast([128, 32, 64]), ALU.mult, [mrk, "wm"], [mk_])
            kt_, kk = kta.next()
            p.dma(kt_[:], KAT[h * 64:(h + 1) * 64, 0:NT], ["KT"], [kk], eng="act")
            q, qk = qta.next()
            p.dma(q[:], QAT[h * 64:(h + 1) * 64, 0:NS], ["QT"], [qk], eng="act")
            ve, vek = va_e.next()
            mset(p, "pool", ve[:], 1.0, [vek])
            for t9 in range(4):
                p.dma(ve[:, t9 * 9:(t9 + 1) * 9, 0:64],
                      VA[t9 * 1152:(t9 + 1) * 1152, h * 64:(h + 1) * 64].rearrange("(t q) d -> q t d", q=128), ["V"], [vek],
                      eng=("act" if t9 % 2 else "sp"))
            vo, vok = va_o.next()
            mset(p, "pool", vo[:], 1.0, [vok])
            for t9 in range(5):
                p.dma(vo[:, t9 * 7:(t9 + 1) * 7, 0:64],
                      VA[64 + t9 * 896:64 + (t9 + 1) * 896, h * 64:(h + 1) * 64].rearrange("(t q) d -> q t d", q=128), ["V"], [vok],
                      eng=("act" if t9 % 2 else "sp"))
            ob, obk = oba.next()
            def na_stage1(r):
                rs = min(max(r - 4, 0), 56)
                off = rs - r + 7
                ps, pk = C.pss.next()
                psv = ps[:].rearrange("q (j c) -> q j c", c=64)
                for j in range(4):
                    k0 = rs * 64 + 128 * j
                    mm(p, psv[:, j, :], kt_[:, k0:k0 + 128], q[:, r * 64:(r + 1) * 64], True, True, [kk, qk], [pk])
                for j in range(4):
                    k0 = NS + 128 * j
                    mm(p, psv[:, 4 + j, :], kt_[:, k0:k0 + 128], q[:, r * 64:(r + 1) * 64], True, True, [kk, qk], [pk])
                e, ek = En.next()
                act(p, e[:], psv, AF.Exp, [pk], [ek], scale=0.125)
                tt(p, "dve", e[:, 0:4, :], e[:, 0:4, :], m_[:, off * 4:off * 4 + 4, ::-1], ALU.mult, [ek, mk_], [ek])
                return e, ek

            def na_stage2(r, e, ek):
                rs = min(max(r - 4, 0), 56)
                acc, ak = C.pacc.next()
                for j in range(8):
                    if j < 4:
                        if rs % 2 == 0:
                            vt = ve[:, rs // 2 + j, :]
                            vkk = vek
                        else:
                            vt = vo[:, (rs - 1) // 2 + j, :]
                            vkk = vok
                    else:
                        vt = ve[:, 32 + (j - 4), :]
                        vkk = vek
                    mm(p, acc[0:64, 0:65], e[:, j, :], vt, j == 0, j == 7, [ek, vkk], [ak])
                sm, sk = C.small.next()
                recip(p, sm[0:64, 0:1], acc[0:64, 64:65], [ak], [sk])
                ts(p, "dve", ob[:, r, :], acc[0:64, 0:64], sm[0:64, 0:1], None, ALU.mult, None, [ak, sk], [obk])

            pend = na_stage1(0)
            for r in range(64):
                nxt = na_stage1(r + 1) if r + 1 < 64 else None
                na_stage2(r, pend[0], pend[1])
                pend = nxt
            for r4 in range(4):
                p.dma(MIXT[r4 * 1024:(r4 + 1) * 1024, h * 64:(h + 1) * 64].rearrange("(r q) d -> q r d", q=64), ob[:, r4 * 16:(r4 + 1) * 16, :],
                      [obk], ["MIXT"])


EPS_T = [None]


def _fm(v, nch):
    v = np.asarray(v, np.float32)
    return np.ascontiguousarray(np.swapaxes(v.reshape(v.shape[:-1] + (nch, 128)), -1, -2))


def _pairs(a):
    a = np.asarray(a, np.float32)
    sh = a.shape[:-2]
    a = a.reshape(sh + (16, 2, 64))
    a = np.moveaxis(a, -3, -1)
    return np.ascontiguousarray(a.reshape(sh + (128, 16)))


def shared_inputs(inp):
    f = lambda a: np.ascontiguousarray(np.asarray(a, np.float32))
    S = {}
    S["w_mod"] = f(inp["w_mod"])
    S["b_mod"] = _fm(inp["b_mod"], 48)
    S["gpre1"] = _fm(inp["g_mix_pre"], 8); S["gpost1"] = _fm(inp["g_mix_post"], 8)
    S["gpre2"] = _fm(inp["g_ffn_pre"], 8); S["gpost2"] = _fm(inp["g_ffn_post"], 8)
    S["wg"] = f(inp["w_ffn_gate"]); S["wu"] = f(inp["w_ffn_up"]); S["wd"] = f(inp["w_ffn_down"])
    S["w_in_e"] = f(inp["w_in_e"]); S["w_out_e"] = f(inp["w_out_e"])
    S["rpb"] = f(inp["na_rpb"])
    S["lamv"] = np.ascontiguousarray(np.stack([f(inp["lam_q1"]), f(inp["lam_k1"]), f(inp["lam_q2"]), f(inp["lam_k2"])], axis=1))
    S["gsub"] = f(inp["g_subln"])
    S["w_in_o"] = f(inp["w_in_o"]); S["w_out_o"] = f(inp["w_out_o"])
    S["s_lre"] = _pairs(inp["ssm_lam_re"]); S["s_lim"] = _pairs(inp["ssm_lam_im"])
    ldt = np.asarray(inp["ssm_log_dt"], np.float32)
    S["s_ldt"] = _pairs(np.repeat(ldt[..., None], 64, axis=-1))
    for nm, src in (("s_bre", "ssm_b_re"), ("s_bim", "ssm_b_im")):
        b = np.asarray(inp[src], np.float32)
        b = np.moveaxis(b, -1, 2)
        b = _pairs(b)
        S[nm] = np.ascontiguousarray(np.moveaxis(b, 2, -1).reshape(2, 2, 128, 256))
    for nm, src in (("s_cre", "ssm_c_re"), ("s_cim", "ssm_c_im")):
        c = np.asarray(inp[src], np.float32)
        c = np.moveaxis(c, 3, 2)
        c = _pairs(c)
        S[nm] = np.ascontiguousarray(np.moveaxis(c, 2, -1).reshape(2, 2, 128, 256))
    S["s_d"] = _fm(inp["ssm_d"], 4)
    S["w_glu"] = f(inp["w_glu"]); S["b_glu"] = _fm(inp["b_glu"], 4)
    cw = np.asarray(inp["hy_conv_w"], np.float32)
    S["hcw"] = np.ascontiguousarray(np.moveaxis(_fm(cw, 12), 1, -1))
    S["hcb"] = _fm(inp["hy_conv_b"], 12)
    S["hw1"] = f(inp["hy_w1"]); S["hb1"] = f(inp["hy_b1"])[..., None]; S["hf1"] = f(inp["hy_fr1"])[..., None]
    S["hw2"] = f(inp["hy_w2"]); S["hb2"] = f(inp["hy_b2"])[..., None]; S["hf2"] = f(inp["hy_fr2"])[..., None]
    S["hw3"] = f(inp["hy_w3"])
    S["hdec"] = np.ascontiguousarray(np.asarray(inp["hy_decay"], np.float32).reshape(2, 4, 512))
    S["hbias"] = f(inp["hy_bias"])
    S.update(consts())
    return S


def core_inputs(inp, cid, S):
    m = dict(S)
    xs = np.asarray(inp["x_sample"][cid], np.float32)
    xp = np.asarray(inp["x_prompt"][2 * cid:2 * cid + 2], np.float32).reshape(512, D)
    m["x0"] = np.ascontiguousarray(np.concatenate([xs, xp], 0))
    for nm, src in (("cak", "cache_a_k"), ("cav", "cache_a_v"), ("cbk", "cache_b_k"), ("cbv", "cache_b_v")):
        m[nm] = np.ascontiguousarray(np.asarray(inp[src][cid], np.float32).reshape(2, 512, 512))
    m["sre"] = _pairs(inp["state_c_re"][cid]); m["sim"] = _pairs(inp["state_c_im"][cid])
    cc = np.stack([np.asarray(inp["c"][cid], np.float32), np.asarray(inp["c_ctx"], np.float32)], -1)
    m["cond"] = np.ascontiguousarray(cc.reshape(8, 128, 2).transpose(1, 0, 2))
    return m


_PROG = {}


def run_device(inputs, nlayers=DEPTH, cores=8):
    if nlayers not in _PROG:
        _PROG[nlayers] = build_program(nlayers)
    nc = _PROG[nlayers]
    S = shared_inputs(inputs)
    in_maps = []
    for cid in range(cores):
        m = core_inputs(inputs, cid, S)
        in_maps.append({k: m[k] for k in IN_SPECS})
    res = run_bass_kernel_spmd(nc, in_maps, core_ids=list(range(cores)))
    return res.results


def kernel(**inputs):
    R = run_device(inputs)
    yp = np.concatenate([r["o_yp"].reshape(2, 256, D) for r in R], 0)
    ys = np.stack([r["o_ys"] for r in R], 0)
    outs = [yp.astype(np.float32), ys.astype(np.float32)]
    for nm, (hh, dd) in (("o_ak", (8, 64)), ("o_av", (8, 64)), ("o_bk", (4, 128)), ("o_bv", (4, 128))):
        a = np.concatenate([r[nm] for r in R], 0)
        outs.append(np.ascontiguousarray(a.reshape(16, 2, 256, hh, dd)).astype(np.float32))
    for nm in ("o_sre", "o_sim"):
        a = np.concatenate([r[nm] for r in R], 0)
        outs.append(np.ascontiguousarray(a.reshape(16, 2, 2, 32, 64)).astype(np.float32))
    return tuple(outs)


def build_odd_proj(p, T, l, li, identb, epsT, modA1, modB1):
    X = T["X"]
    UT32, UTB, HYT = T["UT32"], T["UTB"], T["HYT"]
    with p.phase():
        st = RR(p, 2, [128, 1024], F32, "wst")
        win = p.sbuf([128, 8, 2048], BF16, "win")
        load_w_bf16(p, win, "win", T["w_in_o"][li], 2048, st)
        nctx = NormCtx(p, identb, epsT)
        xs = RR(p, 2, [128, 4, D], F32, "xs")
        hT = RR(p, 2, [128, 8, 512], BF16, "hT")
        o32 = RR(p, 3, [128, 512], F32, "o32")
        obf = RR(p, 2, [128, 512], BF16, "obf")
        pso = RR(p, 4, [128, 512], F32, "pso", psum=True)
        for u in range(9):
            t0 = u * 512
            cd = unit_cd(u)
            xt, xk = xs.next()
            p.dma(xt[:], xview(X, t0), ["X%d" % u], [xk], eng="act")
            h, hk = hT.next()
            nctx.run(xt, xk, modA1, modB1, cd, h, hk)
            for c in range(16):
                ps, pk = pso.next()
                for kc in range(8):
                    mm(p, ps[:], win[:, kc, c * 128:(c + 1) * 128], h[:, kc, :], kc == 0, kc == 7, ["win", hk], [pk])
                o, ok = o32.next()
                cp(p, "act" if c % 2 else "dve", o[:], ps[:], [pk], [ok])
                if c < 4:
                    p.dma(UT32[c * 128:(c + 1) * 128, t0:t0 + 512], o[:], [ok], ["UT32"])
                    b, bk = obf.next()
                    cp(p, "pool", b[:], o[:], [ok], [bk])
                    p.dma(UTB[c * 128:(c + 1) * 128, t0:t0 + 512], b[:], [bk], ["UTB"])
                else:
                    p.dma(HYT[(c - 4) * 128:(c - 3) * 128, t0:t0 + 512], o[:], [ok], ["HYT"])


def sin_turns(p, eng_i, out, turns, tmp_i, tmp_f, keys_r, keys_w, shift=0.0):
    r = list(keys_r)
    if shift != 0.0:
        ts(p, "dve", tmp_f, turns, shift, None, ALU.add, None, r, keys_w)
        src = tmp_f
    else:
        src = turns
    cp(p, "dve", tmp_i, src, r + list(keys_w), keys_w)
    cp(p, "dve", out, tmp_i, keys_w, keys_w)
    tt(p, "dve", out, src, out, ALU.subtract, r + list(keys_w), keys_w)
    act(p, out, out, AF.Sin, keys_w, keys_w, scale=TWO_PI)


SEQS = ((0, 4096, True), (4096, 256, False), (4352, 256, False))


def build_s5(p, T, l, li, identf):
    UTB, UT32, YS, MIXF = T["UTB"], T["UT32"], T["YS"], T["MIXF"]
    with p.phase():
        iota = p.sbuf([128, 128], F32, "iota")
        p.dma(iota[:], T["iota128"], (), ["iota"])
        prm = p.sbuf([128, 2, 3, 16], F32, "prm")
        wk_ = p.sbuf([128, 2, 12, 16], F32, "wk")
        wki = p.sbuf([128, 16], I32, "wki")
        Bl = p.sbuf([128, 2, 16, 2, 128], BF16, "Bl")
        Cl = p.sbuf([128, 2, 16, 2, 128], BF16, "Cl")
        cosT = p.sbuf([128, 2, 16, 128], F32, "cosT")
        sinT = p.sbuf([128, 2, 16, 128], F32, "sinT")
        rT = p.sbuf([128, 2, 16, 128], F32, "rT")
        bc = RR(p, 2, [128, 2, 16, 16], F32, "bc")
        bb = RR(p, 2, [128, 2, 16, 16], F32, "bb")
        spd = RR(p, 2, [128, 128], F32, "spd")
        ti = p.sbuf([128, 128], I32, "ti")
        tf = p.sbuf([128, 128], F32, "tf")
        ptr = RR(p, 1, [128, 512], F32, "ptr", psum=True)
        pbu = RR(p, 4, [128, 512], F32, "pbu", psum=True)
        pcy = RR(p, 2, [128, 512], F32, "pcy", psum=True)
        for d in range(2):
            for j, nm in enumerate(("s_lre", "s_lim", "s_ldt")):
                p.dma(prm[:, d, j, :], T[nm][li, d], (), ["prm"])
            lre, lim, ldt = prm[:, d, 0, :], prm[:, d, 1, :], prm[:, d, 2, :]
            W = lambda i: wk_[:, d, i, :]
            R, Wk = ["prm", "wk"], ["wk"]
            act(p, W(0), ldt, AF.Exp, R, Wk)
            tt(p, "dve", W(1), lre, W(0), ALU.mult, R, Wk)
            act(p, W(1), W(1), AF.Exp, R, Wk)
            tt(p, "dve", W(2), lim, W(0), ALU.mult, R, Wk)
            ts(p, "dve", W(2), W(2), 1.0 / TWO_PI, None, ALU.mult, None, R, Wk)
            cp(p, "dve", wki[:], W(2), R, ["wki"])
            cp(p, "dve", W(3), wki[:], ["wki"], Wk)
            tt(p, "dve", W(2), W(2), W(3), ALU.subtract, R, Wk)
            act(p, W(4), W(2), AF.Sin, R, Wk, scale=TWO_PI)
            ts(p, "dve", W(3), W(2), 0.25, None, ALU.add, None, R, Wk)
            cp(p, "dve", wki[:], W(3), R, ["wki"])
            cp(p, "dve", W(5), wki[:], ["wki"], Wk)
            tt(p, "dve", W(3), W(3), W(5), ALU.subtract, R, Wk)
            act(p, W(5), W(3), AF.Sin, R, Wk, scale=TWO_PI)
            tt(p, "dve", W(6), W(1), W(5), ALU.mult, R, Wk)
            tt(p, "dve", W(7), W(1), W(4), ALU.mult, R, Wk)
            tt(p, "dve", W(3), lre, lre, ALU.mult, R, Wk)
            tt(p, "dve", W(4), lim, lim, ALU.mult, R, Wk)
            tt(p, "dve", W(3), W(3), W(4), ALU.add, R, Wk)
            recip(p, W(3), W(3), R, Wk)
            ts(p, "dve", W(6), W(6), -1.0, None, ALU.add, None, R, Wk)
            tt(p, "dve", W(4), W(6), lre, ALU.mult, R, Wk)
            tt(p, "dve", W(5), W(7), lim, ALU.mult, R, Wk)
            tt(p, "dve", W(4), W(4), W(5), ALU.add, R, Wk)
            tt(p, "dve", W(8), W(4), W(3), ALU.mult, R, Wk)
            tt(p, "dve", W(4), W(7), lre, ALU.mult, R, Wk)
            tt(p, "dve", W(5), W(6), lim, ALU.mult, R, Wk)
            tt(p, "dve", W(4), W(4), W(5), ALU.subtract, R, Wk)
            tt(p, "dve", W(9), W(4), W(3), ALU.mult, R, Wk)
            b_, bk = bc.next()
            p.dma(b_[:, 0].rearrange("q k h -> q (k h)"), T["s_bre"][li, d], (), [bk])
            p.dma(b_[:, 1].rearrange("q k h -> q (k h)"), T["s_bim"][li, d], (), [bk])
            o_, ok = bb.next()
            kre = W(8).unsqueeze(2).to_broadcast([128, 16, 16])
            kim = W(9).unsqueeze(2).to_broadcast([128, 16, 16])
            t1, t1k = bb.next()
            tt(p, "dve", o_[:, 0], b_[:, 0], kre, ALU.mult, [bk] + R, [ok])
            tt(p, "dve", t1[:, 0], b_[:, 1], kim, ALU.mult, [bk] + R, [t1k])
            tt(p, "dve", o_[:, 0], o_[:, 0], t1[:, 0], ALU.subtract, [t1k], [ok])
            tt(p, "dve", o_[:, 1], b_[:, 1], kre, ALU.mult, [bk] + R, [ok])
            tt(p, "dve", t1[:, 1], b_[:, 0], kim, ALU.mult, [bk] + R, [t1k])
            tt(p, "dve", o_[:, 1], o_[:, 1], t1[:, 1], ALU.add, [t1k], [ok])
            c_, ck = bc.next()
            p.dma(c_[:, 0].rearrange("q k h -> q (k h)"), T["s_cre"][li, d], (), [ck])
            p.dma(c_[:, 1].rearrange("q k h -> q (k h)"), T["s_cim"][li, d], (), [ck])
            mset(p, "pool", Cl[:, d].rearrange("q k r m -> q (k r m)"), 0.0, ["Cl"])
            for k in range(16):
                gl0, gl1 = (2 * k) % 8, (2 * k + 1) % 8
                for ri in range(2):
                    s_, sk = spd.next()
                    mset(p, "pool", s_[:], 0.0, [sk])
                    cp(p, "dve", s_[0:64, gl0 * 16:(gl0 + 1) * 16], o_[0:64, ri, k, :], [ok], [sk])
                    cp(p, "dve", s_[64:128, gl1 * 16:(gl1 + 1) * 16], o_[64:128, ri, k, :], [ok], [sk])
                    ps, pk = ptr.next()
                    trp(p, ps[:, 0:128], s_[:], identf[:], [sk, "identf"], [pk])
                    cp(p, "act", Bl[:, d, k, ri, :], ps[:, 0:128], [pk], ["Bl"])
                    if ri == 0:
                        cp(p, "dve", Cl[0:64, d, k, 0, gl0 * 16:(gl0 + 1) * 16], c_[0:64, 0, k, :], [ck, "Cl"], ["Cl"])
                        cp(p, "dve", Cl[64:128, d, k, 0, gl1 * 16:(gl1 + 1) * 16], c_[64:128, 0, k, :], [ck, "Cl"], ["Cl"])
                    else:
                        ts(p, "dve", Cl[0:64, d, k, 1, gl0 * 16:(gl0 + 1) * 16], c_[0:64, 1, k, :], -1.0, None, ALU.mult, None, [ck, "Cl"], ["Cl"])
                        ts(p, "dve", Cl[64:128, d, k, 1, gl1 * 16:(gl1 + 1) * 16], c_[64:128, 1, k, :], -1.0, None, ALU.mult, None, [ck, "Cl"], ["Cl"])
                ts(p, "dve", tf[:], iota[:], wk_[:, d, 2, k:k + 1], None, ALU.mult, None, ["iota"] + R, ["tf"])
                cp(p, "dve", ti[:], tf[:], ["tf"], ["ti"])
                cp(p, "dve", sinT[:, d, k, :], ti[:], ["ti"], ["tab"])
                tt(p, "dve", tf[:], tf[:], sinT[:, d, k, :], ALU.subtract, ["tab"], ["tf"])
                act(p, sinT[:, d, k, :], tf[:], AF.Sin, ["tf"], ["tab"], scale=TWO_PI)
                ts(p, "dve", tf[:], tf[:], 0.25, None, ALU.add, None, ["tf"], ["tf"])
                cp(p, "dve", ti[:], tf[:], ["tf"], ["ti"])
                cp(p, "dve", cosT[:, d, k, :], ti[:], ["ti"], ["tab"])
                tt(p, "dve", tf[:], tf[:], cosT[:, d, k, :], ALU.subtract, ["tab"], ["tf"])
                act(p, cosT[:, d, k, :], tf[:], AF.Sin, ["tf"], ["tab"], scale=TWO_PI)
                cp(p, "pool", rT[:, d, k, :], wk_[:, d, 1, k:k + 1].to_broadcast([128, 128]), R, ["tab"])

        ut = RR(p, 2, [128, 4, 512], BF16, "ut")
        xre = p.sbuf([128, 16, 512], BF16, "xre")
        xim = p.sbuf([128, 16, 512], BF16, "xim")
        m4 = RR(p, 8, [128, 4, 128], F32, "m4")
        wz = RR(p, 16, [128, 4, 128], F32, "wz")
        carry = p.sbuf([128, 2, 16], F32, "carry")
        cst = RR(p, 8, [128, 2], F32, "cst")
        o32 = RR(p, 2, [128, 512], F32, "o32")
        fst = RR(p, 2, [16, 128], F32, "fst")
        for si, (toff, L, has_init) in enumerate(SEQS):
            bs = min(512, L)
            nblk = L // bs
            nch = bs // 128
            for d in range(2):
                if has_init:
                    p.dma(carry[:, 0, :], T["sre"][li, d], (), ["carry%d" % k for k in range(16)])
                    p.dma(carry[:, 1, :], T["sim"][li, d], (), ["carry%d" % k for k in range(16)])
                else:
                    mset(p, "dve", carry[:].rearrange("q a k -> q (a k)"), 0.0, ["carry%d" % k for k in range(16)])
                blks = range(nblk) if d == 0 else range(nblk - 1, -1, -1)
                for blk in blks:
                    c0 = toff + blk * bs
                    u_, uk = ut.next()
                    p.dma(u_[:, :, 0:bs], UTB[:, c0:c0 + bs].rearrange("(c q) t -> q c t", q=128), ["UTB"], [uk])
                    csb_ = lambda k: cosT[:, d, k, :].unsqueeze(1).to_broadcast([128, nch, 128])
                    snb_ = lambda k: sinT[:, d, k, :].unsqueeze(1).to_broadcast([128, nch, 128])

                    def V3(a, d=d):
                        v = a.rearrange("q (c j) -> q c j", j=128)
                        return v if d == 0 else v[:, :, ::-1]
                    chs = list(range(nch)) if d == 0 else list(range(nch - 1, -1, -1))
                    for kg in range(4):
                        ks = list(range(kg * 4, kg * 4 + 4))
                        W_ = {}
                        for k in ks:
                            pr, prk = pbu.next()
                            pi_, pik = pbu.next()
                            mm(p, pr[:, 0:bs], Bl[:, d, k, 0, :], u_[:, k // 4, 0:bs], True, True, ["Bl", uk], [prk])
                            mm(p, pi_[:, 0:bs], Bl[:, d, k, 1, :], u_[:, k // 4, 0:bs], True, True, ["Bl", uk], [pik])
                            bre, bim = V3(pr[:, 0:bs]), V3(pi_[:, 0:bs])
                            a1, a1k = m4.next(); a2, a2k = m4.next(); a3, a3k = m4.next(); a4, a4k = m4.next()
                            tt(p, "dve", a1[:, 0:nch, :], bre, csb_(k), ALU.mult, [prk, "tab"], [a1k])
                            tt(p, "dve", a2[:, 0:nch, :], bim, snb_(k), ALU.mult, [pik, "tab"], [a2k])
                            tt(p, "dve", a3[:, 0:nch, :], bim, csb_(k), ALU.mult, [pik, "tab"], [a3k])
                            tt(p, "dve", a4[:, 0:nch, :], bre, snb_(k), ALU.mult, [prk, "tab"], [a4k])
                            wr, wrk = wz.next(); wi, wik = wz.next()
                            tt(p, "pool", wr[:, 0:nch, :], a1[:, 0:nch, :], a2[:, 0:nch, :], ALU.add, [a1k, a2k], [wrk])
                            tt(p, "pool", wi[:, 0:nch, :], a3[:, 0:nch, :], a4[:, 0:nch, :], ALU.subtract, [a3k, a4k], [wik])
                            zr, zrk = wz.next(); zi, zik = wz.next()
                            W_[k] = (wr, wrk, wi, wik, zr, zrk, zi, zik)
                        for ch in chs:
                            c2s = {}
                            for k in ks:
                                wr, wrk, wi, wik, zr, zrk, zi, zik = W_[k]
                                r_ = rT[:, d, k, :]
                                ck_ = "carry%d" % k
                                p.op("dve", lambda e, zr=zr, wr=wr, r_=r_, k=k, ch=ch: e.tensor_tensor_scan(out=zr[:, ch, :], data0=r_, data1=wr[:, ch, :], initial=carry[:, 0, k:k + 1], op0=ALU.mult, op1=ALU.add),
                                     [wrk, "tab", ck_], [zrk])
                                p.op("dve", lambda e, zi=zi, wi=wi, r_=r_, k=k, ch=ch: e.tensor_tensor_scan(out=zi[:, ch, :], data0=r_, data1=wi[:, ch, :], initial=carry[:, 1, k:k + 1], op0=ALU.mult, op1=ALU.add),
                                     [wik, "tab", ck_], [zik])
                            for k in ks:
                                wr, wrk, wi, wik, zr, zrk, zi, zik = W_[k]
                                cs_, sn_ = cosT[:, d, k, :], sinT[:, d, k, :]
                                c2, c2k = cst.next()
                                c2s[k] = (c2, c2k)
                                tt(p, "dve", c2[:, 0:1], zi[:, ch, 127:128], sn_[:, 127:128], ALU.mult, [zik, "tab"], [c2k])
                                tt(p, "dve", c2[:, 1:2], zi[:, ch, 127:128], cs_[:, 127:128], ALU.mult, [zik, "tab"], [c2k])
                            for k in ks:
                                wr, wrk, wi, wik, zr, zrk, zi, zik = W_[k]
                                cs_, sn_ = cosT[:, d, k, :], sinT[:, d, k, :]
                                c2, c2k = c2s[k]
                                ck_ = "carry%d" % k
                                stt(p, carry[:, 0, k:k + 1], zr[:, ch, 127:128], cs_[:, 127:128], c2[:, 0:1], ALU.mult, ALU.subtract, [zrk, c2k, "tab"], [ck_])
                                stt(p, carry[:, 1, k:k + 1], zr[:, ch, 127:128], sn_[:, 127:128], c2[:, 1:2], ALU.mult, ALU.add, [zrk, c2k, "tab"], [ck_])
                        for k in ks:
                            wr, wrk, wi, wik, zr, zrk, zi, zik = W_[k]
                            n1, n1k = m4.next(); n2, n2k = m4.next(); n3, n3k = m4.next(); n4, n4k = m4.next()
                            tt(p, "pool", n1[:, 0:nch, :], zr[:, 0:nch, :], csb_(k), ALU.mult, [zrk, "tab"], [n1k])
                            tt(p, "dve", n2[:, 0:nch, :], zi[:, 0:nch, :], snb_(k), ALU.mult, [zik, "tab"], [n2k])
                            tt(p, "dve", n3[:, 0:nch, :], zr[:, 0:nch, :], snb_(k), ALU.mult, [zrk, "tab"], [n3k])
                            tt(p, "pool", n4[:, 0:nch, :], zi[:, 0:nch, :], csb_(k), ALU.mult, [zik, "tab"], [n4k])
                            tt(p, "pool", V3(xre[:, k, 0:bs]), n1[:, 0:nch, :], n2[:, 0:nch, :], ALU.subtract, [n1k, n2k], ["xre"])
                            tt(p, "pool", V3(xim[:, k, 0:bs]), n3[:, 0:nch, :], n4[:, 0:nch, :], ALU.add, [n3k, n4k], ["xim"])
                    for cc in range(4):
                        ps, pk = pcy.next()
                        for kk in range(4):
                            k = cc * 4 + kk
                            mm(p, ps[:, 0:bs], Cl[:, d, k, 0, :], xre[:, k, 0:bs], kk == 0, False, ["Cl", "xre"], [pk])
                            mm(p, ps[:, 0:bs], Cl[:, d, k, 1, :], xim[:, k, 0:bs], False, kk == 3, ["Cl", "xim"], [pk])
                        o, ok = o32.next()
                        cp(p, "act", o[:, 0:bs], ps[:, 0:bs], [pk], [ok])
                        p.dma(YS[d, cc * 128:(cc + 1) * 128, c0:c0 + bs], o[:, 0:bs], [ok], ["YS"])
                if not has_init:
                    for ri, nm in enumerate(("o_sre", "o_sim")):
                        ps, pk = ptr.next()
                        trp(p, ps[0:16, 0:128], carry[:, ri, :], identf[:], ["carry%d" % k for k in range(16)] + ["identf"], [pk])
                        f_, fk = fst.next()
                        cp(p, "act", f_[:], ps[0:16, 0:128], [pk], [fk])
                        p.dma(T[nm][si - 1, li, d], f_[:], [fk], [nm + "%d_%d_%d" % (si, li, d)])

    with p.phase():
        st = RR(p, 2, [128, 1024], F32, "wst")
        wgl = p.sbuf([128, 4, 512], BF16, "wgl")
        load_w_bf16(p, wgl, "wgl", T["w_glu"][li], 512, st, nkc=4)
        dsk = p.sbuf([128, 4], F32, "dsk"); bgl = p.sbuf([128, 4], F32, "bgl")
        p.dma(dsk[:], T["s_d"][li], (), ["dsk"]); p.dma(bgl[:], T["b_glu"][li], (), ["bgl"])
        yl = RR(p, 3, [128, 3, 512], F32, "yl")
        g32 = RR(p, 2, [128, 4, 512], F32, "g32")
        gb = RR(p, 2, [128, 4, 512], BF16, "gb")
        sg = RR(p, 2, [128, 512], F32, "sg")
        ob = RR(p, 2, [128, 512], BF16, "ob")
        pso = RR(p, 3, [128, 512], F32, "pso", psum=True)
        for u in range(9):
            t0 = u * 512
            g, gk = g32.next()
            g2, g2k = gb.next()
            for cc in range(4):
                y, yk = yl.next()
                p.dma(y[:, 0, :], YS[0, cc * 128:(cc + 1) * 128, t0:t0 + 512], ["YS"], [yk], eng="act")
                p.dma(y[:, 1, :], YS[1, cc * 128:(cc + 1) * 128, t0:t0 + 512], ["YS"], [yk], eng="act")
                p.dma(y[:, 2, :], UT32[cc * 128:(cc + 1) * 128, t0:t0 + 512], ["UT32"], [yk], eng="sp")
                tt(p, "pool", y[:, 0, :], y[:, 0, :], y[:, 1, :], ALU.add, [yk], [yk])
                stt(p, y[:, 0, :], y[:, 2, :], dsk[:, cc:cc + 1], y[:, 0, :], ALU.mult, ALU.add, [yk, "dsk"], [yk])
                act(p, g[:, cc, :], y[:, 0, :], AF.Gelu, [yk], [gk])
                cp(p, "pool", g2[:, cc, :], g[:, cc, :], [gk], [g2k])
            for oc in range(4):
                ps, pk = pso.next()
                for kc in range(4):
                    mm(p, ps[:], wgl[:, kc, oc * 128:(oc + 1) * 128], g2[:, kc, :], kc == 0, kc == 3, ["wgl", g2k], [pk])
                s, sk = sg.next()
                act(p, s[:], ps[:], AF.Sigmoid, [pk, "bgl"], [sk], bias=bgl[:, oc:oc + 1])
                o, ok = ob.next()
                tt(p, "dve", o[:], g[:, oc, :], s[:], ALU.mult, [gk, sk], [ok])
                p.dma(MIXF[oc * 128:(oc + 1) * 128, t0:t0 + 512], o[:], [ok], ["MIXF"])


def build_hyena(p, T, l, li, identf):
    HYT, HYTM, Z1F, MIXT = T["HYT"], T["HYTM"], T["Z1F"], T["MIXT"]
    with p.phase():
        cw = p.sbuf([128, 12, 3], F32, "cw"); cb = p.sbuf([128, 12], F32, "cb")
        p.dma(cw[:], T["hcw"][li], (), ["cw"]); p.dma(cb[:], T["hcb"][li], (), ["cw"])
        raw = RR(p, 2, [128, 4098], F32, "raw")
        sv = RR(p, 2, [128, 4096], F32, "sv")
        stg = RR(p, 3, [128, 4, 128], F32, "stg")
        ptr = RR(p, 4, [128, 512], F32, "ptr", psum=True)
        for cc in range(12):
            for (toff, L, _) in SEQS:
                r_, rk = raw.next()
                mset(p, "pool", r_[:, 0:1], 0.0, [rk])
                mset(p, "pool", r_[:, L + 1:L + 2], 0.0, [rk])
                p.dma(r_[:, 1:L + 1], HYT[cc * 128:(cc + 1) * 128, toff:toff + L], ["HYT"], [rk], eng="act")
                s_, sk = sv.next()
                act(p, s_[:, 0:L], r_[:, 1:L + 1], AF.Identity, [rk, "cw"], [sk], scale=cw[:, cc, 1:2], bias=cb[:, cc:cc + 1])
                stt(p, s_[:, 0:L], r_[:, 0:L], cw[:, cc, 0:1], s_[:, 0:L], ALU.mult, ALU.add, [rk, "cw"], [sk])
                stt(p, s_[:, 0:L], r_[:, 2:L + 2], cw[:, cc, 2:3], s_[:, 0:L], ALU.mult, ALU.add, [rk, "cw"], [sk])
                for t4 in range(L // 512 if L >= 512 else 1):
                    n = min(4, L // 128)
                    ps, pk = ptr.next()
                    for i in range(n):
                        trp(p, ps[:, i * 128:(i + 1) * 128], s_[:, (t4 * 4 + i) * 128:(t4 * 4 + i + 1) * 128], identf[:], [sk, "identf"], [pk])
                    g, gk = stg.next()
                    cp(p, "act" if t4 % 2 else "dve", g[:, 0:n, :].rearrange("q i c -> q (i c)"), ps[:, 0:n * 128], [pk], [gk])
                    r0 = toff + t4 * 512
                    p.dma(HYTM[r0:r0 + n * 128, cc * 128:(cc + 1) * 128].rearrange("(i q) c -> q i c", q=128), g[:, 0:n, :], [gk], ["HYTM"])

    for L in (4096, 256):
        nb = L // 128
        N = 2 * L
        KSP = T["KSP%d" % L]
        Cb, Sb = T["dftC%d" % L], T["dftS%d" % L]
        with p.phase():
            ft = p.sbuf([33, L], F32, "ft")
            p.dma(ft[:], T["feats%d" % L], (), ["ft"])
            w1 = p.sbuf([33, 64], F32, "w1"); w2 = p.sbuf([64, 64], F32, "w2"); w3 = p.sbuf([64, 2048], F32, "w3")
            p.dma(w1[:], T["hw1"][li], (), ["w1"]); p.dma(w2[:], T["hw2"][li], (), ["w2"]); p.dma(w3[:], T["hw3"][li], (), ["w3"])
            sc = p.sbuf([64, 8], F32, "sc")
            for j, nm in enumerate(("hb1", "hf1", "hb2", "hf2")):
                p.dma(sc[:, j:j + 1], T[nm][li], (), ["sc"])
            ts(p, "dve", sc[:, 4:5], sc[:, 1:2], 1.0 / TWO_PI, None, ALU.mult, None, ["sc"], ["sc"])
            ts(p, "dve", sc[:, 5:6], sc[:, 3:4], 1.0 / TWO_PI, None, ALU.mult, None, ["sc"], ["sc"])
            ntn = p.sbuf([128, nb], F32, "ntn")
            p.dma(ntn[:], T["tn%d" % L], (), ["ntn"])
            ts(p, "dve", ntn[:], ntn[:], -1.0, None, ALU.mult, None, ["ntn"], ["ntn"])
            adec = p.sbuf([128, 4, 512], F32, "adec")
            p.dma(adec[:].rearrange("q a c -> q (a c)"), T["hdec"][li:li + 1].rearrange("o a c -> o (a c)").to_broadcast([128, 2048]), (), ["adec"])
            act(p, adec[:].rearrange("q a c -> q (a c)"), adec[:].rearrange("q a c -> q (a c)"), AF.Abs, ["adec"], ["adec"])
            z1 = p.sbuf([64, L], F32, "z1"); z2 = p.sbuf([64, L], F32, "z2")
            tfm = RR(p, 2, [64, 512], F32, "tfm")
            tim = RR(p, 2, [64, 512], I32, "tim")
            pz = RR(p, 2, [128, 512], F32, "pz", psum=True)
            ph = RR(p, 2, [128, 512], F32, "ph", psum=True)
            pk_ = RR(p, 3, [128, 512], F32, "pk", psum=True)
            bw = min(512, L)
            for (src, srck, W, K, dst, dk, bcol, fcol) in ((ft, "ft", w1, 33, z1, "z1", 0, 4), (z1, "z1", w2, 64, z2, "z2", 2, 5)):
                for b in range(L // bw):
                    ps, pk = pz.next()
                    mm(p, ps[0:64, 0:bw], W[0:K, :], src[0:K, b * bw:(b + 1) * bw], True, True, [srck, "w1", "w2"], [pk])
                    t_, tk = tfm.next()
                    i_, ik = tim.next()
                    ts(p, "dve", t_[:, 0:bw], ps[0:64, 0:bw], sc[:, bcol:bcol + 1], sc[:, fcol:fcol + 1], ALU.add, ALU.mult, [pk, "sc"], [tk])
                    cp(p, "dve", i_[:, 0:bw], t_[:, 0:bw], [tk], [ik])
                    d_ = dst[:, b * bw:(b + 1) * bw]
                    cp(p, "dve", d_, i_[:, 0:bw], [ik], [dk])
                    tt(p, "dve", d_, t_[:, 0:bw], d_, ALU.subtract, [tk, dk], [dk])
                    act(p, d_, d_, AF.Sin, [dk], [dk], scale=TWO_PI)
            ab = [p.sbuf([128, nb, 512], BF16, "ab%d" % j) for j in range(2)]
            hfs = RR(p, 4, [128, 512], F32, "hfs")
            dcy = RR(p, 2, [128, 512], F32, "dcy")
            cbk = RR(p, 2, [128, nb, 128], BF16, "cbk")
            ko = RR(p, 3, [128, 512], F32, "ko")
            sgp = p.sbuf([128, 32], BF16, "sgp")
            p.dma(sgp[:], T["sgnp"], (), ["sgp"])
            for order in range(2):
                for tb in range(nb):
                    hh = []
                    for side in range(2):
                        so = side * 2 + order
                        ps, pk = ph.next()
                        mm(p, ps[:], z2[0:64, tb * 128:(tb + 1) * 128], w3[0:64, so * 512:(so + 1) * 512], True, True, ["z2", "w3"], [pk])
                        dc, dck = dcy.next()
                        act(p, dc[:], adec[:, so, :], AF.Exp, ["adec", "ntn"], [dck], scale=ntn[:, tb:tb + 1])
                        h_, hk = hfs.next()
                        tt(p, "dve", h_[:], ps[:], dc[:], ALU.mult, [pk, dck], [hk])
                        if side == 1 and tb == 0:
                            mset(p, "dve", h_[0:1, :], 0.0, [hk])
                        hh.append((h_, hk))
                    tt(p, "pool", ab[0][:, tb, :], hh[0][0][:], hh[1][0][:], ALU.add, [hh[0][1], hh[1][1]], ["ab0"])
                    tt(p, "pool", ab[1][:, tb, :], hh[0][0][:], hh[1][0][:], ALU.subtract, [hh[0][1], hh[1][1]], ["ab1"])
                for which, (Mb, a_, akey) in enumerate(((Cb, ab[0], "ab0"), (Sb, ab[1], "ab1"))):
                    for fb in range(nb):
                        m_, mk = cbk.next()
                        p.dma(m_[:], Mb[fb], (), [mk], eng=("sp" if fb % 2 else "act"))
                        ps, pk = pk_.next()
                        for sc_ in range(nb):
                            mm(p, ps[:], m_[:, sc_, :], a_[:, sc_, :], sc_ == 0, sc_ == nb - 1, [mk, akey], [pk])
                        o, ok = ko.next()
                        ts(p, "dve", o[:], ps[:], 2.0 / N, None, ALU.mult, None, [pk], [ok])
                        if fb == 0:
                            if which == 0:
                                ts(p, "dve", o[0:1, :], ps[0:1, :], 1.0 / N, None, ALU.mult, None, [pk], [ok])
                            else:
                                ps2, pk2 = pk_.next()
                                for sc_ in range(nb):
                                    mm(p, ps2[0:1, :], sgp[:, 0:1], ab[0][:, sc_, :], sc_ == 0, sc_ == nb - 1, [mk, "ab0", "sgp"], [pk2])
                                ts(p, "dve", o[0:1, :], ps2[0:1, :], 1.0 / N, None, ALU.mult, None, [pk2], [ok])
                        p.dma(KSP[order, which, fb * 128:(fb + 1) * 128, :], o[:], [ok], ["KSP"])

    with p.phase():
        bbc = p.sbuf([128, 2, 512], F32, "bbc")
        p.dma(bbc[:].rearrange("q a c -> q (a c)"), T["hbias"][li:li + 1].rearrange("o a c -> o (a c)").to_broadcast([128, 1024]), (), ["bbc"])
        src_t = p.sbuf([128, 32, 512], BF16, "srct")
        Yre = p.sbuf([128, 32, 512], BF16, "Yre")
        Yp = p.sbuf([128, 32, 512], BF16, "Yp")
        cbk = RR(p, 2, [128, 32, 128], BF16, "cbk")
        sbk = RR(p, 2, [128, 32, 128], BF16, "sbk")
        hv = RR(p, 2, [128, 1536], F32, "hv")
        z1t = RR(p, 2, [128, 512], F32, "z1t")
        t4_ = RR(p, 4, [128, 512], F32, "t4")
        kk_ = RR(p, 2, [128, 2, 512], F32, "kk")
        ob = RR(p, 2, [128, 512], BF16, "ob")
        pf = RR(p, 4, [128, 512], F32, "pf", psum=True)
        pi = RR(p, 2, [128, 512], F32, "pi", psum=True)
        sgp = p.sbuf([128, 32], BF16, "sgp")
        sgj = p.sbuf([1, 128], BF16, "sgj")
        p.dma(sgp[:], T["sgnp"], (), ["sgp"])
        p.dma(sgj[:], T["sgnj"], (), ["sgp"])
        for (toff, L, _) in SEQS:
            nb = L // 128
            KSP = T["KSP%d" % L]
            Cb, Sb = T["dftC%d" % L], T["dftS%d" % L]
            for tb in range(nb):
                h_, hk = hv.next()
                p.dma(h_[:, 0:512], HYTM[toff + tb * 128:toff + (tb + 1) * 128, 0:512], ["HYTM"], [hk], eng="act")
                cp(p, "pool", src_t[:, tb, :], h_[:, 0:512], [hk], ["srct"])
            for order in range(2):
                for fb in range(nb):
                    c_, ck = cbk.next(); s_, sk = sbk.next()
                    p.dma(c_[:, 0:nb, :], Cb[fb], (), [ck], eng="act")
                    p.dma(s_[:, 0:nb, :], Sb[fb], (), [sk], eng="sp")
                    if fb == 0:
                        cp(p, "dve", s_[:, 0:nb, 0], sgp[:, 0:nb], [sk, "sgp"], [sk])
                    k_, kk = kk_.next()
                    p.dma(k_[:, 0, :], KSP[order, 0, fb * 128:(fb + 1) * 128, :], ["KSP"], [kk], eng="act")
                    p.dma(k_[:, 1, :], KSP[order, 1, fb * 128:(fb + 1) * 128, :], ["KSP"], [kk], eng="sp")
                    pre, prk = pf.next(); pp, ppk = pf.next()
                    for sc_ in range(nb):
                        mm(p, pre[:], c_[:, sc_, :], src_t[:, sc_, :], sc_ == 0, sc_ == nb - 1, [ck, "srct"], [prk])
                    for sc_ in range(nb):
                        mm(p, pp[:], s_[:, sc_, :], src_t[:, sc_, :], sc_ == 0, sc_ == nb - 1, [sk, "srct"], [ppk])
                    a1, a1k = t4_.next(); a2, a2k = t4_.next(); a3, a3k = t4_.next(); a4, a4k = t4_.next()
                    tt(p, "dve", a1[:], pre[:], k_[:, 0, :], ALU.mult, [prk, kk], [a1k])
                    tt(p, "dve", a2[:], pp[:], k_[:, 1, :], ALU.mult, [ppk, kk], [a2k])
                    tt(p, "dve", a3[:], pre[:], k_[:, 1, :], ALU.mult, [prk, kk], [a3k])
                    tt(p, "dve", a4[:], pp[:], k_[:, 0, :], ALU.mult, [ppk, kk], [a4k])
                    tt(p, "pool", Yre[:, fb, :], a1[:], a2[:], ALU.subtract, [a1k, a2k], ["Yre"])
                    tt(p, "pool", Yp[:, fb, :], a3[:], a4[:], ALU.add, [a3k, a4k], ["Yp"])
                    if fb == 0:
                        cp(p, "pool", Yre[0:1, 0, :], a1[0:1, :], [a1k, "Yre"], ["Yre"])
                        cp(p, "pool", Yp[0:1, 0, :], a2[0:1, :], [a2k, "Yp"], ["Yp"])
                for tb in range(nb):
                    c_, ck = cbk.next(); s_, sk = sbk.next()
                    p.dma(c_[:, 0:nb, :], Cb[tb], (), [ck], eng="act")
                    p.dma(s_[:, 0:nb, :], Sb[tb], (), [sk], eng="sp")
                    cp(p, "dve", s_[0:1, 0, :], sgj[0:1, :], [sk, "sgp"], [sk])
                    ps, pk = pi.next()
                    for fc in range(nb):
                        mm(p, ps[:], c_[:, fc, :], Yre[:, fc, :], fc == 0, False, [ck, "Yre"], [pk])
                    for fc in range(nb):
                        mm(p, ps[:], s_[:, fc, :], Yp[:, fc, :], False, fc == nb - 1, [sk, "Yp"], [pk])
                    r0 = toff + tb * 128
                    h_, hk = hv.next()
                    p.dma(h_[:], HYTM[r0:r0 + 128, :], ["HYTM"], [hk], eng="act")
                    a1, a1k = t4_.next(); a2, a2k = t4_.next()
                    if order == 0:
                        tt(p, "pool", a1[:], h_[:, 0:512], bbc[:, 0, :], ALU.mult, [hk, "bbc"], [a1k])
                        tt(p, "dve", a2[:], ps[:], a1[:], ALU.add, [pk, a1k], [a2k])
                        z_, zk = z1t.next()
                        tt(p, "dve", z_[:], a2[:], h_[:, 512:1024], ALU.mult, [a2k, hk], [zk])
                        p.dma(Z1F[r0:r0 + 128, :], z_[:], [zk], ["Z1F"])
                        cp(p, "pool", src_t[:, tb, :], z_[:], [zk], ["srct"])
                    else:
                        z_, zk = z1t.next()
                        p.dma(z_[:], Z1F[r0:r0 + 128, :], ["Z1F"], [zk], eng="sp")
                        tt(p, "pool", a1[:], z_[:], bbc[:, 1, :], ALU.mult, [zk, "bbc"], [a1k])
                        tt(p, "dve", a2[:], ps[:], a1[:], ALU.add, [pk, a1k], [a2k])
                        o, ok = ob.next()
                        tt(p, "dve", o[:], a2[:], h_[:, 1024:1536], ALU.mult, [a2k, hk], [ok])
                        p.dma(MIXT[r0:r0 + 128, 512:1024], o[:], [ok], ["MIXT"])
```

```python
import math
import contextlib
import numpy as np
import ml_dtypes
import concourse.bass as bass
import concourse.mybir as mybir
from concourse.bass_utils import run_bass_kernel_spmd

F32 = mybir.dt.float32
BF16 = mybir.dt.bfloat16
I32 = mybir.dt.int32
AF = mybir.ActivationFunctionType
ALU = mybir.AluOpType

D = 1024
NS = 4096
NP_ = 512
NT = NS + NP_
DFF = 2816
NFC = 22
EPS = 1e-6
DEPTH = 4
ENGS = ("pe", "dve", "act", "pool", "sp")
SEM_ROLL = 12000
NDMA_SEM = 28
TWO_PI = 2.0 * math.pi


class Prog:
    def __init__(self, nc, top):
        self.nc = nc
        self.top = top
        self.stack = top
        self.q = {e: [] for e in ENGS}
        self.cnt = {e: 0 for e in ENGS}
        self.esems = {e: [] for e in ENGS}
        self.last_tok = {e: None for e in ENGS}
        self.last_w = {}
        self.readers = {}
        self.waited = {e: {} for e in ENGS}
        self.dma_sems = [top.enter_context(nc.semaphore("dq%d" % i)) for i in range(NDMA_SEM)]
        self.dma_uses = [0] * NDMA_SEM
        self.dma_i = 0
        self.sem_ids = {}
        self.nbuf = 0
        self.ninst = 0

    def sbuf(self, shape, dtype, name="sb"):
        self.nbuf += 1
        return self.stack.enter_context(self.nc.sbuf_tensor("%s_%d" % (name, self.nbuf), list(shape), dtype))

    def psum(self, shape, dtype=F32, name="ps"):
        self.nbuf += 1
        return self.stack.enter_context(self.nc.psum_tensor("%s_%d" % (name, self.nbuf), list(shape), dtype))

    def _esem(self, eng, k):
        while len(self.esems[eng]) <= k:
            s = self.top.enter_context(self.nc.semaphore("e_%s_%d" % (eng, len(self.esems[eng]))))
            self.esems[eng].append(s)
        return self.esems[eng][k]

    def _need(self, eng, tok, waits):
        if tok is None:
            return
        sem, val, teng = tok
        if teng == "pe" and eng == "pe":
            return
        key = id(sem)
        self.sem_ids[key] = sem
        if self.waited[eng].get(key, 0) >= val:
            return
        if val > waits.get(key, 0):
            waits[key] = val

    def op(self, eng, fn, reads=(), writes=(), dma=False):
        waits = {}
        for r in reads:
            self._need(eng, self.last_w.get(r), waits)
        for w in writes:
            self._need(eng, self.last_w.get(w), waits)
            for t in self.readers.get(w, ()):
                self._need(eng, t, waits)
        if dma:
            slot = self.dma_i % NDMA_SEM
            self.dma_i += 1
            sem = self.dma_sems[slot]
            if self.dma_uses[slot] > 0:
                self._need(eng, (sem, 16 * self.dma_uses[slot], "dma"), waits)
            self.dma_uses[slot] += 1
            tok = (sem, 16 * self.dma_uses[slot], "dma")
            inc = 16
        else:
            n = self.cnt[eng]
            self.cnt[eng] += 1
            sem = self._esem(eng, n // SEM_ROLL)
            tok = (sem, (n % SEM_ROLL) + 1, eng)
            inc = 1
            self.last_tok[eng] = tok
        wl = []
        for key, val in waits.items():
            self.waited[eng][key] = val
            wl.append((self.sem_ids[key], val))
        self.q[eng].append((wl, fn, tok[0], inc))
        self.ninst += 1
        for r in reads:
            self.readers.setdefault(r, []).append(tok)
        for w in writes:
            self.last_w[w] = tok
            self.readers[w] = []
        return tok

    def dma(self, out, in_, reads=(), writes=(), eng="sp", **kw):
        return self.op(eng, lambda e: e.dma_start(out=out, in_=in_, **kw), reads, writes, dma=True)

    def barrier(self):
        toks = [self.last_tok[e] for e in ENGS if self.last_tok[e] is not None]
        for i, s in enumerate(self.dma_sems):
            if self.dma_uses[i] > 0:
                toks.append((s, 16 * self.dma_uses[i], "dma"))
        for e in ENGS:
            waits = {}
            for t in toks:
                if t[2] == e:
                    continue
                self._need(e, t, waits)
            wl = []
            for key, val in waits.items():
                self.waited[e][key] = val
                wl.append((self.sem_ids[key], val))
            if wl:
                self.q[e].append((wl, None, None, 0))
        self.last_w.clear()
        self.readers.clear()

    def flush(self):
        q = self.q

        def run(engname, e):
            for wl, fn, sem, inc in q[engname]:
                for s, v in wl:
                    e.wait_ge(s, v)
                if fn is not None:
                    fn(e).then_inc(sem, inc)

        with self.nc.Block() as block:
            @block.tensor
            def _(e):
                run("pe", e)

            @block.vector
            def _(e):
                run("dve", e)

            @block.scalar
            def _(e):
                run("act", e)

            @block.gpsimd
            def _(e):
                run("pool", e)

            @block.sync
            def _(e):
                run("sp", e)
        self.q = {e: [] for e in ENGS}

    @contextlib.contextmanager
    def phase(self):
        with contextlib.ExitStack() as st:
            old = self.stack
            self.stack = st
            yield
            self.barrier()
            self.flush()
            self.stack = old


class RR:
    def __init__(self, p, n, shape, dtype, name, psum=False):
        self.t = [(p.psum(shape, dtype, name) if psum else p.sbuf(shape, dtype, name)) for _ in range(n)]
        self.k = ["%s@%d_%d" % (name, p.nbuf, i) for i in range(n)]
        self.n = n
        self.i = 0

    def next(self):
        j = self.i % self.n
        self.i += 1
        return self.t[j], self.k[j]


def mm(p, out, lhsT, rhs, start, stop, r, w):
    p.op("pe", lambda e: e.matmul(out, lhsT=lhsT, rhs=rhs, start=start, stop=stop), r, w)


def trp(p, out, in_, ident, r, w):
    p.op("pe", lambda e: e.transpose(out, in_, ident), r, w)


def act(p, out, in_, func, r, w, scale=None, bias=None, accum=None):
    kw = {}
    if scale is not None:
        kw["scale"] = scale
    if bias is not None:
        kw["bias"] = bias
    if accum is not None:
        kw["accum_out"] = accum
    p.op("act", lambda e: e.activation(out=out, in_=in_, func=func, **kw), r, w)


def tt(p, eng, out, a, b, op, r, w):
    p.op(eng, lambda e: e.tensor_tensor(out=out, in0=a, in1=b, op=op), r, w)


def ts(p, eng, out, a, s1, s2, op0, op1, r, w):
    if op1 is None:
        p.op(eng, lambda e: e.tensor_scalar(out=out, in0=a, scalar1=s1, scalar2=None, op0=op0), r, w)
    else:
        p.op(eng, lambda e: e.tensor_scalar(out=out, in0=a, scalar1=s1, scalar2=s2, op0=op0, op1=op1), r, w)


def stt(p, out, a, s, b, op0, op1, r, w):
    p.op("dve", lambda e: e.scalar_tensor_tensor(out=out, in0=a, scalar=s, in1=b, op0=op0, op1=op1), r, w)


def cp(p, eng, out, in_, r, w):
    if eng == "act":
        p.op("act", lambda e: e.activation(out=out, in_=in_, func=AF.Copy), r, w)
    else:
        p.op(eng, lambda e: e.tensor_copy(out=out, in_=in_), r, w)


def mset(p, eng, ap, val, w):
    p.op(eng, lambda e: e.memset(ap, val), (), w)


def recip(p, out, in_, r, w):
    p.op("dve", lambda e: e.reciprocal(out=out, in_=in_), r, w)


_CONST = {}


def _dft_blocks(L):
    N = 2 * L
    nb = L // 128
    s = np.arange(L, dtype=np.int64)
    idx = (s[:, None] * s[None, :]) % N
    ang = idx.astype(np.float64) * (TWO_PI / N)
    C = np.cos(ang)
    S = np.sin(ang)

    def blk(M):
        return np.ascontiguousarray(M.reshape(nb, 128, nb, 128).transpose(2, 1, 0, 3)).astype(ml_dtypes.bfloat16)

    return blk(C), blk(S)


def _feats(L):
    tn = (np.arange(L, dtype=np.float32) / np.float32(L)).astype(np.float32)
    bands = np.linspace(1e-4, 15.0, 16, dtype=np.float32)
    ang = (np.float32(TWO_PI) * tn[:, None]).astype(np.float32) * bands
    f = np.concatenate([tn[:, None], np.cos(ang), np.sin(ang)], axis=-1).astype(np.float32)
    tnl = np.ascontiguousarray(tn.reshape(L // 128, 128).T)
    return np.ascontiguousarray(f.T), tnl


def consts():
    if _CONST:
        return _CONST
    c = _CONST
    c["identb"] = np.eye(128, dtype=np.float32).astype(ml_dtypes.bfloat16)
    c["identf"] = np.eye(128, dtype=np.float32)
    t = np.arange(NS)
    inv = (10000.0 ** (-np.arange(16, dtype=np.float32) / 16)).astype(np.float32)
    rc = np.zeros((64, NS), np.float32)
    rs = np.zeros((64, NS), np.float32)
    for d in range(64):
        pos = (t // 64) if d < 32 else (t % 64)
        ang = pos.astype(np.float32) * inv[d % 16]
        rc[d] = np.cos(ang)
        rs[d] = np.sin(ang)
    c["ropeC"] = np.concatenate([rc, rc], 0)
    c["ropeS"] = np.concatenate([rs, rs], 0)
    kc = np.arange(64)[:, None]
    qc = 63 - np.arange(64)[None, :]
    cs = np.clip(qc - 8, 0, 48)
    wm = ((kc >= cs) & (kc < cs + 16)).astype(np.float32)
    c["winmask"] = np.concatenate([wm, wm], 0)
    c["dftC4096"], c["dftS4096"] = _dft_blocks(4096)
    c["dftC256"], c["dftS256"] = _dft_blocks(256)
    c["feats4096"], c["tn4096"] = _feats(4096)
    c["feats256"], c["tn256"] = _feats(256)
    sg = np.where(np.arange(128) % 2 == 0, 1.0, -1.0).astype(np.float32)
    c["sgnp"] = np.tile(sg[:, None], (1, 32)).astype(ml_dtypes.bfloat16)
    c["sgnj"] = sg[None, :].astype(ml_dtypes.bfloat16)
    c["iota128"] = np.tile(np.arange(1, 129, dtype=np.float32)[None, :], (128, 1))
    return c


IN_SPECS = {}


def build_program(nlayers=DEPTH):
    nc = bass.Bass("TRN2", target_bir_lowering=False)
    T = {}

    def din(name, shape, dt=F32):
        T[name] = nc.dram_tensor(name, list(shape), dt, kind="ExternalInput").ap()
        IN_SPECS[name] = (tuple(shape), dt)
        return T[name]

    def dout(name, shape):
        T[name] = nc.dram_tensor(name, list(shape), F32, kind="ExternalOutput").ap()
        return T[name]

    def dscr(name, shape, dt=F32):
        T[name] = nc.dram_tensor(name, list(shape), dt).ap()
        return T[name]

    x0 = din("x0", [NT, D])
    cak = din("cak", [2, 512, 512]); cav = din("cav", [2, 512, 512])
    cbk = din("cbk", [2, 512, 512]); cbv = din("cbv", [2, 512, 512])
    sre = din("sre", [2, 2, 128, 16]); sim = din("sim", [2, 2, 128, 16])
    cond = din("cond", [128, 8, 2])
    w_mod = din("w_mod", [4, D, 6 * D]); b_mod = din("b_mod", [4, 128, 48])
    gpre1 = din("gpre1", [4, 128, 8]); gpost1 = din("gpost1", [4, 128, 8])
    gpre2 = din("gpre2", [4, 128, 8]); gpost2 = din("gpost2", [4, 128, 8])
    wg_in = din("wg", [4, D, DFF]); wu_in = din("wu", [4, D, DFF]); wd_in = din("wd", [4, DFF, D])
    w_in_e = din("w_in_e", [2, D, 3072]); w_out_e = din("w_out_e", [2, D, D])
    rpb = din("rpb", [2, 8, 15, 31])
    lamv = din("lamv", [2, 4, 64]); gsub = din("gsub", [2, 128])
    w_in_o = din("w_in_o", [2, D, 2048]); w_out_o = din("w_out_o", [2, D, D])
    s_lre = din("s_lre", [2, 2, 128, 16]); s_lim = din("s_lim", [2, 2, 128, 16]); s_ldt = din("s_ldt", [2, 2, 128, 16])
    s_bre = din("s_bre", [2, 2, 128, 256]); s_bim = din("s_bim", [2, 2, 128, 256])
    s_cre = din("s_cre", [2, 2, 128, 256]); s_cim = din("s_cim", [2, 2, 128, 256])
    s_d = din("s_d", [2, 128, 4])
    w_glu = din("w_glu", [2, 512, 512]); b_glu = din("b_glu", [2, 128, 4])
    hcw = din("hcw", [2, 128, 12, 3]); hcb = din("hcb", [2, 128, 12])
    hw1 = din("hw1", [2, 33, 64]); hb1 = din("hb1", [2, 64, 1]); hf1 = din("hf1", [2, 64, 1])
    hw2 = din("hw2", [2, 64, 64]); hb2 = din("hb2", [2, 64, 1]); hf2 = din("hf2", [2, 64, 1])
    hw3 = din("hw3", [2, 64, 2048]); hdec = din("hdec", [2, 4, 512]); hbias = din("hbias", [2, 2, 512])
    identb_d = din("identb", [128, 128], BF16); identf_d = din("identf", [128, 128])
    ropeC = din("ropeC", [128, NS]); ropeS = din("ropeS", [128, NS])
    winmask = din("winmask", [128, 64])
    dftC = {4096: din("dftC4096", [32, 128, 32, 128], BF16), 256: din("dftC256", [2, 128, 2, 128], BF16)}
    dftS = {4096: din("dftS4096", [32, 128, 32, 128], BF16), 256: din("dftS256", [2, 128, 2, 128], BF16)}
    feats = {4096: din("feats4096", [33, 4096]), 256: din("feats256", [33, 256])}
    tnl = {4096: din("tn4096", [128, 32]), 256: din("tn256", [128, 2])}
    iota_d = din("iota128", [128, 128])
    din("sgnp", [128, 32], BF16); din("sgnj", [1, 128], BF16)

    o_yp = dout("o_yp", [NP_, D]); o_ys = dout("o_ys", [NS, D])
    o_ak = dout("o_ak", [2, 2, 256, 512]); o_av = dout("o_av", [2, 2, 256, 512])
    o_bk = dout("o_bk", [2, 2, 256, 512]); o_bv = dout("o_bv", [2, 2, 256, 512])
    o_sre = dout("o_sre", [2, 2, 2, 16, 128]); o_sim = dout("o_sim", [2, 2, 2, 16, 128])

    X = dscr("X", [NT, D])
    WG = dscr("WG", [4, 11, 128, 8, 256], BF16); WU = dscr("WU", [4, 11, 128, 8, 256], BF16)
    QAT = dscr("QAT", [512, 5120], BF16); KAT = dscr("KAT", [512, 5120], BF16); VA = dscr("VA", [5120, 512], BF16)
    QBT = dscr("QBT", [512, 5120], BF16); KBT = dscr("KBT", [512, 5120], BF16); VB = dscr("VB", [5120, 512], BF16)
    MIXT = dscr("MIXT", [NT, D], BF16)
    MIXF = dscr("MIXF", [D, NT], BF16)
    RPBP = dscr("RPBP", [8, 15, 288])
    UT32 = dscr("UT32", [512, NT]); UTB = dscr("UTB", [512, NT], BF16)
    HYT = dscr("HYT", [1536, NT]); HYTM = dscr("HYTM", [NT, 1536])
    YS = dscr("YS", [2, 512, NT])
    KSP = {4096: dscr("KSP4096", [2, 2, 4096, 512]), 256: dscr("KSP256", [2, 2, 256, 512])}
    Z1F = dscr("Z1F", [NT, 512])

    fin_keys = []

    with contextlib.ExitStack() as top:
        p = Prog(nc, top)
        identb = p.sbuf([128, 128], BF16, "identb")
        identf = p.sbuf([128, 128], F32, "identf")
        condT = p.sbuf([128, 8, 2], F32, "condT")
        modA1 = p.sbuf([128, 8, 2], F32, "modA1"); modB1 = p.sbuf([128, 8, 2], F32, "modB1")
        modA2 = p.sbuf([128, 8, 2], F32, "modA2"); modB2 = p.sbuf([128, 8, 2], F32, "modB2")
        ggbc1 = [p.sbuf([128, D], F32, "ggbc1") for _ in range(2)]
        ggbc2 = [p.sbuf([128, D], F32, "ggbc2") for _ in range(2)]
        epsT = p.sbuf([128, 1], F32, "epsT")
        EPS_T[0] = epsT

        with p.phase():
            p.dma(identb[:], identb_d, (), ["identb"])
            p.dma(identf[:], identf_d, (), ["identf"])
            p.dma(condT[:], cond, (), ["condT"])
            mset(p, "dve", epsT[:], EPS, ["epsT"])
            act(p, condT[:], condT[:], AF.Silu, ["condT"], ["condT"])
            for i in range(9):
                p.dma(X[i * 512:(i + 1) * 512, :], x0[i * 512:(i + 1) * 512, :], (), ["X%d" % i], eng=("sp" if i % 2 else "act"))
            st32 = RR(p, 3, [128, DFF], F32, "st32")
            st16 = RR(p, 3, [128, DFF], BF16, "st16")
            k = 0
            for l in range(nlayers):
                for (src, dst) in ((wg_in, WG), (wu_in, WU)):
                    for rb in range(8):
                        a, ak = st32.next()
                        b, bk = st16.next()
                        p.dma(a[:], src[l][rb * 128:(rb + 1) * 128, :], (), [ak], eng=("sp" if k % 2 else "act"))
                        eng = ("dve", "pool", "act")[k % 3]
                        cp(p, eng, b[:], a[:], [ak], [bk])
                        p.dma(dst[l].rearrange("pc q kc f -> q kc pc f")[:, rb], b[:].rearrange("q (pc f) -> q pc f", f=256), [bk], ["wscr"], eng="sp")
                        k += 1

        for l in range(nlayers):
            even = (l % 2 == 0)
            li = l // 2
            lam_init = 0.8 - 0.6 * math.exp(-0.3 * l)
            build_mod(p, T, l, condT, identf, (modA1, modB1, modA2, modB2, ggbc1, ggbc2))
            if even:
                build_even_proj(p, T, l, li, identb, identf, epsT, modA1, modB1)
                build_even_attn(p, T, l, li, lam_init)
                tm_chunks = list(range(8))
            else:
                build_odd_proj(p, T, l, li, identb, epsT, modA1, modB1)
                build_s5(p, T, l, li, identf)
                build_hyena(p, T, l, li, identf)
                tm_chunks = [4, 5, 6, 7]
            build_out_ffn(p, T, l, li, even, tm_chunks, identb, epsT, modA2, modB2, ggbc1, ggbc2)

        with p.phase():
            for i in range(8):
                p.dma(o_ys[i * 512:(i + 1) * 512, :], X[i * 512:(i + 1) * 512, :], (), ["oys%d" % i], eng=("sp" if i % 2 else "act"))
            p.dma(o_yp[:, :], X[NS:NT, :], (), ["oyp"])
    return nc


def build_mod(p, T, l, condT, identf, outs):
    modA1, modB1, modA2, modB2, ggbc1, ggbc2 = outs
    w_mod, b_mod = T["w_mod"], T["b_mod"]
    with p.phase():
        mfm = p.sbuf([128, 48, 2], F32, "mfm")
        bm = p.sbuf([128, 48], F32, "bm")
        g4 = p.sbuf([128, 4, 8], F32, "g4")
        gg = p.sbuf([128, 8, 2], F32, "gg")
        lhs = RR(p, 2, [128, 128], F32, "bclhs")
        wp = RR(p, 2, [128, 8, 512], F32, "wmodp")
        pm = RR(p, 2, [128, 512], F32, "pm", psum=True)
        pb = RR(p, 2, [128, 512], F32, "pbc", psum=True)
        p.dma(bm[:], b_mod[l], (), ["bm"])
        for j, nm in enumerate(("gpre1", "gpost1", "gpre2", "gpost2")):
            p.dma(g4[:, j, :], T[nm][l], (), ["g4"], eng="act")
        for pc in range(12):
            w, wk = wp.next()
            p.dma(w[:], w_mod[l].rearrange("(kc q) n -> q kc n", q=128)[:, :, pc * 512:(pc + 1) * 512], (), [wk],
                  eng=("sp" if pc % 2 else "act"))
            ps, pk = pm.next()
            for jc in range(4):
                for kc in range(8):
                    mm(p, ps[:, jc * 2:jc * 2 + 2], w[:, kc, jc * 128:(jc + 1) * 128], condT[:, kc, :], kc == 0, kc == 7,
                       [wk, "condT"], [pk])
            for cd in range(2):
                tt(p, "dve", mfm[:, pc * 4:pc * 4 + 4, cd], ps[:, cd:8:2], bm[:, pc * 4:pc * 4 + 4], ALU.add,
                   [pk, "bm"], ["mfm"])
        for cd in range(2):
            ts(p, "dve", modA1[:, :, cd], mfm[:, 8:16, cd], 1.0, None, ALU.add, None, ["mfm"], ["modA1"])
            tt(p, "dve", modA1[:, :, cd], modA1[:, :, cd], g4[:, 0, :], ALU.mult, ["modA1", "g4"], ["modA1"])
            cp(p, "dve", modB1[:, :, cd], mfm[:, 0:8, cd], ["mfm"], ["modB1"])
            ts(p, "dve", modA2[:, :, cd], mfm[:, 32:40, cd], 1.0, None, ALU.add, None, ["mfm"], ["modA2"])
            tt(p, "dve", modA2[:, :, cd], modA2[:, :, cd], g4[:, 2, :], ALU.mult, ["modA2", "g4"], ["modA2"])
            cp(p, "dve", modB2[:, :, cd], mfm[:, 24:32, cd], ["mfm"], ["modB2"])
            for which, (sp_, gj, dst) in enumerate(((2, 1, ggbc1), (5, 3, ggbc2))):
                tt(p, "dve", gg[:, :, cd], mfm[:, sp_ * 8:sp_ * 8 + 8, cd], g4[:, gj, :], ALU.mult, ["mfm", "g4"], ["gg"])
                for half in range(2):
                    ps, pk = pb.next()
                    for kk in range(4):
                        kc = half * 4 + kk
                        lt, lk = lhs.next()
                        cp(p, "dve", lt[:], gg[:, kc, cd:cd + 1].to_broadcast([128, 128]), ["gg"], [lk])
                        mm(p, ps[:, kk * 128:(kk + 1) * 128], lt[:], identf[:], True, True, [lk, "identf"], [pk])
                    cp(p, "act", dst[cd][:, half * 512:(half + 1) * 512], ps[:], [pk], ["ggbc"])


class NormCtx:
    def __init__(self, p, identb, epsT):
        self.p = p
        self.identb = identb
        self.epsT = epsT
        self.junk = RR(p, 2, [128, D], BF16, "junk")
        self.ss = RR(p, 2, [128, 4], F32, "ss")
        self.xn = RR(p, 2, [128, 4, D], BF16, "xn")
        self.pst = RR(p, 2, [128, 1024], BF16, "pst", psum=True)

    def run(self, xs, xkey, A, B, cd, hT, hkey, ncol=512):
        p = self.p
        nt = ncol // 128
        ss, sk = self.ss.next()
        xn, xk = self.xn.next()
        for i in range(nt):
            j, jk = self.junk.next()
            act(p, j[:], xs[:, i, :], AF.Square, [xkey], [jk, sk], accum=ss[:, i:i + 1])
        act(p, ss[:, 0:nt], ss[:, 0:nt], AF.Sqrt, [sk], [sk], scale=1.0 / D, bias=self.epsT[:])
        recip(p, ss[:, 0:nt], ss[:, 0:nt], [sk], [sk])
        for i in range(nt):
            ts(p, "dve" if i % 2 == 0 else "pool", xn[:, i, :], xs[:, i, :], ss[:, i:i + 1], None, ALU.mult, None,
               [xkey, sk], [xk])
        for kc in range(8):
            ps, pk = self.pst.next()
            for i in range(nt):
                trp(p, ps[:, i * 128:(i + 1) * 128], xn[:, i, kc * 128:(kc + 1) * 128], self.identb[:], [xk, "identb"], [pk])
            act(p, hT[:, kc, 0:ncol], ps[:, 0:ncol], AF.Identity, [pk, "modA", "modB"], [hkey],
                scale=A[:, kc, cd:cd + 1], bias=B[:, kc, cd:cd + 1])


def load_w_bf16(p, dst, dkey, src2d, ncols, st, nkc=8):
    k = 0
    for kc in range(nkc):
        c0 = 0
        while c0 < ncols:
            cw = min(1024, ncols - c0)
            a, ak = st.next()
            p.dma(a[:, 0:cw], src2d[kc * 128:(kc + 1) * 128, c0:c0 + cw], (), [ak], eng=("sp" if k % 2 else "act"))
            cp(p, ("dve", "pool")[k % 2], dst[:, kc, c0:c0 + cw], a[:, 0:cw], [ak], [dkey])
            c0 += cw
            k += 1


def unit_cd(u):
    return 0 if u < 8 else 1


def xview(X, t0, n=512):
    return X[t0:t0 + n, :].rearrange("(i q) c -> q i c", q=128)


class PostNorm:
    def __init__(self, p, epsT):
        self.p = p
        self.epsT = epsT
        self.ss = RR(p, 4, [128, 4], F32, "pnss")
        self.junk = RR(p, 2, [128, D], BF16, "pnjunk")
        self.tmp = RR(p, 2, [128, D], F32, "pntmp")

    def run(self, po0, k0, po1, k1, x, xk, gg):
        p = self.p
        ss, sk = self.ss.next()
        j, jk = self.junk.next()
        act(p, j[:, 0:512], po0[:], AF.Square, [k0], [jk, sk], accum=ss[:, 0:1])
        act(p, j[:, 512:1024], po1[:], AF.Square, [k1], [jk, sk], accum=ss[:, 1:2])
        tt(p, "dve", ss[:, 2:3], ss[:, 0:1], ss[:, 1:2], ALU.add, [sk], [sk])
        act(p, ss[:, 2:3], ss[:, 2:3], AF.Sqrt, [sk], [sk], scale=1.0 / D, bias=self.epsT[:])
        recip(p, ss[:, 3:4], ss[:, 2:3], [sk], [sk])
        t, tk = self.tmp.next()
        stt(p, t[:, 0:512], po0[:], ss[:, 3:4], gg[:, 0:512], ALU.mult, ALU.mult, [k0, sk, "ggbc"], [tk])
        stt(p, t[:, 512:1024], po1[:], ss[:, 3:4], gg[:, 512:1024], ALU.mult, ALU.mult, [k1, sk, "ggbc"], [tk])
        tt(p, "pool", x, x, t[:], ALU.add, [tk, xk], [xk])


def build_out_ffn(p, T, l, li, even, tm_chunks, identb, epsT, modA2, modB2, ggbc1, ggbc2):
    X, MIXT, MIXF = T["X"], T["MIXT"], T["MIXF"]
    w_out = T["w_out_e"][li] if even else T["w_out_o"][li]
    ntm = len(tm_chunks)
    c0 = tm_chunks[0]
    with p.phase():
        st = RR(p, 2, [128, 1024], F32, "wst")
        wout = p.sbuf([128, 8, D], BF16, "wout")
        load_w_bf16(p, wout, "wout", w_out, D, st)
        pn = PostNorm(p, epsT)
        xs = RR(p, 2, [128, 4, D], F32, "xs")
        mT = RR(p, 2, [128, 8, 512], BF16, "mT")
        mx = RR(p, 2, [128, 4, ntm * 128], BF16, "mx")
        pso = RR(p, 4, [128, 512], F32, "pso", psum=True)
        pst = RR(p, 2, [128, 1024], BF16, "pst", psum=True)
        for u in range(9):
            t0 = u * 512
            cd = unit_cd(u)
            xt, xk = xs.next()
            p.dma(xt[:], xview(X, t0), ["X%d" % u], [xk], eng="act")
            m, mk = mT.next()
            for kc in range(8):
                if kc not in tm_chunks:
                    p.dma(m[:, kc, :], MIXF[kc * 128:(kc + 1) * 128, t0:t0 + 512], ["MIXF"], [mk], eng="sp")
            mxt, mxk = mx.next()
            p.dma(mxt[:], MIXT[t0:t0 + 512, c0 * 128:(c0 + ntm) * 128].rearrange("(i q) c -> q i c", q=128), ["MIXT"], [mxk])
            for kc in tm_chunks:
                ps, pk = pst.next()
                for i in range(4):
                    trp(p, ps[:, i * 128:(i + 1) * 128], mxt[:, i, (kc - c0) * 128:(kc - c0 + 1) * 128], identb[:], [mxk, "identb"], [pk])
                cp(p, "act" if kc % 2 else "dve", m[:, kc, :], ps[:, 0:512], [pk], [mk])
            for i in range(4):
                po0, k0 = pso.next()
                po1, k1 = pso.next()
                for half, (po, pk) in enumerate(((po0, k0), (po1, k1))):
                    for kc in range(8):
                        mm(p, po[:], m[:, kc, i * 128:(i + 1) * 128], wout[:, kc, half * 512:(half + 1) * 512], kc == 0, kc == 7,
                           [mk, "wout"], [pk])
                pn.run(po0, k0, po1, k1, xt[:, i, :], xk, ggbc1[cd])
            p.dma(xview(X, t0), xt[:], [xk], ["X%d" % u], eng="sp")
    WG, WU = T["WG"][l], T["WU"][l]
    with p.phase():
        st = RR(p, 2, [128, 1024], F32, "wst")
        wd = p.sbuf([128, NFC, D], BF16, "wd")
        load_w_bf16(p, wd, "wd", T["wd"][l], D, st, nkc=NFC)
        pn = PostNorm(p, epsT)
        nctx = NormCtx(p, identb, epsT)
        xs = RR(p, 2, [128, 4, D], F32, "xs")
        hT = RR(p, 1, [128, 8, 512], BF16, "hT")
        actT = p.sbuf([128, NFC, 512], BF16, "actT")
        wgu = RR(p, 2, [128, 2, 8, 256], BF16, "wgu")
        sg = RR(p, 2, [128, 512], F32, "sg")
        pso = RR(p, 4, [128, 512], F32, "pso", psum=True)
        for u in range(9):
            t0 = u * 512
            cd = unit_cd(u)
            xt, xk = xs.next()
            p.dma(xt[:], xview(X, t0), ["X%d" % u], [xk], eng="act")
            h, hk = hT.next()
            nctx.run(xt, xk, modA2, modB2, cd, h, hk)
            for pc in range(11):
                w, wk = wgu.next()
                p.dma(w[:, 0], WG[pc], ["wscr"], [wk], eng="sp")
                p.dma(w[:, 1], WU[pc], ["wscr"], [wk], eng="sp")
                for f2 in range(2):
                    fc = pc * 2 + f2
                    pg, kg = pso.next()
                    pu, ku = pso.next()
                    for kc in range(8):
                        mm(p, pg[:], w[:, 0, kc, f2 * 128:(f2 + 1) * 128], h[:, kc, :], kc == 0, kc == 7, [wk, hk], [kg])
                    for kc in range(8):
                        mm(p, pu[:], w[:, 1, kc, f2 * 128:(f2 + 1) * 128], h[:, kc, :], kc == 0, kc == 7, [wk, hk], [ku])
                    s, sk = sg.next()
                    act(p, s[:], pg[:], AF.Silu, [kg], [sk])
                    tt(p, "dve", actT[:, fc, :], pu[:], s[:], ALU.mult, [ku, sk], ["actT"])
            for i in range(4):
                po0, k0 = pso.next()
                po1, k1 = pso.next()
                for half, (po, pk) in enumerate(((po0, k0), (po1, k1))):
                    for fc in range(NFC):
                        mm(p, po[:], actT[:, fc, i * 128:(i + 1) * 128], wd[:, fc, half * 512:(half + 1) * 512], fc == 0, fc == NFC - 1,
                           ["actT", "wd"], [pk])
                pn.run(po0, k0, po1, k1, xt[:, i, :], xk, ggbc2[cd])
            p.dma(xview(X, t0), xt[:], [xk], ["X%d" % u], eng="sp")


def build_even_proj(p, T, l, li, identb, identf, epsT, modA1, modB1):
    X = T["X"]
    QAT, KAT, VA, QBT, KBT, VB = T["QAT"], T["KAT"], T["VA"], T["QBT"], T["KBT"], T["VB"]
    with p.phase():
        st = RR(p, 2, [128, 1024], F32, "wst")
        win = p.sbuf([128, 8, 3072], BF16, "win")
        wrot = p.sbuf([128, 8, 1024], BF16, "wrot")
        load_w_bf16(p, win, "win", T["w_in_e"][li], 3072, st)
        for (s0, d0) in ((1536, 0), (2048, 512)):
            sv = win[:, :, s0:s0 + 512].rearrange("q k (b two s) -> q k b two s", b=16, two=2, s=16)
            dv = wrot[:, :, d0:d0 + 512].rearrange("q k (b two s) -> q k b two s", b=16, two=2, s=16)
            ts(p, "dve", dv[:, :, :, 0, :], sv[:, :, :, 1, :], -1.0, None, ALU.mult, None, ["win"], ["wrot"])
            cp(p, "pool", dv[:, :, :, 1, :], sv[:, :, :, 0, :], ["win"], ["wrot"])
        nctx = NormCtx(p, identb, epsT)
        xs = RR(p, 1, [128, 4, D], F32, "xs")
        hT = RR(p, 2, [128, 8, 512], BF16, "hT")
        rcs = RR(p, 2, [128, 2, 512], F32, "rcs")
        obf = RR(p, 4, [128, 512], BF16, "obf")
        o32 = RR(p, 2, [128, 512], F32, "o32")
        t12 = RR(p, 4, [128, 512], F32, "t12")
        pso = RR(p, 5, [128, 512], F32, "pso", psum=True)
        pst = nctx.pst
        ctile = RR(p, 2, [128, 512], F32, "ctile")
        cb16 = RR(p, 2, [128, 512], BF16, "cb16")
        ktmp = RR(p, 2, [128, 4, 128], BF16, "ktmp")
        for (ck, cv, KTd, Vd) in ((T["cak"], T["cav"], KAT, VA), (T["cbk"], T["cbv"], KBT, VB)):
            for i in range(4):
                a, ak = ctile.next()
                p.dma(a[:], ck[li, i * 128:(i + 1) * 128, :], (), [ak], eng="act")
                b, bk = cb16.next()
                cp(p, "dve", b[:], a[:], [ak], [bk])
                kt, kk = ktmp.next()
                ps, pk = pst.next()
                for c in range(4):
                    trp(p, ps[:, c * 128:(c + 1) * 128], b[:, c * 128:(c + 1) * 128], identb[:], [bk, "identb"], [pk])
                cp(p, "act", kt[:].rearrange("q c t -> q (c t)"), ps[:, 0:512], [pk], [kk])
                p.dma(KTd[:, NS + i * 128:NS + (i + 1) * 128].rearrange("(c q) t -> q c t", q=128), kt[:], [kk], ["KT"])
                a, ak = ctile.next()
                p.dma(a[:], cv[li, i * 128:(i + 1) * 128, :], (), [ak], eng="act")
                b, bk = cb16.next()
                cp(p, "pool", b[:], a[:], [ak], [bk])
                p.dma(Vd[NS + i * 128:NS + (i + 1) * 128, :], b[:], [bk], ["V"])

        def fm_group(W, col0, h, hk, n):
            ps, pk = pso.next()
            for kc in range(8):
                mm(p, ps[:, 0:n], W[:, kc, col0:col0 + 128], h[:, kc, 0:n], kc == 0, kc == 7, ["win", "wrot", hk], [pk])
            return ps, pk

        for u in range(9):
            t0 = u * 512
            cd = unit_cd(u)
            xt, xk = xs.next()
            p.dma(xt[:], xview(X, t0), ["X%d" % u], [xk], eng="act")
            h, hk = hT.next()
            nctx.run(xt, xk, modA1, modB1, cd, h, hk)
            dcol = t0 if cd == 0 else NT
            if cd == 0:
                rc, rk = rcs.next()
                p.dma(rc[:, 0, :], T["ropeC"][:, t0:t0 + 512], (), [rk])
                p.dma(rc[:, 1, :], T["ropeS"][:, t0:t0 + 512], (), [rk])
            k = 0
            for (dst, wc0, rot0, dkey) in ((QAT, 0, None, "QT"), (KAT, 512, None, "KT"), (QBT, 1536, 0, "QT"), (KBT, 2048, 512, "KT")):
                for c in range(4):
                    ps, pk = fm_group(win, wc0 + c * 128, h, hk, 512)
                    o, ok = obf.next()
                    if cd == 0 and rot0 is not None:
                        ps2, pk2 = fm_group(wrot, rot0 + c * 128, h, hk, 512)
                        a, ak = t12.next()
                        b, bk = t12.next()
                        tt(p, "dve", a[:], ps[:], rc[:, 0, :], ALU.mult, [pk, rk], [ak])
                        tt(p, "dve", b[:], ps2[:], rc[:, 1, :], ALU.mult, [pk2, rk], [bk])
                        tt(p, "pool", o[:], a[:], b[:], ALU.add, [ak, bk], [ok])
                    else:
                        cp(p, "act" if k % 2 else "dve", o[:], ps[:], [pk], [ok])
                    k += 1
                    p.dma(dst[c * 128:(c + 1) * 128, dcol:dcol + 512], o[:], [ok], [dkey])
            for i in range(4):
                for (wc0, Vd, outk, outv) in ((512, None, "o_ak", None), (1024, VA, None, "o_av"), (2048, None, "o_bk", None), (2560, VB, None, "o_bv")):
                    if cd == 0 and Vd is None:
                        continue
                    ps, pk = pso.next()
                    for kc in range(8):
                        mm(p, ps[:], h[:, kc, i * 128:(i + 1) * 128], win[:, kc, wc0:wc0 + 512], kc == 0, kc == 7, ["win", hk], [pk])
                    if Vd is not None:
                        o, ok = obf.next()
                        cp(p, "act", o[:], ps[:], [pk], [ok])
                        p.dma(Vd[dcol + i * 128:dcol + (i + 1) * 128, :], o[:], [ok], ["V"])
                    if cd == 1:
                        o2, ok2 = o32.next()
                        cp(p, "act", o2[:], ps[:], [pk], [ok2])
                        nm = outk or outv
                        p.dma(T[nm][i // 2, li, (i % 2) * 128:(i % 2 + 1) * 128, :], o2[:], [ok2], [nm + str(li)])


class AttnCtx:
    def __init__(self, p):
        self.p = p
        self.pss = RR(p, 3, [128, 512], F32, "pss", psum=True)
        self.pacc = RR(p, 4, [128, 512], F32, "pacc", psum=True)
        self.E = RR(p, 3, [128, 512], BF16, "E")
        self.small = RR(p, 8, [128, 4], F32, "asmall")


def attn_block(p, C, KT, ktkey, QT, qkey, nq, Vaug, vkey, nkt, dv, finish):
    nqt = (nq + 127) // 128
    accs = [C.pacc.next() for _ in range(nqt)]

    def qk(kt):
        ps, pk = C.pss.next()
        mm(p, ps[:, 0:nq], KT[:, kt * 128:(kt + 1) * 128], QT[:, 0:nq], True, True, [ktkey, qkey], [pk])
        e, ek = C.E.next()
        act(p, e[:, 0:nq], ps[:, 0:nq], AF.Exp, [pk], [ek], scale=0.125)
        return e, ek

    pend = qk(0)
    for kt in range(nkt):
        nxt = qk(kt + 1) if kt + 1 < nkt else None
        e, ek = pend
        for i in range(nqt):
            mm(p, accs[i][0][:, 0:dv + 1], e[:, i * 128:(i + 1) * 128], Vaug[:, kt, :], kt == 0, kt == nkt - 1,
               [ek, vkey], [accs[i][1]])
        pend = nxt
    for i in range(nqt):
        finish(i, accs[i][0], accs[i][1])


def build_even_attn(p, T, l, li, lam_init):
    QAT, KAT, VA, QBT, KBT, VB = T["QAT"], T["KAT"], T["VA"], T["QBT"], T["KBT"], T["VB"]
    MIXT, RPBP = T["MIXT"], T["RPBP"]
    with p.phase():
        C = AttnCtx(p)
        lamt = p.sbuf([128, 4, 64], F32, "lamt")
        lsm = p.sbuf([128, 8], F32, "lsm")
        gsb = p.sbuf([128, 128], F32, "gsb")
        lj = p.sbuf([128, 64], F32, "lj")
        p.dma(lamt[:].rearrange("q a b -> q (a b)"), T["lamv"][li:li + 1].rearrange("o a b -> o (a b)").to_broadcast([128, 256]), (), ["lamt"])
        p.dma(gsb[:], T["gsub"][li:li + 1, :].to_broadcast([128, 128]), (), ["gsb"])
        for j in range(2):
            tt(p, "dve", lj[:], lamt[:, 2 * j, :], lamt[:, 2 * j + 1, :], ALU.mult, ["lamt"], ["lj"])
            act(p, lj[:], lj[:], AF.Identity, ["lj"], ["lj", "lsm"], accum=lsm[:, j:j + 1])
        act(p, lsm[:, 2:4], lsm[:, 0:2], AF.Exp, ["lsm"], ["lsm"])
        tt(p, "dve", lsm[:, 4:5], lsm[:, 3:4], lsm[:, 2:3], ALU.subtract, ["lsm"], ["lsm"])
        ts(p, "dve", lsm[:, 5:6], lsm[:, 4:5], -lam_init, None, ALU.add, None, ["lsm"], ["lsm"])
        ts(p, "dve", gsb[:], gsb[:], 1.0 - lam_init, None, ALU.mult, None, ["gsb"], ["gsb"])
        neglam = lsm[:, 5:6]

        om = [p.sbuf([128, 4, 128], F32, "om%d" % s) for s in range(2)]
        ocomb = RR(p, 2, [128, 128], F32, "ocomb")
        ojunk = RR(p, 2, [128, 128], F32, "ojunk")

        def diff_finish(s):
            def f(i, acc, ak):
                sm, sk = C.small.next()
                recip(p, sm[:, 0:1], acc[:, 128:129], [ak], [sk])
                ts(p, "dve", om[s][:, i, :], acc[:, 0:128], sm[:, 0:1], None, ALU.mult, None, [ak, sk], ["om%d" % s])
            return f

        def diff_combine(i, dst, dkey):
            o, ok = ocomb.next()
            stt(p, o[:], om[1][:, i, :], neglam, om[0][:, i, :], ALU.mult, ALU.add, ["om0", "om1", "lsm"], [ok])
            sm, sk = C.small.next()
            j, jk = ojunk.next()
            act(p, j[:], o[:], AF.Square, [ok], [jk, sk], accum=sm[:, 0:1])
            act(p, sm[:, 1:2], sm[:, 0:1], AF.Sqrt, [sk], [sk], scale=1.0 / 128, bias=EPS_T[0][:])
            recip(p, sm[:, 2:3], sm[:, 1:2], [sk], [sk])
            stt(p, dst, o[:], sm[:, 2:3], gsb[:], ALU.mult, ALU.mult, [ok, sk, "gsb"], [dkey])

        ktb = RR(p, 2, [64, NT], BF16, "ktb")
        vaug = RR(p, 2, [128, 36, 129], BF16, "vaug")
        qtb = RR(p, 2, [64, 512], BF16, "qtb")
        obuf = RR(p, 2, [128, 4, 128], BF16, "obufB")
        for h in range(4):
            v, vk = vaug.next()
            mset(p, "pool", v[:], 1.0, [vk])
            for t9 in range(4):
                p.dma(v[:, t9 * 9:(t9 + 1) * 9, 0:128],
                      VB[t9 * 1152:(t9 + 1) * 1152, h * 128:(h + 1) * 128].rearrange("(t q) d -> q t d", q=128), ["V"], [vk],
                      eng=("act" if t9 % 2 else "sp"))
            kts = []
            for s in range(2):
                kt_, kk = ktb.next()
                p.dma(kt_[:], KBT[h * 128 + s * 64:h * 128 + (s + 1) * 64, 0:NT], ["KT"], [kk], eng="act")
                kts.append((kt_, kk))
            for qb in range(8):
                for s in range(2):
                    q, qk = qtb.next()
                    p.dma(q[:], QBT[h * 128 + s * 64:h * 128 + (s + 1) * 64, qb * 512:(qb + 1) * 512], ["QT"], [qk])
                    attn_block(p, C, kts[s][0], kts[s][1], q, qk, 512, v, vk, 36, 128, diff_finish(s))
                ob, obk = obuf.next()
                for i in range(4):
                    diff_combine(i, ob[:, i, :], obk)
                p.dma(MIXT[qb * 512:(qb + 1) * 512, 512 + h * 128:512 + (h + 1) * 128].rearrange("(i q) c -> q i c", q=128), ob[:],
                      [obk], ["MIXT"])

        ktp = RR(p, 2, [64, 256], BF16, "ktp")
        qtp = RR(p, 2, [64, 256], BF16, "qtp")
        vap = RR(p, 2, [128, 2, 65], BF16, "vap")
        vbp = RR(p, 2, [128, 2, 129], BF16, "vbp")
        obp = RR(p, 2, [128, 2, D], BF16, "obp")
        for js in range(2):
            tb = NT + 256 * js
            ob, obk = obp.next()
            for h in range(8):
                v, vk = vap.next()
                mset(p, "pool", v[:], 1.0, [vk])
                p.dma(v[:, :, 0:64], VA[tb:tb + 256, h * 64:(h + 1) * 64].rearrange("(t q) d -> q t d", q=128), ["V"], [vk], eng="act")
                kt_, kk = ktp.next()
                p.dma(kt_[:], KAT[h * 64:(h + 1) * 64, tb:tb + 256], ["KT"], [kk], eng="act")
                q, qk = qtp.next()
                p.dma(q[:], QAT[h * 64:(h + 1) * 64, tb:tb + 256], ["QT"], [qk])

                def fin_a(i, acc, ak, h=h, ob=ob, obk=obk):
                    sm, sk = C.small.next()
                    recip(p, sm[:, 0:1], acc[:, 64:65], [ak], [sk])
                    ts(p, "dve", ob[:, i, h * 64:(h + 1) * 64], acc[:, 0:64], sm[:, 0:1], None, ALU.mult, None, [ak, sk], [obk])
                attn_block(p, C, kt_, kk, q, qk, 256, v, vk, 2, 64, fin_a)
            for h in range(4):
                v, vk = vbp.next()
                mset(p, "pool", v[:], 1.0, [vk])
                p.dma(v[:, :, 0:128], VB[tb:tb + 256, h * 128:(h + 1) * 128].rearrange("(t q) d -> q t d", q=128), ["V"], [vk], eng="act")
                for s in range(2):
                    kt_, kk = ktp.next()
                    p.dma(kt_[:], KBT[h * 128 + s * 64:h * 128 + (s + 1) * 64, tb:tb + 256], ["KT"], [kk], eng="act")
                    q, qk = qtp.next()
                    p.dma(q[:], QBT[h * 128 + s * 64:h * 128 + (s + 1) * 64, tb:tb + 256], ["QT"], [qk])
                    attn_block(p, C, kt_, kk, q, qk, 256, v, vk, 2, 128, diff_finish(s))
                for i in range(2):
                    diff_combine(i, ob[:, i, 512 + h * 128:512 + (h + 1) * 128], obk)
            p.dma(MIXT[NS + 256 * js:NS + 256 * (js + 1), :].rearrange("(i q) c -> q i c", q=128), ob[:], [obk], ["MIXT"])

        zt = p.sbuf([120, 288], F32, "zt")
        mset(p, "dve", zt[:], 0.0, ["zt"])
        p.dma(RPBP.rearrange("h r c -> (h r) c"), zt[:], ["zt"], ["RPBP"])
        p.dma(RPBP[:, :, 128:159], T["rpb"][li], (), ["RPBP"])
        wm = p.sbuf([128, 64], F32, "wm")
        p.dma(wm[:], T["winmask"], (), ["wm"])
        mraw = RR(p, 2, [128, 32, 64], F32, "mraw")
        mh = RR(p, 2, [128, 32, 64], BF16, "mh")
        kta = RR(p, 2, [64, NT], BF16, "kta")
        qta = RR(p, 2, [64, NS], BF16, "qta")
        va_e = RR(p, 2, [128, 36, 65], BF16, "va_e")
        va_o = RR(p, 2, [128, 35, 65], BF16, "va_o")
        oba = RR(p, 2, [64, 64, 64], BF16, "oba")
        En = RR(p, 3, [128, 8, 64], BF16, "En")
        for h in range(8):
            mr, mrk = mraw.next()
            kq = 0
            for off in range(8):
                for j in range(4):
                    for mlow in range(2):
                        dr = 2 * j + mlow + off
                        base = RPBP[h, dr, 80:144]
                        src = bass.AP(base.tensor, base.offset, [[1, 64], [1, 64]])
                        p.dma(mr[mlow * 64:(mlow + 1) * 64, off * 4 + j, :], src, ["RPBP"], [mrk], eng=("sp" if kq % 2 else "act"))
                        kq += 1
            act(p, mr[:], mr[:], AF.Exp, [mrk], [mrk])
            m_, mk_ = mh.next()
            tt(p, "pool", m_[:], mr[:], wm[:].unsqueeze(1).to_broadcast([128, 32, 64]), ALU.mult, [mrk, "wm"], [mk_])
            kt_, kk = kta.next()
            p.dma(kt_[:], KAT[h * 64:(h + 1) * 64, 0:NT], ["KT"], [kk], eng="act")
            q, qk = qta.next()
            p.dma(q[:], QAT[h * 64:(h + 1) * 64, 0:NS], ["QT"], [qk], eng="act")
            ve, vek = va_e.next()
            mset(p, "pool", ve[:], 1.0, [vek])
            for t9 in range(4):
                p.dma(ve[:, t9 * 9:(t9 + 1) * 9, 0:64],
                      VA[t9 * 1152:(t9 + 1) * 1152, h * 64:(h + 1) * 64].rearrange("(t q) d -> q t d", q=128), ["V"], [vek],
                      eng=("act" if t9 % 2 else "sp"))
            vo, vok = va_o.next()
            mset(p, "pool", vo[:], 1.0, [vok])
            for t9 in range(5):
                p.dma(vo[:, t9 * 7:(t9 + 1) * 7, 0:64],
                      VA[64 + t9 * 896:64 + (t9 + 1) * 896, h * 64:(h + 1) * 64].rearrange("(t q) d -> q t d", q=128), ["V"], [vok],
                      eng=("act" if t9 % 2 else "sp"))
            ob, obk = oba.next()
            def na_stage1(r):
                rs = min(max(r - 4, 0), 56)
                off = rs - r + 7
                ps, pk = C.pss.next()
                psv = ps[:].rearrange("q (j c) -> q j c", c=64)
                for j in range(4):
                    k0 = rs * 64 + 128 * j
                    mm(p, psv[:, j, :], kt_[:, k0:k0 + 128], q[:, r * 64:(r + 1) * 64], True, True, [kk, qk], [pk])
                for j in range(4):
                    k0 = NS + 128 * j
                    mm(p, psv[:, 4 + j, :], kt_[:, k0:k0 + 128], q[:, r * 64:(r + 1) * 64], True, True, [kk, qk], [pk])
                e, ek = En.next()
                act(p, e[:], psv, AF.Exp, [pk], [ek], scale=0.125)
                tt(p, "dve", e[:, 0:4, :], e[:, 0:4, :], m_[:, off * 4:off * 4 + 4, ::-1], ALU.mult, [ek, mk_], [ek])
                return e, ek

            def na_stage2(r, e, ek):
                rs = min(max(r - 4, 0), 56)
                acc, ak = C.pacc.next()
                for j in range(8):
                    if j < 4:
                        if rs % 2 == 0:
                            vt = ve[:, rs // 2 + j, :]
                            vkk = vek
                        else:
                            vt = vo[:, (rs - 1) // 2 + j, :]
                            vkk = vok
                    else:
                        vt = ve[:, 32 + (j - 4), :]
                        vkk = vek
                    mm(p, acc[0:64, 0:65], e[:, j, :], vt, j == 0, j == 7, [ek, vkk], [ak])
                sm, sk = C.small.next()
                recip(p, sm[0:64, 0:1], acc[0:64, 64:65], [ak], [sk])
                ts(p, "dve", ob[:, r, :], acc[0:64, 0:64], sm[0:64, 0:1], None, ALU.mult, None, [ak, sk], [obk])

            pend = na_stage1(0)
            for r in range(64):
                nxt = na_stage1(r + 1) if r + 1 < 64 else None
                na_stage2(r, pend[0], pend[1])
                pend = nxt
            for r4 in range(4):
                p.dma(MIXT[r4 * 1024:(r4 + 1) * 1024, h * 64:(h + 1) * 64].rearrange("(r q) d -> q r d", q=64), ob[:, r4 * 16:(r4 + 1) * 16, :],
                      [obk], ["MIXT"])


EPS_T = [None]


def _fm(v, nch):
    v = np.asarray(v, np.float32)
    return np.ascontiguousarray(np.swapaxes(v.reshape(v.shape[:-1] + (nch, 128)), -1, -2))


def _pairs(a):
    a = np.asarray(a, np.float32)
    sh = a.shape[:-2]
    a = a.reshape(sh + (16, 2, 64))
    a = np.moveaxis(a, -3, -1)
    return np.ascontiguousarray(a.reshape(sh + (128, 16)))


def shared_inputs(inp):
    f = lambda a: np.ascontiguousarray(np.asarray(a, np.float32))
    S = {}
    S["w_mod"] = f(inp["w_mod"])
    S["b_mod"] = _fm(inp["b_mod"], 48)
    S["gpre1"] = _fm(inp["g_mix_pre"], 8); S["gpost1"] = _fm(inp["g_mix_post"], 8)
    S["gpre2"] = _fm(inp["g_ffn_pre"], 8); S["gpost2"] = _fm(inp["g_ffn_post"], 8)
    S["wg"] = f(inp["w_ffn_gate"]); S["wu"] = f(inp["w_ffn_up"]); S["wd"] = f(inp["w_ffn_down"])
    S["w_in_e"] = f(inp["w_in_e"]); S["w_out_e"] = f(inp["w_out_e"])
    S["rpb"] = f(inp["na_rpb"])
    S["lamv"] = np.ascontiguousarray(np.stack([f(inp["lam_q1"]), f(inp["lam_k1"]), f(inp["lam_q2"]), f(inp["lam_k2"])], axis=1))
    S["gsub"] = f(inp["g_subln"])
    S["w_in_o"] = f(inp["w_in_o"]); S["w_out_o"] = f(inp["w_out_o"])
    S["s_lre"] = _pairs(inp["ssm_lam_re"]); S["s_lim"] = _pairs(inp["ssm_lam_im"])
    ldt = np.asarray(inp["ssm_log_dt"], np.float32)
    S["s_ldt"] = _pairs(np.repeat(ldt[..., None], 64, axis=-1))
    for nm, src in (("s_bre", "ssm_b_re"), ("s_bim", "ssm_b_im")):
        b = np.asarray(inp[src], np.float32)
        b = np.moveaxis(b, -1, 2)
        b = _pairs(b)
        S[nm] = np.ascontiguousarray(np.moveaxis(b, 2, -1).reshape(2, 2, 128, 256))
    for nm, src in (("s_cre", "ssm_c_re"), ("s_cim", "ssm_c_im")):
        c = np.asarray(inp[src], np.float32)
        c = np.moveaxis(c, 3, 2)
        c = _pairs(c)
        S[nm] = np.ascontiguousarray(np.moveaxis(c, 2, -1).reshape(2, 2, 128, 256))
    S["s_d"] = _fm(inp["ssm_d"], 4)
    S["w_glu"] = f(inp["w_glu"]); S["b_glu"] = _fm(inp["b_glu"], 4)
    cw = np.asarray(inp["hy_conv_w"], np.float32)
    S["hcw"] = np.ascontiguousarray(np.moveaxis(_fm(cw, 12), 1, -1))
    S["hcb"] = _fm(inp["hy_conv_b"], 12)
    S["hw1"] = f(inp["hy_w1"]); S["hb1"] = f(inp["hy_b1"])[..., None]; S["hf1"] = f(inp["hy_fr1"])[..., None]
    S["hw2"] = f(inp["hy_w2"]); S["hb2"] = f(inp["hy_b2"])[..., None]; S["hf2"] = f(inp["hy_fr2"])[..., None]
    S["hw3"] = f(inp["hy_w3"])
    S["hdec"] = np.ascontiguousarray(np.asarray(inp["hy_decay"], np.float32).reshape(2, 4, 512))
    S["hbias"] = f(inp["hy_bias"])
    S.update(consts())
    return S


def core_inputs(inp, cid, S):
    m = dict(S)
    xs = np.asarray(inp["x_sample"][cid], np.float32)
    xp = np.asarray(inp["x_prompt"][2 * cid:2 * cid + 2], np.float32).reshape(512, D)
    m["x0"] = np.ascontiguousarray(np.concatenate([xs, xp], 0))
    for nm, src in (("cak", "cache_a_k"), ("cav", "cache_a_v"), ("cbk", "cache_b_k"), ("cbv", "cache_b_v")):
        m[nm] = np.ascontiguousarray(np.asarray(inp[src][cid], np.float32).reshape(2, 512, 512))
    m["sre"] = _pairs(inp["state_c_re"][cid]); m["sim"] = _pairs(inp["state_c_im"][cid])
    cc = np.stack([np.asarray(inp["c"][cid], np.float32), np.asarray(inp["c_ctx"], np.float32)], -1)
    m["cond"] = np.ascontiguousarray(cc.reshape(8, 128, 2).transpose(1, 0, 2))
    return m


_PROG = {}


def run_device(inputs, nlayers=DEPTH, cores=8):
    if nlayers not in _PROG:
        _PROG[nlayers] = build_program(nlayers)
    nc = _PROG[nlayers]
    S = shared_inputs(inputs)
    in_maps = []
    for cid in range(cores):
        m = core_inputs(inputs, cid, S)
        in_maps.append({k: m[k] for k in IN_SPECS})
    res = run_bass_kernel_spmd(nc, in_maps, core_ids=list(range(cores)))
    return res.results


def kernel(**inputs):
    R = run_device(inputs)
    yp = np.concatenate([r["o_yp"].reshape(2, 256, D) for r in R], 0)
    ys = np.stack([r["o_ys"] for r in R], 0)
    outs = [yp.astype(np.float32), ys.astype(np.float32)]
    for nm, (hh, dd) in (("o_ak", (8, 64)), ("o_av", (8, 64)), ("o_bk", (4, 128)), ("o_bv", (4, 128))):
        a = np.concatenate([r[nm] for r in R], 0)
        outs.append(np.ascontiguousarray(a.reshape(16, 2, 256, hh, dd)).astype(np.float32))
    for nm in ("o_sre", "o_sim"):
        a = np.concatenate([r[nm] for r in R], 0)
        outs.append(np.ascontiguousarray(a.reshape(16, 2, 2, 32, 64)).astype(np.float32))
    return tuple(outs)


def build_odd_proj(p, T, l, li, identb, epsT, modA1, modB1):
    X = T["X"]
    UT32, UTB, HYT = T["UT32"], T["UTB"], T["HYT"]
    with p.phase():
        st = RR(p, 2, [128, 1024], F32, "wst")
        win = p.sbuf([128, 8, 2048], BF16, "win")
        load_w_bf16(p, win, "win", T["w_in_o"][li], 2048, st)
        nctx = NormCtx(p, identb, epsT)
        xs = RR(p, 2, [128, 4, D], F32, "xs")
        hT = RR(p, 2, [128, 8, 512], BF16, "hT")
        o32 = RR(p, 3, [128, 512], F32, "o32")
        obf = RR(p, 2, [128, 512], BF16, "obf")
        pso = RR(p, 4, [128, 512], F32, "pso", psum=True)
        for u in range(9):
            t0 = u * 512
            cd = unit_cd(u)
            xt, xk = xs.next()
            p.dma(xt[:], xview(X, t0), ["X%d" % u], [xk], eng="act")
            h, hk = hT.next()
            nctx.run(xt, xk, modA1, modB1, cd, h, hk)
            for c in range(16):
                ps, pk = pso.next()
                for kc in range(8):
                    mm(p, ps[:], win[:, kc, c * 128:(c + 1) * 128], h[:, kc, :], kc == 0, kc == 7, ["win", hk], [pk])
                o, ok = o32.next()
                cp(p, "act" if c % 2 else "dve", o[:], ps[:], [pk], [ok])
                if c < 4:
                    p.dma(UT32[c * 128:(c + 1) * 128, t0:t0 + 512], o[:], [ok], ["UT32"])
                    b, bk = obf.next()
                    cp(p, "pool", b[:], o[:], [ok], [bk])
                    p.dma(UTB[c * 128:(c + 1) * 128, t0:t0 + 512], b[:], [bk], ["UTB"])
                else:
                    p.dma(HYT[(c - 4) * 128:(c - 3) * 128, t0:t0 + 512], o[:], [ok], ["HYT"])


def sin_turns(p, eng_i, out, turns, tmp_i, tmp_f, keys_r, keys_w, shift=0.0):
    r = list(keys_r)
    if shift != 0.0:
        ts(p, "dve", tmp_f, turns, shift, None, ALU.add, None, r, keys_w)
        src = tmp_f
    else:
        src = turns
    cp(p, "dve", tmp_i, src, r + list(keys_w), keys_w)
    cp(p, "dve", out, tmp_i, keys_w, keys_w)
    tt(p, "dve", out, src, out, ALU.subtract, r + list(keys_w), keys_w)
    act(p, out, out, AF.Sin, keys_w, keys_w, scale=TWO_PI)


SEQS = ((0, 4096, True), (4096, 256, False), (4352, 256, False))


def build_s5(p, T, l, li, identf):
    UTB, UT32, YS, MIXF = T["UTB"], T["UT32"], T["YS"], T["MIXF"]
    with p.phase():
        iota = p.sbuf([128, 128], F32, "iota")
        p.dma(iota[:], T["iota128"], (), ["iota"])
        prm = p.sbuf([128, 2, 3, 16], F32, "prm")
        wk_ = p.sbuf([128, 2, 12, 16], F32, "wk")
        wki = p.sbuf([128, 16], I32, "wki")
        Bl = p.sbuf([128, 2, 16, 2, 128], BF16, "Bl")
        Cl = p.sbuf([128, 2, 16, 2, 128], BF16, "Cl")
        cosT = p.sbuf([128, 2, 16, 128], F32, "cosT")
        sinT = p.sbuf([128, 2, 16, 128], F32, "sinT")
        rT = p.sbuf([128, 2, 16, 128], F32, "rT")
        bc = RR(p, 2, [128, 2, 16, 16], F32, "bc")
        bb = RR(p, 2, [128, 2, 16, 16], F32, "bb")
        spd = RR(p, 2, [128, 128], F32, "spd")
        ti = p.sbuf([128, 128], I32, "ti")
        tf = p.sbuf([128, 128], F32, "tf")
        ptr = RR(p, 1, [128, 512], F32, "ptr", psum=True)
        pbu = RR(p, 4, [128, 512], F32, "pbu", psum=True)
        pcy = RR(p, 2, [128, 512], F32, "pcy", psum=True)
        for d in range(2):
            for j, nm in enumerate(("s_lre", "s_lim", "s_ldt")):
                p.dma(prm[:, d, j, :], T[nm][li, d], (), ["prm"])
            lre, lim, ldt = prm[:, d, 0, :], prm[:, d, 1, :], prm[:, d, 2, :]
            W = lambda i: wk_[:, d, i, :]
            R, Wk = ["prm", "wk"], ["wk"]
            act(p, W(0), ldt, AF.Exp, R, Wk)
            tt(p, "dve", W(1), lre, W(0), ALU.mult, R, Wk)
            act(p, W(1), W(1), AF.Exp, R, Wk)
            tt(p, "dve", W(2), lim, W(0), ALU.mult, R, Wk)
            ts(p, "dve", W(2), W(2), 1.0 / TWO_PI, None, ALU.mult, None, R, Wk)
            cp(p, "dve", wki[:], W(2), R, ["wki"])
            cp(p, "dve", W(3), wki[:], ["wki"], Wk)
            tt(p, "dve", W(2), W(2), W(3), ALU.subtract, R, Wk)
            act(p, W(4), W(2), AF.Sin, R, Wk, scale=TWO_PI)
            ts(p, "dve", W(3), W(2), 0.25, None, ALU.add, None, R, Wk)
            cp(p, "dve", wki[:], W(3), R, ["wki"])
            cp(p, "dve", W(5), wki[:], ["wki"], Wk)
            tt(p, "dve", W(3), W(3), W(5), ALU.subtract, R, Wk)
            act(p, W(5), W(3), AF.Sin, R, Wk, scale=TWO_PI)
            tt(p, "dve", W(6), W(1), W(5), ALU.mult, R, Wk)
            tt(p, "dve", W(7), W(1), W(4), ALU.mult, R, Wk)
            tt(p, "dve", W(3), lre, lre, ALU.mult, R, Wk)
            tt(p, "dve", W(4), lim, lim, ALU.mult, R, Wk)
            tt(p, "dve", W(3), W(3), W(4), ALU.add, R, Wk)
            recip(p, W(3), W(3), R, Wk)
            ts(p, "dve", W(6), W(6), -1.0, None, ALU.add, None, R, Wk)
            tt(p, "dve", W(4), W(6), lre, ALU.mult, R, Wk)
            tt(p, "dve", W(5), W(7), lim, ALU.mult, R, Wk)
            tt(p, "dve", W(4), W(4), W(5), ALU.add, R, Wk)
            tt(p, "dve", W(8), W(4), W(3), ALU.mult, R, Wk)
            tt(p, "dve", W(4), W(7), lre, ALU.mult, R, Wk)
            tt(p, "dve", W(5), W(6), lim, ALU.mult, R, Wk)
            tt(p, "dve", W(4), W(4), W(5), ALU.subtract, R, Wk)
            tt(p, "dve", W(9), W(4), W(3), ALU.mult, R, Wk)
            b_, bk = bc.next()
            p.dma(b_[:, 0].rearrange("q k h -> q (k h)"), T["s_bre"][li, d], (), [bk])
            p.dma(b_[:, 1].rearrange("q k h -> q (k h)"), T["s_bim"][li, d], (), [bk])
            o_, ok = bb.next()
            kre = W(8).unsqueeze(2).to_broadcast([128, 16, 16])
            kim = W(9).unsqueeze(2).to_broadcast([128, 16, 16])
            t1, t1k = bb.next()
            tt(p, "dve", o_[:, 0], b_[:, 0], kre, ALU.mult, [bk] + R, [ok])
            tt(p, "dve", t1[:, 0], b_[:, 1], kim, ALU.mult, [bk] + R, [t1k])
            tt(p, "dve", o_[:, 0], o_[:, 0], t1[:, 0], ALU.subtract, [t1k], [ok])
            tt(p, "dve", o_[:, 1], b_[:, 1], kre, ALU.mult, [bk] + R, [ok])
            tt(p, "dve", t1[:, 1], b_[:, 0], kim, ALU.mult, [bk] + R, [t1k])
            tt(p, "dve", o_[:, 1], o_[:, 1], t1[:, 1], ALU.add, [t1k], [ok])
            c_, ck = bc.next()
            p.dma(c_[:, 0].rearrange("q k h -> q (k h)"), T["s_cre"][li, d], (), [ck])
            p.dma(c_[:, 1].rearrange("q k h -> q (k h)"), T["s_cim"][li, d], (), [ck])
            mset(p, "pool", Cl[:, d].rearrange("q k r m -> q (k r m)"), 0.0, ["Cl"])
            for k in range(16):
                gl0, gl1 = (2 * k) % 8, (2 * k + 1) % 8
                for ri in range(2):
                    s_, sk = spd.next()
                    mset(p, "pool", s_[:], 0.0, [sk])
                    cp(p, "dve", s_[0:64, gl0 * 16:(gl0 + 1) * 16], o_[0:64, ri, k, :], [ok], [sk])
                    cp(p, "dve", s_[64:128, gl1 * 16:(gl1 + 1) * 16], o_[64:128, ri, k, :], [ok], [sk])
                    ps, pk = ptr.next()
                    trp(p, ps[:, 0:128], s_[:], identf[:], [sk, "identf"], [pk])
                    cp(p, "act", Bl[:, d, k, ri, :], ps[:, 0:128], [pk], ["Bl"])
                    if ri == 0:
                        cp(p, "dve", Cl[0:64, d, k, 0, gl0 * 16:(gl0 + 1) * 16], c_[0:64, 0, k, :], [ck, "Cl"], ["Cl"])
                        cp(p, "dve", Cl[64:128, d, k, 0, gl1 * 16:(gl1 + 1) * 16], c_[64:128, 0, k, :], [ck, "Cl"], ["Cl"])
                    else:
                        ts(p, "dve", Cl[0:64, d, k, 1, gl0 * 16:(gl0 + 1) * 16], c_[0:64, 1, k, :], -1.0, None, ALU.mult, None, [ck, "Cl"], ["Cl"])
                        ts(p, "dve", Cl[64:128, d, k, 1, gl1 * 16:(gl1 + 1) * 16], c_[64:128, 1, k, :], -1.0, None, ALU.mult, None, [ck, "Cl"], ["Cl"])
                ts(p, "dve", tf[:], iota[:], wk_[:, d, 2, k:k + 1], None, ALU.mult, None, ["iota"] + R, ["tf"])
                cp(p, "dve", ti[:], tf[:], ["tf"], ["ti"])
                cp(p, "dve", sinT[:, d, k, :], ti[:], ["ti"], ["tab"])
                tt(p, "dve", tf[:], tf[:], sinT[:, d, k, :], ALU.subtract, ["tab"], ["tf"])
                act(p, sinT[:, d, k, :], tf[:], AF.Sin, ["tf"], ["tab"], scale=TWO_PI)
                ts(p, "dve", tf[:], tf[:], 0.25, None, ALU.add, None, ["tf"], ["tf"])
                cp(p, "dve", ti[:], tf[:], ["tf"], ["ti"])
                cp(p, "dve", cosT[:, d, k, :], ti[:], ["ti"], ["tab"])
                tt(p, "dve", tf[:], tf[:], cosT[:, d, k, :], ALU.subtract, ["tab"], ["tf"])
                act(p, cosT[:, d, k, :], tf[:], AF.Sin, ["tf"], ["tab"], scale=TWO_PI)
                cp(p, "pool", rT[:, d, k, :], wk_[:, d, 1, k:k + 1].to_broadcast([128, 128]), R, ["tab"])

        ut = RR(p, 2, [128, 4, 512], BF16, "ut")
        xre = p.sbuf([128, 16, 512], BF16, "xre")
        xim = p.sbuf([128, 16, 512], BF16, "xim")
        m4 = RR(p, 8, [128, 4, 128], F32, "m4")
        wz = RR(p, 16, [128, 4, 128], F32, "wz")
        carry = p.sbuf([128, 2, 16], F32, "carry")
        cst = RR(p, 8, [128, 2], F32, "cst")
        o32 = RR(p, 2, [128, 512], F32, "o32")
        fst = RR(p, 2, [16, 128], F32, "fst")
        for si, (toff, L, has_init) in enumerate(SEQS):
            bs = min(512, L)
            nblk = L // bs
            nch = bs // 128
            for d in range(2):
                if has_init:
                    p.dma(carry[:, 0, :], T["sre"][li, d], (), ["carry%d" % k for k in range(16)])
                    p.dma(carry[:, 1, :], T["sim"][li, d], (), ["carry%d" % k for k in range(16)])
                else:
                    mset(p, "dve", carry[:].rearrange("q a k -> q (a k)"), 0.0, ["carry%d" % k for k in range(16)])
                blks = range(nblk) if d == 0 else range(nblk - 1, -1, -1)
                for blk in blks:
                    c0 = toff + blk * bs
                    u_, uk = ut.next()
                    p.dma(u_[:, :, 0:bs], UTB[:, c0:c0 + bs].rearrange("(c q) t -> q c t", q=128), ["UTB"], [uk])
                    csb_ = lambda k: cosT[:, d, k, :].unsqueeze(1).to_broadcast([128, nch, 128])
                    snb_ = lambda k: sinT[:, d, k, :].unsqueeze(1).to_broadcast([128, nch, 128])

                    def V3(a, d=d):
                        v = a.rearrange("q (c j) -> q c j", j=128)
                        return v if d == 0 else v[:, :, ::-1]
                    chs = list(range(nch)) if d == 0 else list(range(nch - 1, -1, -1))
                    for kg in range(4):
                        ks = list(range(kg * 4, kg * 4 + 4))
                        W_ = {}
                        for k in ks:
                            pr, prk = pbu.next()
                            pi_, pik = pbu.next()
                            mm(p, pr[:, 0:bs], Bl[:, d, k, 0, :], u_[:, k // 4, 0:bs], True, True, ["Bl", uk], [prk])
                            mm(p, pi_[:, 0:bs], Bl[:, d, k, 1, :], u_[:, k // 4, 0:bs], True, True, ["Bl", uk], [pik])
                            bre, bim = V3(pr[:, 0:bs]), V3(pi_[:, 0:bs])
                            a1, a1k = m4.next(); a2, a2k = m4.next(); a3, a3k = m4.next(); a4, a4k = m4.next()
                            tt(p, "dve", a1[:, 0:nch, :], bre, csb_(k), ALU.mult, [prk, "tab"], [a1k])
                            tt(p, "dve", a2[:, 0:nch, :], bim, snb_(k), ALU.mult, [pik, "tab"], [a2k])
                            tt(p, "dve", a3[:, 0:nch, :], bim, csb_(k), ALU.mult, [pik, "tab"], [a3k])
                            tt(p, "dve", a4[:, 0:nch, :], bre, snb_(k), ALU.mult, [prk, "tab"], [a4k])
                            wr, wrk = wz.next(); wi, wik = wz.next()
                            tt(p, "pool", wr[:, 0:nch, :], a1[:, 0:nch, :], a2[:, 0:nch, :], ALU.add, [a1k, a2k], [wrk])
                            tt(p, "pool", wi[:, 0:nch, :], a3[:, 0:nch, :], a4[:, 0:nch, :], ALU.subtract, [a3k, a4k], [wik])
                            zr, zrk = wz.next(); zi, zik = wz.next()
                            W_[k] = (wr, wrk, wi, wik, zr, zrk, zi, zik)
                        for ch in chs:
                            c2s = {}
                            for k in ks:
                                wr, wrk, wi, wik, zr, zrk, zi, zik = W_[k]
                                r_ = rT[:, d, k, :]
                                ck_ = "carry%d" % k
                                p.op("dve", lambda e, zr=zr, wr=wr, r_=r_, k=k, ch=ch: e.tensor_tensor_scan(out=zr[:, ch, :], data0=r_, data1=wr[:, ch, :], initial=carry[:, 0, k:k + 1], op0=ALU.mult, op1=ALU.add),
                                     [wrk, "tab", ck_], [zrk])
                                p.op("dve", lambda e, zi=zi, wi=wi, r_=r_, k=k, ch=ch: e.tensor_tensor_scan(out=zi[:, ch, :], data0=r_, data1=wi[:, ch, :], initial=carry[:, 1, k:k + 1], op0=ALU.mult, op1=ALU.add),
                                     [wik, "tab", ck_], [zik])
                            for k in ks:
                                wr, wrk, wi, wik, zr, zrk, zi, zik = W_[k]
                                cs_, sn_ = cosT[:, d, k, :], sinT[:, d, k, :]
                                c2, c2k = cst.next()
                                c2s[k] = (c2, c2k)
                                tt(p, "dve", c2[:, 0:1], zi[:, ch, 127:128], sn_[:, 127:128], ALU.mult, [zik, "tab"], [c2k])
                                tt(p, "dve", c2[:, 1:2], zi[:, ch, 127:128], cs_[:, 127:128], ALU.mult, [zik, "tab"], [c2k])
                            for k in ks:
                                wr, wrk, wi, wik, zr, zrk, zi, zik = W_[k]
                                cs_, sn_ = cosT[:, d, k, :], sinT[:, d, k, :]
                                c2, c2k = c2s[k]
                                ck_ = "carry%d" % k
                                stt(p, carry[:, 0, k:k + 1], zr[:, ch, 127:128], cs_[:, 127:128], c2[:, 0:1], ALU.mult, ALU.subtract, [zrk, c2k, "tab"], [ck_])
                                stt(p, carry[:, 1, k:k + 1], zr[:, ch, 127:128], sn_[:, 127:128], c2[:, 1:2], ALU.mult, ALU.add, [zrk, c2k, "tab"], [ck_])
                        for k in ks:
                            wr, wrk, wi, wik, zr, zrk, zi, zik = W_[k]
                            n1, n1k = m4.next(); n2, n2k = m4.next(); n3, n3k = m4.next(); n4, n4k = m4.next()
                            tt(p, "pool", n1[:, 0:nch, :], zr[:, 0:nch, :], csb_(k), ALU.mult, [zrk, "tab"], [n1k])
                            tt(p, "dve", n2[:, 0:nch, :], zi[:, 0:nch, :], snb_(k), ALU.mult, [zik, "tab"], [n2k])
                            tt(p, "dve", n3[:, 0:nch, :], zr[:, 0:nch, :], snb_(k), ALU.mult, [zrk, "tab"], [n3k])
                            tt(p, "pool", n4[:, 0:nch, :], zi[:, 0:nch, :], csb_(k), ALU.mult, [zik, "tab"], [n4k])
                            tt(p, "pool", V3(xre[:, k, 0:bs]), n1[:, 0:nch, :], n2[:, 0:nch, :], ALU.subtract, [n1k, n2k], ["xre"])
                            tt(p, "pool", V3(xim[:, k, 0:bs]), n3[:, 0:nch, :], n4[:, 0:nch, :], ALU.add, [n3k, n4k], ["xim"])
                    for cc in range(4):
                        ps, pk = pcy.next()
                        for kk in range(4):
                            k = cc * 4 + kk
                            mm(p, ps[:, 0:bs], Cl[:, d, k, 0, :], xre[:, k, 0:bs], kk == 0, False, ["Cl", "xre"], [pk])
                            mm(p, ps[:, 0:bs], Cl[:, d, k, 1, :], xim[:, k, 0:bs], False, kk == 3, ["Cl", "xim"], [pk])
                        o, ok = o32.next()
                        cp(p, "act", o[:, 0:bs], ps[:, 0:bs], [pk], [ok])
                        p.dma(YS[d, cc * 128:(cc + 1) * 128, c0:c0 + bs], o[:, 0:bs], [ok], ["YS"])
                if not has_init:
                    for ri, nm in enumerate(("o_sre", "o_sim")):
                        ps, pk = ptr.next()
                        trp(p, ps[0:16, 0:128], carry[:, ri, :], identf[:], ["carry%d" % k for k in range(16)] + ["identf"], [pk])
                        f_, fk = fst.next()
                        cp(p, "act", f_[:], ps[0:16, 0:128], [pk], [fk])
                        p.dma(T[nm][si - 1, li, d], f_[:], [fk], [nm + "%d_%d_%d" % (si, li, d)])

    with p.phase():
        st = RR(p, 2, [128, 1024], F32, "wst")
        wgl = p.sbuf([128, 4, 512], BF16, "wgl")
        load_w_bf16(p, wgl, "wgl", T["w_glu"][li], 512, st, nkc=4)
        dsk = p.sbuf([128, 4], F32, "dsk"); bgl = p.sbuf([128, 4], F32, "bgl")
        p.dma(dsk[:], T["s_d"][li], (), ["dsk"]); p.dma(bgl[:], T["b_glu"][li], (), ["bgl"])
        yl = RR(p, 3, [128, 3, 512], F32, "yl")
        g32 = RR(p, 2, [128, 4, 512], F32, "g32")
        gb = RR(p, 2, [128, 4, 512], BF16, "gb")
        sg = RR(p, 2, [128, 512], F32, "sg")
        ob = RR(p, 2, [128, 512], BF16, "ob")
        pso = RR(p, 3, [128, 512], F32, "pso", psum=True)
        for u in range(9):
            t0 = u * 512
            g, gk = g32.next()
            g2, g2k = gb.next()
            for cc in range(4):
                y, yk = yl.next()
                p.dma(y[:, 0, :], YS[0, cc * 128:(cc + 1) * 128, t0:t0 + 512], ["YS"], [yk], eng="act")
                p.dma(y[:, 1, :], YS[1, cc * 128:(cc + 1) * 128, t0:t0 + 512], ["YS"], [yk], eng="act")
                p.dma(y[:, 2, :], UT32[cc * 128:(cc + 1) * 128, t0:t0 + 512], ["UT32"], [yk], eng="sp")
                tt(p, "pool", y[:, 0, :], y[:, 0, :], y[:, 1, :], ALU.add, [yk], [yk])
                stt(p, y[:, 0, :], y[:, 2, :], dsk[:, cc:cc + 1], y[:, 0, :], ALU.mult, ALU.add, [yk, "dsk"], [yk])
                act(p, g[:, cc, :], y[:, 0, :], AF.Gelu, [yk], [gk])
                cp(p, "pool", g2[:, cc, :], g[:, cc, :], [gk], [g2k])
            for oc in range(4):
                ps, pk = pso.next()
                for kc in range(4):
                    mm(p, ps[:], wgl[:, kc, oc * 128:(oc + 1) * 128], g2[:, kc, :], kc == 0, kc == 3, ["wgl", g2k], [pk])
                s, sk = sg.next()
                act(p, s[:], ps[:], AF.Sigmoid, [pk, "bgl"], [sk], bias=bgl[:, oc:oc + 1])
                o, ok = ob.next()
                tt(p, "dve", o[:], g[:, oc, :], s[:], ALU.mult, [gk, sk], [ok])
                p.dma(MIXF[oc * 128:(oc + 1) * 128, t0:t0 + 512], o[:], [ok], ["MIXF"])


def build_hyena(p, T, l, li, identf):
    HYT, HYTM, Z1F, MIXT = T["HYT"], T["HYTM"], T["Z1F"], T["MIXT"]
    with p.phase():
        cw = p.sbuf([128, 12, 3], F32, "cw"); cb = p.sbuf([128, 12], F32, "cb")
        p.dma(cw[:], T["hcw"][li], (), ["cw"]); p.dma(cb[:], T["hcb"][li], (), ["cw"])
        raw = RR(p, 2, [128, 4098], F32, "raw")
        sv = RR(p, 2, [128, 4096], F32, "sv")
        stg = RR(p, 3, [128, 4, 128], F32, "stg")
        ptr = RR(p, 4, [128, 512], F32, "ptr", psum=True)
        for cc in range(12):
            for (toff, L, _) in SEQS:
                r_, rk = raw.next()
                mset(p, "pool", r_[:, 0:1], 0.0, [rk])
                mset(p, "pool", r_[:, L + 1:L + 2], 0.0, [rk])
                p.dma(r_[:, 1:L + 1], HYT[cc * 128:(cc + 1) * 128, toff:toff + L], ["HYT"], [rk], eng="act")
                s_, sk = sv.next()
                act(p, s_[:, 0:L], r_[:, 1:L + 1], AF.Identity, [rk, "cw"], [sk], scale=cw[:, cc, 1:2], bias=cb[:, cc:cc + 1])
                stt(p, s_[:, 0:L], r_[:, 0:L], cw[:, cc, 0:1], s_[:, 0:L], ALU.mult, ALU.add, [rk, "cw"], [sk])
                stt(p, s_[:, 0:L], r_[:, 2:L + 2], cw[:, cc, 2:3], s_[:, 0:L], ALU.mult, ALU.add, [rk, "cw"], [sk])
                for t4 in range(L // 512 if L >= 512 else 1):
                    n = min(4, L // 128)
                    ps, pk = ptr.next()
                    for i in range(n):
                        trp(p, ps[:, i * 128:(i + 1) * 128], s_[:, (t4 * 4 + i) * 128:(t4 * 4 + i + 1) * 128], identf[:], [sk, "identf"], [pk])
                    g, gk = stg.next()
                    cp(p, "act" if t4 % 2 else "dve", g[:, 0:n, :].rearrange("q i c -> q (i c)"), ps[:, 0:n * 128], [pk], [gk])
                    r0 = toff + t4 * 512
                    p.dma(HYTM[r0:r0 + n * 128, cc * 128:(cc + 1) * 128].rearrange("(i q) c -> q i c", q=128), g[:, 0:n, :], [gk], ["HYTM"])

    for L in (4096, 256):
        nb = L // 128
        N = 2 * L
        KSP = T["KSP%d" % L]
        Cb, Sb = T["dftC%d" % L], T["dftS%d" % L]
        with p.phase():
            ft = p.sbuf([33, L], F32, "ft")
            p.dma(ft[:], T["feats%d" % L], (), ["ft"])
            w1 = p.sbuf([33, 64], F32, "w1"); w2 = p.sbuf([64, 64], F32, "w2"); w3 = p.sbuf([64, 2048], F32, "w3")
            p.dma(w1[:], T["hw1"][li], (), ["w1"]); p.dma(w2[:], T["hw2"][li], (), ["w2"]); p.dma(w3[:], T["hw3"][li], (), ["w3"])
            sc = p.sbuf([64, 8], F32, "sc")
            for j, nm in enumerate(("hb1", "hf1", "hb2", "hf2")):
                p.dma(sc[:, j:j + 1], T[nm][li], (), ["sc"])
            ts(p, "dve", sc[:, 4:5], sc[:, 1:2], 1.0 / TWO_PI, None, ALU.mult, None, ["sc"], ["sc"])
            ts(p, "dve", sc[:, 5:6], sc[:, 3:4], 1.0 / TWO_PI, None, ALU.mult, None, ["sc"], ["sc"])
            ntn = p.sbuf([128, nb], F32, "ntn")
            p.dma(ntn[:], T["tn%d" % L], (), ["ntn"])
            ts(p, "dve", ntn[:], ntn[:], -1.0, None, ALU.mult, None, ["ntn"], ["ntn"])
            adec = p.sbuf([128, 4, 512], F32, "adec")
            p.dma(adec[:].rearrange("q a c -> q (a c)"), T["hdec"][li:li + 1].rearrange("o a c -> o (a c)").to_broadcast([128, 2048]), (), ["adec"])
            act(p, adec[:].rearrange("q a c -> q (a c)"), adec[:].rearrange("q a c -> q (a c)"), AF.Abs, ["adec"], ["adec"])
            z1 = p.sbuf([64, L], F32, "z1"); z2 = p.sbuf([64, L], F32, "z2")
            tfm = RR(p, 2, [64, 512], F32, "tfm")
            tim = RR(p, 2, [64, 512], I32, "tim")
            pz = RR(p, 2, [128, 512], F32, "pz", psum=True)
            ph = RR(p, 2, [128, 512], F32, "ph", psum=True)
            pk_ = RR(p, 3, [128, 512], F32, "pk", psum=True)
            bw = min(512, L)
            for (src, srck, W, K, dst, dk, bcol, fcol) in ((ft, "ft", w1, 33, z1, "z1", 0, 4), (z1, "z1", w2, 64, z2, "z2", 2, 5)):
                for b in range(L // bw):
                    ps, pk = pz.next()
                    mm(p, ps[0:64, 0:bw], W[0:K, :], src[0:K, b * bw:(b + 1) * bw], True, True, [srck, "w1", "w2"], [pk])
                    t_, tk = tfm.next()
                    i_, ik = tim.next()
                    ts(p, "dve", t_[:, 0:bw], ps[0:64, 0:bw], sc[:, bcol:bcol + 1], sc[:, fcol:fcol + 1], ALU.add, ALU.mult, [pk, "sc"], [tk])
                    cp(p, "dve", i_[:, 0:bw], t_[:, 0:bw], [tk], [ik])
                    d_ = dst[:, b * bw:(b + 1) * bw]
                    cp(p, "dve", d_, i_[:, 0:bw], [ik], [dk])
                    tt(p, "dve", d_, t_[:, 0:bw], d_, ALU.subtract, [tk, dk], [dk])
                    act(p, d_, d_, AF.Sin, [dk], [dk], scale=TWO_PI)
            ab = [p.sbuf([128, nb, 512], BF16, "ab%d" % j) for j in range(2)]
            hfs = RR(p, 4, [128, 512], F32, "hfs")
            dcy = RR(p, 2, [128, 512], F32, "dcy")
            cbk = RR(p, 2, [128, nb, 128], BF16, "cbk")
            ko = RR(p, 3, [128, 512], F32, "ko")
            sgp = p.sbuf([128, 32], BF16, "sgp")
            p.dma(sgp[:], T["sgnp"], (), ["sgp"])
            for order in range(2):
                for tb in range(nb):
                    hh = []
                    for side in range(2):
                        so = side * 2 + order
                        ps, pk = ph.next()
                        mm(p, ps[:], z2[0:64, tb * 128:(tb + 1) * 128], w3[0:64, so * 512:(so + 1) * 512], True, True, ["z2", "w3"], [pk])
                        dc, dck = dcy.next()
                        act(p, dc[:], adec[:, so, :], AF.Exp, ["adec", "ntn"], [dck], scale=ntn[:, tb:tb + 1])
                        h_, hk = hfs.next()
                        tt(p, "dve", h_[:], ps[:], dc[:], ALU.mult, [pk, dck], [hk])
                        if side == 1 and tb == 0:
                            mset(p, "dve", h_[0:1, :], 0.0, [hk])
                        hh.append((h_, hk))
                    tt(p, "pool", ab[0][:, tb, :], hh[0][0][:], hh[1][0][:], ALU.add, [hh[0][1], hh[1][1]], ["ab0"])
                    tt(p, "pool", ab[1][:, tb, :], hh[0][0][:], hh[1][0][:], ALU.subtract, [hh[0][1], hh[1][1]], ["ab1"])
                for which, (Mb, a_, akey) in enumerate(((Cb, ab[0], "ab0"), (Sb, ab[1], "ab1"))):
                    for fb in range(nb):
                        m_, mk = cbk.next()
                        p.dma(m_[:], Mb[fb], (), [mk], eng=("sp" if fb % 2 else "act"))
                        ps, pk = pk_.next()
                        for sc_ in range(nb):
                            mm(p, ps[:], m_[:, sc_, :], a_[:, sc_, :], sc_ == 0, sc_ == nb - 1, [mk, akey], [pk])
                        o, ok = ko.next()
                        ts(p, "dve", o[:], ps[:], 2.0 / N, None, ALU.mult, None, [pk], [ok])
                        if fb == 0:
                            if which == 0:
                                ts(p, "dve", o[0:1, :], ps[0:1, :], 1.0 / N, None, ALU.mult, None, [pk], [ok])
                            else:
                                ps2, pk2 = pk_.next()
                                for sc_ in range(nb):
                                    mm(p, ps2[0:1, :], sgp[:, 0:1], ab[0][:, sc_, :], sc_ == 0, sc_ == nb - 1, [mk, "ab0", "sgp"], [pk2])
                                ts(p, "dve", o[0:1, :], ps2[0:1, :], 1.0 / N, None, ALU.mult, None, [pk2], [ok])
                        p.dma(KSP[order, which, fb * 128:(fb + 1) * 128, :], o[:], [ok], ["KSP"])

    with p.phase():
        bbc = p.sbuf([128, 2, 512], F32, "bbc")
        p.dma(bbc[:].rearrange("q a c -> q (a c)"), T["hbias"][li:li + 1].rearrange("o a c -> o (a c)").to_broadcast([128, 1024]), (), ["bbc"])
        src_t = p.sbuf([128, 32, 512], BF16, "srct")
        Yre = p.sbuf([128, 32, 512], BF16, "Yre")
        Yp = p.sbuf([128, 32, 512], BF16, "Yp")
        cbk = RR(p, 2, [128, 32, 128], BF16, "cbk")
        sbk = RR(p, 2, [128, 32, 128], BF16, "sbk")
        hv = RR(p, 2, [128, 1536], F32, "hv")
        z1t = RR(p, 2, [128, 512], F32, "z1t")
        t4_ = RR(p, 4, [128, 512], F32, "t4")
        kk_ = RR(p, 2, [128, 2, 512], F32, "kk")
        ob = RR(p, 2, [128, 512], BF16, "ob")
        pf = RR(p, 4, [128, 512], F32, "pf", psum=True)
        pi = RR(p, 2, [128, 512], F32, "pi", psum=True)
        sgp = p.sbuf([128, 32], BF16, "sgp")
        sgj = p.sbuf([1, 128], BF16, "sgj")
        p.dma(sgp[:], T["sgnp"], (), ["sgp"])
        p.dma(sgj[:], T["sgnj"], (), ["sgp"])
        for (toff, L, _) in SEQS:
            nb = L // 128
            KSP = T["KSP%d" % L]
            Cb, Sb = T["dftC%d" % L], T["dftS%d" % L]
            for tb in range(nb):
                h_, hk = hv.next()
                p.dma(h_[:, 0:512], HYTM[toff + tb * 128:toff + (tb + 1) * 128, 0:512], ["HYTM"], [hk], eng="act")
                cp(p, "pool", src_t[:, tb, :], h_[:, 0:512], [hk], ["srct"])
            for order in range(2):
                for fb in range(nb):
                    c_, ck = cbk.next(); s_, sk = sbk.next()
                    p.dma(c_[:, 0:nb, :], Cb[fb], (), [ck], eng="act")
                    p.dma(s_[:, 0:nb, :], Sb[fb], (), [sk], eng="sp")
                    if fb == 0:
                        cp(p, "dve", s_[:, 0:nb, 0], sgp[:, 0:nb], [sk, "sgp"], [sk])
                    k_, kk = kk_.next()
                    p.dma(k_[:, 0, :], KSP[order, 0, fb * 128:(fb + 1) * 128, :], ["KSP"], [kk], eng="act")
                    p.dma(k_[:, 1, :], KSP[order, 1, fb * 128:(fb + 1) * 128, :], ["KSP"], [kk], eng="sp")
                    pre, prk = pf.next(); pp, ppk = pf.next()
                    for sc_ in range(nb):
                        mm(p, pre[:], c_[:, sc_, :], src_t[:, sc_, :], sc_ == 0, sc_ == nb - 1, [ck, "srct"], [prk])
                    for sc_ in range(nb):
                        mm(p, pp[:], s_[:, sc_, :], src_t[:, sc_, :], sc_ == 0, sc_ == nb - 1, [sk, "srct"], [ppk])
                    a1, a1k = t4_.next(); a2, a2k = t4_.next(); a3, a3k = t4_.next(); a4, a4k = t4_.next()
                    tt(p, "dve", a1[:], pre[:], k_[:, 0, :], ALU.mult, [prk, kk], [a1k])
                    tt(p, "dve", a2[:], pp[:], k_[:, 1, :], ALU.mult, [ppk, kk], [a2k])
                    tt(p, "dve", a3[:], pre[:], k_[:, 1, :], ALU.mult, [prk, kk], [a3k])
                    tt(p, "dve", a4[:], pp[:], k_[:, 0, :], ALU.mult, [ppk, kk], [a4k])
                    tt(p, "pool", Yre[:, fb, :], a1[:], a2[:], ALU.subtract, [a1k, a2k], ["Yre"])
                    tt(p, "pool", Yp[:, fb, :], a3[:], a4[:], ALU.add, [a3k, a4k], ["Yp"])
                    if fb == 0:
                        cp(p, "pool", Yre[0:1, 0, :], a1[0:1, :], [a1k, "Yre"], ["Yre"])
                        cp(p, "pool", Yp[0:1, 0, :], a2[0:1, :], [a2k, "Yp"], ["Yp"])
                for tb in range(nb):
                    c_, ck = cbk.next(); s_, sk = sbk.next()
                    p.dma(c_[:, 0:nb, :], Cb[tb], (), [ck], eng="act")
                    p.dma(s_[:, 0:nb, :], Sb[tb], (), [sk], eng="sp")
                    cp(p, "dve", s_[0:1, 0, :], sgj[0:1, :], [sk, "sgp"], [sk])
                    ps, pk = pi.next()
                    for fc in range(nb):
                        mm(p, ps[:], c_[:, fc, :], Yre[:, fc, :], fc == 0, False, [ck, "Yre"], [pk])
                    for fc in range(nb):
                        mm(p, ps[:], s_[:, fc, :], Yp[:, fc, :], False, fc == nb - 1, [sk, "Yp"], [pk])
                    r0 = toff + tb * 128
                    h_, hk = hv.next()
                    p.dma(h_[:], HYTM[r0:r0 + 128, :], ["HYTM"], [hk], eng="act")
                    a1, a1k = t4_.next(); a2, a2k = t4_.next()
                    if order == 0:
                        tt(p, "pool", a1[:], h_[:, 0:512], bbc[:, 0, :], ALU.mult, [hk, "bbc"], [a1k])
                        tt(p, "dve", a2[:], ps[:], a1[:], ALU.add, [pk, a1k], [a2k])
                        z_, zk = z1t.next()
                        tt(p, "dve", z_[:], a2[:], h_[:, 512:1024], ALU.mult, [a2k, hk], [zk])
                        p.dma(Z1F[r0:r0 + 128, :], z_[:], [zk], ["Z1F"])
                        cp(p, "pool", src_t[:, tb, :], z_[:], [zk], ["srct"])
                    else:
                        z_, zk = z1t.next()
                        p.dma(z_[:], Z1F[r0:r0 + 128, :], ["Z1F"], [zk], eng="sp")
                        tt(p, "pool", a1[:], z_[:], bbc[:, 1, :], ALU.mult, [zk, "bbc"], [a1k])
                        tt(p, "dve", a2[:], ps[:], a1[:], ALU.add, [pk, a1k], [a2k])
                        o, ok = ob.next()
                        tt(p, "dve", o[:], a2[:], h_[:, 1024:1536], ALU.mult, [a2k, hk], [ok])
                        p.dma(MIXT[r0:r0 + 128, 512:1024], o[:], [ok], ["MIXT"])
```

```python
import math
import contextlib
import numpy as np
import ml_dtypes
import concourse.bass as bass
import concourse.mybir as mybir
from concourse.bass_utils import run_bass_kernel_spmd

F32 = mybir.dt.float32
BF16 = mybir.dt.bfloat16
I32 = mybir.dt.int32
AF = mybir.ActivationFunctionType
ALU = mybir.AluOpType

D = 1024
NS = 4096
NP_ = 512
NT = NS + NP_
DFF = 2816
NFC = 22
EPS = 1e-6
DEPTH = 4
ENGS = ("pe", "dve", "act", "pool", "sp")
SEM_ROLL = 12000
NDMA_SEM = 28
TWO_PI = 2.0 * math.pi


class Prog:
    def __init__(self, nc, top):
        self.nc = nc
        self.top = top
        self.stack = top
        self.q = {e: [] for e in ENGS}
        self.cnt = {e: 0 for e in ENGS}
        self.esems = {e: [] for e in ENGS}
        self.last_tok = {e: None for e in ENGS}
        self.last_w = {}
        self.readers = {}
        self.waited = {e: {} for e in ENGS}
        self.dma_sems = [top.enter_context(nc.semaphore("dq%d" % i)) for i in range(NDMA_SEM)]
        self.dma_uses = [0] * NDMA_SEM
        self.dma_i = 0
        self.sem_ids = {}
        self.nbuf = 0
        self.ninst = 0

    def sbuf(self, shape, dtype, name="sb"):
        self.nbuf += 1
        return self.stack.enter_context(self.nc.sbuf_tensor("%s_%d" % (name, self.nbuf), list(shape), dtype))

    def psum(self, shape, dtype=F32, name="ps"):
        self.nbuf += 1
        return self.stack.enter_context(self.nc.psum_tensor("%s_%d" % (name, self.nbuf), list(shape), dtype))

    def _esem(self, eng, k):
        while len(self.esems[eng]) <= k:
            s = self.top.enter_context(self.nc.semaphore("e_%s_%d" % (eng, len(self.esems[eng]))))
            self.esems[eng].append(s)
        return self.esems[eng][k]

    def _need(self, eng, tok, waits):
        if tok is None:
            return
        sem, val, teng = tok
        if teng == "pe" and eng == "pe":
            return
        key = id(sem)
        self.sem_ids[key] = sem
        if self.waited[eng].get(key, 0) >= val:
            return
        if val > waits.get(key, 0):
            waits[key] = val

    def op(self, eng, fn, reads=(), writes=(), dma=False):
        waits = {}
        for r in reads:
            self._need(eng, self.last_w.get(r), waits)
        for w in writes:
            self._need(eng, self.last_w.get(w), waits)
            for t in self.readers.get(w, ()):
                self._need(eng, t, waits)
        if dma:
            slot = self.dma_i % NDMA_SEM
            self.dma_i += 1
            sem = self.dma_sems[slot]
            if self.dma_uses[slot] > 0:
                self._need(eng, (sem, 16 * self.dma_uses[slot], "dma"), waits)
            self.dma_uses[slot] += 1
            tok = (sem, 16 * self.dma_uses[slot], "dma")
            inc = 16
        else:
            n = self.cnt[eng]
            self.cnt[eng] += 1
            sem = self._esem(eng, n // SEM_ROLL)
            tok = (sem, (n % SEM_ROLL) + 1, eng)
            inc = 1
            self.last_tok[eng] = tok
        wl = []
        for key, val in waits.items():
            self.waited[eng][key] = val
            wl.append((self.sem_ids[key], val))
        self.q[eng].append((wl, fn, tok[0], inc))
        self.ninst += 1
        for r in reads:
            self.readers.setdefault(r, []).append(tok)
        for w in writes:
            self.last_w[w] = tok
            self.readers[w] = []
        return tok

    def dma(self, out, in_, reads=(), writes=(), eng="sp", **kw):
        return self.op(eng, lambda e: e.dma_start(out=out, in_=in_, **kw), reads, writes, dma=True)

    def barrier(self):
        toks = [self.last_tok[e] for e in ENGS if self.last_tok[e] is not None]
        for i, s in enumerate(self.dma_sems):
            if self.dma_uses[i] > 0:
                toks.append((s, 16 * self.dma_uses[i], "dma"))
        for e in ENGS:
            waits = {}
            for t in toks:
                if t[2] == e:
                    continue
                self._need(e, t, waits)
            wl = []
            for key, val in waits.items():
                self.waited[e][key] = val
                wl.append((self.sem_ids[key], val))
            if wl:
                self.q[e].append((wl, None, None, 0))
        self.last_w.clear()
        self.readers.clear()

    def flush(self):
        q = self.q

        def run(engname, e):
            for wl, fn, sem, inc in q[engname]:
                for s, v in wl:
                    e.wait_ge(s, v)
                if fn is not None:
                    fn(e).then_inc(sem, inc)

        with self.nc.Block() as block:
            @block.tensor
            def _(e):
                run("pe", e)

            @block.vector
            def _(e):
                run("dve", e)

            @block.scalar
            def _(e):
                run("act", e)

            @block.gpsimd
            def _(e):
                run("pool", e)

            @block.sync
            def _(e):
                run("sp", e)
        self.q = {e: [] for e in ENGS}

    @contextlib.contextmanager
    def phase(self):
        with contextlib.ExitStack() as st:
            old = self.stack
            self.stack = st
            yield
            self.barrier()
            self.flush()
            self.stack = old


class RR:
    def __init__(self, p, n, shape, dtype, name, psum=False):
        self.t = [(p.psum(shape, dtype, name) if psum else p.sbuf(shape, dtype, name)) for _ in range(n)]
        self.k = ["%s@%d_%d" % (name, p.nbuf, i) for i in range(n)]
        self.n = n
        self.i = 0

    def next(self):
        j = self.i % self.n
        self.i += 1
        return self.t[j], self.k[j]


def mm(p, out, lhsT, rhs, start, stop, r, w):
    p.op("pe", lambda e: e.matmul(out, lhsT=lhsT, rhs=rhs, start=start, stop=stop), r, w)


def trp(p, out, in_, ident, r, w):
    p.op("pe", lambda e: e.transpose(out, in_, ident), r, w)


def act(p, out, in_, func, r, w, scale=None, bias=None, accum=None):
    kw = {}
    if scale is not None:
        kw["scale"] = scale
    if bias is not None:
        kw["bias"] = bias
    if accum is not None:
        kw["accum_out"] = accum
    p.op("act", lambda e: e.activation(out=out, in_=in_, func=func, **kw), r, w)


def tt(p, eng, out, a, b, op, r, w):
    p.op(eng, lambda e: e.tensor_tensor(out=out, in0=a, in1=b, op=op), r, w)


def ts(p, eng, out, a, s1, s2, op0, op1, r, w):
    if op1 is None:
        p.op(eng, lambda e: e.tensor_scalar(out=out, in0=a, scalar1=s1, scalar2=None, op0=op0), r, w)
    else:
        p.op(eng, lambda e: e.tensor_scalar(out=out, in0=a, scalar1=s1, scalar2=s2, op0=op0, op1=op1), r, w)


def stt(p, out, a, s, b, op0, op1, r, w):
    p.op("dve", lambda e: e.scalar_tensor_tensor(out=out, in0=a, scalar=s, in1=b, op0=op0, op1=op1), r, w)


def cp(p, eng, out, in_, r, w):
    if eng == "act":
        p.op("act", lambda e: e.activation(out=out, in_=in_, func=AF.Copy), r, w)
    else:
        p.op(eng, lambda e: e.tensor_copy(out=out, in_=in_), r, w)


def mset(p, eng, ap, val, w):
    p.op(eng, lambda e: e.memset(ap, val), (), w)


def recip(p, out, in_, r, w):
    p.op("dve", lambda e: e.reciprocal(out=out, in_=in_), r, w)


_CONST = {}


def _dft_blocks(L):
    N = 2 * L
    nb = L // 128
    s = np.arange(L, dtype=np.int64)
    idx = (s[:, None] * s[None, :]) % N
    ang = idx.astype(np.float64) * (TWO_PI / N)
    C = np.cos(ang)
    S = np.sin(ang)

    def blk(M):
        return np.ascontiguousarray(M.reshape(nb, 128, nb, 128).transpose(2, 1, 0, 3)).astype(ml_dtypes.bfloat16)

    return blk(C), blk(S)


def _feats(L):
    tn = (np.arange(L, dtype=np.float32) / np.float32(L)).astype(np.float32)
    bands = np.linspace(1e-4, 15.0, 16, dtype=np.float32)
    ang = (np.float32(TWO_PI) * tn[:, None]).astype(np.float32) * bands
    f = np.concatenate([tn[:, None], np.cos(ang), np.sin(ang)], axis=-1).astype(np.float32)
    tnl = np.ascontiguousarray(tn.reshape(L // 128, 128).T)
    return np.ascontiguousarray(f.T), tnl


def consts():
    if _CONST:
        return _CONST
    c = _CONST
    c["identb"] = np.eye(128, dtype=np.float32).astype(ml_dtypes.bfloat16)
    c["identf"] = np.eye(128, dtype=np.float32)
    t = np.arange(NS)
    inv = (10000.0 ** (-np.arange(16, dtype=np.float32) / 16)).astype(np.float32)
    rc = np.zeros((64, NS), np.float32)
    rs = np.zeros((64, NS), np.float32)
    for d in range(64):
        pos = (t // 64) if d < 32 else (t % 64)
        ang = pos.astype(np.float32) * inv[d % 16]
        rc[d] = np.cos(ang)
        rs[d] = np.sin(ang)
    c["ropeC"] = np.concatenate([rc, rc], 0)
    c["ropeS"] = np.concatenate([rs, rs], 0)
    kc = np.arange(64)[:, None]
    qc = 63 - np.arange(64)[None, :]
    cs = np.clip(qc - 8, 0, 48)
    wm = ((kc >= cs) & (kc < cs + 16)).astype(np.float32)
    c["winmask"] = np.concatenate([wm, wm], 0)
    c["dftC4096"], c["dftS4096"] = _dft_blocks(4096)
    c["dftC256"], c["dftS256"] = _dft_blocks(256)
    c["feats4096"], c["tn4096"] = _feats(4096)
    c["feats256"], c["tn256"] = _feats(256)
    sg = np.where(np.arange(128) % 2 == 0, 1.0, -1.0).astype(np.float32)
    c["sgnp"] = np.tile(sg[:, None], (1, 32)).astype(ml_dtypes.bfloat16)
    c["sgnj"] = sg[None, :].astype(ml_dtypes.bfloat16)
    c["iota128"] = np.tile(np.arange(1, 129, dtype=np.float32)[None, :], (128, 1))
    return c


IN_SPECS = {}


def build_program(nlayers=DEPTH):
    nc = bass.Bass("TRN2", target_bir_lowering=False)
    T = {}

    def din(name, shape, dt=F32):
        T[name] = nc.dram_tensor(name, list(shape), dt, kind="ExternalInput").ap()
        IN_SPECS[name] = (tuple(shape), dt)
        return T[name]

    def dout(name, shape):
        T[name] = nc.dram_tensor(name, list(shape), F32, kind="ExternalOutput").ap()
        return T[name]

    def dscr(name, shape, dt=F32):
        T[name] = nc.dram_tensor(name, list(shape), dt).ap()
        return T[name]

    x0 = din("x0", [NT, D])
    cak = din("cak", [2, 512, 512]); cav = din("cav", [2, 512, 512])
    cbk = din("cbk", [2, 512, 512]); cbv = din("cbv", [2, 512, 512])
    sre = din("sre", [2, 2, 128, 16]); sim = din("sim", [2, 2, 128, 16])
    cond = din("cond", [128, 8, 2])
    w_mod = din("w_mod", [4, D, 6 * D]); b_mod = din("b_mod", [4, 128, 48])
    gpre1 = din("gpre1", [4, 128, 8]); gpost1 = din("gpost1", [4, 128, 8])
    gpre2 = din("gpre2", [4, 128, 8]); gpost2 = din("gpost2", [4, 128, 8])
    wg_in = din("wg", [4, D, DFF]); wu_in = din("wu", [4, D, DFF]); wd_in = din("wd", [4, DFF, D])
    w_in_e = din("w_in_e", [2, D, 3072]); w_out_e = din("w_out_e", [2, D, D])
    rpb = din("rpb", [2, 8, 15, 31])
    lamv = din("lamv", [2, 4, 64]); gsub = din("gsub", [2, 128])
    w_in_o = din("w_in_o", [2, D, 2048]); w_out_o = din("w_out_o", [2, D, D])
    s_lre = din("s_lre", [2, 2, 128, 16]); s_lim = din("s_lim", [2, 2, 128, 16]); s_ldt = din("s_ldt", [2, 2, 128, 16])
    s_bre = din("s_bre", [2, 2, 128, 256]); s_bim = din("s_bim", [2, 2, 128, 256])
    s_cre = din("s_cre", [2, 2, 128, 256]); s_cim = din("s_cim", [2, 2, 128, 256])
    s_d = din("s_d", [2, 128, 4])
    w_glu = din("w_glu", [2, 512, 512]); b_glu = din("b_glu", [2, 128, 4])
    hcw = din("hcw", [2, 128, 12, 3]); hcb = din("hcb", [2, 128, 12])
    hw1 = din("hw1", [2, 33, 64]); hb1 = din("hb1", [2, 64, 1]); hf1 = din("hf1", [2, 64, 1])
    hw2 = din("hw2", [2, 64, 64]); hb2 = din("hb2", [2, 64, 1]); hf2 = din("hf2", [2, 64, 1])
    hw3 = din("hw3", [2, 64, 2048]); hdec = din("hdec", [2, 4, 512]); hbias = din("hbias", [2, 2, 512])
    identb_d = din("identb", [128, 128], BF16); identf_d = din("identf", [128, 128])
    ropeC = din("ropeC", [128, NS]); ropeS = din("ropeS", [128, NS])
    winmask = din("winmask", [128, 64])
    dftC = {4096: din("dftC4096", [32, 128, 32, 128], BF16), 256: din("dftC256", [2, 128, 2, 128], BF16)}
    dftS = {4096: din("dftS4096", [32, 128, 32, 128], BF16), 256: din("dftS256", [2, 128, 2, 128], BF16)}
    feats = {4096: din("feats4096", [33, 4096]), 256: din("feats256", [33, 256])}
    tnl = {4096: din("tn4096", [128, 32]), 256: din("tn256", [128, 2])}
    iota_d = din("iota128", [128, 128])
    din("sgnp", [128, 32], BF16); din("sgnj", [1, 128], BF16)

    o_yp = dout("o_yp", [NP_, D]); o_ys = dout("o_ys", [NS, D])
    o_ak = dout("o_ak", [2, 2, 256, 512]); o_av = dout("o_av", [2, 2, 256, 512])
    o_bk = dout("o_bk", [2, 2, 256, 512]); o_bv = dout("o_bv", [2, 2, 256, 512])
    o_sre = dout("o_sre", [2, 2, 2, 16, 128]); o_sim = dout("o_sim", [2, 2, 2, 16, 128])

    X = dscr("X", [NT, D])
    WG = dscr("WG", [4, 11, 128, 8, 256], BF16); WU = dscr("WU", [4, 11, 128, 8, 256], BF16)
    QAT = dscr("QAT", [512, 5120], BF16); KAT = dscr("KAT", [512, 5120], BF16); VA = dscr("VA", [5120, 512], BF16)
    QBT = dscr("QBT", [512, 5120], BF16); KBT = dscr("KBT", [512, 5120], BF16); VB = dscr("VB", [5120, 512], BF16)
    MIXT = dscr("MIXT", [NT, D], BF16)
    MIXF = dscr("MIXF", [D, NT], BF16)
    RPBP = dscr("RPBP", [8, 15, 288])
    UT32 = dscr("UT32", [512, NT]); UTB = dscr("UTB", [512, NT], BF16)
    HYT = dscr("HYT", [1536, NT]); HYTM = dscr("HYTM", [NT, 1536])
    YS = dscr("YS", [2, 512, NT])
    KSP = {4096: dscr("KSP4096", [2, 2, 4096, 512]), 256: dscr("KSP256", [2, 2, 256, 512])}
    Z1F = dscr("Z1F", [NT, 512])

    fin_keys = []

    with contextlib.ExitStack() as top:
        p = Prog(nc, top)
        identb = p.sbuf([128, 128], BF16, "identb")
        identf = p.sbuf([128, 128], F32, "identf")
        condT = p.sbuf([128, 8, 2], F32, "condT")
        modA1 = p.sbuf([128, 8, 2], F32, "modA1"); modB1 = p.sbuf([128, 8, 2], F32, "modB1")
        modA2 = p.sbuf([128, 8, 2], F32, "modA2"); modB2 = p.sbuf([128, 8, 2], F32, "modB2")
        ggbc1 = [p.sbuf([128, D], F32, "ggbc1") for _ in range(2)]
        ggbc2 = [p.sbuf([128, D], F32, "ggbc2") for _ in range(2)]
        epsT = p.sbuf([128, 1], F32, "epsT")
        EPS_T[0] = epsT

        with p.phase():
            p.dma(identb[:], identb_d, (), ["identb"])
            p.dma(identf[:], identf_d, (), ["identf"])
            p.dma(condT[:], cond, (), ["condT"])
            mset(p, "dve", epsT[:], EPS, ["epsT"])
            act(p, condT[:], condT[:], AF.Silu, ["condT"], ["condT"])
            for i in range(9):
                p.dma(X[i * 512:(i + 1) * 512, :], x0[i * 512:(i + 1) * 512, :], (), ["X%d" % i], eng=("sp" if i % 2 else "act"))
            st32 = RR(p, 3, [128, DFF], F32, "st32")
            st16 = RR(p, 3, [128, DFF], BF16, "st16")
            k = 0
            for l in range(nlayers):
                for (src, dst) in ((wg_in, WG), (wu_in, WU)):
                    for rb in range(8):
                        a, ak = st32.next()
                        b, bk = st16.next()
                        p.dma(a[:], src[l][rb * 128:(rb + 1) * 128, :], (), [ak], eng=("sp" if k % 2 else "act"))
                        eng = ("dve", "pool", "act")[k % 3]
                        cp(p, eng, b[:], a[:], [ak], [bk])
                        p.dma(dst[l].rearrange("pc q kc f -> q kc pc f")[:, rb], b[:].rearrange("q (pc f) -> q pc f", f=256), [bk], ["wscr"], eng="sp")
                        k += 1

        for l in range(nlayers):
            even = (l % 2 == 0)
            li = l // 2
            lam_init = 0.8 - 0.6 * math.exp(-0.3 * l)
            build_mod(p, T, l, condT, identf, (modA1, modB1, modA2, modB2, ggbc1, ggbc2))
            if even:
                build_even_proj(p, T, l, li, identb, identf, epsT, modA1, modB1)
                build_even_attn(p, T, l, li, lam_init)
                tm_chunks = list(range(8))
            else:
                build_odd_proj(p, T, l, li, identb, epsT, modA1, modB1)
                build_s5(p, T, l, li, identf)
                build_hyena(p, T, l, li, identf)
                tm_chunks = [4, 5, 6, 7]
            build_out_ffn(p, T, l, li, even, tm_chunks, identb, epsT, modA2, modB2, ggbc1, ggbc2)

        with p.phase():
            for i in range(8):
                p.dma(o_ys[i * 512:(i + 1) * 512, :], X[i * 512:(i + 1) * 512, :], (), ["oys%d" % i], eng=("sp" if i % 2 else "act"))
            p.dma(o_yp[:, :], X[NS:NT, :], (), ["oyp"])
    return nc


def build_mod(p, T, l, condT, identf, outs):
    modA1, modB1, modA2, modB2, ggbc1, ggbc2 = outs
    w_mod, b_mod = T["w_mod"], T["b_mod"]
    with p.phase():
        mfm = p.sbuf([128, 48, 2], F32, "mfm")
        bm = p.sbuf([128, 48], F32, "bm")
        g4 = p.sbuf([128, 4, 8], F32, "g4")
        gg = p.sbuf([128, 8, 2], F32, "gg")
        lhs = RR(p, 2, [128, 128], F32, "bclhs")
        wp = RR(p, 2, [128, 8, 512], F32, "wmodp")
        pm = RR(p, 2, [128, 512], F32, "pm", psum=True)
        pb = RR(p, 2, [128, 512], F32, "pbc", psum=True)
        p.dma(bm[:], b_mod[l], (), ["bm"])
        for j, nm in enumerate(("gpre1", "gpost1", "gpre2", "gpost2")):
            p.dma(g4[:, j, :], T[nm][l], (), ["g4"], eng="act")
        for pc in range(12):
            w, wk = wp.next()
            p.dma(w[:], w_mod[l].rearrange("(kc q) n -> q kc n", q=128)[:, :, pc * 512:(pc + 1) * 512], (), [wk],
                  eng=("sp" if pc % 2 else "act"))
            ps, pk = pm.next()
            for jc in range(4):
                for kc in range(8):
                    mm(p, ps[:, jc * 2:jc * 2 + 2], w[:, kc, jc * 128:(jc + 1) * 128], condT[:, kc, :], kc == 0, kc == 7,
                       [wk, "condT"], [pk])
            for cd in range(2):
                tt(p, "dve", mfm[:, pc * 4:pc * 4 + 4, cd], ps[:, cd:8:2], bm[:, pc * 4:pc * 4 + 4], ALU.add,
                   [pk, "bm"], ["mfm"])
        for cd in range(2):
            ts(p, "dve", modA1[:, :, cd], mfm[:, 8:16, cd], 1.0, None, ALU.add, None, ["mfm"], ["modA1"])
            tt(p, "dve", modA1[:, :, cd], modA1[:, :, cd], g4[:, 0, :], ALU.mult, ["modA1", "g4"], ["modA1"])
            cp(p, "dve", modB1[:, :, cd], mfm[:, 0:8, cd], ["mfm"], ["modB1"])
            ts(p, "dve", modA2[:, :, cd], mfm[:, 32:40, cd], 1.0, None, ALU.add, None, ["mfm"], ["modA2"])
            tt(p, "dve", modA2[:, :, cd], modA2[:, :, cd], g4[:, 2, :], ALU.mult, ["modA2", "g4"], ["modA2"])
            cp(p, "dve", modB2[:, :, cd], mfm[:, 24:32, cd], ["mfm"], ["modB2"])
            for which, (sp_, gj, dst) in enumerate(((2, 1, ggbc1), (5, 3, ggbc2))):
                tt(p, "dve", gg[:, :, cd], mfm[:, sp_ * 8:sp_ * 8 + 8, cd], g4[:, gj, :], ALU.mult, ["mfm", "g4"], ["gg"])
                for half in range(2):
                    ps, pk = pb.next()
                    for kk in range(4):
                        kc = half * 4 + kk
                        lt, lk = lhs.next()
                        cp(p, "dve", lt[:], gg[:, kc, cd:cd + 1].to_broadcast([128, 128]), ["gg"], [lk])
                        mm(p, ps[:, kk * 128:(kk + 1) * 128], lt[:], identf[:], True, True, [lk, "identf"], [pk])
                    cp(p, "act", dst[cd][:, half * 512:(half + 1) * 512], ps[:], [pk], ["ggbc"])


class NormCtx:
    def __init__(self, p, identb, epsT):
        self.p = p
        self.identb = identb
        self.epsT = epsT
        self.junk = RR(p, 2, [128, D], BF16, "junk")
        self.ss = RR(p, 2, [128, 4], F32, "ss")
        self.xn = RR(p, 2, [128, 4, D], BF16, "xn")
        self.pst = RR(p, 2, [128, 1024], BF16, "pst", psum=True)

    def run(self, xs, xkey, A, B, cd, hT, hkey, ncol=512):
        p = self.p
        nt = ncol // 128
        ss, sk = self.ss.next()
        xn, xk = self.xn.next()
        for i in range(nt):
            j, jk = self.junk.next()
            act(p, j[:], xs[:, i, :], AF.Square, [xkey], [jk, sk], accum=ss[:, i:i + 1])
        act(p, ss[:, 0:nt], ss[:, 0:nt], AF.Sqrt, [sk], [sk], scale=1.0 / D, bias=self.epsT[:])
        recip(p, ss[:, 0:nt], ss[:, 0:nt], [sk], [sk])
        for i in range(nt):
            ts(p, "dve" if i % 2 == 0 else "pool", xn[:, i, :], xs[:, i, :], ss[:, i:i + 1], None, ALU.mult, None,
               [xkey, sk], [xk])
        for kc in range(8):
            ps, pk = self.pst.next()
            for i in range(nt):
                trp(p, ps[:, i * 128:(i + 1) * 128], xn[:, i, kc * 128:(kc + 1) * 128], self.identb[:], [xk, "identb"], [pk])
            act(p, hT[:, kc, 0:ncol], ps[:, 0:ncol], AF.Identity, [pk, "modA", "modB"], [hkey],
                scale=A[:, kc, cd:cd + 1], bias=B[:, kc, cd:cd + 1])


def load_w_bf16(p, dst, dkey, src2d, ncols, st, nkc=8):
    k = 0
    for kc in range(nkc):
        c0 = 0
        while c0 < ncols:
            cw = min(1024, ncols - c0)
            a, ak = st.next()
            p.dma(a[:, 0:cw], src2d[kc * 128:(kc + 1) * 128, c0:c0 + cw], (), [ak], eng=("sp" if k % 2 else "act"))
            cp(p, ("dve", "pool")[k % 2], dst[:, kc, c0:c0 + cw], a[:, 0:cw], [ak], [dkey])
            c0 += cw
            k += 1


def unit_cd(u):
    return 0 if u < 8 else 1


def xview(X, t0, n=512):
    return X[t0:t0 + n, :].rearrange("(i q) c -> q i c", q=128)


class PostNorm:
    def __init__(self, p, epsT):
        self.p = p
        self.epsT = epsT
        self.ss = RR(p, 4, [128, 4], F32, "pnss")
        self.junk = RR(p, 2, [128, D], BF16, "pnjunk")
        self.tmp = RR(p, 2, [128, D], F32, "pntmp")

    def run(self, po0, k0, po1, k1, x, xk, gg):
        p = self.p
        ss, sk = self.ss.next()
        j, jk = self.junk.next()
        act(p, j[:, 0:512], po0[:], AF.Square, [k0], [jk, sk], accum=ss[:, 0:1])
        act(p, j[:, 512:1024], po1[:], AF.Square, [k1], [jk, sk], accum=ss[:, 1:2])
        tt(p, "dve", ss[:, 2:3], ss[:, 0:1], ss[:, 1:2], ALU.add, [sk], [sk])
        act(p, ss[:, 2:3], ss[:, 2:3], AF.Sqrt, [sk], [sk], scale=1.0 / D, bias=self.epsT[:])
        recip(p, ss[:, 3:4], ss[:, 2:3], [sk], [sk])
        t, tk = self.tmp.next()
        stt(p, t[:, 0:512], po0[:], ss[:, 3:4], gg[:, 0:512], ALU.mult, ALU.mult, [k0, sk, "ggbc"], [tk])
        stt(p, t[:, 512:1024], po1[:], ss[:, 3:4], gg[:, 512:1024], ALU.mult, ALU.mult, [k1, sk, "ggbc"], [tk])
        tt(p, "pool", x, x, t[:], ALU.add, [tk, xk], [xk])


def build_out_ffn(p, T, l, li, even, tm_chunks, identb, epsT, modA2, modB2, ggbc1, ggbc2):
    X, MIXT, MIXF = T["X"], T["MIXT"], T["MIXF"]
    w_out = T["w_out_e"][li] if even else T["w_out_o"][li]
    ntm = len(tm_chunks)
    c0 = tm_chunks[0]
    with p.phase():
        st = RR(p, 2, [128, 1024], F32, "wst")
        wout = p.sbuf([128, 8, D], BF16, "wout")
        load_w_bf16(p, wout, "wout", w_out, D, st)
        pn = PostNorm(p, epsT)
        xs = RR(p, 2, [128, 4, D], F32, "xs")
        mT = RR(p, 2, [128, 8, 512], BF16, "mT")
        mx = RR(p, 2, [128, 4, ntm * 128], BF16, "mx")
        pso = RR(p, 4, [128, 512], F32, "pso", psum=True)
        pst = RR(p, 2, [128, 1024], BF16, "pst", psum=True)
        def ldx(u):
            xt, xk = xs.next()
            p.dma(xt[:], xview(X, u * 512), ["X%d" % u], [xk], eng="sp")
            return xt, xk
        pre = ldx(0)
        for u in range(9):
            t0 = u * 512
            cd = unit_cd(u)
            xt, xk = pre
            m, mk = mT.next()
            for kc in range(8):
                if kc not in tm_chunks:
                    p.dma(m[:, kc, :], MIXF[kc * 128:(kc + 1) * 128, t0:t0 + 512], ["MIXF"], [mk], eng="sp")
            mxt, mxk = mx.next()
            p.dma(mxt[:], MIXT[t0:t0 + 512, c0 * 128:(c0 + ntm) * 128].rearrange("(i q) c -> q i c", q=128), ["MIXT"], [mxk])
            for kc in tm_chunks:
                ps, pk = pst.next()
                for i in range(4):
                    trp(p, ps[:, i * 128:(i + 1) * 128], mxt[:, i, (kc - c0) * 128:(kc - c0 + 1) * 128], identb[:], [mxk, "identb"], [pk])
                cp(p, "act" if kc % 2 else "dve", m[:, kc, :], ps[:, 0:512], [pk], [mk])
            for i in range(4):
                po0, k0 = pso.next()
                po1, k1 = pso.next()
                for half, (po, pk) in enumerate(((po0, k0), (po1, k1))):
                    for kc in range(8):
                        mm(p, po[:], m[:, kc, i * 128:(i + 1) * 128], wout[:, kc, half * 512:(half + 1) * 512], kc == 0, kc == 7,
                           [mk, "wout"], [pk])
                pn.run(po0, k0, po1, k1, xt[:, i, :], xk, ggbc1[cd])
            p.dma(xview(X, t0), xt[:], [xk], ["X%d" % u], eng="act")
            if u + 1 < 9:
                pre = ldx(u + 1)
    WG, WU = T["WG"][l], T["WU"][l]
    with p.phase():
        st = RR(p, 2, [128, 1024], F32, "wst")
        wd = p.sbuf([128, NFC, D], BF16, "wd")
        load_w_bf16(p, wd, "wd", T["wd"][l], D, st, nkc=NFC)
        pn = PostNorm(p, epsT)
        nctx = NormCtx(p, identb, epsT)
        xs = RR(p, 2, [128, 4, D], F32, "xs")
        hT = RR(p, 1, [128, 8, 512], BF16, "hT")
        actT = p.sbuf([128, NFC, 512], BF16, "actT")
        wgu = RR(p, 2, [128, 2, 8, 256], BF16, "wgu")
        sg = RR(p, 2, [128, 512], F32, "sg")
        pso = RR(p, 4, [128, 512], F32, "pso", psum=True)
        def ldx2(u):
            xt, xk = xs.next()
            p.dma(xt[:], xview(X, u * 512), ["X%d" % u], [xk], eng="sp")
            return xt, xk
        pre = ldx2(0)
        for u in range(9):
            t0 = u * 512
            cd = unit_cd(u)
            xt, xk = pre
            if u + 1 < 9:
                pre = ldx2(u + 1)
            h, hk = hT.next()
            nctx.run(xt, xk, modA2, modB2, cd, h, hk)
            for pc in range(11):
                w, wk = wgu.next()
                p.dma(w[:, 0], WG[pc], ["wscr"], [wk], eng="sp")
                p.dma(w[:, 1], WU[pc], ["wscr"], [wk], eng="sp")
                for f2 in range(2):
                    fc = pc * 2 + f2
                    pg, kg = pso.next()
                    pu, ku = pso.next()
                    for kc in range(8):
                        mm(p, pg[:], w[:, 0, kc, f2 * 128:(f2 + 1) * 128], h[:, kc, :], kc == 0, kc == 7, [wk, hk], [kg])
                    for kc in range(8):
                        mm(p, pu[:], w[:, 1, kc, f2 * 128:(f2 + 1) * 128], h[:, kc, :], kc == 0, kc == 7, [wk, hk], [ku])
                    s, sk = sg.next()
                    act(p, s[:], pg[:], AF.Silu, [kg], [sk])
                    tt(p, "dve", actT[:, fc, :], pu[:], s[:], ALU.mult, [ku, sk], ["actT"])
            for i in range(4):
                po0, k0 = pso.next()
                po1, k1 = pso.next()
                for half, (po, pk) in enumerate(((po0, k0), (po1, k1))):
                    for fc in range(NFC):
                        mm(p, po[:], actT[:, fc, i * 128:(i + 1) * 128], wd[:, fc, half * 512:(half + 1) * 512], fc == 0, fc == NFC - 1,
                           ["actT", "wd"], [pk])
                pn.run(po0, k0, po1, k1, xt[:, i, :], xk, ggbc2[cd])
            p.dma(xview(X, t0), xt[:], [xk], ["X%d" % u], eng="act")


def build_even_proj(p, T, l, li, identb, identf, epsT, modA1, modB1):
    X = T["X"]
    QAT, KAT, VA, QBT, KBT, VB = T["QAT"], T["KAT"], T["VA"], T["QBT"], T["KBT"], T["VB"]
    with p.phase():
        st = RR(p, 2, [128, 1024], F32, "wst")
        win = p.sbuf([128, 8, 3072], BF16, "win")
        wrot = p.sbuf([128, 8, 1024], BF16, "wrot")
        load_w_bf16(p, win, "win", T["w_in_e"][li], 3072, st)
        for (s0, d0) in ((1536, 0), (2048, 512)):
            sv = win[:, :, s0:s0 + 512].rearrange("q k (b two s) -> q k b two s", b=16, two=2, s=16)
            dv = wrot[:, :, d0:d0 + 512].rearrange("q k (b two s) -> q k b two s", b=16, two=2, s=16)
            ts(p, "dve", dv[:, :, :, 0, :], sv[:, :, :, 1, :], -1.0, None, ALU.mult, None, ["win"], ["wrot"])
            cp(p, "pool", dv[:, :, :, 1, :], sv[:, :, :, 0, :], ["win"], ["wrot"])
        nctx = NormCtx(p, identb, epsT)
        xs = RR(p, 1, [128, 4, D], F32, "xs")
        hT = RR(p, 2, [128, 8, 512], BF16, "hT")
        rcs = RR(p, 2, [128, 2, 512], F32, "rcs")
        obf = RR(p, 4, [128, 512], BF16, "obf")
        o32 = RR(p, 2, [128, 512], F32, "o32")
        t12 = RR(p, 4, [128, 512], F32, "t12")
        pso = RR(p, 5, [128, 512], F32, "pso", psum=True)
        pst = nctx.pst
        ctile = RR(p, 2, [128, 512], F32, "ctile")
        cb16 = RR(p, 2, [128, 512], BF16, "cb16")
        ktmp = RR(p, 2, [128, 4, 128], BF16, "ktmp")
        for (ck, cv, KTd, Vd) in ((T["cak"], T["cav"], KAT, VA), (T["cbk"], T["cbv"], KBT, VB)):
            for i in range(4):
                a, ak = ctile.next()
                p.dma(a[:], ck[li, i * 128:(i + 1) * 128, :], (), [ak], eng="act")
                b, bk = cb16.next()
                cp(p, "dve", b[:], a[:], [ak], [bk])
                kt, kk = ktmp.next()
                ps, pk = pst.next()
                for c in range(4):
                    trp(p, ps[:, c * 128:(c + 1) * 128], b[:, c * 128:(c + 1) * 128], identb[:], [bk, "identb"], [pk])
                cp(p, "act", kt[:].rearrange("q c t -> q (c t)"), ps[:, 0:512], [pk], [kk])
                p.dma(KTd[:, NS + i * 128:NS + (i + 1) * 128].rearrange("(c q) t -> q c t", q=128), kt[:], [kk], ["KT"])
                a, ak = ctile.next()
                p.dma(a[:], cv[li, i * 128:(i + 1) * 128, :], (), [ak], eng="act")
                b, bk = cb16.next()
                cp(p, "pool", b[:], a[:], [ak], [bk])
                p.dma(Vd[NS + i * 128:NS + (i + 1) * 128, :], b[:], [bk], ["V"])

        def fm_group(W, col0, h, hk, n):
            ps, pk = pso.next()
            for kc in range(8):
                mm(p, ps[:, 0:n], W[:, kc, col0:col0 + 128], h[:, kc, 0:n], kc == 0, kc == 7, ["win", "wrot", hk], [pk])
            return ps, pk

        for u in range(9):
            t0 = u * 512
            cd = unit_cd(u)
            xt, xk = xs.next()
            p.dma(xt[:], xview(X, t0), ["X%d" % u], [xk], eng="act")
            h, hk = hT.next()
            nctx.run(xt, xk, modA1, modB1, cd, h, hk)
            dcol = t0 if cd == 0 else NT
            if cd == 0:
                rc, rk = rcs.next()
                p.dma(rc[:, 0, :], T["ropeC"][:, t0:t0 + 512], (), [rk])
                p.dma(rc[:, 1, :], T["ropeS"][:, t0:t0 + 512], (), [rk])
            k = 0
            for (dst, wc0, rot0, dkey) in ((QAT, 0, None, "QT"), (KAT, 512, None, "KT"), (QBT, 1536, 0, "QT"), (KBT, 2048, 512, "KT")):
                for c in range(4):
                    ps, pk = fm_group(win, wc0 + c * 128, h, hk, 512)
                    o, ok = obf.next()
                    if cd == 0 and rot0 is not None:
                        ps2, pk2 = fm_group(wrot, rot0 + c * 128, h, hk, 512)
                        a, ak = t12.next()
                        b, bk = t12.next()
                        tt(p, "dve", a[:], ps[:], rc[:, 0, :], ALU.mult, [pk, rk], [ak])
                        tt(p, "dve", b[:], ps2[:], rc[:, 1, :], ALU.mult, [pk2, rk], [bk])
                        tt(p, "pool", o[:], a[:], b[:], ALU.add, [ak, bk], [ok])
                    else:
                        cp(p, "act" if k % 2 else "dve", o[:], ps[:], [pk], [ok])
                    k += 1
                    p.dma(dst[c * 128:(c + 1) * 128, dcol:dcol + 512], o[:], [ok], [dkey])
            for i in range(4):
                for (wc0, Vd, outk, outv) in ((512, None, "o_ak", None), (1024, VA, None, "o_av"), (2048, None, "o_bk", None), (2560, VB, None, "o_bv")):
                    if cd == 0 and Vd is None:
                        continue
                    ps, pk = pso.next()
                    for kc in range(8):
                        mm(p, ps[:], h[:, kc, i * 128:(i + 1) * 128], win[:, kc, wc0:wc0 + 512], kc == 0, kc == 7, ["win", hk], [pk])
                    if Vd is not None:
                        o, ok = obf.next()
                        cp(p, "act", o[:], ps[:], [pk], [ok])
                        p.dma(Vd[dcol + i * 128:dcol + (i + 1) * 128, :], o[:], [ok], ["V"])
                    if cd == 1:
                        o2, ok2 = o32.next()
                        cp(p, "act", o2[:], ps[:], [pk], [ok2])
                        nm = outk or outv
                        p.dma(T[nm][i // 2, li, (i % 2) * 128:(i % 2 + 1) * 128, :], o2[:], [ok2], [nm + str(li)])


class AttnCtx:
    def __init__(self, p):
        self.p = p
        self.pss = RR(p, 3, [128, 512], F32, "pss", psum=True)
        self.pacc = RR(p, 4, [128, 512], F32, "pacc", psum=True)
        self.E = RR(p, 3, [128, 512], BF16, "E")
        self.small = RR(p, 8, [128, 4], F32, "asmall")


def attn_block(p, C, KT, ktkey, QT, qkey, nq, Vaug, vkey, nkt, dv, finish):
    nqt = (nq + 127) // 128
    accs = [C.pacc.next() for _ in range(nqt)]

    def qk(kt):
        ps, pk = C.pss.next()
        mm(p, ps[:, 0:nq], KT[:, kt * 128:(kt + 1) * 128], QT[:, 0:nq], True, True, [ktkey, qkey], [pk])
        e, ek = C.E.next()
        act(p, e[:, 0:nq], ps[:, 0:nq], AF.Exp, [pk], [ek], scale=0.125)
        return e, ek

    pend = qk(0)
    for kt in range(nkt):
        nxt = qk(kt + 1) if kt + 1 < nkt else None
        e, ek = pend
        for i in range(nqt):
            mm(p, accs[i][0][:, 0:dv + 1], e[:, i * 128:(i + 1) * 128], Vaug[:, kt, :], kt == 0, kt == nkt - 1,
               [ek, vkey], [accs[i][1]])
        pend = nxt
    for i in range(nqt):
        finish(i, accs[i][0], accs[i][1])


def build_even_attn(p, T, l, li, lam_init):
    QAT, KAT, VA, QBT, KBT, VB = T["QAT"], T["KAT"], T["VA"], T["QBT"], T["KBT"], T["VB"]
    MIXT, RPBP = T["MIXT"], T["RPBP"]
    with p.phase():
        C = AttnCtx(p)
        lamt = p.sbuf([128, 4, 64], F32, "lamt")
        lsm = p.sbuf([128, 8], F32, "lsm")
        gsb = p.sbuf([128, 128], F32, "gsb")
        lj = p.sbuf([128, 64], F32, "lj")
        p.dma(lamt[:].rearrange("q a b -> q (a b)"), T["lamv"][li:li + 1].rearrange("o a b -> o (a b)").to_broadcast([128, 256]), (), ["lamt"])
        p.dma(gsb[:], T["gsub"][li:li + 1, :].to_broadcast([128, 128]), (), ["gsb"])
        for j in range(2):
            tt(p, "dve", lj[:], lamt[:, 2 * j, :], lamt[:, 2 * j + 1, :], ALU.mult, ["lamt"], ["lj"])
            act(p, lj[:], lj[:], AF.Identity, ["lj"], ["lj", "lsm"], accum=lsm[:, j:j + 1])
        act(p, lsm[:, 2:4], lsm[:, 0:2], AF.Exp, ["lsm"], ["lsm"])
        tt(p, "dve", lsm[:, 4:5], lsm[:, 3:4], lsm[:, 2:3], ALU.subtract, ["lsm"], ["lsm"])
        ts(p, "dve", lsm[:, 5:6], lsm[:, 4:5], -lam_init, None, ALU.add, None, ["lsm"], ["lsm"])
        ts(p, "dve", gsb[:], gsb[:], 1.0 - lam_init, None, ALU.mult, None, ["gsb"], ["gsb"])
        neglam = lsm[:, 5:6]

        om = [p.sbuf([128, 4, 128], F32, "om%d" % s) for s in range(2)]
        ocomb = RR(p, 2, [128, 128], F32, "ocomb")
        ojunk = RR(p, 2, [128, 128], F32, "ojunk")

        def diff_finish(s):
            def f(i, acc, ak):
                sm, sk = C.small.next()
                recip(p, sm[:, 0:1], acc[:, 128:129], [ak], [sk])
                ts(p, "dve", om[s][:, i, :], acc[:, 0:128], sm[:, 0:1], None, ALU.mult, None, [ak, sk], ["om%d" % s])
            return f

        def diff_combine(i, dst, dkey):
            o, ok = ocomb.next()
            stt(p, o[:], om[1][:, i, :], neglam, om[0][:, i, :], ALU.mult, ALU.add, ["om0", "om1", "lsm"], [ok])
            sm, sk = C.small.next()
            j, jk = ojunk.next()
            act(p, j[:], o[:], AF.Square, [ok], [jk, sk], accum=sm[:, 0:1])
            act(p, sm[:, 1:2], sm[:, 0:1], AF.Sqrt, [sk], [sk], scale=1.0 / 128, bias=EPS_T[0][:])
            recip(p, sm[:, 2:3], sm[:, 1:2], [sk], [sk])
            stt(p, dst, o[:], sm[:, 2:3], gsb[:], ALU.mult, ALU.mult, [ok, sk, "gsb"], [dkey])

        ktb = RR(p, 2, [64, NT], BF16, "ktb")
        vaug = RR(p, 2, [128, 36, 129], BF16, "vaug")
        qtb = RR(p, 2, [64, 512], BF16, "qtb")
        obuf = RR(p, 2, [128, 4, 128], BF16, "obufB")
        for h in range(4):
            v, vk = vaug.next()
            mset(p, "pool", v[:], 1.0, [vk])
            for t9 in range(4):
                p.dma(v[:, t9 * 9:(t9 + 1) * 9, 0:128],
                      VB[t9 * 1152:(t9 + 1) * 1152, h * 128:(h + 1) * 128].rearrange("(t q) d -> q t d", q=128), ["V"], [vk],
                      eng=("act" if t9 % 2 else "sp"))
            kts = []
            for s in range(2):
                kt_, kk = ktb.next()
                p.dma(kt_[:], KBT[h * 128 + s * 64:h * 128 + (s + 1) * 64, 0:NT], ["KT"], [kk], eng="act")
                kts.append((kt_, kk))
            for qb in range(8):
                for s in range(2):
                    q, qk = qtb.next()
                    p.dma(q[:], QBT[h * 128 + s * 64:h * 128 + (s + 1) * 64, qb * 512:(qb + 1) * 512], ["QT"], [qk])
                    attn_block(p, C, kts[s][0], kts[s][1], q, qk, 512, v, vk, 36, 128, diff_finish(s))
                ob, obk = obuf.next()
                for i in range(4):
                    diff_combine(i, ob[:, i, :], obk)
                p.dma(MIXT[qb * 512:(qb + 1) * 512, 512 + h * 128:512 + (h + 1) * 128].rearrange("(i q) c -> q i c", q=128), ob[:],
                      [obk], ["MIXT"])

        ktp = RR(p, 2, [64, 256], BF16, "ktp")
        qtp = RR(p, 2, [64, 256], BF16, "qtp")
        vap = RR(p, 2, [128, 2, 65], BF16, "vap")
        vbp = RR(p, 2, [128, 2, 129], BF16, "vbp")
        obp = RR(p, 2, [128, 2, D], BF16, "obp")
        for js in range(2):
            tb = NT + 256 * js
            ob, obk = obp.next()
            for h in range(8):
                v, vk = vap.next()
                mset(p, "pool", v[:], 1.0, [vk])
                p.dma(v[:, :, 0:64], VA[tb:tb + 256, h * 64:(h + 1) * 64].rearrange("(t q) d -> q t d", q=128), ["V"], [vk], eng="act")
                kt_, kk = ktp.next()
                p.dma(kt_[:], KAT[h * 64:(h + 1) * 64, tb:tb + 256], ["KT"], [kk], eng="act")
                q, qk = qtp.next()
                p.dma(q[:], QAT[h * 64:(h + 1) * 64, tb:tb + 256], ["QT"], [qk])

                def fin_a(i, acc, ak, h=h, ob=ob, obk=obk):
                    sm, sk = C.small.next()
                    recip(p, sm[:, 0:1], acc[:, 64:65], [ak], [sk])
                    ts(p, "dve", ob[:, i, h * 64:(h + 1) * 64], acc[:, 0:64], sm[:, 0:1], None, ALU.mult, None, [ak, sk], [obk])
                attn_block(p, C, kt_, kk, q, qk, 256, v, vk, 2, 64, fin_a)
            for h in range(4):
                v, vk = vbp.next()
                mset(p, "pool", v[:], 1.0, [vk])
                p.dma(v[:, :, 0:128], VB[tb:tb + 256, h * 128:(h + 1) * 128].rearrange("(t q) d -> q t d", q=128), ["V"], [vk], eng="act")
                for s in range(2):
                    kt_, kk = ktp.next()
                    p.dma(kt_[:], KBT[h * 128 + s * 64:h * 128 + (s + 1) * 64, tb:tb + 256], ["KT"], [kk], eng="act")
                    q, qk = qtp.next()
                    p.dma(q[:], QBT[h * 128 + s * 64:h * 128 + (s + 1) * 64, tb:tb + 256], ["QT"], [qk])
                    attn_block(p, C, kt_, kk, q, qk, 256, v, vk, 2, 128, diff_finish(s))
                for i in range(2):
                    diff_combine(i, ob[:, i, 512 + h * 128:512 + (h + 1) * 128], obk)
            p.dma(MIXT[NS + 256 * js:NS + 256 * (js + 1), :].rearrange("(i q) c -> q i c", q=128), ob[:], [obk], ["MIXT"])

        zt = p.sbuf([120, 288], F32, "zt")
        mset(p, "dve", zt[:], 0.0, ["zt"])
        p.dma(RPBP.rearrange("h r c -> (h r) c"), zt[:], ["zt"], ["RPBP"])
        p.dma(RPBP[:, :, 128:159], T["rpb"][li], (), ["RPBP"])
        wm = p.sbuf([128, 64], F32, "wm")
        p.dma(wm[:], T["winmask"], (), ["wm"])
        mraw = RR(p, 2, [128, 32, 64], F32, "mraw")
        mh = RR(p, 2, [128, 32, 64], BF16, "mh")
        kta = RR(p, 2, [64, NT], BF16, "kta")
        qta = RR(p, 2, [64, NS], BF16, "qta")
        va_e = RR(p, 2, [128, 36, 65], BF16, "va_e")
        va_o = RR(p, 2, [128, 35, 65], BF16, "va_o")
        oba = RR(p, 2, [64, 64, 64], BF16, "oba")
        En = RR(p, 3, [128, 8, 64], BF16, "En")
        for h in range(8):
            mr, mrk = mraw.next()
            kq = 0
            for off in range(8):
                for j in range(4):
                    for mlow in range(2):
                        dr = 2 * j + mlow + off
                        base = RPBP[h, dr, 80:144]
                        src = bass.AP(base.tensor, base.offset, [[1, 64], [1, 64]])
                        p.dma(mr[mlow * 64:(mlow + 1) * 64, off * 4 + j, :], src, ["RPBP"], [mrk], eng=("sp" if kq % 2 else "act"))
                        kq += 1
            act(p, mr[:], mr[:], AF.Exp, [mrk], [mrk])
            m_, mk_ = mh.next()
            tt(p, "pool", m_[:], mr[:], wm[:].unsqueeze(1).to_broadcast([128, 32, 64]), ALU.mult, [mrk, "wm"], [mk_])
            kt_, kk = kta.next()
            p.dma(kt_[:], KAT[h * 64:(h + 1) * 64, 0:NT], ["KT"], [kk], eng="act")
            q, qk = qta.next()
            p.dma(q[:], QAT[h * 64:(h + 1) * 64, 0:NS], ["QT"], [qk], eng="act")
            ve, vek = va_e.next()
            mset(p, "pool", ve[:], 1.0, [vek])
            for t9 in range(4):
                p.dma(ve[:, t9 * 9:(t9 + 1) * 9, 0:64],
                      VA[t9 * 1152:(t9 + 1) * 1152, h * 64:(h + 1) * 64].rearrange("(t q) d -> q t d", q=128), ["V"], [vek],
                      eng=("act" if t9 % 2 else "sp"))
            vo, vok = va_o.next()
            mset(p, "pool", vo[:], 1.0, [vok])
            for t9 in range(5):
                p.dma(vo[:, t9 * 7:(t9 + 1) * 7, 0:64],
                      VA[64 + t9 * 896:64 + (t9 + 1) * 896, h * 64:(h + 1) * 64].rearrange("(t q) d -> q t d", q=128), ["V"], [vok],
                      eng=("act" if t9 % 2 else "sp"))
            ob, obk = oba.next()
            def na_stage1(r):
                rs = min(max(r - 4, 0), 56)
                off = rs - r + 7
                ps, pk = C.pss.next()
                psv = ps[:].rearrange("q (j c) -> q j c", c=64)
                for j in range(4):
                    k0 = rs * 64 + 128 * j
                    mm(p, psv[:, j, :], kt_[:, k0:k0 + 128], q[:, r * 64:(r + 1) * 64], True, True, [kk, qk], [pk])
                for j in range(4):
                    k0 = NS + 128 * j
                    mm(p, psv[:, 4 + j, :], kt_[:, k0:k0 + 128], q[:, r * 64:(r + 1) * 64], True, True, [kk, qk], [pk])
                e, ek = En.next()
                act(p, e[:], psv, AF.Exp, [pk], [ek], scale=0.125)
                tt(p, "dve", e[:, 0:4, :], e[:, 0:4, :], m_[:, off * 4:off * 4 + 4, ::-1], ALU.mult, [ek, mk_], [ek])
                return e, ek

            def na_stage2(r, e, ek):
                rs = min(max(r - 4, 0), 56)
                acc, ak = C.pacc.next()
                for j in range(8):
                    if j < 4:
                        if rs % 2 == 0:
                            vt = ve[:, rs // 2 + j, :]
                            vkk = vek
                        else:
                            vt = vo[:, (rs - 1) // 2 + j, :]
                            vkk = vok
                    else:
                        vt = ve[:, 32 + (j - 4), :]
                        vkk = vek
                    mm(p, acc[0:64, 0:65], e[:, j, :], vt, j == 0, j == 7, [ek, vkk], [ak])
                sm, sk = C.small.next()
                recip(p, sm[0:64, 0:1], acc[0:64, 64:65], [ak], [sk])
                ts(p, "dve", ob[:, r, :], acc[0:64, 0:64], sm[0:64, 0:1], None, ALU.mult, None, [ak, sk], [obk])

            pend = na_stage1(0)
            for r in range(64):
                nxt = na_stage1(r + 1) if r + 1 < 64 else None
                na_stage2(r, pend[0], pend[1])
                pend = nxt
            for r4 in range(4):
                p.dma(MIXT[r4 * 1024:(r4 + 1) * 1024, h * 64:(h + 1) * 64].rearrange("(r q) d -> q r d", q=64), ob[:, r4 * 16:(r4 + 1) * 16, :],
                      [obk], ["MIXT"])


EPS_T = [None]


def _fm(v, nch):
    v = np.asarray(v, np.float32)
    return np.ascontiguousarray(np.swapaxes(v.reshape(v.shape[:-1] + (nch, 128)), -1, -2))


def _pairs(a):
    a = np.asarray(a, np.float32)
    sh = a.shape[:-2]
    a = a.reshape(sh + (16, 2, 64))
    a = np.moveaxis(a, -3, -1)
    return np.ascontiguousarray(a.reshape(sh + (128, 16)))


def shared_inputs(inp):
    f = lambda a: np.ascontiguousarray(np.asarray(a, np.float32))
    S = {}
    S["w_mod"] = f(inp["w_mod"])
    S["b_mod"] = _fm(inp["b_mod"], 48)
    S["gpre1"] = _fm(inp["g_mix_pre"], 8); S["gpost1"] = _fm(inp["g_mix_post"], 8)
    S["gpre2"] = _fm(inp["g_ffn_pre"], 8); S["gpost2"] = _fm(inp["g_ffn_post"], 8)
    S["wg"] = f(inp["w_ffn_gate"]); S["wu"] = f(inp["w_ffn_up"]); S["wd"] = f(inp["w_ffn_down"])
    S["w_in_e"] = f(inp["w_in_e"]); S["w_out_e"] = f(inp["w_out_e"])
    S["rpb"] = f(inp["na_rpb"])
    S["lamv"] = np.ascontiguousarray(np.stack([f(inp["lam_q1"]), f(inp["lam_k1"]), f(inp["lam_q2"]), f(inp["lam_k2"])], axis=1))
    S["gsub"] = f(inp["g_subln"])
    S["w_in_o"] = f(inp["w_in_o"]); S["w_out_o"] = f(inp["w_out_o"])
    S["s_lre"] = _pairs(inp["ssm_lam_re"]); S["s_lim"] = _pairs(inp["ssm_lam_im"])
    ldt = np.asarray(inp["ssm_log_dt"], np.float32)
    S["s_ldt"] = _pairs(np.repeat(ldt[..., None], 64, axis=-1))
    for nm, src in (("s_bre", "ssm_b_re"), ("s_bim", "ssm_b_im")):
        b = np.asarray(inp[src], np.float32)
        b = np.moveaxis(b, -1, 2)
        b = _pairs(b)
        S[nm] = np.ascontiguousarray(np.moveaxis(b, 2, -1).reshape(2, 2, 128, 256))
    for nm, src in (("s_cre", "ssm_c_re"), ("s_cim", "ssm_c_im")):
        c = np.asarray(inp[src], np.float32)
        c = np.moveaxis(c, 3, 2)
        c = _pairs(c)
        S[nm] = np.ascontiguousarray(np.moveaxis(c, 2, -1).reshape(2, 2, 128, 256))
    S["s_d"] = _fm(inp["ssm_d"], 4)
    S["w_glu"] = f(inp["w_glu"]); S["b_glu"] = _fm(inp["b_glu"], 4)
    cw = np.asarray(inp["hy_conv_w"], np.float32)
    S["hcw"] = np.ascontiguousarray(np.moveaxis(_fm(cw, 12), 1, -1))
    S["hcb"] = _fm(inp["hy_conv_b"], 12)
    S["hw1"] = f(inp["hy_w1"]); S["hb1"] = f(inp["hy_b1"])[..., None]; S["hf1"] = f(inp["hy_fr1"])[..., None]
    S["hw2"] = f(inp["hy_w2"]); S["hb2"] = f(inp["hy_b2"])[..., None]; S["hf2"] = f(inp["hy_fr2"])[..., None]
    S["hw3"] = f(inp["hy_w3"])
    S["hdec"] = np.ascontiguousarray(np.asarray(inp["hy_decay"], np.float32).reshape(2, 4, 512))
    S["hbias"] = f(inp["hy_bias"])
    S.update(consts())
    return S


def core_inputs(inp, cid, S):
    m = dict(S)
    xs = np.asarray(inp["x_sample"][cid], np.float32)
    xp = np.asarray(inp["x_prompt"][2 * cid:2 * cid + 2], np.float32).reshape(512, D)
    m["x0"] = np.ascontiguousarray(np.concatenate([xs, xp], 0))
    for nm, src in (("cak", "cache_a_k"), ("cav", "cache_a_v"), ("cbk", "cache_b_k"), ("cbv", "cache_b_v")):
        m[nm] = np.ascontiguousarray(np.asarray(inp[src][cid], np.float32).reshape(2, 512, 512))
    m["sre"] = _pairs(inp["state_c_re"][cid]); m["sim"] = _pairs(inp["state_c_im"][cid])
    cc = np.stack([np.asarray(inp["c"][cid], np.float32), np.asarray(inp["c_ctx"], np.float32)], -1)
    m["cond"] = np.ascontiguousarray(cc.reshape(8, 128, 2).transpose(1, 0, 2))
    return m


_PROG = {}


def run_device(inputs, nlayers=DEPTH, cores=8):
    if nlayers not in _PROG:
        _PROG[nlayers] = build_program(nlayers)
    nc = _PROG[nlayers]
    S = shared_inputs(inputs)
    in_maps = []
    for cid in range(cores):
        m = core_inputs(inputs, cid, S)
        in_maps.append({k: m[k] for k in IN_SPECS})
    res = run_bass_kernel_spmd(nc, in_maps, core_ids=list(range(cores)))
    return res.results


def kernel(**inputs):
    R = run_device(inputs)
    yp = np.concatenate([r["o_yp"].reshape(2, 256, D) for r in R], 0)
    ys = np.stack([r["o_ys"] for r in R], 0)
    outs = [yp.astype(np.float32), ys.astype(np.float32)]
    for nm, (hh, dd) in (("o_ak", (8, 64)), ("o_av", (8, 64)), ("o_bk", (4, 128)), ("o_bv", (4, 128))):
        a = np.concatenate([r[nm] for r in R], 0)
        outs.append(np.ascontiguousarray(a.reshape(16, 2, 256, hh, dd)).astype(np.float32))
    for nm in ("o_sre", "o_sim"):
        a = np.concatenate([r[nm] for r in R], 0)
        outs.append(np.ascontiguousarray(a.reshape(16, 2, 2, 32, 64)).astype(np.float32))
    return tuple(outs)


def build_odd_proj(p, T, l, li, identb, epsT, modA1, modB1):
    X = T["X"]
    UT32, UTB, HYT = T["UT32"], T["UTB"], T["HYT"]
    with p.phase():
        st = RR(p, 2, [128, 1024], F32, "wst")
        win = p.sbuf([128, 8, 2048], BF16, "win")
        load_w_bf16(p, win, "win", T["w_in_o"][li], 2048, st)
        nctx = NormCtx(p, identb, epsT)
        xs = RR(p, 2, [128, 4, D], F32, "xs")
        hT = RR(p, 2, [128, 8, 512], BF16, "hT")
        o32 = RR(p, 3, [128, 512], F32, "o32")
        obf = RR(p, 2, [128, 512], BF16, "obf")
        pso = RR(p, 4, [128, 512], F32, "pso", psum=True)
        for u in range(9):
            t0 = u * 512
            cd = unit_cd(u)
            xt, xk = xs.next()
            p.dma(xt[:], xview(X, t0), ["X%d" % u], [xk], eng="act")
            h, hk = hT.next()
            nctx.run(xt, xk, modA1, modB1, cd, h, hk)
            for c in range(16):
                ps, pk = pso.next()
                for kc in range(8):
                    mm(p, ps[:], win[:, kc, c * 128:(c + 1) * 128], h[:, kc, :], kc == 0, kc == 7, ["win", hk], [pk])
                o, ok = o32.next()
                cp(p, "act" if c % 2 else "dve", o[:], ps[:], [pk], [ok])
                if c < 4:
                    p.dma(UT32[c * 128:(c + 1) * 128, t0:t0 + 512], o[:], [ok], ["UT32"])
                    b, bk = obf.next()
                    cp(p, "pool", b[:], o[:], [ok], [bk])
                    p.dma(UTB[c * 128:(c + 1) * 128, t0:t0 + 512], b[:], [bk], ["UTB"])
                else:
                    p.dma(HYT[(c - 4) * 128:(c - 3) * 128, t0:t0 + 512], o[:], [ok], ["HYT"])


def sin_turns(p, eng_i, out, turns, tmp_i, tmp_f, keys_r, keys_w, shift=0.0):
    r = list(keys_r)
    if shift != 0.0:
        ts(p, "dve", tmp_f, turns, shift, None, ALU.add, None, r, keys_w)
        src = tmp_f
    else:
        src = turns
    cp(p, "dve", tmp_i, src, r + list(keys_w), keys_w)
    cp(p, "dve", out, tmp_i, keys_w, keys_w)
    tt(p, "dve", out, src, out, ALU.subtract, r + list(keys_w), keys_w)
    act(p, out, out, AF.Sin, keys_w, keys_w, scale=TWO_PI)


SEQS = ((0, 4096, True), (4096, 256, False), (4352, 256, False))


def build_s5(p, T, l, li, identf):
    UTB, UT32, YS, MIXF = T["UTB"], T["UT32"], T["YS"], T["MIXF"]
    with p.phase():
        iota = p.sbuf([128, 128], F32, "iota")
        p.dma(iota[:], T["iota128"], (), ["iota"])
        prm = p.sbuf([128, 2, 3, 16], F32, "prm")
        wk_ = p.sbuf([128, 2, 12, 16], F32, "wk")
        wki = p.sbuf([128, 16], I32, "wki")
        Bl = p.sbuf([128, 2, 16, 2, 128], BF16, "Bl")
        Cl = p.sbuf([128, 2, 16, 2, 128], BF16, "Cl")
        cosT = p.sbuf([128, 2, 16, 128], F32, "cosT")
        sinT = p.sbuf([128, 2, 16, 128], F32, "sinT")
        rT = p.sbuf([128, 2, 16, 128], F32, "rT")
        bc = RR(p, 2, [128, 2, 16, 16], F32, "bc")
        bb = RR(p, 2, [128, 2, 16, 16], F32, "bb")
        spd = RR(p, 2, [128, 128], F32, "spd")
        ti = p.sbuf([128, 128], I32, "ti")
        tf = p.sbuf([128, 128], F32, "tf")
        ptr = RR(p, 1, [128, 512], F32, "ptr", psum=True)
        pbu = RR(p, 4, [128, 512], F32, "pbu", psum=True)
        pcy = RR(p, 2, [128, 512], F32, "pcy", psum=True)
        for d in range(2):
            for j, nm in enumerate(("s_lre", "s_lim", "s_ldt")):
                p.dma(prm[:, d, j, :], T[nm][li, d], (), ["prm"])
            lre, lim, ldt = prm[:, d, 0, :], prm[:, d, 1, :], prm[:, d, 2, :]
            W = lambda i: wk_[:, d, i, :]
            R, Wk = ["prm", "wk"], ["wk"]
            act(p, W(0), ldt, AF.Exp, R, Wk)
            tt(p, "dve", W(1), lre, W(0), ALU.mult, R, Wk)
            act(p, W(1), W(1), AF.Exp, R, Wk)
            tt(p, "dve", W(2), lim, W(0), ALU.mult, R, Wk)
            ts(p, "dve", W(2), W(2), 1.0 / TWO_PI, None, ALU.mult, None, R, Wk)
            cp(p, "dve", wki[:], W(2), R, ["wki"])
            cp(p, "dve", W(3), wki[:], ["wki"], Wk)
            tt(p, "dve", W(2), W(2), W(3), ALU.subtract, R, Wk)
            act(p, W(4), W(2), AF.Sin, R, Wk, scale=TWO_PI)
            ts(p, "dve", W(3), W(2), 0.25, None, ALU.add, None, R, Wk)
            cp(p, "dve", wki[:], W(3), R, ["wki"])
            cp(p, "dve", W(5), wki[:], ["wki"], Wk)
            tt(p, "dve", W(3), W(3), W(5), ALU.subtract, R, Wk)
            act(p, W(5), W(3), AF.Sin, R, Wk, scale=TWO_PI)
            tt(p, "dve", W(6), W(1), W(5), ALU.mult, R, Wk)
            tt(p, "dve", W(7), W(1), W(4), ALU.mult, R, Wk)
            tt(p, "dve", W(3), lre, lre, ALU.mult, R, Wk)
            tt(p, "dve", W(4), lim, lim, ALU.mult, R, Wk)
            tt(p, "dve", W(3), W(3), W(4), ALU.add, R, Wk)
            recip(p, W(3), W(3), R, Wk)
            ts(p, "dve", W(6), W(6), -1.0, None, ALU.add, None, R, Wk)
            tt(p, "dve", W(4), W(6), lre, ALU.mult, R, Wk)
            tt(p, "dve", W(5), W(7), lim, ALU.mult, R, Wk)
            tt(p, "dve", W(4), W(4), W(5), ALU.add, R, Wk)
            tt(p, "dve", W(8), W(4), W(3), ALU.mult, R, Wk)
            tt(p, "dve", W(4), W(7), lre, ALU.mult, R, Wk)
            tt(p, "dve", W(5), W(6), lim, ALU.mult, R, Wk)
            tt(p, "dve", W(4), W(4), W(5), ALU.subtract, R, Wk)
            tt(p, "dve", W(9), W(4), W(3), ALU.mult, R, Wk)
            b_, bk = bc.next()
            p.dma(b_[:, 0].rearrange("q k h -> q (k h)"), T["s_bre"][li, d], (), [bk])
            p.dma(b_[:, 1].rearrange("q k h -> q (k h)"), T["s_bim"][li, d], (), [bk])
            o_, ok = bb.next()
            kre = W(8).unsqueeze(2).to_broadcast([128, 16, 16])
            kim = W(9).unsqueeze(2).to_broadcast([128, 16, 16])
            t1, t1k = bb.next()
            tt(p, "dve", o_[:, 0], b_[:, 0], kre, ALU.mult, [bk] + R, [ok])
            tt(p, "dve", t1[:, 0], b_[:, 1], kim, ALU.mult, [bk] + R, [t1k])
            tt(p, "dve", o_[:, 0], o_[:, 0], t1[:, 0], ALU.subtract, [t1k], [ok])
            tt(p, "dve", o_[:, 1], b_[:, 1], kre, ALU.mult, [bk] + R, [ok])
            tt(p, "dve", t1[:, 1], b_[:, 0], kim, ALU.mult, [bk] + R, [t1k])
            tt(p, "dve", o_[:, 1], o_[:, 1], t1[:, 1], ALU.add, [t1k], [ok])
            c_, ck = bc.next()
            p.dma(c_[:, 0].rearrange("q k h -> q (k h)"), T["s_cre"][li, d], (), [ck])
            p.dma(c_[:, 1].rearrange("q k h -> q (k h)"), T["s_cim"][li, d], (), [ck])
            mset(p, "pool", Cl[:, d].rearrange("q k r m -> q (k r m)"), 0.0, ["Cl"])
            for k in range(16):
                gl0, gl1 = (2 * k) % 8, (2 * k + 1) % 8
                for ri in range(2):
                    s_, sk = spd.next()
                    mset(p, "pool", s_[:], 0.0, [sk])
                    cp(p, "dve", s_[0:64, gl0 * 16:(gl0 + 1) * 16], o_[0:64, ri, k, :], [ok], [sk])
                    cp(p, "dve", s_[64:128, gl1 * 16:(gl1 + 1) * 16], o_[64:128, ri, k, :], [ok], [sk])
                    ps, pk = ptr.next()
                    trp(p, ps[:, 0:128], s_[:], identf[:], [sk, "identf"], [pk])
                    cp(p, "act", Bl[:, d, k, ri, :], ps[:, 0:128], [pk], ["Bl"])
                    if ri == 0:
                        cp(p, "dve", Cl[0:64, d, k, 0, gl0 * 16:(gl0 + 1) * 16], c_[0:64, 0, k, :], [ck, "Cl"], ["Cl"])
                        cp(p, "dve", Cl[64:128, d, k, 0, gl1 * 16:(gl1 + 1) * 16], c_[64:128, 0, k, :], [ck, "Cl"], ["Cl"])
                    else:
                        ts(p, "dve", Cl[0:64, d, k, 1, gl0 * 16:(gl0 + 1) * 16], c_[0:64, 1, k, :], -1.0, None, ALU.mult, None, [ck, "Cl"], ["Cl"])
                        ts(p, "dve", Cl[64:128, d, k, 1, gl1 * 16:(gl1 + 1) * 16], c_[64:128, 1, k, :], -1.0, None, ALU.mult, None, [ck, "Cl"], ["Cl"])
                ts(p, "dve", tf[:], iota[:], wk_[:, d, 2, k:k + 1], None, ALU.mult, None, ["iota"] + R, ["tf"])
                cp(p, "dve", ti[:], tf[:], ["tf"], ["ti"])
                cp(p, "dve", sinT[:, d, k, :], ti[:], ["ti"], ["tab"])
                tt(p, "dve", tf[:], tf[:], sinT[:, d, k, :], ALU.subtract, ["tab"], ["tf"])
                act(p, sinT[:, d, k, :], tf[:], AF.Sin, ["tf"], ["tab"], scale=TWO_PI)
                ts(p, "dve", tf[:], tf[:], 0.25, None, ALU.add, None, ["tf"], ["tf"])
                cp(p, "dve", ti[:], tf[:], ["tf"], ["ti"])
                cp(p, "dve", cosT[:, d, k, :], ti[:], ["ti"], ["tab"])
                tt(p, "dve", tf[:], tf[:], cosT[:, d, k, :], ALU.subtract, ["tab"], ["tf"])
                act(p, cosT[:, d, k, :], tf[:], AF.Sin, ["tf"], ["tab"], scale=TWO_PI)
                cp(p, "pool", rT[:, d, k, :], wk_[:, d, 1, k:k + 1].to_broadcast([128, 128]), R, ["tab"])

        ut = RR(p, 2, [128, 4, 512], BF16, "ut")
        xre = p.sbuf([128, 16, 512], BF16, "xre")
        xim = p.sbuf([128, 16, 512], BF16, "xim")
        m4 = RR(p, 8, [128, 4, 128], F32, "m4")
        wz = RR(p, 16, [128, 4, 128], F32, "wz")
        carry = p.sbuf([128, 2, 16], F32, "carry")
        cst = RR(p, 8, [128, 2], F32, "cst")
        o32 = RR(p, 2, [128, 512], F32, "o32")
        fst = RR(p, 2, [16, 128], F32, "fst")
        for si, (toff, L, has_init) in enumerate(SEQS):
            bs = min(512, L)
            nblk = L // bs
            nch = bs // 128
            for d in range(2):
                if has_init:
                    p.dma(carry[:, 0, :], T["sre"][li, d], (), ["carry%d" % k for k in range(16)])
                    p.dma(carry[:, 1, :], T["sim"][li, d], (), ["carry%d" % k for k in range(16)])
                else:
                    mset(p, "dve", carry[:].rearrange("q a k -> q (a k)"), 0.0, ["carry%d" % k for k in range(16)])
                blks = range(nblk) if d == 0 else range(nblk - 1, -1, -1)
                for blk in blks:
                    c0 = toff + blk * bs
                    u_, uk = ut.next()
                    p.dma(u_[:, :, 0:bs], UTB[:, c0:c0 + bs].rearrange("(c q) t -> q c t", q=128), ["UTB"], [uk])
                    csb_ = lambda k: cosT[:, d, k, :].unsqueeze(1).to_broadcast([128, nch, 128])
                    snb_ = lambda k: sinT[:, d, k, :].unsqueeze(1).to_broadcast([128, nch, 128])

                    def V3(a, d=d):
                        v = a.rearrange("q (c j) -> q c j", j=128)
                        return v if d == 0 else v[:, :, ::-1]
                    chs = list(range(nch)) if d == 0 else list(range(nch - 1, -1, -1))
                    for kg in range(4):
                        ks = list(range(kg * 4, kg * 4 + 4))
                        W_ = {}
                        for k in ks:
                            pr, prk = pbu.next()
                            pi_, pik = pbu.next()
                            mm(p, pr[:, 0:bs], Bl[:, d, k, 0, :], u_[:, k // 4, 0:bs], True, True, ["Bl", uk], [prk])
                            mm(p, pi_[:, 0:bs], Bl[:, d, k, 1, :], u_[:, k // 4, 0:bs], True, True, ["Bl", uk], [pik])
                            bre, bim = V3(pr[:, 0:bs]), V3(pi_[:, 0:bs])
                            a1, a1k = m4.next(); a2, a2k = m4.next(); a3, a3k = m4.next(); a4, a4k = m4.next()
                            tt(p, "dve", a1[:, 0:nch, :], bre, csb_(k), ALU.mult, [prk, "tab"], [a1k])
                            tt(p, "dve", a2[:, 0:nch, :], bim, snb_(k), ALU.mult, [pik, "tab"], [a2k])
                            tt(p, "dve", a3[:, 0:nch, :], bim, csb_(k), ALU.mult, [pik, "tab"], [a3k])
                            tt(p, "dve", a4[:, 0:nch, :], bre, snb_(k), ALU.mult, [prk, "tab"], [a4k])
                            wr, wrk = wz.next(); wi, wik = wz.next()
                            tt(p, "pool", wr[:, 0:nch, :], a1[:, 0:nch, :], a2[:, 0:nch, :], ALU.add, [a1k, a2k], [wrk])
                            tt(p, "pool", wi[:, 0:nch, :], a3[:, 0:nch, :], a4[:, 0:nch, :], ALU.subtract, [a3k, a4k], [wik])
                            zr, zrk = wz.next(); zi, zik = wz.next()
                            W_[k] = (wr, wrk, wi, wik, zr, zrk, zi, zik)
                        for ch in chs:
                            c2s = {}
                            for k in ks:
                                wr, wrk, wi, wik, zr, zrk, zi, zik = W_[k]
                                r_ = rT[:, d, k, :]
                                ck_ = "carry%d" % k
                                p.op("dve", lambda e, zr=zr, wr=wr, r_=r_, k=k, ch=ch: e.tensor_tensor_scan(out=zr[:, ch, :], data0=r_, data1=wr[:, ch, :], initial=carry[:, 0, k:k + 1], op0=ALU.mult, op1=ALU.add),
                                     [wrk, "tab", ck_], [zrk])
                                p.op("dve", lambda e, zi=zi, wi=wi, r_=r_, k=k, ch=ch: e.tensor_tensor_scan(out=zi[:, ch, :], data0=r_, data1=wi[:, ch, :], initial=carry[:, 1, k:k + 1], op0=ALU.mult, op1=ALU.add),
                                     [wik, "tab", ck_], [zik])
                            for k in ks:
                                wr, wrk, wi, wik, zr, zrk, zi, zik = W_[k]
                                cs_, sn_ = cosT[:, d, k, :], sinT[:, d, k, :]
                                c2, c2k = cst.next()
                                c2s[k] = (c2, c2k)
                                tt(p, "dve", c2[:, 0:1], zi[:, ch, 127:128], sn_[:, 127:128], ALU.mult, [zik, "tab"], [c2k])
                                tt(p, "dve", c2[:, 1:2], zi[:, ch, 127:128], cs_[:, 127:128], ALU.mult, [zik, "tab"], [c2k])
                            for k in ks:
                                wr, wrk, wi, wik, zr, zrk, zi, zik = W_[k]
                                cs_, sn_ = cosT[:, d, k, :], sinT[:, d, k, :]
                                c2, c2k = c2s[k]
                                ck_ = "carry%d" % k
                                stt(p, carry[:, 0, k:k + 1], zr[:, ch, 127:128], cs_[:, 127:128], c2[:, 0:1], ALU.mult, ALU.subtract, [zrk, c2k, "tab"], [ck_])
                                stt(p, carry[:, 1, k:k + 1], zr[:, ch, 127:128], sn_[:, 127:128], c2[:, 1:2], ALU.mult, ALU.add, [zrk, c2k, "tab"], [ck_])
                        for k in ks:
                            wr, wrk, wi, wik, zr, zrk, zi, zik = W_[k]
                            n1, n1k = m4.next(); n2, n2k = m4.next(); n3, n3k = m4.next(); n4, n4k = m4.next()
                            tt(p, "pool", n1[:, 0:nch, :], zr[:, 0:nch, :], csb_(k), ALU.mult, [zrk, "tab"], [n1k])
                            tt(p, "dve", n2[:, 0:nch, :], zi[:, 0:nch, :], snb_(k), ALU.mult, [zik, "tab"], [n2k])
                            tt(p, "dve", n3[:, 0:nch, :], zr[:, 0:nch, :], snb_(k), ALU.mult, [zrk, "tab"], [n3k])
                            tt(p, "pool", n4[:, 0:nch, :], zi[:, 0:nch, :], csb_(k), ALU.mult, [zik, "tab"], [n4k])
                            tt(p, "pool", V3(xre[:, k, 0:bs]), n1[:, 0:nch, :], n2[:, 0:nch, :], ALU.subtract, [n1k, n2k], ["xre"])
                            tt(p, "pool", V3(xim[:, k, 0:bs]), n3[:, 0:nch, :], n4[:, 0:nch, :], ALU.add, [n3k, n4k], ["xim"])
                    for cc in range(4):
                        ps, pk = pcy.next()
                        for kk in range(4):
                            k = cc * 4 + kk
                            mm(p, ps[:, 0:bs], Cl[:, d, k, 0, :], xre[:, k, 0:bs], kk == 0, False, ["Cl", "xre"], [pk])
                            mm(p, ps[:, 0:bs], Cl[:, d, k, 1, :], xim[:, k, 0:bs], False, kk == 3, ["Cl", "xim"], [pk])
                        o, ok = o32.next()
                        cp(p, "act", o[:, 0:bs], ps[:, 0:bs], [pk], [ok])
                        p.dma(YS[d, cc * 128:(cc + 1) * 128, c0:c0 + bs], o[:, 0:bs], [ok], ["YS"])
                if not has_init:
                    for ri, nm in enumerate(("o_sre", "o_sim")):
                        ps, pk = ptr.next()
                        trp(p, ps[0:16, 0:128], carry[:, ri, :], identf[:], ["carry%d" % k for k in range(16)] + ["identf"], [pk])
                        f_, fk = fst.next()
                        cp(p, "act", f_[:], ps[0:16, 0:128], [pk], [fk])
                        p.dma(T[nm][si - 1, li, d], f_[:], [fk], [nm + "%d_%d_%d" % (si, li, d)])

    with p.phase():
        st = RR(p, 2, [128, 1024], F32, "wst")
        wgl = p.sbuf([128, 4, 512], BF16, "wgl")
        load_w_bf16(p, wgl, "wgl", T["w_glu"][li], 512, st, nkc=4)
        dsk = p.sbuf([128, 4], F32, "dsk"); bgl = p.sbuf([128, 4], F32, "bgl")
        p.dma(dsk[:], T["s_d"][li], (), ["dsk"]); p.dma(bgl[:], T["b_glu"][li], (), ["bgl"])
        yl = RR(p, 3, [128, 3, 512], F32, "yl")
        g32 = RR(p, 2, [128, 4, 512], F32, "g32")
        gb = RR(p, 2, [128, 4, 512], BF16, "gb")
        sg = RR(p, 2, [128, 512], F32, "sg")
        ob = RR(p, 2, [128, 512], BF16, "ob")
        pso = RR(p, 3, [128, 512], F32, "pso", psum=True)
        for u in range(9):
            t0 = u * 512
            g, gk = g32.next()
            g2, g2k = gb.next()
            for cc in range(4):
                y, yk = yl.next()
                p.dma(y[:, 0, :], YS[0, cc * 128:(cc + 1) * 128, t0:t0 + 512], ["YS"], [yk], eng="act")
                p.dma(y[:, 1, :], YS[1, cc * 128:(cc + 1) * 128, t0:t0 + 512], ["YS"], [yk], eng="act")
                p.dma(y[:, 2, :], UT32[cc * 128:(cc + 1) * 128, t0:t0 + 512], ["UT32"], [yk], eng="sp")
                tt(p, "pool", y[:, 0, :], y[:, 0, :], y[:, 1, :], ALU.add, [yk], [yk])
                stt(p, y[:, 0, :], y[:, 2, :], dsk[:, cc:cc + 1], y[:, 0, :], ALU.mult, ALU.add, [yk, "dsk"], [yk])
                act(p, g[:, cc, :], y[:, 0, :], AF.Gelu, [yk], [gk])
                cp(p, "pool", g2[:, cc, :], g[:, cc, :], [gk], [g2k])
            for oc in range(4):
                ps, pk = pso.next()
                for kc in range(4):
                    mm(p, ps[:], wgl[:, kc, oc * 128:(oc + 1) * 128], g2[:, kc, :], kc == 0, kc == 3, ["wgl", g2k], [pk])
                s, sk = sg.next()
                act(p, s[:], ps[:], AF.Sigmoid, [pk, "bgl"], [sk], bias=bgl[:, oc:oc + 1])
                o, ok = ob.next()
                tt(p, "dve", o[:], g[:, oc, :], s[:], ALU.mult, [gk, sk], [ok])
                p.dma(MIXF[oc * 128:(oc + 1) * 128, t0:t0 + 512], o[:], [ok], ["MIXF"])


def build_hyena(p, T, l, li, identf):
    HYT, HYTM, Z1F, MIXT = T["HYT"], T["HYTM"], T["Z1F"], T["MIXT"]
    with p.phase():
        cw = p.sbuf([128, 12, 3], F32, "cw"); cb = p.sbuf([128, 12], F32, "cb")
        p.dma(cw[:], T["hcw"][li], (), ["cw"]); p.dma(cb[:], T["hcb"][li], (), ["cw"])
        raw = RR(p, 2, [128, 4098], F32, "raw")
        sv = RR(p, 2, [128, 4096], F32, "sv")
        stg = RR(p, 3, [128, 4, 128], F32, "stg")
        ptr = RR(p, 4, [128, 512], F32, "ptr", psum=True)
        for cc in range(12):
            for (toff, L, _) in SEQS:
                r_, rk = raw.next()
                mset(p, "pool", r_[:, 0:1], 0.0, [rk])
                mset(p, "pool", r_[:, L + 1:L + 2], 0.0, [rk])
                p.dma(r_[:, 1:L + 1], HYT[cc * 128:(cc + 1) * 128, toff:toff + L], ["HYT"], [rk], eng="act")
                s_, sk = sv.next()
                act(p, s_[:, 0:L], r_[:, 1:L + 1], AF.Identity, [rk, "cw"], [sk], scale=cw[:, cc, 1:2], bias=cb[:, cc:cc + 1])
                stt(p, s_[:, 0:L], r_[:, 0:L], cw[:, cc, 0:1], s_[:, 0:L], ALU.mult, ALU.add, [rk, "cw"], [sk])
                stt(p, s_[:, 0:L], r_[:, 2:L + 2], cw[:, cc, 2:3], s_[:, 0:L], ALU.mult, ALU.add, [rk, "cw"], [sk])
                for t4 in range(L // 512 if L >= 512 else 1):
                    n = min(4, L // 128)
                    ps, pk = ptr.next()
                    for i in range(n):
                        trp(p, ps[:, i * 128:(i + 1) * 128], s_[:, (t4 * 4 + i) * 128:(t4 * 4 + i + 1) * 128], identf[:], [sk, "identf"], [pk])
                    g, gk = stg.next()
                    cp(p, "act" if t4 % 2 else "dve", g[:, 0:n, :].rearrange("q i c -> q (i c)"), ps[:, 0:n * 128], [pk], [gk])
                    r0 = toff + t4 * 512
                    p.dma(HYTM[r0:r0 + n * 128, cc * 128:(cc + 1) * 128].rearrange("(i q) c -> q i c", q=128), g[:, 0:n, :], [gk], ["HYTM"])

    for L in (4096, 256):
        nb = L // 128
        N = 2 * L
        KSP = T["KSP%d" % L]
        Cb, Sb = T["dftC%d" % L], T["dftS%d" % L]
        with p.phase():
            ft = p.sbuf([33, L], F32, "ft")
            p.dma(ft[:], T["feats%d" % L], (), ["ft"])
            w1 = p.sbuf([33, 64], F32, "w1"); w2 = p.sbuf([64, 64], F32, "w2"); w3 = p.sbuf([64, 2048], F32, "w3")
            p.dma(w1[:], T["hw1"][li], (), ["w1"]); p.dma(w2[:], T["hw2"][li], (), ["w2"]); p.dma(w3[:], T["hw3"][li], (), ["w3"])
            sc = p.sbuf([64, 8], F32, "sc")
            for j, nm in enumerate(("hb1", "hf1", "hb2", "hf2")):
                p.dma(sc[:, j:j + 1], T[nm][li], (), ["sc"])
            ts(p, "dve", sc[:, 4:5], sc[:, 1:2], 1.0 / TWO_PI, None, ALU.mult, None, ["sc"], ["sc"])
            ts(p, "dve", sc[:, 5:6], sc[:, 3:4], 1.0 / TWO_PI, None, ALU.mult, None, ["sc"], ["sc"])
            ntn = p.sbuf([128, nb], F32, "ntn")
            p.dma(ntn[:], T["tn%d" % L], (), ["ntn"])
            ts(p, "dve", ntn[:], ntn[:], -1.0, None, ALU.mult, None, ["ntn"], ["ntn"])
            adec = p.sbuf([128, 4, 512], F32, "adec")
            p.dma(adec[:].rearrange("q a c -> q (a c)"), T["hdec"][li:li + 1].rearrange("o a c -> o (a c)").to_broadcast([128, 2048]), (), ["adec"])
            act(p, adec[:].rearrange("q a c -> q (a c)"), adec[:].rearrange("q a c -> q (a c)"), AF.Abs, ["adec"], ["adec"])
            z1 = p.sbuf([64, L], F32, "z1"); z2 = p.sbuf([64, L], F32, "z2")
            tfm = RR(p, 2, [64, 512], F32, "tfm")
            tim = RR(p, 2, [64, 512], I32, "tim")
            pz = RR(p, 2, [128, 512], F32, "pz", psum=True)
            ph = RR(p, 2, [128, 512], F32, "ph", psum=True)
            pk_ = RR(p, 3, [128, 512], F32, "pk", psum=True)
            bw = min(512, L)
            for (src, srck, W, K, dst, dk, bcol, fcol) in ((ft, "ft", w1, 33, z1, "z1", 0, 4), (z1, "z1", w2, 64, z2, "z2", 2, 5)):
                for b in range(L // bw):
                    ps, pk = pz.next()
                    mm(p, ps[0:64, 0:bw], W[0:K, :], src[0:K, b * bw:(b + 1) * bw], True, True, [srck, "w1", "w2"], [pk])
                    t_, tk = tfm.next()
                    i_, ik = tim.next()
                    ts(p, "dve", t_[:, 0:bw], ps[0:64, 0:bw], sc[:, bcol:bcol + 1], sc[:, fcol:fcol + 1], ALU.add, ALU.mult, [pk, "sc"], [tk])
                    cp(p, "dve", i_[:, 0:bw], t_[:, 0:bw], [tk], [ik])
                    d_ = dst[:, b * bw:(b + 1) * bw]
                    cp(p, "dve", d_, i_[:, 0:bw], [ik], [dk])
                    tt(p, "dve", d_, t_[:, 0:bw], d_, ALU.subtract, [tk, dk], [dk])
                    act(p, d_, d_, AF.Sin, [dk], [dk], scale=TWO_PI)
            ab = [p.sbuf([128, nb, 512], BF16, "ab%d" % j) for j in range(2)]
            hfs = RR(p, 4, [128, 512], F32, "hfs")
            dcy = RR(p, 2, [128, 512], F32, "dcy")
            cbk = RR(p, 2, [128, nb, 128], BF16, "cbk")
            ko = RR(p, 3, [128, 512], F32, "ko")
            sgp = p.sbuf([128, 32], BF16, "sgp")
            p.dma(sgp[:], T["sgnp"], (), ["sgp"])
            for order in range(2):
                for tb in range(nb):
                    hh = []
                    for side in range(2):
                        so = side * 2 + order
                        ps, pk = ph.next()
                        mm(p, ps[:], z2[0:64, tb * 128:(tb + 1) * 128], w3[0:64, so * 512:(so + 1) * 512], True, True, ["z2", "w3"], [pk])
                        dc, dck = dcy.next()
                        act(p, dc[:], adec[:, so, :], AF.Exp, ["adec", "ntn"], [dck], scale=ntn[:, tb:tb + 1])
                        h_, hk = hfs.next()
                        tt(p, "dve", h_[:], ps[:], dc[:], ALU.mult, [pk, dck], [hk])
                        if side == 1 and tb == 0:
                            mset(p, "dve", h_[0:1, :], 0.0, [hk])
                        hh.append((h_, hk))
                    tt(p, "pool", ab[0][:, tb, :], hh[0][0][:], hh[1][0][:], ALU.add, [hh[0][1], hh[1][1]], ["ab0"])
                    tt(p, "pool", ab[1][:, tb, :], hh[0][0][:], hh[1][0][:], ALU.subtract, [hh[0][1], hh[1][1]], ["ab1"])
                for which, (Mb, a_, akey) in enumerate(((Cb, ab[0], "ab0"), (Sb, ab[1], "ab1"))):
                    for fb in range(nb):
                        m_, mk = cbk.next()
                        p.dma(m_[:], Mb[fb], (), [mk], eng=("sp" if fb % 2 else "act"))
                        ps, pk = pk_.next()
                        for sc_ in range(nb):
                            mm(p, ps[:], m_[:, sc_, :], a_[:, sc_, :], sc_ == 0, sc_ == nb - 1, [mk, akey], [pk])
                        o, ok = ko.next()
                        ts(p, "dve", o[:], ps[:], 2.0 / N, None, ALU.mult, None, [pk], [ok])
                        if fb == 0:
                            if which == 0:
                                ts(p, "dve", o[0:1, :], ps[0:1, :], 1.0 / N, None, ALU.mult, None, [pk], [ok])
                            else:
                                ps2, pk2 = pk_.next()
                                for sc_ in range(nb):
                                    mm(p, ps2[0:1, :], sgp[:, 0:1], ab[0][:, sc_, :], sc_ == 0, sc_ == nb - 1, [mk, "ab0", "sgp"], [pk2])
                                ts(p, "dve", o[0:1, :], ps2[0:1, :], 1.0 / N, None, ALU.mult, None, [pk2], [ok])
                        p.dma(KSP[order, which, fb * 128:(fb + 1) * 128, :], o[:], [ok], ["KSP"])

    with p.phase():
        bbc = p.sbuf([128, 2, 512], F32, "bbc")
        p.dma(bbc[:].rearrange("q a c -> q (a c)"), T["hbias"][li:li + 1].rearrange("o a c -> o (a c)").to_broadcast([128, 1024]), (), ["bbc"])
        src_t = p.sbuf([128, 32, 512], BF16, "srct")
        Yre = p.sbuf([128, 32, 512], BF16, "Yre")
        Yp = p.sbuf([128, 32, 512], BF16, "Yp")
        cbk = RR(p, 2, [128, 32, 128], BF16, "cbk")
        sbk = RR(p, 2, [128, 32, 128], BF16, "sbk")
        hv = RR(p, 2, [128, 1536], F32, "hv")
        z1t = RR(p, 2, [128, 512], F32, "z1t")
        t4_ = RR(p, 4, [128, 512], F32, "t4")
        kk_ = RR(p, 2, [128, 2, 512], F32, "kk")
        ob = RR(p, 2, [128, 512], BF16, "ob")
        pf = RR(p, 4, [128, 512], F32, "pf", psum=True)
        pi = RR(p, 2, [128, 512], F32, "pi", psum=True)
        sgp = p.sbuf([128, 32], BF16, "sgp")
        sgj = p.sbuf([1, 128], BF16, "sgj")
        p.dma(sgp[:], T["sgnp"], (), ["sgp"])
        p.dma(sgj[:], T["sgnj"], (), ["sgp"])
        for (toff, L, _) in SEQS:
            nb = L // 128
            KSP = T["KSP%d" % L]
            Cb, Sb = T["dftC%d" % L], T["dftS%d" % L]
            for tb in range(nb):
                h_, hk = hv.next()
                p.dma(h_[:, 0:512], HYTM[toff + tb * 128:toff + (tb + 1) * 128, 0:512], ["HYTM"], [hk], eng="act")
                cp(p, "pool", src_t[:, tb, :], h_[:, 0:512], [hk], ["srct"])
            for order in range(2):
                for fb in range(nb):
                    c_, ck = cbk.next(); s_, sk = sbk.next()
                    p.dma(c_[:, 0:nb, :], Cb[fb], (), [ck], eng="act")
                    p.dma(s_[:, 0:nb, :], Sb[fb], (), [sk], eng="sp")
                    if fb == 0:
                        cp(p, "dve", s_[:, 0:nb, 0], sgp[:, 0:nb], [sk, "sgp"], [sk])
                    k_, kk = kk_.next()
                    p.dma(k_[:, 0, :], KSP[order, 0, fb * 128:(fb + 1) * 128, :], ["KSP"], [kk], eng="act")
                    p.dma(k_[:, 1, :], KSP[order, 1, fb * 128:(fb + 1) * 128, :], ["KSP"], [kk], eng="sp")
                    pre, prk = pf.next(); pp, ppk = pf.next()
                    for sc_ in range(nb):
                        mm(p, pre[:], c_[:, sc_, :], src_t[:, sc_, :], sc_ == 0, sc_ == nb - 1, [ck, "srct"], [prk])
                    for sc_ in range(nb):
                        mm(p, pp[:], s_[:, sc_, :], src_t[:, sc_, :], sc_ == 0, sc_ == nb - 1, [sk, "srct"], [ppk])
                    a1, a1k = t4_.next(); a2, a2k = t4_.next(); a3, a3k = t4_.next(); a4, a4k = t4_.next()
                    tt(p, "dve", a1[:], pre[:], k_[:, 0, :], ALU.mult, [prk, kk], [a1k])
                    tt(p, "dve", a2[:], pp[:], k_[:, 1, :], ALU.mult, [ppk, kk], [a2k])
                    tt(p, "dve", a3[:], pre[:], k_[:, 1, :], ALU.mult, [prk, kk], [a3k])
                    tt(p, "dve", a4[:], pp[:], k_[:, 0, :], ALU.mult, [ppk, kk], [a4k])
                    tt(p, "pool", Yre[:, fb, :], a1[:], a2[:], ALU.subtract, [a1k, a2k], ["Yre"])
                    tt(p, "pool", Yp[:, fb, :], a3[:], a4[:], ALU.add, [a3k, a4k], ["Yp"])
                    if fb == 0:
                        cp(p, "pool", Yre[0:1, 0, :], a1[0:1, :], [a1k, "Yre"], ["Yre"])
                        cp(p, "pool", Yp[0:1, 0, :], a2[0:1, :], [a2k, "Yp"], ["Yp"])
                for tb in range(nb):
                    c_, ck = cbk.next(); s_, sk = sbk.next()
                    p.dma(c_[:, 0:nb, :], Cb[tb], (), [ck], eng="act")
                    p.dma(s_[:, 0:nb, :], Sb[tb], (), [sk], eng="sp")
                    cp(p, "dve", s_[0:1, 0, :], sgj[0:1, :], [sk, "sgp"], [sk])
                    ps, pk = pi.next()
                    for fc in range(nb):
                        mm(p, ps[:], c_[:, fc, :], Yre[:, fc, :], fc == 0, False, [ck, "Yre"], [pk])
                    for fc in range(nb):
                        mm(p, ps[:], s_[:, fc, :], Yp[:, fc, :], False, fc == nb - 1, [sk, "Yp"], [pk])
                    r0 = toff + tb * 128
                    h_, hk = hv.next()
                    p.dma(h_[:], HYTM[r0:r0 + 128, :], ["HYTM"], [hk], eng="act")
                    a1, a1k = t4_.next(); a2, a2k = t4_.next()
                    if order == 0:
                        tt(p, "pool", a1[:], h_[:, 0:512], bbc[:, 0, :], ALU.mult, [hk, "bbc"], [a1k])
                        tt(p, "dve", a2[:], ps[:], a1[:], ALU.add, [pk, a1k], [a2k])
                        z_, zk = z1t.next()
                        tt(p, "dve", z_[:], a2[:], h_[:, 512:1024], ALU.mult, [a2k, hk], [zk])
                        p.dma(Z1F[r0:r0 + 128, :], z_[:], [zk], ["Z1F"])
                        cp(p, "pool", src_t[:, tb, :], z_[:], [zk], ["srct"])
                    else:
                        z_, zk = z1t.next()
                        p.dma(z_[:], Z1F[r0:r0 + 128, :], ["Z1F"], [zk], eng="sp")
                        tt(p, "pool", a1[:], z_[:], bbc[:, 1, :], ALU.mult, [zk, "bbc"], [a1k])
                        tt(p, "dve", a2[:], ps[:], a1[:], ALU.add, [pk, a1k], [a2k])
                        o, ok = ob.next()
                        tt(p, "dve", o[:], a2[:], h_[:, 1024:1536], ALU.mult, [a2k, hk], [ok])
                        p.dma(MIXT[r0:r0 + 128, 512:1024], o[:], [ok], ["MIXT"])
```
